# Optimizing a Trainium2 kernel written in Bass

```python
import math
import jax, jax.numpy as jnp
from jax import lax
import numpy as np

D_MODEL = 1024
BATCH = 8
SEQ = 4096
DEPTH = 2

LRU_WIDTH = D_MODEL // 4
LRU_BLOCKS = 4
LRU_BLOCK_W = LRU_WIDTH // LRU_BLOCKS
LRU_CONV = 4
LRU_C = 8.0
GLA_HEADS = 4
GLA_DV = (D_MODEL // 4) // GLA_HEADS
GLA_DK = GLA_DV // 2
GLA_GATE_RANK = 16
GLA_TAU = 16.0
GLA_CHUNK = 64
MOBA_HEADS = 8
MOBA_DH = (D_MODEL // 2) // MOBA_HEADS
MOBA_BLOCK = 256
MOBA_TOPK = 3
MOBA_Q_CHUNK = 128
REL_BUCKETS = 32
REL_MAX_DIST = 128
D_FF = -(-8 * D_MODEL // (3 * 256)) * 256

GLA_QK_W = GLA_HEADS * GLA_DK
GLA_V_W = GLA_HEADS * GLA_DV
MOBA_W = MOBA_HEADS * MOBA_DH
D_MIX = LRU_WIDTH + GLA_V_W + MOBA_W
IN_SPLITS = (LRU_WIDTH, LRU_WIDTH, GLA_QK_W, GLA_QK_W, GLA_V_W, GLA_GATE_RANK, GLA_V_W, MOBA_W, MOBA_W, MOBA_W)
D_IN = sum(IN_SPLITS)
RMS_EPS = 1e-6
NEG_INF = -1e30

kernel_name = "hymba_rglru_gla_moba_sandwich"


def rms_norm(x, g):
    xf = x.astype(jnp.float32)
    y = xf * lax.rsqrt(jnp.mean(xf * xf, axis=-1, keepdims=True) + RMS_EPS)
    return (y * g.astype(jnp.float32)).astype(x.dtype)


def split_heads(t, n):
    b, s, w = t.shape
    return t.astype(jnp.float32).reshape(b, s, n, w // n).transpose(0, 2, 1, 3)


def t5_bucket(rel):
    n = jnp.maximum(rel, 0)
    max_exact = REL_BUCKETS // 2
    nf = jnp.maximum(n, 1).astype(jnp.float32)
    large = max_exact + (jnp.log(nf / max_exact) / math.log(REL_MAX_DIST / max_exact)
                         * (REL_BUCKETS - max_exact)).astype(jnp.int32)
    large = jnp.minimum(large, REL_BUCKETS - 1)
    return jnp.where(n < max_exact, n, large)


def rg_lru_branch(xb, gb, conv_w, conv_b, wa, ba, wx, bx, lam):
    b, s, c = xb.shape
    xc = lax.conv_general_dilated(xb, conv_w[:, None, :].astype(xb.dtype), window_strides=(1,),
                                  padding=[(LRU_CONV - 1, 0)],
                                  dimension_numbers=('NWC', 'WIO', 'NWC'),
                                  feature_group_count=c)
    xc = xc.astype(jnp.float32) + conv_b.astype(jnp.float32)
    xg = xc.reshape(b, s, LRU_BLOCKS, LRU_BLOCK_W)
    r = jax.nn.sigmoid(jnp.einsum('bsgi,gij->bsgj', xg, wa.astype(jnp.float32)).reshape(b, s, c)
                       + ba.astype(jnp.float32))
    i = jax.nn.sigmoid(jnp.einsum('bsgi,gij->bsgj', xg, wx.astype(jnp.float32)).reshape(b, s, c)
                       + bx.astype(jnp.float32))
    log_a = -LRU_C * r * jax.nn.softplus(-lam.astype(jnp.float32))
    a = jnp.exp(log_a)
    mult = jnp.sqrt(-jnp.expm1(2.0 * log_a))
    mult = mult.at[:, 0].set(1.0)
    u = mult * (i * xc)

    def combine(left, right):
        a1, b1 = left
        a2, b2 = right
        return a1 * a2, a2 * b1 + b2

    _, h = lax.associative_scan(combine, (a, u), axis=1)
    return h * jax.nn.gelu(gb.astype(jnp.float32))


def gla_chunked(q, k, v, g):
    b, h, s, dk = q.shape
    dv = v.shape[-1]
    c = GLA_CHUNK
    n = s // c
    q = q.reshape(b, h, n, c, dk) * (dk ** -0.5)
    k = k.reshape(b, h, n, c, dk)
    v = v.reshape(b, h, n, c, dv)
    cum = jnp.cumsum(g.reshape(b, h, n, c, dk), axis=3)
    last = cum[:, :, :, -1:, :]
    q_d = q * jnp.exp(cum)
    k_d = k * jnp.exp(-cum)
    causal = jnp.tril(jnp.ones((c, c), dtype=bool))
    attn = jnp.where(causal, jnp.einsum('bhncd,bhned->bhnce', q_d, k_d), 0.0)
    intra = jnp.einsum('bhnce,bhnev->bhncv', attn, v)
    kv = jnp.einsum('bhncd,bhncv->bhndv', k * jnp.exp(last - cum), v)
    decay = jnp.exp(last[:, :, :, 0, :])

    def step(state, inp):
        kv_n, d_n = inp
        return d_n[..., None] * state + kv_n, state

    init = jnp.zeros((b, h, dk, dv), jnp.float32)
    _, s_prev = lax.scan(step, init, (kv.transpose(2, 0, 1, 3, 4), decay.transpose(2, 0, 1, 3)))
    s_prev = s_prev.transpose(1, 2, 0, 3, 4)
    inter = jnp.einsum('bhncd,bhndv->bhncv', q_d, s_prev)
    return (intra + inter).reshape(b, h, s, dv)


def moba_attention(q, k, v, rel_bias):
    b, h, s, d = q.shape
    nb = -(-s // MOBA_BLOCK)
    pad = nb * MOBA_BLOCK - s
    k_blk = jnp.pad(k, ((0, 0), (0, 0), (0, pad), (0, 0))).reshape(b, h, nb, MOBA_BLOCK, d)
    v_blk = jnp.pad(v, ((0, 0), (0, 0), (0, pad), (0, 0))).reshape(b, h, nb, MOBA_BLOCK, d)
    k_mean = jnp.mean(k_blk, axis=3)
    topk = min(MOBA_TOPK, nb)
    scale = d ** -0.5
    bias_h = rel_bias.T.astype(jnp.float32)
    bi = jnp.arange(b)[:, None, None]
    hi = jnp.arange(h)[None, :, None]
    blk_ids = jnp.arange(nb)
    offs = jnp.arange(MOBA_BLOCK)

    def chunk(ci):
        q0 = ci * MOBA_Q_CHUNK
        qc = lax.dynamic_slice_in_dim(q, q0, MOBA_Q_CHUNK, axis=2)
        qpos = q0 + jnp.arange(MOBA_Q_CHUNK)
        n_past = q0 // MOBA_BLOCK
        gate = jnp.einsum('bhqd,bhnd->bhqn', qc, k_mean)
        gate = jnp.where(blk_ids < n_past, gate, -jnp.inf)
        _, sel = lax.top_k(gate, topk)
        k_own = lax.dynamic_index_in_dim(k_blk, n_past, axis=2, keepdims=False)
        v_own = lax.dynamic_index_in_dim(v_blk, n_past, axis=2, keepdims=False)
        rel_own = qpos[:, None] - (n_past * MOBA_BLOCK + offs)[None, :]
        logit_own = (jnp.einsum('bhqd,bhkd->bhqk', qc, k_own) * scale
                     + bias_h[:, t5_bucket(rel_own)])
        logit_own = jnp.where(rel_own >= 0, logit_own, NEG_INF)
        logits = []
        for j in range(topk):
            idx = sel[..., j]
            kj = k_blk[bi, hi, idx]
            rel = qpos[:, None] - (idx[..., None] * MOBA_BLOCK + offs)
            bias = bias_h[hi[..., None], t5_bucket(rel)]
            lj = jnp.einsum('bhqd,bhqkd->bhqk', qc, kj) * scale + bias
            logits.append(jnp.where(j < n_past, lj, NEG_INF))
        logits.append(logit_own)
        p = jax.nn.softmax(jnp.concatenate(logits, axis=-1), axis=-1)
        p = p.reshape(b, h, MOBA_Q_CHUNK, topk + 1, MOBA_BLOCK)
        out = jnp.einsum('bhqk,bhkd->bhqd', p[..., topk, :], v_own)
        for j in range(topk):
            vj = v_blk[bi, hi, sel[..., j]]
            out = out + jnp.einsum('bhqk,bhqkd->bhqd', p[..., j, :], vj)
        return out

    out = lax.map(chunk, jnp.arange(s // MOBA_Q_CHUNK))
    return out.transpose(1, 2, 0, 3, 4).reshape(b, h, s, d)


def hybrid_mixer(h, w_in, conv_w, conv_b, wa, ba, wx, bx, lam, gate_w2, gate_b, gla_gain, rel_bias):
    bsz, s, _ = h.shape
    proj = h @ w_in
    split_points = [int(p) for p in np.cumsum(IN_SPLITS)[:-1]]
    lru_x, lru_g, gq, gk, gv, g_lr, g_out, mq, mk, mv = jnp.split(proj, split_points, axis=-1)
    y_lru = rg_lru_branch(lru_x, lru_g, conv_w, conv_b, wa, ba, wx, bx, lam)
    log_alpha = jax.nn.log_sigmoid((g_lr @ gate_w2 + gate_b).astype(jnp.float32)) / GLA_TAU
    o = gla_chunked(split_heads(gq, GLA_HEADS), split_heads(gk, GLA_HEADS),
                    split_heads(gv, GLA_HEADS), split_heads(log_alpha, GLA_HEADS))
    o = o.transpose(0, 2, 1, 3)
    o = o * lax.rsqrt(jnp.mean(o * o, axis=-1, keepdims=True) + RMS_EPS)
    y_gla = (o.reshape(bsz, s, GLA_V_W) * gla_gain.astype(jnp.float32)
             * jax.nn.silu(g_out.astype(jnp.float32)))
    y_moba = moba_attention(split_heads(mq, MOBA_HEADS), split_heads(mk, MOBA_HEADS),
                            split_heads(mv, MOBA_HEADS), rel_bias)
    y_moba = y_moba.transpose(0, 2, 1, 3).reshape(bsz, s, MOBA_W)
    return jnp.concatenate([y_lru, y_gla, y_moba], axis=-1).astype(h.dtype)


def setup_inputs(seed: int = 0) -> dict:
    key = jax.random.key(seed)
    ks = jax.random.split(key, 24)
    f32 = jnp.float32
    L = DEPTH

    def nrm(k, shape, scale):
        return jax.random.normal(k, shape, f32) * scale

    u = jax.random.uniform(ks[12], (L, LRU_WIDTH), f32, 0.9, 0.999) ** (1.0 / LRU_C)
    return {
        'x': nrm(ks[0], (BATCH, SEQ, D_MODEL), 1.0),
        'pre_mix_norm': 1.0 + nrm(ks[1], (L, D_MODEL), 0.02),
        'post_mix_norm': 1.0 + nrm(ks[2], (L, D_MODEL), 0.02),
        'pre_ffn_norm': 1.0 + nrm(ks[3], (L, D_MODEL), 0.02),
        'post_ffn_norm': 1.0 + nrm(ks[4], (L, D_MODEL), 0.02),
        'w_in': nrm(ks[5], (L, D_MODEL, D_IN), D_MODEL ** -0.5),
        'w_out': nrm(ks[6], (L, D_MIX, D_MODEL), D_MIX ** -0.5),
        'lru_conv_w': nrm(ks[7], (L, LRU_CONV, LRU_WIDTH), LRU_CONV ** -0.5),
        'lru_conv_b': nrm(ks[8], (L, LRU_WIDTH), 0.01),
        'lru_wa': nrm(ks[9], (L, LRU_BLOCKS, LRU_BLOCK_W, LRU_BLOCK_W), LRU_BLOCK_W ** -0.5),
        'lru_ba': nrm(ks[10], (L, LRU_WIDTH), 0.01),
        'lru_wx': nrm(ks[11], (L, LRU_BLOCKS, LRU_BLOCK_W, LRU_BLOCK_W), LRU_BLOCK_W ** -0.5),
        'lru_bx': nrm(ks[13], (L, LRU_WIDTH), 0.01),
        'lru_lambda': jnp.log(u) - jnp.log1p(-u),
        'gla_gate_w2': nrm(ks[14], (L, GLA_GATE_RANK, GLA_QK_W), GLA_GATE_RANK ** -0.5),
        'gla_gate_b': nrm(ks[15], (L, GLA_QK_W), 0.01),
        'gla_norm': 1.0 + nrm(ks[16], (L, GLA_V_W), 0.02),
        'rel_bias': nrm(ks[17], (REL_BUCKETS, MOBA_HEADS), 0.2),
        'w_ffn_gate': nrm(ks[18], (L, D_MODEL, D_FF), D_MODEL ** -0.5),
        'w_ffn_up': nrm(ks[19], (L, D_MODEL, D_FF), D_MODEL ** -0.5),
        'w_ffn_down': nrm(ks[20], (L, D_FF, D_MODEL), D_FF ** -0.5),
    }


def reference(x, pre_mix_norm, post_mix_norm, pre_ffn_norm, post_ffn_norm, w_in, w_out,
              lru_conv_w, lru_conv_b, lru_wa, lru_ba, lru_wx, lru_bx, lru_lambda,
              gla_gate_w2, gla_gate_b, gla_norm, rel_bias, w_ffn_gate, w_ffn_up, w_ffn_down):
    for l in range(DEPTH):
        h = rms_norm(x, pre_mix_norm[l])
        mix = hybrid_mixer(h, w_in[l], lru_conv_w[l], lru_conv_b[l], lru_wa[l], lru_ba[l],
                           lru_wx[l], lru_bx[l], lru_lambda[l], gla_gate_w2[l], gla_gate_b[l],
                           gla_norm[l], rel_bias)
        x = x + rms_norm(mix @ w_out[l], post_mix_norm[l])
        h = rms_norm(x, pre_ffn_norm[l])
        f = (jax.nn.silu(h @ w_ffn_gate[l]) * (h @ w_ffn_up[l])) @ w_ffn_down[l]
        x = x + rms_norm(f, post_ffn_norm[l])
    return x
```

```python
from contextlib import ExitStack
import math
import numpy as np
import ml_dtypes
import concourse.bass as bass
import concourse.mybir as mybir
from concourse.bass_utils import run_bass_kernel_spmd

F32 = mybir.dt.float32
BF16 = mybir.dt.bfloat16
ALU = mybir.AluOpType
AF = mybir.ActivationFunctionType
AX = mybir.AxisListType

S = 4096
D = 1024
L = 2
DIN = 2832
DFF = 2816
NFC = DFF // 128
EPS = 1e-6
NEG = -30000.0
ENGS = ("pe", "act", "dve", "pool", "sp")


class KB:
    def __init__(self, nc, stack, sync_same=True):
        self.nc = nc
        self.stack = stack
        self.sync_same = sync_same
        self.ops = {e: [] for e in ENGS}
        self.sem = {}
        self.cnt = {}
        self.step = {}
        self.known = {e: {} for e in ENGS}
        self.lw = {}
        self.rd = {}
        for e in ENGS:
            self._dom(e, 1)

    def _dom(self, name, step):
        if name not in self.sem:
            self.sem[name] = self.stack.enter_context(self.nc.semaphore("s_" + name))
            self.cnt[name] = 0
            self.step[name] = step
        return name

    def op(self, eng, fn, reads=(), writes=(), dma=None):
        dom = eng if dma is None else self._dom("d_" + dma, 16)
        deps = {}

        def add(d):
            if d is not None and deps.get(d[0], 0) < d[1]:
                deps[d[0]] = d[1]

        for k in reads:
            add(self.lw.get(k))
        for k in writes:
            add(self.lw.get(k))
            for dm, c in self.rd.get(k, {}).items():
                add((dm, c))
        if dma is not None and self.cnt[dom] > 0:
            add((dom, self.cnt[dom]))
        kn = self.known[eng]
        for d, c in deps.items():
            if d == eng and (eng == "pe" or not self.sync_same):
                continue
            if kn.get(d, 0) >= c:
                continue
            self.ops[eng].append(("w", self.sem[d], c))
            kn[d] = c
        self.cnt[dom] += self.step[dom]
        me = (dom, self.cnt[dom])
        self.ops[eng].append(("o", fn, self.sem[dom], self.step[dom]))
        for k in writes:
            self.lw[k] = me
            self.rd[k] = {}
        for k in reads:
            r = self.rd.setdefault(k, {})
            if r.get(dom, 0) < me[1]:
                r[dom] = me[1]
        return me

    def barrier(self):
        for eng in ENGS:
            kn = self.known[eng]
            for dom, c in self.cnt.items():
                if c > 0 and dom != eng and kn.get(dom, 0) < c:
                    self.ops[eng].append(("w", self.sem[dom], c))
                    kn[dom] = c
        self.lw = {}
        self.rd = {}

    def emit(self):
        nc = self.nc
        ops = self.ops

        def run(lst, e):
            for it in lst:
                if it[0] == "w":
                    e.wait_ge(it[1], it[2])
                else:
                    it[1](e).then_inc(it[2], it[3])

        with nc.Block() as block:
            @block.tensor
            def _(e):
                run(ops["pe"], e)

            @block.scalar
            def _(e):
                run(ops["act"], e)

            @block.vector
            def _(e):
                run(ops["dve"], e)

            @block.gpsimd
            def _(e):
                run(ops["pool"], e)

            @block.sync
            def _(e):
                run(ops["sp"], e)
        self.ops = {e: [] for e in ENGS}


def _t5_bucket(n):
    n = np.maximum(n, 0)
    nf = np.maximum(n, 1).astype(np.float32)
    large = 16 + (np.log(nf / np.float32(16)) / np.float32(math.log(128 / 16)) * np.float32(16)).astype(np.int32)
    large = np.minimum(large, 31)
    return np.where(n < 16, n, large)


def _consts():
    c = {}
    c["ident"] = np.eye(128, dtype=np.float32).astype(ml_dtypes.bfloat16)
    e = np.arange(128)
    c["tri"] = (e[:, None] <= e[None, :]).astype(np.float32)
    c["caus"] = np.where(e[None, :] >= e[:, None], 0.0, NEG).astype(np.float32)
    keys = np.arange(S)
    c["e16"] = (keys[None, :] // 256 == np.arange(16)[:, None]).astype(np.float32).astype(ml_dtypes.bfloat16)
    npast = np.arange(16)[:, None]
    nn = np.arange(16)[None, :]
    c["cm"] = np.where(nn < npast, 0.0, -1e30).astype(np.float32).reshape(1, 256)
    c["pm"] = (nn < npast).astype(np.float32).reshape(1, 256)
    p = np.arange(128)[:, None]
    c["bmask"] = (p // 32 == (np.arange(256)[None, :] // 64)).astype(np.float32)
    c["hm"] = (p // 32 == np.arange(4)[None, :]).astype(np.float32)
    return c


def _bias_idx():
    k = np.arange(128)[:, None]
    q = np.arange(128)[None, :]
    idx_diag = _t5_bucket(q - k)
    idx_off1 = _t5_bucket(q + 128 - k)
    return idx_diag, idx_off1


def build(debug=False, stop_after=None):
    nc = bass.Bass("TRN2", target_bir_lowering=False)
    dr = lambda name, shape, dt, kind="Internal": nc.dram_tensor(name, list(shape), dt, kind=kind).ap()
    IN = "ExternalInput"
    x_d = dr("x", [S, D], F32, IN)
    norms_d = dr("norms", [L, 4, D], F32, IN)
    w_in_d = dr("w_in", [L, D, DIN], F32, IN)
    w_out_d = dr("w_out", [L, D, D], F32, IN)
    wg_d = dr("w_ffn_gate", [L, D, DFF], F32, IN)
    wu_d = dr("w_ffn_up", [L, D, DFF], F32, IN)
    wd_d = dr("w_ffn_down", [L, DFF, D], F32, IN)
    lcols_d = dr("lru_cols", [L, 2, 128, 8], F32, IN)
    lwa_d = dr("lru_wa", [L, 4, 64, 64], F32, IN)
    lwx_d = dr("lru_wx", [L, 4, 64, 64], F32, IN)
    gw2_d = dr("gla_gate_w2", [L, 16, 128], F32, IN)
    gb_d = dr("gla_gate_b", [L, 128, 1], F32, IN)
    gn_d = dr("gla_norm", [L, 256], F32, IN)
    rb31_d = dr("rb31", [1, 8], F32, IN)
    tdg_d = dr("tdg", [128, 8, 128], F32, IN)
    tof_d = dr("tof", [128, 8, 128], F32, IN)
    ident_d = dr("ident", [128, 128], BF16, IN)
    tri_d = dr("tri", [128, 128], F32, IN)
    caus_d = dr("caus", [128, 128], F32, IN)
    e16_d = dr("e16", [16, S], BF16, IN)
    cm_d = dr("cm", [1, 256], F32, IN)
    pm_d = dr("pm", [1, 256], F32, IN)
    bmask_d = dr("bmask", [128, 256], F32, IN)
    hm_d = dr("hm", [128, 4], F32, IN)
    out_d = dr("out", [S, D], F32, "ExternalOutput")

    dk = "ExternalOutput" if debug else "Internal"
    winb_d = dr("winb", [L, D, DIN], BF16)
    woutb_d = dr("woutb", [L, D, D], BF16)
    wgub_d = dr("wgub", [L, NFC, 128, 2, 8, 128], BF16)
    wdb_d = dr("wdb", [L, DFF, D], BF16)
    xs1_d = dr("xs1", [S, D], F32, dk)
    lruT_d = dr("lruT", [512, S], F32, dk)
    gqT_d = dr("gqT", [128, S], F32, dk)
    gkT_d = dr("gkT", [128, S], F32, dk)
    glrT_d = dr("glrT", [16, S], BF16, dk)
    mqT_d = dr("mqT", [512, S], BF16, dk)
    mkT_d = dr("mkT", [512, S], BF16, dk)
    gv_d = dr("gv", [S, 256], BF16, dk)
    gout_d = dr("gout", [S, 256], F32, dk)
    mvp_d = dr("mvp", [S, 520], BF16, dk)
    mixT_d = dr("mixT", [D, S], BF16, dk)

    with ExitStack() as st:
        kb = KB(nc, st)
        uid = [0]

        def sbt(ctx, shape, dt, name=None):
            uid[0] += 1
            return ctx.enter_context(nc.sbuf_tensor("%s_%d" % (name or "t", uid[0]), list(shape), dt))

        def pst(ctx, shape, dt, name=None):
            uid[0] += 1
            return ctx.enter_context(nc.psum_tensor("%s_%d" % (name or "p", uid[0]), list(shape), dt))

        rr = {}

        def dmaname(stream, n):
            i = rr.get(stream, 0)
            rr[stream] = i + 1
            return "%s%d" % (stream, i % n)

        def dma(eng, out, in_, reads=(), writes=(), stream="g", n=4):
            kb.op(eng, lambda e: e.dma_start(out=out, in_=in_), reads=reads, writes=writes, dma=dmaname(stream, n))

        def mm(out, lhsT, rhs, start, stop, reads, writes, **kw):
            kb.op("pe", lambda e: e.matmul(out, lhsT=lhsT, rhs=rhs, start=start, stop=stop, **kw),
                  reads=reads, writes=writes)

        def tp(out, in_, ident, reads, writes):
            kb.op("pe", lambda e: e.transpose(out, in_, ident), reads=reads, writes=writes)

        ident = sbt(st, [128, 128], BF16, "ident")
        dma("sp", ident[:], ident_d, writes=["ident"])

        def end_phase():
            kb.barrier()
            kb.emit()

        def phase0():
            for l in range(L):
                for kc in range(8):
                    r0 = kc * 128
                    for c0 in range(0, DIN, 944):
                        dma("pool", winb_d[l, r0:r0 + 128, c0:c0 + 944], w_in_d[l, r0:r0 + 128, c0:c0 + 944], writes=[("winb", l, kc, c0)], stream="cast", n=4)
                if l == 0:
                    continue
            for l in range(L):
                for kc in range(8):
                    r0 = kc * 128
                    dma("pool", woutb_d[l, r0:r0 + 128, :], w_out_d[l, r0:r0 + 128, :], stream="cast", n=4)
                for fc in range(NFC):
                    for gu, wsrc in enumerate((wg_d, wu_d)):
                        dma("pool", wgub_d[l, fc, :, gu, :, :],
                            wsrc[l].rearrange("(kc p) f -> p kc f", p=128)[:, :, fc * 128:(fc + 1) * 128],
                            stream="cast", n=4)
                    dma("pool", wdb_d[l, fc * 128:(fc + 1) * 128, :], wd_d[l, fc * 128:(fc + 1) * 128, :], stream="cast", n=4)
            kb.emit()

        def rstd_from_ssq(ssq, rstd, n, tag):
            kb.op("dve", lambda e: e.tensor_scalar(out=rstd, in0=ssq, scalar1=1.0 / n, scalar2=EPS, op0=ALU.mult, op1=ALU.add),
                  reads=[tag + "ssq"], writes=[tag + "rs"])
            kb.op("act", lambda e: e.sqrt(out=rstd, in_=rstd), reads=[tag + "rs"], writes=[tag + "rs"])
            kb.op("dve", lambda e: e.reciprocal(out=rstd, in_=rstd), reads=[tag + "rs"], writes=[tag + "rs"])

        def phase1(l, xin_d):
            with ExitStack() as ph:
                win = sbt(ph, [128, 8, DIN], BF16, "win")
                gpre = sbt(ph, [128, D], F32, "gpre")
                xt = [sbt(ph, [128, D], F32, "xt") for _ in range(3)]
                hb = [sbt(ph, [128, D], BF16, "hb") for _ in range(2)]
                junk = sbt(ph, [128, D], BF16, "junk")
                hT = [sbt(ph, [128, 8, 512], BF16, "hT") for _ in range(2)]
                ssq = sbt(ph, [128, 8], F32, "ssq")
                rst = sbt(ph, [128, 8], F32, "rst")
                sf = [sbt(ph, [128, 512], F32, "sf") for _ in range(6)]
                sbf = [sbt(ph, [128, 512], BF16, "sbf") for _ in range(6)]
                sgv = [sbt(ph, [128, 256], BF16, "sgv") for _ in range(4)]
                sgo = [sbt(ph, [128, 256], F32, "sgo") for _ in range(4)]
                smv = [sbt(ph, [128, 8, 65], BF16, "smv") for _ in range(4)]
                tps = [pst(ph, [128, D], BF16, "tps") for _ in range(2)]
                aps = [pst(ph, [128, 512], F32, "aps") for _ in range(5)]
                for kc in range(8):
                    dma("sp", win[:, kc, :], winb_d[l, kc * 128:(kc + 1) * 128, :], reads=[("winb", l, kc, c0_) for c0_ in range(0, DIN, 944)], writes=[("win", kc)], stream="w", n=8)
                dma("sp", gpre[:], norms_d[l, 0:1, :].partition_broadcast(128), writes=["gpre"])
                for i in range(4):
                    kb.op("pool", lambda e, i=i: e.memset(smv[i][:], 1.0), writes=[("smv", i)])

                WINK = [("win", kc_) for kc_ in range(8)]
                flist = [("lru", lruT_d, 0, 0, 128, F32), ("lru", lruT_d, 128, 128, 128, F32),
                         ("lru", lruT_d, 256, 256, 128, F32), ("lru", lruT_d, 384, 384, 128, F32),
                         ("gq", gqT_d, 0, 512, 128, F32), ("gk", gkT_d, 0, 640, 128, F32),
                         ("glr", glrT_d, 0, 1024, 16, BF16)]
                for i in range(4):
                    flist.append(("mq", mqT_d, i * 128, 1296 + i * 128, 128, BF16))
                for i in range(4):
                    flist.append(("mk", mkT_d, i * 128, 1808 + i * 128, 128, BF16))

                def load(t):
                    dma("sp", xt[t % 3][:], xin_d[t * 128:(t + 1) * 128, :], writes=[("xt", t % 3)], stream="x", n=3)

                NT = S // 128
                load(0)
                load(1)
                pi = 0
                ev = 0
                import os as _os
                _ng = int(_os.environ.get("P1_GROUPS", S // 512))
                _parts = int(_os.environ.get("P1_PARTS", 7))
                for g in range(_ng):
                    hTg = hT[g % 2]
                    for s in range(4):
                        t = g * 4 + s
                        if t + 2 < NT:
                            load(t + 2)
                        xs = xt[t % 3]
                        hbs = hb[t % 2]
                        c = t % 8
                        kb.op("act", lambda e, xs=xs, c=c: e.activation(out=junk[:], in_=xs[:], func=AF.Square, accum_out=ssq[:, c:c + 1]),
                              reads=[("xt", t % 3)], writes=["junk", "p1ssq"])
                        rstd_from_ssq(ssq[:, c:c + 1], rst[:, c:c + 1], D, "p1")
                        kb.op("dve", lambda e, xs=xs, hbs=hbs, c=c: e.scalar_tensor_tensor(out=hbs[:], in0=xs[:], scalar=rst[:, c:c + 1], in1=gpre[:], op0=ALU.mult, op1=ALU.mult),
                              reads=[("xt", t % 3), "p1rs", "gpre"], writes=[("hb", t % 2)])
                        tpp = tps[t % 2]
                        for kc in range(8):
                            tp(tpp[:, kc * 128:(kc + 1) * 128], hbs[:, kc * 128:(kc + 1) * 128], ident[:],
                               reads=[("hb", t % 2), "ident"], writes=[("tps", t % 2)])
                        kb.op("act", lambda e, tpp=tpp, hTg=hTg, s=s: e.copy(out=hTg[:, :, s * 128:(s + 1) * 128], in_=tpp[:].rearrange("p (k c) -> p k c", k=8)),
                              reads=[("tps", t % 2)], writes=[("hT", g % 2)])
                    for (nm, dst, drow, wcol, wid, dt) in (flist if _parts & 2 else []):
                        ps = aps[pi % 5]
                        pk = ("aps", pi % 5)
                        pi += 1
                        for kc in range(8):
                            mm(ps[0:wid, :], win[:, kc, wcol:wcol + wid], hTg[:, kc, :], kc == 0, kc == 7,
                               reads=WINK + [("hT", g % 2)], writes=[pk])
                        if dt == F32:
                            stg = sf[ev % 6]
                            sk = ("sf", ev % 6)
                        else:
                            stg = sbf[ev % 6]
                            sk = ("sbf", ev % 6)
                        eng = "act" if ev % 2 == 0 else "dve"
                        ev += 1
                        if nm == "mq":
                            if eng == "act":
                                kb.op("act", lambda e, stg=stg, ps=ps, wid=wid: e.mul(out=stg[0:wid, :], in_=ps[0:wid, :], mul=0.125), reads=[pk], writes=[sk])
                            else:
                                kb.op("dve", lambda e, stg=stg, ps=ps, wid=wid: e.tensor_scalar(out=stg[0:wid, :], in0=ps[0:wid, :], scalar1=0.125, scalar2=None, op0=ALU.mult), reads=[pk], writes=[sk])
                        else:
                            if eng == "act":
                                kb.op("act", lambda e, stg=stg, ps=ps, wid=wid: e.copy(out=stg[0:wid, :], in_=ps[0:wid, :]), reads=[pk], writes=[sk])
                            else:
                                kb.op("dve", lambda e, stg=stg, ps=ps, wid=wid: e.tensor_copy(out=stg[0:wid, :], in_=ps[0:wid, :]), reads=[pk], writes=[sk])
                        dma("sp", dst[drow:drow + wid, g * 512:(g + 1) * 512], stg[0:wid, :], reads=[sk], stream="o1", n=12)
                    for s in (range(4) if _parts & 4 else []):
                        t = g * 4 + s
                        _tm = int(_os.environ.get("TM_SKIP", 0))
                        if not _tm & 1:
                            ps = aps[pi % 5]
                            pk = ("aps", pi % 5)
                            pi += 1
                            ps2 = aps[pi % 5]
                            pk2 = ("aps", pi % 5)
                            pi += 1
                            for kc in range(8):
                                mm(ps[:, 0:256], hTg[:, kc, s * 128:(s + 1) * 128], win[:, kc, 768:1024], kc == 0, kc == 7,
                                   reads=WINK + [("hT", g % 2)], writes=[pk])
                            for kc in range(8):
                                mm(ps2[:, 0:256], hTg[:, kc, s * 128:(s + 1) * 128], win[:, kc, 1040:1296], kc == 0, kc == 7,
                                   reads=WINK + [("hT", g % 2)], writes=[pk2])
                            a, b = sgv[t % 4], sgo[t % 4]
                            kb.op("act", lambda e, a=a, ps=ps: e.copy(out=a[:], in_=ps[:, 0:256]), reads=[pk], writes=[("sgv", t % 4)])
                            kb.op("dve", lambda e, b=b, ps2=ps2: e.tensor_copy(out=b[:], in_=ps2[:, 0:256]), reads=[pk2], writes=[("sgo", t % 4)])
                            dma("sp", gv_d[t * 128:(t + 1) * 128, :], a[:], reads=[("sgv", t % 4)], stream="o1", n=12)
                            dma("sp", gout_d[t * 128:(t + 1) * 128, :], b[:], reads=[("sgo", t % 4)], stream="o1", n=12)
                        if not _tm & 2:
                            ps = aps[pi % 5]
                            pk = ("aps", pi % 5)
                            pi += 1
                            for kc in range(8):
                                mm(ps[:, :], hTg[:, kc, s * 128:(s + 1) * 128], win[:, kc, 2320:2832], kc == 0, kc == 7,
                                   reads=WINK + [("hT", g % 2)], writes=[pk])
                            m = smv[t % 4]
                            psv = ps[:].rearrange("p (h d) -> p h d", h=8)
                            if _tm & 4:
                                pass
                            elif t % 2:
                                kb.op("act", lambda e, m=m, psv=psv: e.copy(out=m[:, :, 0:64], in_=psv), reads=[pk], writes=[("smv", t % 4)])
                            else:
                                kb.op("dve", lambda e, m=m, psv=psv: e.tensor_copy(out=m[:, :, 0:64], in_=psv), reads=[pk], writes=[("smv", t % 4)])
                            if not _tm & 8:
                                dma("sp", mvp_d[t * 128:(t + 1) * 128, :], m[:].rearrange("p h d -> p (h d)"), reads=[("smv", t % 4)], stream="o1", n=12)
                if _os.environ.get("P1_TAILSTORE"):
                    dma("sp", lruT_d[0:128, 0:8], rst[:], reads=["p1rs"], stream="o1", n=12)
                end_phase()

        def phase2a(l):
            TB = 1024
            with ExitStack() as ph:
                cols = sbt(ph, [128, 2, 8], F32, "lcols")
                ccol = sbt(ph, [128, 2], F32, "ccol")
                wstage = sbt(ph, [128, 2, 2, 128], F32, "wstage")
                wbd = sbt(ph, [128, 2, 2, 128], BF16, "wbd")
                xin = [sbt(ph, [128, TB + 3], F32, "xin") for _ in range(2)]
                gin = [sbt(ph, [128, TB], F32, "gin") for _ in range(2)]
                xc = sbt(ph, [128, TB], F32, "xc")
                xcb = sbt(ph, [128, TB], BF16, "xcb")
                rr_ = sbt(ph, [128, TB], F32, "r")
                ii_ = sbt(ph, [128, TB], F32, "i")
                aa = sbt(ph, [128, TB], F32, "a")
                mmul = sbt(ph, [128, TB], F32, "mult")
                uu = sbt(ph, [128, TB], F32, "u")
                hh = [sbt(ph, [128, TB], F32, "h") for _ in range(2)]
                gt = sbt(ph, [128, TB], F32, "gt")
                gs = sbt(ph, [128, TB], F32, "gs")
                yb = [sbt(ph, [128, TB], BF16, "yb") for _ in range(2)]
                gps = [pst(ph, [128, 512], F32, "gps") for _ in range(4)]
                for h in range(2):
                    dma("sp", cols[:, h, :], lcols_d[l, h], writes=["lcols"])
                kb.op("pool", lambda e: e.memset(wstage[:], 0.0), writes=["wstage"])
                for ax, src in enumerate((lwa_d, lwx_d)):
                    for h in range(2):
                        for b in range(2):
                            dma("sp", wstage[b * 64:(b + 1) * 64, ax, h, b * 64:(b + 1) * 64], src[l, 2 * h + b],
                                reads=[], writes=["wstage"])
                kb.op("dve", lambda e: e.tensor_copy(out=wbd[:], in_=wstage[:]), reads=["wstage"], writes=["wbd"])
                kb.op("act", lambda e: e.activation(out=ccol[:], in_=cols[:, :, 7], func=AF.Exp, scale=-1.0), reads=["lcols"], writes=["ccol"])
                kb.op("act", lambda e: e.activation(out=ccol[:], in_=ccol[:], func=AF.Ln, bias=1.0), reads=["ccol"], writes=["ccol"])
                kb.op("dve", lambda e: e.tensor_scalar(out=ccol[:], in0=ccol[:], scalar1=-8.0, scalar2=None, op0=ALU.mult), reads=["ccol"], writes=["ccol"])

                nb = S // TB
                it = 0
                for h in range(2):
                    for b in range(nb):
                        t0 = b * TB
                        xi = xin[it % 2]
                        gi = gin[it % 2]
                        hcur = hh[it % 2]
                        hprev = hh[(it + 1) % 2]
                        ybs = yb[it % 2]
                        kx, kg, ky = ("xin", it % 2), ("gin", it % 2), ("yb", it % 2)
                        kh, khp = ("h", it % 2), ("h", (it + 1) % 2)
                        it += 1
                        if b == 0:
                            kb.op("pool", lambda e, xi=xi: e.memset(xi[:, 0:3], 0.0), writes=[kx])
                            dma("sp", xi[:, 3:], lruT_d[h * 128:(h + 1) * 128, 0:TB], writes=[kx], stream="x", n=3)
                        else:
                            dma("sp", xi[:], lruT_d[h * 128:(h + 1) * 128, t0 - 3:t0 + TB], writes=[kx], stream="x", n=3)
                        dma("sp", gi[:], lruT_d[256 + h * 128:256 + (h + 1) * 128, t0:t0 + TB], writes=[kg], stream="x", n=3)
                        kb.op("dve", lambda e, xi=xi, h=h: e.tensor_scalar(out=xc[:], in0=xi[:, 3:TB + 3], scalar1=cols[:, h, 3:4], scalar2=cols[:, h, 4:5], op0=ALU.mult, op1=ALU.add),
                              reads=[kx, "lcols"], writes=["xc"])
                        for j in range(3):
                            kb.op("dve", lambda e, xi=xi, h=h, j=j: e.scalar_tensor_tensor(out=xc[:], in0=xi[:, j:TB + j], scalar=cols[:, h, j:j + 1], in1=xc[:], op0=ALU.mult, op1=ALU.add),
                                  reads=[kx, "lcols", "xc"], writes=["xc"])
                        kb.op("pool", lambda e: e.tensor_copy(out=xcb[:], in_=xc[:]), reads=["xc"], writes=["xcb"])
                        for sblk in range(TB // 512):
                            cs = slice(sblk * 512, (sblk + 1) * 512)
                            pa, px = gps[(2 * sblk) % 4], gps[(2 * sblk + 1) % 4]
                            ka, kx_ = ("gps", (2 * sblk) % 4), ("gps", (2 * sblk + 1) % 4)
                            mm(pa[:], wbd[:, 0, h, :], xcb[:, cs], True, True, reads=["wbd", "xcb"], writes=[ka])
                            mm(px[:], wbd[:, 1, h, :], xcb[:, cs], True, True, reads=["wbd", "xcb"], writes=[kx_])
                            kb.op("act", lambda e, pa=pa, cs=cs, h=h: e.activation(out=rr_[:, cs], in_=pa[:], func=AF.Sigmoid, bias=cols[:, h, 5:6]),
                                  reads=[ka, "lcols"], writes=["r"])
                            kb.op("act", lambda e, px=px, cs=cs, h=h: e.activation(out=ii_[:, cs], in_=px[:], func=AF.Sigmoid, bias=cols[:, h, 6:7]),
                                  reads=[kx_, "lcols"], writes=["i"])
                        kb.op("pool", lambda e, gi=gi: e.tensor_tensor(out=gt[:], in0=gi[:], in1=gi[:], op=ALU.mult), reads=[kg], writes=["gt"])
                        kb.op("pool", lambda e: e.tensor_scalar(out=gt[:], in0=gt[:], scalar1=0.044715, scalar2=1.0, op0=ALU.mult, op1=ALU.add), reads=["gt"], writes=["gt"])
                        kb.op("pool", lambda e, gi=gi: e.tensor_tensor(out=gt[:], in0=gt[:], in1=gi[:], op=ALU.mult), reads=["gt", kg], writes=["gt"])
                        kb.op("act", lambda e: e.activation(out=gs[:], in_=gt[:], func=AF.Sigmoid, scale=1.5957691216057308), reads=["gt"], writes=["gs"])
                        kb.op("pool", lambda e, gi=gi: e.tensor_tensor(out=gs[:], in0=gs[:], in1=gi[:], op=ALU.mult), reads=["gs", kg], writes=["gs"])
                        kb.op("act", lambda e, h=h: e.activation(out=aa[:], in_=rr_[:], func=AF.Exp, scale=ccol[:, h:h + 1]), reads=["r", "ccol"], writes=["a"])
                        kb.op("pool", lambda e: e.tensor_tensor(out=mmul[:], in0=aa[:], in1=aa[:], op=ALU.mult), reads=["a"], writes=["mult"])
                        kb.op("act", lambda e: e.activation(out=mmul[:], in_=mmul[:], func=AF.Sqrt, scale=-1.0, bias=1.0), reads=["mult"], writes=["mult"])
                        if b == 0:
                            kb.op("dve", lambda e: e.memset(mmul[:, 0:1], 1.0), reads=["mult"], writes=["mult"])
                        kb.op("dve", lambda e: e.tensor_tensor(out=uu[:], in0=ii_[:], in1=xc[:], op=ALU.mult), reads=["i", "xc"], writes=["u"])
                        kb.op("dve", lambda e: e.tensor_tensor(out=uu[:], in0=uu[:], in1=mmul[:], op=ALU.mult), reads=["u", "mult"], writes=["u"])
                        if b == 0:
                            kb.op("dve", lambda e, hcur=hcur: e.tensor_tensor_scan(out=hcur[:], data0=aa[:], data1=uu[:], initial=0.0, op0=ALU.mult, op1=ALU.add),
                                  reads=["a", "u"], writes=[kh])
                        else:
                            kb.op("dve", lambda e, hcur=hcur, hprev=hprev: e.tensor_tensor_scan(out=hcur[:], data0=aa[:], data1=uu[:], initial=hprev[:, TB - 1:TB], op0=ALU.mult, op1=ALU.add),
                                  reads=["a", "u", khp], writes=[kh])
                        kb.op("dve", lambda e, hcur=hcur, ybs=ybs: e.tensor_tensor(out=ybs[:], in0=hcur[:], in1=gs[:], op=ALU.mult), reads=[kh, "gs"], writes=[ky])
                        dma("sp", mixT_d[h * 128:(h + 1) * 128, t0:t0 + TB], ybs[:], reads=[ky], stream="o", n=4)
                end_phase()

        def phase2b(l):
            TB = 1024
            NCH = TB // 128
            with ExitStack() as ph:
                w2s = sbt(ph, [16, 128], F32, "w2s")
                w2b = sbt(ph, [16, 128], BF16, "w2b")
                negb = sbt(ph, [128, 1], F32, "negb")
                gn = sbt(ph, [128, 256], F32, "gn")
                tri = sbt(ph, [128, 128], F32, "tri")
                bmask = sbt(ph, [128, 256], F32, "bmask")
                hm = sbt(ph, [128, 4], F32, "hm")
                ones = sbt(ph, [128, 128], F32, "ones")
                glr = [sbt(ph, [16, TB], BF16, "glr") for _ in range(2)]
                qT = [sbt(ph, [128, TB], F32, "qT") for _ in range(2)]
                kT = [sbt(ph, [128, TB], F32, "kT") for _ in range(2)]
                vv = [sbt(ph, [128, NCH, 256], BF16, "vv") for _ in range(2)]
                go = [sbt(ph, [128, NCH, 256], F32, "go") for _ in range(2)]
                ee = sbt(ph, [128, TB], F32, "ee")
                cum = sbt(ph, [128, TB], F32, "cum")
                ex = sbt(ph, [128, TB], F32, "ex")
                dd = sbt(ph, [128, TB], F32, "dd")
                qd = sbt(ph, [128, TB], BF16, "qd")
                kdm = sbt(ph, [128, 4, TB], BF16, "kdm")
                kdec = sbt(ph, [128, TB], BF16, "kdec")
                dcol = sbt(ph, [128, NCH], F32, "dcol")
                kdtm = [sbt(ph, [128, 128], BF16, "kdtm") for _ in range(2)]
                am = [sbt(ph, [128, 4, 128], BF16, "am") for _ in range(2)]
                Sst = sbt(ph, [128, 256], F32, "Sst")
                Sbf = sbt(ph, [128, 256], BF16, "Sbf")
                kvm = sbt(ph, [128, 256], F32, "kvm")
                ob = sbt(ph, [128, NCH, 256], F32, "ob")
                osq = sbt(ph, [128, NCH, 256], F32, "osq")
                ssq = sbt(ph, [128, NCH * 4], F32, "gssq")
                rst = sbt(ph, [128, NCH * 4], F32, "grst")
                sg = sbt(ph, [128, NCH, 256], F32, "sg")
                yb = sbt(ph, [128, NCH, 256], BF16, "yb")
                yT = [sbt(ph, [128, 2, TB], BF16, "yT") for _ in range(2)]
                zps = [pst(ph, [128, 512], F32, "zps") for _ in range(2)]
                tps = pst(ph, [128, 1024], BF16, "gtps")
                aps_ = [pst(ph, [128, 512], F32, "gaps") for _ in range(2)]
                ops_ = [pst(ph, [128, 512], F32, "gops") for _ in range(2)]
                kvps = pst(ph, [128, 512], F32, "kvps")

                dma("sp", w2s[:], gw2_d[l], writes=["w2s"])
                kb.op("dve", lambda e: e.tensor_copy(out=w2b[:], in_=w2s[:]), reads=["w2s"], writes=["w2b"])
                dma("sp", negb[:], gb_d[l], writes=["negb"])
                kb.op("dve", lambda e: e.tensor_scalar(out=negb[:], in0=negb[:], scalar1=-1.0, scalar2=None, op0=ALU.mult), reads=["negb"], writes=["negb"])
                dma("sp", gn[:], gn_d[l:l + 1, :].partition_broadcast(128), writes=["gn"])
                dma("sp", tri[:], tri_d, writes=["tri"])
                dma("sp", bmask[:], bmask_d, writes=["bmask"])
                dma("sp", hm[:], hm_d, writes=["hm"])
                kb.op("pool", lambda e: e.memset(ones[:], 1.0), writes=["ones"])
                kb.op("pool", lambda e: e.memset(Sst[:], 0.0), writes=["Sst"])
                kb.op("pool", lambda e: e.memset(Sbf[:], 0.0), writes=["Sbf"])

                def load(b):
                    i = b % 2
                    t0 = b * TB
                    dma("sp", glr[i][:], glrT_d[:, t0:t0 + TB], writes=[("glr", i)], stream="x", n=3)
                    dma("sp", qT[i][:], gqT_d[:, t0:t0 + TB], writes=[("qT", i)], stream="x", n=3)
                    dma("sp", kT[i][:], gkT_d[:, t0:t0 + TB], writes=[("kT", i)], stream="x", n=3)
                    dma("sp", vv[i][:], gv_d[t0:t0 + TB, :].rearrange("(c p) f -> p c f", p=128), writes=[("vv", i)], stream="x", n=3)
                    dma("sp", go[i][:], gout_d[t0:t0 + TB, :].rearrange("(c p) f -> p c f", p=128), writes=[("go", i)], stream="x", n=3)

                nb = S // TB
                load(0)
                for b in range(nb):
                    if b + 1 < nb:
                        load(b + 1)
                    i = b % 2
                    t0 = b * TB
                    q_, k_, v_, g_, r_ = qT[i], kT[i], vv[i], go[i], glr[i]
                    kq, kk, kv, kg, kr = ("qT", i), ("kT", i), ("vv", i), ("go", i), ("glr", i)
                    for sblk in range(TB // 512):
                        cs = slice(sblk * 512, (sblk + 1) * 512)
                        zp = zps[sblk % 2]
                        mm(zp[:], w2b[:], r_[:, cs], True, True, reads=["w2b", kr], writes=[("zps", sblk % 2)])
                        kb.op("act", lambda e, zp=zp, cs=cs: e.activation(out=ee[:, cs], in_=zp[:], func=AF.Exp, scale=-1.0, bias=negb[:]),
                              reads=[("zps", sblk % 2), "negb"], writes=["ee"])
                    kb.op("act", lambda e: e.activation(out=ee[:], in_=ee[:], func=AF.Ln, bias=1.0), reads=["ee"], writes=["ee"])
                    for c in range(NCH):
                        cs = slice(c * 128, (c + 1) * 128)
                        kb.op("dve", lambda e, cs=cs: e.tensor_tensor_scan(out=cum[:, cs], data0=ones[:], data1=ee[:, cs], initial=0.0, op0=ALU.mult, op1=ALU.add),
                              reads=["ones", "ee"], writes=["cum"])
                    kb.op("act", lambda e: e.activation(out=ex[:], in_=cum[:], func=AF.Exp, scale=-1.0 / 16.0), reads=["cum"], writes=["ex"])
                    kb.op("dve", lambda e, q_=q_: e.scalar_tensor_tensor(out=qd[:], in0=q_[:], scalar=32.0 ** -0.5, in1=ex[:], op0=ALU.mult, op1=ALU.mult),
                          reads=[kq, "ex"], writes=["qd"])
                    kb.op("act", lambda e: e.activation(out=dcol[:], in_=cum[:].rearrange("p (c t) -> p c t", t=128)[:, :, 127], func=AF.Exp, scale=-1.0 / 16.0),
                          reads=["cum"], writes=["dcol"])
                    for c in range(NCH):
                        cs = slice(c * 128, (c + 1) * 128)
                        kb.op("pool", lambda e, cs=cs, c=c: e.tensor_scalar(out=dd[:, cs], in0=cum[:, cs], scalar1=cum[:, c * 128 + 127:c * 128 + 128], scalar2=None, op0=ALU.subtract),
                              reads=["cum"], writes=["dd"])
                    kb.op("act", lambda e: e.activation(out=ex[:], in_=cum[:], func=AF.Exp, scale=1.0 / 16.0), reads=["cum", "qd"], writes=["ex"])
                    for hh_ in range(4):
                        kb.op("dve", lambda e, k_=k_, hh_=hh_: e.scalar_tensor_tensor(out=kdm[:, hh_, :], in0=k_[:], scalar=hm[:, hh_:hh_ + 1], in1=ex[:], op0=ALU.mult, op1=ALU.mult),
                              reads=[kk, "ex", "hm"], writes=["kdm"])
                    kb.op("act", lambda e: e.activation(out=dd[:], in_=dd[:], func=AF.Exp, scale=1.0 / 16.0), reads=["dd"], writes=["dd"])
                    kb.op("pool", lambda e, k_=k_: e.tensor_tensor(out=kdec[:], in0=k_[:], in1=dd[:], op=ALU.mult), reads=[kk, "dd"], writes=["kdec"])
                    kb.op("act", lambda e, g_=g_: e.activation(out=sg[:], in_=g_[:], func=AF.Silu), reads=[kg], writes=["sg"])
                    for c in range(NCH):
                        cs = slice(c * 128, (c + 1) * 128)
                        j = c % 2
                        tp(tps[:, j * 128:(j + 1) * 128], kdec[:, cs], ident[:], reads=["kdec", "ident"], writes=["gtps"])
                        kb.op("act", lambda e, j=j: e.copy(out=kdtm[j][:], in_=tps[:, j * 128:(j + 1) * 128]), reads=["gtps"], writes=[("kdtm", j)])
                        ap_ = aps_[j]
                        for hh_ in range(4):
                            mm(ap_[:, hh_ * 128:(hh_ + 1) * 128], kdm[:, hh_, cs], qd[:, cs], True, True, reads=["kdm", "qd"], writes=[("gaps", j)])
                        kb.op("dve", lambda e, ap_=ap_, j=j: e.tensor_tensor(out=am[j][:], in0=ap_[:].rearrange("p (h c) -> p h c", h=4),
                                                                             in1=tri[:].unsqueeze(1).broadcast_to([128, 4, 128]), op=ALU.mult),
                              reads=[("gaps", j), "tri"], writes=[("am", j)])
                        op_ = ops_[j]
                        mm(op_[:, 0:256], qd[:, cs], Sbf[:], True, True, reads=["qd", "Sbf"], writes=[("gops", j)])
                        for hh_ in range(4):
                            mm(op_[:, hh_ * 64:(hh_ + 1) * 64], am[j][:, hh_, :], v_[:, c, hh_ * 64:(hh_ + 1) * 64], False, True,
                               reads=[("am", j), kv], writes=[("gops", j)], skip_group_check=True)
                        kb.op("act", lambda e, op_=op_, c=c: e.copy(out=ob[:, c, :], in_=op_[:, 0:256]), reads=[("gops", j)], writes=["ob"])
                        mm(kvps[:, 0:256], kdtm[j][:], v_[:, c, :], True, True, reads=[("kdtm", j), kv], writes=["kvps"])
                        kb.op("dve", lambda e: e.tensor_tensor(out=kvm[:], in0=kvps[:, 0:256], in1=bmask[:], op=ALU.mult), reads=["kvps", "bmask"], writes=["kvm"])
                        kb.op("dve", lambda e, c=c: e.scalar_tensor_tensor(out=Sst[:], in0=Sst[:], scalar=dcol[:, c:c + 1], in1=kvm[:], op0=ALU.mult, op1=ALU.add),
                              reads=["Sst", "dcol", "kvm"], writes=["Sst"])
                        kb.op("pool", lambda e: e.tensor_copy(out=Sbf[:], in_=Sst[:]), reads=["Sst"], writes=["Sbf"])
                    kb.op("pool", lambda e: e.tensor_tensor(out=osq[:], in0=ob[:], in1=ob[:], op=ALU.mult), reads=["ob"], writes=["osq"])
                    kb.op("dve", lambda e: e.tensor_reduce(out=ssq[:], in_=osq[:].rearrange("p c (h v) -> p (c h) v", h=4), axis=AX.X, op=ALU.add),
                          reads=["osq"], writes=["p2bssq"])
                    rstd_from_ssq(ssq[:], rst[:], 64, "p2b")
                    kb.op("dve", lambda e: e.tensor_tensor(out=ob[:].rearrange("p c (h v) -> p (c h) v", h=4), in0=ob[:].rearrange("p c (h v) -> p (c h) v", h=4),
                                                           in1=rst[:].unsqueeze(2).broadcast_to([128, NCH * 4, 64]), op=ALU.mult),
                          reads=["ob", "p2brs"], writes=["ob"])
                    kb.op("pool", lambda e: e.tensor_tensor(out=sg[:], in0=sg[:], in1=gn[:].unsqueeze(1).broadcast_to([128, NCH, 256]), op=ALU.mult),
                          reads=["sg", "gn"], writes=["sg"])
                    kb.op("dve", lambda e: e.tensor_tensor(out=yb[:], in0=ob[:], in1=sg[:], op=ALU.mult), reads=["ob", "sg"], writes=["yb"])
                    yTb = yT[b % 2]
                    for c in range(NCH):
                        for f in range(2):
                            jj = (c * 2 + f) % 4
                            tp(tps[:, jj * 128:(jj + 1) * 128], yb[:, c, f * 128:(f + 1) * 128], ident[:], reads=["yb", "ident"], writes=["gtps"])
                            kb.op("act", lambda e, jj=jj, c=c, f=f, yTb=yTb: e.copy(out=yTb[:, f, c * 128:(c + 1) * 128], in_=tps[:, jj * 128:(jj + 1) * 128]),
                                  reads=["gtps"], writes=[("yT", b % 2)])
                    dma("sp", mixT_d[256:512, t0:t0 + TB].rearrange("(f p) t -> p f t", p=128), yTb[:], reads=[("yT", b % 2)], stream="o", n=4)
                end_phase()

        def phase2c(l):
            with ExitStack() as ph:
                kaug = sbt(ph, [128, 8, S], BF16, "kaug")
                vp = sbt(ph, [128, 32, 520], BF16, "vp")
                qaug = [sbt(ph, [128, 8, 512], BF16, "qaug") for _ in range(2)]
                km = sbt(ph, [64, 8, 16], F32, "km")
                kmb = sbt(ph, [64, 8, 16], BF16, "kmb")
                cm = sbt(ph, [128, 16, 16], F32, "cm")
                pm = sbt(ph, [128, 16, 16], F32, "pm")
                b31 = sbt(ph, [128, 8], F32, "b31")
                caus = sbt(ph, [128, 128], F32, "caus")
                tstage = sbt(ph, [128, 2, 8, 128], F32, "tstage")
                tdT = sbt(ph, [128, 8, 128], BF16, "tdT")
                toT = sbt(ph, [128, 8, 128], BF16, "toT")
                tomT = sbt(ph, [128, 8, 128], BF16, "tomT")
                zer = sbt(ph, [128, 260], BF16, "zer")
                gm = sbt(ph, [128, 4, 8, 16], F32, "gm")
                m8 = sbt(ph, [128, 4, 8, 8], F32, "m8")
                sel = sbt(ph, [128, 4, 8, 16], F32, "sel")
                mpad = [sbt(ph, [128, 4, 8, 80], BF16, "mpad") for _ in range(2)]
                pT = [sbt(ph, [128, 512], BF16, "pT") for _ in range(4)]
                rcp = sbt(ph, [128, 4], F32, "rcp")
                ymo = [sbt(ph, [128, 4, 512], BF16, "ymo") for _ in range(2)]
                ymT = [sbt(ph, [128, 4, 512], BF16, "ymT") for _ in range(2)]
                gps = pst(ph, [128, 512], F32, "mgps")
                mtps = [pst(ph, [128, 512], F32, "mtps") for _ in range(1)]
                mtps_b = [pst(ph, [128, 1024], BF16, "mtpsb") for _ in range(1)]
                sps = [pst(ph, [128, 512], F32, "sps") for _ in range(3)]
                accs_full = [pst(ph, [128, 512], F32, "acc") for _ in range(2)]
                accs = [a_[:, 0:260].rearrange("p (s d) -> p s d", s=4) for a_ in accs_full]

                for h in range(8):
                    dma("sp", kaug[0:64, h, :], mkT_d[h * 64:(h + 1) * 64, :], writes=["kaug"], stream="x", n=3)
                    dma("sp", kaug[64:80, h, :], e16_d, writes=["kaug"], stream="x", n=3)
                for c in range(4):
                    dma("sp", vp[:, c * 8:(c + 1) * 8, :], mvp_d[c * 1024:(c + 1) * 1024, :].rearrange("(c p) f -> p c f", p=128), writes=["vp"], stream="x", n=3)
                dma("sp", cm[:].rearrange("p a b -> p (a b)"), cm_d.partition_broadcast(128), writes=["cm"])
                dma("sp", pm[:].rearrange("p a b -> p (a b)"), pm_d.partition_broadcast(128), writes=["pm"])
                dma("sp", b31[:], rb31_d.partition_broadcast(128), writes=["b31"])
                dma("sp", caus[:], caus_d, writes=["caus"])
                dma("sp", tstage[:, 0], tdg_d, writes=["tstage"])
                dma("sp", tstage[:, 1], tof_d, writes=["tstage"])
                kb.op("dve", lambda e: e.tensor_tensor(out=tdT[:], in0=tstage[:, 0], in1=caus[:].unsqueeze(1).broadcast_to([128, 8, 128]), op=ALU.add),
                      reads=["tstage", "caus"], writes=["tdT"])
                kb.op("dve", lambda e: e.tensor_copy(out=toT[:], in_=tstage[:, 1]), reads=["tstage"], writes=["toT"])
                kb.op("dve", lambda e: e.tensor_tensor(out=tomT[:], in0=tstage[:, 1], in1=b31[:].unsqueeze(2).broadcast_to([128, 8, 128]), op=ALU.subtract),
                      reads=["tstage", "b31"], writes=["tomT"])
                kb.op("pool", lambda e: e.memset(zer[:], 0.0), writes=["zer"])
                for i in range(2):
                    kb.op("pool", lambda e, i=i: e.memset(mpad[i][:], 0.0), writes=[("mpad", i)])
                kb.op("dve", lambda e: e.tensor_reduce(out=km[:].rearrange("p h n -> p (h n)"), in_=kaug[0:64, :, :].rearrange("p h (n t) -> p (h n) t", t=256), axis=AX.X, op=ALU.add),
                      reads=["kaug"], writes=["km"])
                kb.op("dve", lambda e: e.tensor_scalar(out=kmb[:], in0=km[:], scalar1=1.0 / 256.0, scalar2=None, op0=ALU.mult), reads=["km"], writes=["kmb"])

                def loadq(G):
                    i = G % 2
                    dma("sp", qaug[i][0:64, :, :], mqT_d.rearrange("(h d) t -> d h t", d=64)[:, :, G * 512:(G + 1) * 512], writes=[("qaug", i)], stream="q", n=2)

                NG = S // 512
                loadq(0)
                si = 0
                ai = 0
                for G in range(NG):
                    if G + 1 < NG:
                        loadq(G + 1)
                    qi = G % 2
                    qa = qaug[qi]
                    kqa = ("qaug", qi)
                    mp = mpad[qi]
                    for s in range(4):
                        for h in range(8):
                            mm(gps[:, (s * 8 + h) * 16:(s * 8 + h + 1) * 16], qa[0:64, h, s * 128:(s + 1) * 128], kmb[:, h, :], True, True,
                               reads=[kqa, "kmb"], writes=["mgps"])
                    np0 = 2 * G
                    for a in range(2):
                        cmv = cm[:, np0 + a, :].unsqueeze(1).unsqueeze(1).broadcast_to([128, 2, 8, 16])
                        kb.op("dve", lambda e, cmv=cmv, a=a: e.tensor_tensor(out=gm[:, 2 * a:2 * a + 2], in0=gps[:].rearrange("p (s h n) -> p s h n", s=4, h=8)[:, 2 * a:2 * a + 2],
                                                                             in1=cmv, op=ALU.add),
                              reads=["mgps", "cm"], writes=["gm"])
                    for s in range(4):
                        for h in range(8):
                            kb.op("dve", lambda e, s=s, h=h: e.max(out=m8[:, s, h, :], in_=gm[:, s, h, :]), reads=["gm"], writes=["m8"])
                    kb.op("dve", lambda e: e.tensor_tensor(out=sel[:], in0=gm[:], in1=m8[:, :, :, 2:3].broadcast_to([128, 4, 8, 16]), op=ALU.is_ge),
                          reads=["gm", "m8"], writes=["sel"])
                    kb.op("dve", lambda e: e.tensor_scalar(out=sel[:], in0=sel[:], scalar1=-NEG, scalar2=NEG, op0=ALU.mult, op1=ALU.add), reads=["sel"], writes=["sel"])
                    kb.op("dve", lambda e: e.tensor_tensor(out=sel[:], in0=sel[:], in1=b31[:].unsqueeze(1).unsqueeze(3).broadcast_to([128, 4, 8, 16]), op=ALU.add),
                          reads=["sel", "b31"], writes=["sel"])
                    for a in range(2):
                        pmv = pm[:, np0 + a, :].unsqueeze(1).unsqueeze(1).broadcast_to([128, 2, 8, 16])
                        kb.op("dve", lambda e, pmv=pmv, mp=mp, a=a: e.tensor_tensor(out=mp[:, 2 * a:2 * a + 2, :, 64:80], in0=sel[:, 2 * a:2 * a + 2], in1=pmv, op=ALU.mult),
                              reads=["sel", "pm"], writes=[("mpad", qi)])
                    for h in range(8):
                        mt = mtps[0]
                        for s in range(4):
                            mm(mt[0:80, s * 128:(s + 1) * 128], mp[:, s, h, :], ident[:], True, True, reads=[("mpad", qi), "ident"], writes=[("mtps", 0)])
                        kb.op("act", lambda e, mt=mt, h=h, qa=qa: e.copy(out=qa[64:80, h, :], in_=mt[64:80, :]),
                              reads=[("mtps", 0)], writes=[kqa])
                    ym = ymo[G % 2]
                    nj = 4 * G + 4
                    DEPTH = 2

                    def stageA(h, j):
                        nonlocal si
                        acc = accs[h % 2]
                        ka = ("acc", h % 2)
                        if j == 0:
                            mm(accs_full[h % 2][:, 0:260], zer[:, 0:128], zer[:, :], True, True, reads=["zer"], writes=[ka])
                        r = j - 4 * G
                        c0 = max(r, 0) * 128
                        sp_ = sps[si % 3]
                        ks = ("sps", si % 3)
                        pt = pT[si % 4]
                        kp = ("pT", si % 4)
                        si += 1
                        mm(sp_[:, c0:512], kaug[0:80, h, j * 128:(j + 1) * 128], qa[0:80, h, c0:512], True, True,
                           reads=["kaug", kqa], writes=[ks])
                        if r == -1:
                            mm(sp_[:, 0:128], ident[:], tomT[:, h, :], False, True, reads=["ident", "tomT"], writes=[ks], skip_group_check=True)
                        if r >= 0:
                            mm(sp_[:, r * 128:(r + 1) * 128], ident[:], tdT[:, h, :], False, True, reads=["ident", "tdT"], writes=[ks], skip_group_check=True)
                            if r < 3:
                                tt = toT if r % 2 == 0 else tomT
                                mm(sp_[:, (r + 1) * 128:(r + 2) * 128], ident[:], tt[:, h, :], False, True, reads=["ident", "toT", "tomT"], writes=[ks], skip_group_check=True)
                        kb.op("act", lambda e, pt=pt, sp_=sp_, c0=c0: e.activation(out=pt[:, c0:512], in_=sp_[:, c0:512], func=AF.Exp),
                              reads=[ks], writes=[kp])
                        return (h, j, r, pt, kp, acc, ka)

                    def stageB(info):
                        h, j, r, pt, kp, acc, ka = info
                        for s in range(max(r, 0), 4):
                            mm(acc[:, s, :], pt[:, s * 128:(s + 1) * 128], vp[:, j, h * 65:(h + 1) * 65], False, True,
                               reads=[kp, "vp"], writes=[ka], skip_group_check=True)
                        if j == nj - 1:
                            kb.op("dve", lambda e, acc=acc: e.reciprocal(out=rcp[:], in_=acc[:, :, 64]), reads=[ka], writes=["rcp"])
                            kb.op("dve", lambda e, acc=acc, h=h, ym=ym: e.tensor_tensor(out=ym[:, :, h * 64:(h + 1) * 64], in0=acc[:, :, 0:64],
                                                                                  in1=rcp[:].unsqueeze(2).broadcast_to([128, 4, 64]), op=ALU.mult),
                                  reads=[ka, "rcp"], writes=[("ymo", G % 2)])

                    pend = []
                    for h in range(8):
                        for j in range(nj):
                            pend.append(stageA(h, j))
                            if len(pend) > DEPTH:
                                stageB(pend.pop(0))
                    while pend:
                        stageB(pend.pop(0))
                    yt = ymT[G % 2]
                    for s in range(4):
                        tpb = mtps_b[0]
                        for f in range(4):
                            tp(tpb[:, f * 128:(f + 1) * 128], ym[:, s, f * 128:(f + 1) * 128], ident[:], reads=[("ymo", G % 2), "ident"], writes=[("mtpsb", 0)])
                        kb.op("act", lambda e, tpb=tpb, yt=yt, s=s: e.copy(out=yt[:, :, s * 128:(s + 1) * 128], in_=tpb[:, 0:512].rearrange("p (f q) -> p f q", f=4)),
                              reads=[("mtpsb", 0)], writes=[("ymT", G % 2)])
                    dma("sp", mixT_d[512:1024, G * 512:(G + 1) * 512].rearrange("(f p) t -> p f t", p=128), yt[:], reads=[("ymT", G % 2)], stream="o", n=4)
                end_phase()

        def phase3(l, xin_d, xout_d):
            with ExitStack() as ph:
                wout = sbt(ph, [128, 8, D], BF16, "wout")
                wdn = sbt(ph, [128, NFC, D], BF16, "wdn")
                g3 = sbt(ph, [128, 3, D], F32, "g3")
                wgu = [sbt(ph, [128, 2, 8, 128], BF16, "wgu") for _ in range(8)]
                mixT = [sbt(ph, [128, 8, 512], BF16, "mixT") for _ in range(2)]
                xt = [sbt(ph, [128, D], F32, "xt3") for _ in range(2)]
                x1 = [sbt(ph, [128, D], F32, "x1") for _ in range(4)]
                tmp = [sbt(ph, [128, D], F32, "tmp3") for _ in range(2)]
                junk = sbt(ph, [128, D], BF16, "junk3")
                hb = [sbt(ph, [128, D], BF16, "hb3") for _ in range(4)]
                hT = sbt(ph, [128, 8, 512], BF16, "hT3")
                actT = sbt(ph, [128, NFC, 512], BF16, "actT")
                sgt = [sbt(ph, [128, 512], F32, "sgt") for _ in range(2)]
                ssq = sbt(ph, [128, 16], F32, "ssq3")
                rst = sbt(ph, [128, 16], F32, "rst3")
                xo = [sbt(ph, [128, D], F32, "xo") for _ in range(2)]
                ops_ = [pst(ph, [128, 2, 512], F32, "p3o") for _ in range(2)]
                tps = pst(ph, [128, D], BF16, "p3t")
                gus = [pst(ph, [128, 512], F32, "p3gu") for _ in range(3)]
                for kc in range(8):
                    dma("sp", wout[:, kc, :], woutb_d[l, kc * 128:(kc + 1) * 128, :], writes=["wout"], stream="w", n=4)
                for fc in range(NFC):
                    dma("sp", wdn[:, fc, :], wdb_d[l, fc * 128:(fc + 1) * 128, :], writes=["wdn"], stream="w", n=4)
                for i in range(3):
                    dma("sp", g3[:, i, :], norms_d[l, i + 1:i + 2, :].partition_broadcast(128), writes=["g3"])

                wi = [0]

                def loadw(fc):
                    i = wi[0] % 8
                    wi[0] += 1
                    dma("sp", wgu[i][:], wgub_d[l, fc], writes=[("wgu", i)], stream="wgu", n=8)
                    return i

                NG = S // 512
                sq = 0

                def loadg(G):
                    i = G % 2
                    dma("sp", mixT[i][:], mixT_d[:, G * 512:(G + 1) * 512].rearrange("(k p) t -> p k t", p=128), writes=[("mixT", i)], stream="m", n=2)

                def loadx(t):
                    dma("sp", xt[t % 2][:], xin_d[t * 128:(t + 1) * 128, :], writes=[("xt3", t % 2)], stream="x", n=3)

                loadg(0)
                loadx(0)
                wq = []
                PRE = 7
                for G in range(NG):
                    if G + 1 < NG:
                        loadg(G + 1)
                    mT = mixT[G % 2]
                    while len(wq) < PRE:
                        wq.append(loadw(len(wq)))
                    for s in range(4):
                        t = G * 4 + s
                        if t + 1 < S // 128:
                            loadx(t + 1)
                        xs = xt[t % 2]
                        x1s = x1[s]
                        op_ = ops_[s % 2]
                        ko = ("p3o", s % 2)
                        tm_ = tmp[s % 2]
                        kt = ("tmp3", s % 2)
                        for hf in range(2):
                            for kc in range(8):
                                mm(op_[:, hf, :], mT[:, kc, s * 128:(s + 1) * 128], wout[:, kc, hf * 512:(hf + 1) * 512], kc == 0, kc == 7,
                                   reads=[("mixT", G % 2), "wout"], writes=[ko])
                        c = sq % 16
                        sq += 1
                        kb.op("act", lambda e, c=c, op_=op_: e.activation(out=junk[:], in_=op_[:].rearrange("p a b -> p (a b)"), func=AF.Square, accum_out=ssq[:, c:c + 1]),
                              reads=[ko], writes=["junk3", "p3ssq"])
                        rstd_from_ssq(ssq[:, c:c + 1], rst[:, c:c + 1], D, "p3")
                        kb.op("dve", lambda e, c=c, op_=op_, tm_=tm_: e.scalar_tensor_tensor(out=tm_[:], in0=op_[:].rearrange("p a b -> p (a b)"), scalar=rst[:, c:c + 1], in1=g3[:, 0, :], op0=ALU.mult, op1=ALU.mult),
                              reads=[ko, "p3rs", "g3"], writes=[kt])
                        kb.op("pool", lambda e, xs=xs, x1s=x1s, tm_=tm_: e.tensor_tensor(out=x1s[:], in0=xs[:], in1=tm_[:], op=ALU.add),
                              reads=[("xt3", t % 2), kt], writes=[("x1", s)])
                        c2 = sq % 16
                        sq += 1
                        kb.op("act", lambda e, c2=c2, x1s=x1s: e.activation(out=junk[:], in_=x1s[:], func=AF.Square, accum_out=ssq[:, c2:c2 + 1]),
                              reads=[("x1", s)], writes=["junk3", "p3ssq"])
                        rstd_from_ssq(ssq[:, c2:c2 + 1], rst[:, c2:c2 + 1], D, "p3")
                        hbs = hb[s]
                        kb.op("dve", lambda e, c2=c2, x1s=x1s, hbs=hbs: e.scalar_tensor_tensor(out=hbs[:], in0=x1s[:], scalar=rst[:, c2:c2 + 1], in1=g3[:, 1, :], op0=ALU.mult, op1=ALU.mult),
                              reads=[("x1", s), "p3rs", "g3"], writes=[("hb3", s)])
                    for s in range(4):
                        hbs = hb[s]
                        for kc in range(8):
                            tp(tps[:, kc * 128:(kc + 1) * 128], hbs[:, kc * 128:(kc + 1) * 128], ident[:], reads=[("hb3", s), "ident"], writes=["p3t"])
                        kb.op("act", lambda e, s=s: e.copy(out=hT[:, :, s * 128:(s + 1) * 128], in_=tps[:].rearrange("p (k c) -> p k c", k=8)),
                              reads=["p3t"], writes=["hT3"])
                    for fc in range(NFC):
                        wslot = wq.pop(0)
                        nxt = fc + PRE
                        if nxt < NFC:
                            wq.append(loadw(nxt))
                        w_ = wgu[wslot]
                        gp, up = gus[(2 * fc) % 3], gus[(2 * fc + 1) % 3]
                        kgp, kup = ("p3gu", (2 * fc) % 3), ("p3gu", (2 * fc + 1) % 3)
                        for kc in range(8):
                            mm(gp[:], w_[:, 0, kc, :], hT[:, kc, :], kc == 0, kc == 7, reads=[("wgu", wslot), "hT3"], writes=[kgp])
                        for kc in range(8):
                            mm(up[:], w_[:, 1, kc, :], hT[:, kc, :], kc == 0, kc == 7, reads=[("wgu", wslot), "hT3"], writes=[kup])
                        sg_ = sgt[fc % 2]
                        kb.op("act", lambda e, sg_=sg_, gp=gp: e.activation(out=sg_[:], in_=gp[:], func=AF.Silu), reads=[kgp], writes=[("sgt", fc % 2)])
                        kb.op("dve", lambda e, sg_=sg_, up=up, fc=fc: e.tensor_tensor(out=actT[:, fc, :], in0=up[:], in1=sg_[:], op=ALU.mult),
                              reads=[kup, ("sgt", fc % 2)], writes=["actT"])
                    if G + 1 < NG:
                        while len(wq) < PRE:
                            wq.append(loadw(len(wq)))
                    for s in range(4):
                        t = G * 4 + s
                        op_ = ops_[s % 2]
                        ko = ("p3o", s % 2)
                        tm_ = tmp[s % 2]
                        kt = ("tmp3", s % 2)
                        for hf in range(2):
                            for fc in range(NFC):
                                mm(op_[:, hf, :], actT[:, fc, s * 128:(s + 1) * 128], wdn[:, fc, hf * 512:(hf + 1) * 512], fc == 0, fc == NFC - 1,
                                   reads=["actT", "wdn"], writes=[ko])
                        c = sq % 16
                        sq += 1
                        kb.op("act", lambda e, c=c, op_=op_: e.activation(out=junk[:], in_=op_[:].rearrange("p a b -> p (a b)"), func=AF.Square, accum_out=ssq[:, c:c + 1]),
                              reads=[ko], writes=["junk3", "p3ssq"])
                        rstd_from_ssq(ssq[:, c:c + 1], rst[:, c:c + 1], D, "p3")
                        kb.op("dve", lambda e, c=c, op_=op_, tm_=tm_: e.scalar_tensor_tensor(out=tm_[:], in0=op_[:].rearrange("p a b -> p (a b)"), scalar=rst[:, c:c + 1], in1=g3[:, 2, :], op0=ALU.mult, op1=ALU.mult),
                              reads=[ko, "p3rs", "g3"], writes=[kt])
                        xos = xo[t % 2]
                        kb.op("pool", lambda e, xos=xos, s=s, tm_=tm_: e.tensor_tensor(out=xos[:], in0=x1[s][:], in1=tm_[:], op=ALU.add),
                              reads=[("x1", s), kt], writes=[("xo", t % 2)])
                        dma("sp", xout_d[t * 128:(t + 1) * 128, :], xos[:], reads=[("xo", t % 2)], stream="o", n=4)
                end_phase()

        kb.barrier()
        import os as _os2
        if not _os2.environ.get("SKIP_P0"):
            phase0()
        done = stop_after == "p0"
        for l in range(L):
            if done:
                break
            xin = x_d if l == 0 else xs1_d
            xout = xs1_d if l == 0 else out_d
            for nm, fn in (("p1", lambda: phase1(l, xin)), ("p2a", lambda: phase2a(l)), ("p2b", lambda: phase2b(l)),
                           ("p2c", lambda: phase2c(l)), ("p3", lambda: phase3(l, xin, xout))):
                fn()
                if stop_after == (l, nm):
                    done = True
                    break
            if done:
                break
        kb.barrier()
        kb.emit()

    return nc


def _host_inputs(inputs):
    f = lambda a: np.ascontiguousarray(np.asarray(a, dtype=np.float32))
    c = _consts()
    idx_diag, idx_off1 = _bias_idx()
    rel = f(inputs["rel_bias"])
    shared = {
        "norms": f(np.stack([inputs["pre_mix_norm"], inputs["post_mix_norm"], inputs["pre_ffn_norm"], inputs["post_ffn_norm"]], axis=1)),
        "w_in": f(inputs["w_in"]), "w_out": f(inputs["w_out"]),
        "w_ffn_gate": f(inputs["w_ffn_gate"]), "w_ffn_up": f(inputs["w_ffn_up"]), "w_ffn_down": f(inputs["w_ffn_down"]),
        "lru_wa": f(inputs["lru_wa"]), "lru_wx": f(inputs["lru_wx"]),
        "gla_gate_w2": f(inputs["gla_gate_w2"]),
        "gla_gate_b": f(np.asarray(inputs["gla_gate_b"]).reshape(L, 128, 1)),
        "gla_norm": f(inputs["gla_norm"]),
        "rb31": f(rel[31:32, :]),
        "tdg": f(np.transpose(rel[idx_diag], (0, 2, 1))),
        "tof": f(np.transpose(rel[idx_off1], (0, 2, 1))),
        "ident": c["ident"], "tri": c["tri"], "caus": c["caus"], "e16": c["e16"],
        "cm": c["cm"], "pm": c["pm"], "bmask": c["bmask"], "hm": c["hm"],
    }
    cw = np.transpose(np.asarray(inputs["lru_conv_w"], dtype=np.float32), (0, 2, 1))
    cols = np.concatenate([cw] + [np.asarray(inputs[k], dtype=np.float32)[:, :, None]
                                  for k in ("lru_conv_b", "lru_ba", "lru_bx", "lru_lambda")], axis=2)
    shared["lru_cols"] = f(cols.reshape(L, 2, 128, 8))
    x = np.asarray(inputs["x"], dtype=np.float32)
    return [dict(shared, x=np.ascontiguousarray(x[b])) for b in range(x.shape[0])]


_NC_CACHE = {}


def kernel(**inputs):
    in_maps = _host_inputs(inputs)
    if "nc" not in _NC_CACHE:
        _NC_CACHE["nc"] = build()
    nc = _NC_CACHE["nc"]
    n = len(in_maps)
    res = run_bass_kernel_spmd(nc, in_maps, core_ids=list(range(n)))
    return np.stack([np.asarray(r["out"], dtype=np.float32) for r in res.results], axis=0)
```

```python
from contextlib import ExitStack
import math
import numpy as np
import ml_dtypes
import concourse.bass as bass
import concourse.mybir as mybir
from concourse.bass_utils import run_bass_kernel_spmd

F32 = mybir.dt.float32
BF16 = mybir.dt.bfloat16
ALU = mybir.AluOpType
AF = mybir.ActivationFunctionType
AX = mybir.AxisListType

S = 4096
D = 1024
L = 2
DIN = 2832
DFF = 2816
NFC = DFF // 128
EPS = 1e-6
NEG = -30000.0
ENGS = ("pe", "act", "dve", "pool", "sp")


class KB:
    def __init__(self, nc, stack, sync_same=True):
        self.nc = nc
        self.stack = stack
        self.sync_same = sync_same
        self.ops = {e: [] for e in ENGS}
        self.sem = {}
        self.cnt = {}
        self.step = {}
        self.known = {e: {} for e in ENGS}
        self.lw = {}
        self.rd = {}
        for e in ENGS:
            self._dom(e, 1)

    def _dom(self, name, step):
        if name not in self.sem:
            self.sem[name] = self.stack.enter_context(self.nc.semaphore("s_" + name))
            self.cnt[name] = 0
            self.step[name] = step
        return name

    def op(self, eng, fn, reads=(), writes=(), dma=None):
        dom = eng if dma is None else self._dom("d_" + dma, 16)
        deps = {}

        def add(d):
            if d is not None and deps.get(d[0], 0) < d[1]:
                deps[d[0]] = d[1]

        for k in reads:
            add(self.lw.get(k))
        for k in writes:
            add(self.lw.get(k))
            for dm, c in self.rd.get(k, {}).items():
                add((dm, c))
        if dma is not None and self.cnt[dom] > 0:
            add((dom, self.cnt[dom]))
        kn = self.known[eng]
        for d, c in deps.items():
            if d == eng and (eng == "pe" or not self.sync_same):
                continue
            if kn.get(d, 0) >= c:
                continue
            self.ops[eng].append(("w", self.sem[d], c))
            kn[d] = c
        self.cnt[dom] += self.step[dom]
        me = (dom, self.cnt[dom])
        self.ops[eng].append(("o", fn, self.sem[dom], self.step[dom]))
        for k in writes:
            self.lw[k] = me
            self.rd[k] = {}
        for k in reads:
            r = self.rd.setdefault(k, {})
            if r.get(dom, 0) < me[1]:
                r[dom] = me[1]
        return me

    def barrier(self):
        for eng in ENGS:
            kn = self.known[eng]
            for dom, c in self.cnt.items():
                if c > 0 and dom != eng and kn.get(dom, 0) < c:
                    self.ops[eng].append(("w", self.sem[dom], c))
                    kn[dom] = c
        self.lw = {}
        self.rd = {}

    def emit(self):
        nc = self.nc
        ops = self.ops

        def run(lst, e):
            for it in lst:
                if it[0] == "w":
                    e.wait_ge(it[1], it[2])
                else:
                    it[1](e).then_inc(it[2], it[3])

        with nc.Block() as block:
            @block.tensor
            def _(e):
                run(ops["pe"], e)

            @block.scalar
            def _(e):
                run(ops["act"], e)

            @block.vector
            def _(e):
                run(ops["dve"], e)

            @block.gpsimd
            def _(e):
                run(ops["pool"], e)

            @block.sync
            def _(e):
                run(ops["sp"], e)
        self.ops = {e: [] for e in ENGS}


def _t5_bucket(n):
    n = np.maximum(n, 0)
    nf = np.maximum(n, 1).astype(np.float32)
    large = 16 + (np.log(nf / np.float32(16)) / np.float32(math.log(128 / 16)) * np.float32(16)).astype(np.int32)
    large = np.minimum(large, 31)
    return np.where(n < 16, n, large)


def _consts():
    c = {}
    c["ident"] = np.eye(128, dtype=np.float32).astype(ml_dtypes.bfloat16)
    e = np.arange(128)
    c["tri"] = (e[:, None] <= e[None, :]).astype(np.float32)
    c["caus"] = np.where(e[None, :] >= e[:, None], 0.0, NEG).astype(np.float32)
    keys = np.arange(S)
    c["e16"] = (keys[None, :] // 256 == np.arange(16)[:, None]).astype(np.float32).astype(ml_dtypes.bfloat16)
    npast = np.arange(16)[:, None]
    nn = np.arange(16)[None, :]
    c["cm"] = np.where(nn < npast, 0.0, -1e30).astype(np.float32).reshape(1, 256)
    c["pm"] = (nn < npast).astype(np.float32).reshape(1, 256)
    p = np.arange(128)[:, None]
    c["bmask"] = (p // 32 == (np.arange(256)[None, :] // 64)).astype(np.float32)
    c["hm"] = (p // 32 == np.arange(4)[None, :]).astype(np.float32)
    return c


def _bias_idx():
    k = np.arange(128)[:, None]
    q = np.arange(128)[None, :]
    idx_diag = _t5_bucket(q - k)
    idx_off1 = _t5_bucket(q + 128 - k)
    return idx_diag, idx_off1


def build(debug=False, stop_after=None):
    nc = bass.Bass("TRN2", target_bir_lowering=False)
    dr = lambda name, shape, dt, kind="Internal": nc.dram_tensor(name, list(shape), dt, kind=kind).ap()
    IN = "ExternalInput"
    x_d = dr("x", [S, D], F32, IN)
    norms_d = dr("norms", [L, 4, D], F32, IN)
    w_in_d = dr("w_in", [L, D, DIN], F32, IN)
    w_out_d = dr("w_out", [L, D, D], F32, IN)
    wg_d = dr("w_ffn_gate", [L, D, DFF], F32, IN)
    wu_d = dr("w_ffn_up", [L, D, DFF], F32, IN)
    wd_d = dr("w_ffn_down", [L, DFF, D], F32, IN)
    lcols_d = dr("lru_cols", [L, 2, 128, 8], F32, IN)
    lwa_d = dr("lru_wa", [L, 4, 64, 64], F32, IN)
    lwx_d = dr("lru_wx", [L, 4, 64, 64], F32, IN)
    gw2_d = dr("gla_gate_w2", [L, 16, 128], F32, IN)
    gb_d = dr("gla_gate_b", [L, 128, 1], F32, IN)
    gn_d = dr("gla_norm", [L, 256], F32, IN)
    rb31_d = dr("rb31", [1, 8], F32, IN)
    tdg_d = dr("tdg", [128, 8, 128], F32, IN)
    tof_d = dr("tof", [128, 8, 128], F32, IN)
    ident_d = dr("ident", [128, 128], BF16, IN)
    tri_d = dr("tri", [128, 128], F32, IN)
    caus_d = dr("caus", [128, 128], F32, IN)
    e16_d = dr("e16", [16, S], BF16, IN)
    cm_d = dr("cm", [1, 256], F32, IN)
    pm_d = dr("pm", [1, 256], F32, IN)
    bmask_d = dr("bmask", [128, 256], F32, IN)
    hm_d = dr("hm", [128, 4], F32, IN)
    out_d = dr("out", [S, D], F32, "ExternalOutput")

    dk = "ExternalOutput" if debug else "Internal"
    winb_d = dr("winb", [L, D, DIN], BF16)
    woutb_d = dr("woutb", [L, D, D], BF16)
    wgub_d = dr("wgub", [L, NFC, 128, 2, 8, 128], BF16)
    wdb_d = dr("wdb", [L, DFF, D], BF16)
    xs1_d = dr("xs1", [S, D], F32, dk)
    lruT_d = dr("lruT", [512, S], F32, dk)
    gqT_d = dr("gqT", [128, S], F32, dk)
    gkT_d = dr("gkT", [128, S], F32, dk)
    glrT_d = dr("glrT", [16, S], BF16, dk)
    mqT_d = dr("mqT", [512, S], BF16, dk)
    mkT_d = dr("mkT", [512, S], BF16, dk)
    gv_d = dr("gv", [S, 256], BF16, dk)
    gout_d = dr("gout", [S, 256], F32, dk)
    mvp_d = dr("mvp", [S, 520], BF16, dk)
    mixT_d = dr("mixT", [D, S], BF16, dk)

    with ExitStack() as st:
        kb = KB(nc, st)
        uid = [0]

        def sbt(ctx, shape, dt, name=None):
            uid[0] += 1
            return ctx.enter_context(nc.sbuf_tensor("%s_%d" % (name or "t", uid[0]), list(shape), dt))

        def pst(ctx, shape, dt, name=None):
            uid[0] += 1
            return ctx.enter_context(nc.psum_tensor("%s_%d" % (name or "p", uid[0]), list(shape), dt))

        rr = {}

        def dmaname(stream, n):
            i = rr.get(stream, 0)
            rr[stream] = i + 1
            return "%s%d" % (stream, i % n)

        def dma(eng, out, in_, reads=(), writes=(), stream="g", n=4):
            kb.op(eng, lambda e: e.dma_start(out=out, in_=in_), reads=reads, writes=writes, dma=dmaname(stream, n))

        def mm(out, lhsT, rhs, start, stop, reads, writes, **kw):
            kb.op("pe", lambda e: e.matmul(out, lhsT=lhsT, rhs=rhs, start=start, stop=stop, **kw),
                  reads=reads, writes=writes)

        def tp(out, in_, ident, reads, writes):
            kb.op("pe", lambda e: e.transpose(out, in_, ident), reads=reads, writes=writes)

        ident = sbt(st, [128, 128], BF16, "ident")
        dma("sp", ident[:], ident_d, writes=["ident"])

        def end_phase():
            kb.barrier()
            kb.emit()

        def phase0():
            for l in range(L):
                for kc in range(8):
                    r0 = kc * 128
                    for c0 in range(0, DIN, 944):
                        dma("pool", winb_d[l, r0:r0 + 128, c0:c0 + 944], w_in_d[l, r0:r0 + 128, c0:c0 + 944], writes=[("winb", l, kc, c0)], stream="cast", n=4)
                if l == 0:
                    continue
            for l in range(L):
                for kc in range(8):
                    r0 = kc * 128
                    dma("pool", woutb_d[l, r0:r0 + 128, :], w_out_d[l, r0:r0 + 128, :], stream="cast", n=4)
                for fc in range(NFC):
                    for gu, wsrc in enumerate((wg_d, wu_d)):
                        dma("pool", wgub_d[l, fc, :, gu, :, :],
                            wsrc[l].rearrange("(kc p) f -> p kc f", p=128)[:, :, fc * 128:(fc + 1) * 128],
                            stream="cast", n=4)
                    dma("pool", wdb_d[l, fc * 128:(fc + 1) * 128, :], wd_d[l, fc * 128:(fc + 1) * 128, :], stream="cast", n=4)
            kb.emit()

        def rstd_from_ssq(ssq, rstd, n, tag):
            kb.op("dve", lambda e: e.tensor_scalar(out=rstd, in0=ssq, scalar1=1.0 / n, scalar2=EPS, op0=ALU.mult, op1=ALU.add),
                  reads=[tag + "ssq"], writes=[tag + "rs"])
            kb.op("act", lambda e: e.sqrt(out=rstd, in_=rstd), reads=[tag + "rs"], writes=[tag + "rs"])
            kb.op("dve", lambda e: e.reciprocal(out=rstd, in_=rstd), reads=[tag + "rs"], writes=[tag + "rs"])

        def phase1(l, xin_d):
            with ExitStack() as ph:
                win = sbt(ph, [128, 8, DIN], BF16, "win")
                gpre = sbt(ph, [128, D], F32, "gpre")
                xt = [sbt(ph, [128, D], F32, "xt") for _ in range(3)]
                hb = [sbt(ph, [128, D], BF16, "hb") for _ in range(2)]
                junk = sbt(ph, [128, D], BF16, "junk")
                hT = [sbt(ph, [128, 8, 512], BF16, "hT") for _ in range(2)]
                ssq = sbt(ph, [128, 8], F32, "ssq")
                rst = sbt(ph, [128, 8], F32, "rst")
                sf = [sbt(ph, [128, 512], F32, "sf") for _ in range(6)]
                sbf = [sbt(ph, [128, 512], BF16, "sbf") for _ in range(6)]
                sgv = [sbt(ph, [128, 256], BF16, "sgv") for _ in range(4)]
                sgo = [sbt(ph, [128, 256], F32, "sgo") for _ in range(4)]
                smv = [sbt(ph, [128, 8, 65], BF16, "smv") for _ in range(4)]
                tps = [pst(ph, [128, D], BF16, "tps") for _ in range(2)]
                aps = [pst(ph, [128, 512], F32, "aps") for _ in range(5)]
                for kc in range(8):
                    dma("sp", win[:, kc, :], winb_d[l, kc * 128:(kc + 1) * 128, :], reads=[("winb", l, kc, c0_) for c0_ in range(0, DIN, 944)], writes=[("win", kc)], stream="w", n=8)
                dma("sp", gpre[:], norms_d[l, 0:1, :].partition_broadcast(128), writes=["gpre"])
                for i in range(4):
                    kb.op("pool", lambda e, i=i: e.memset(smv[i][:], 1.0), writes=[("smv", i)])

                WINK = [("win", kc_) for kc_ in range(8)]
                flist = [("lru", lruT_d, 0, 0, 128, F32), ("lru", lruT_d, 128, 128, 128, F32),
                         ("lru", lruT_d, 256, 256, 128, F32), ("lru", lruT_d, 384, 384, 128, F32),
                         ("gq", gqT_d, 0, 512, 128, F32), ("gk", gkT_d, 0, 640, 128, F32),
                         ("glr", glrT_d, 0, 1024, 16, BF16)]
                for i in range(4):
                    flist.append(("mq", mqT_d, i * 128, 1296 + i * 128, 128, BF16))
                for i in range(4):
                    flist.append(("mk", mkT_d, i * 128, 1808 + i * 128, 128, BF16))

                def load(t):
                    dma("sp", xt[t % 3][:], xin_d[t * 128:(t + 1) * 128, :], writes=[("xt", t % 3)], stream="x", n=3)

                NT = S // 128
                load(0)
                load(1)
                pi = 0
                ev = 0
                import os as _os
                _ng = int(_os.environ.get("P1_GROUPS", S // 512))
                _parts = int(_os.environ.get("P1_PARTS", 7))
                for g in range(_ng):
                    hTg = hT[g % 2]
                    for s in range(4):
                        t = g * 4 + s
                        if t + 2 < NT:
                            load(t + 2)
                        xs = xt[t % 3]
                        hbs = hb[t % 2]
                        c = t % 8
                        kb.op("act", lambda e, xs=xs, c=c: e.activation(out=junk[:], in_=xs[:], func=AF.Square, accum_out=ssq[:, c:c + 1]),
                              reads=[("xt", t % 3)], writes=["junk", "p1ssq"])
                        rstd_from_ssq(ssq[:, c:c + 1], rst[:, c:c + 1], D, "p1")
                        kb.op("dve", lambda e, xs=xs, hbs=hbs, c=c: e.scalar_tensor_tensor(out=hbs[:], in0=xs[:], scalar=rst[:, c:c + 1], in1=gpre[:], op0=ALU.mult, op1=ALU.mult),
                              reads=[("xt", t % 3), "p1rs", "gpre"], writes=[("hb", t % 2)])
                        tpp = tps[t % 2]
                        for kc in range(8):
                            tp(tpp[:, kc * 128:(kc + 1) * 128], hbs[:, kc * 128:(kc + 1) * 128], ident[:],
                               reads=[("hb", t % 2), "ident"], writes=[("tps", t % 2)])
                        kb.op("act", lambda e, tpp=tpp, hTg=hTg, s=s: e.copy(out=hTg[:, :, s * 128:(s + 1) * 128], in_=tpp[:].rearrange("p (k c) -> p k c", k=8)),
                              reads=[("tps", t % 2)], writes=[("hT", g % 2)])
                    for (nm, dst, drow, wcol, wid, dt) in (flist if _parts & 2 else []):
                        ps = aps[pi % 5]
                        pk = ("aps", pi % 5)
                        pi += 1
                        for kc in range(8):
                            mm(ps[0:wid, :], win[:, kc, wcol:wcol + wid], hTg[:, kc, :], kc == 0, kc == 7,
                               reads=WINK + [("hT", g % 2)], writes=[pk])
                        if dt == F32:
                            stg = sf[ev % 6]
                            sk = ("sf", ev % 6)
                        else:
                            stg = sbf[ev % 6]
                            sk = ("sbf", ev % 6)
                        eng = "act" if ev % 2 == 0 else "dve"
                        ev += 1
                        if nm == "mq":
                            if eng == "act":
                                kb.op("act", lambda e, stg=stg, ps=ps, wid=wid: e.mul(out=stg[0:wid, :], in_=ps[0:wid, :], mul=0.125), reads=[pk], writes=[sk])
                            else:
                                kb.op("dve", lambda e, stg=stg, ps=ps, wid=wid: e.tensor_scalar(out=stg[0:wid, :], in0=ps[0:wid, :], scalar1=0.125, scalar2=None, op0=ALU.mult), reads=[pk], writes=[sk])
                        else:
                            if eng == "act":
                                kb.op("act", lambda e, stg=stg, ps=ps, wid=wid: e.copy(out=stg[0:wid, :], in_=ps[0:wid, :]), reads=[pk], writes=[sk])
                            else:
                                kb.op("dve", lambda e, stg=stg, ps=ps, wid=wid: e.tensor_copy(out=stg[0:wid, :], in_=ps[0:wid, :]), reads=[pk], writes=[sk])
                        dma("sp", dst[drow:drow + wid, g * 512:(g + 1) * 512], stg[0:wid, :], reads=[sk], stream="o1", n=12)
                    for s in (range(4) if _parts & 4 else []):
                        t = g * 4 + s
                        _tm = int(_os.environ.get("TM_SKIP", 0))
                        if not _tm & 1:
                            ps = aps[pi % 5]
                            pk = ("aps", pi % 5)
                            pi += 1
                            ps2 = aps[pi % 5]
                            pk2 = ("aps", pi % 5)
                            pi += 1
                            for kc in range(8):
                                mm(ps[:, 0:256], hTg[:, kc, s * 128:(s + 1) * 128], win[:, kc, 768:1024], kc == 0, kc == 7,
                                   reads=WINK + [("hT", g % 2)], writes=[pk])
                            for kc in range(8):
                                mm(ps2[:, 0:256], hTg[:, kc, s * 128:(s + 1) * 128], win[:, kc, 1040:1296], kc == 0, kc == 7,
                                   reads=WINK + [("hT", g % 2)], writes=[pk2])
                            a, b = sgv[t % 4], sgo[t % 4]
                            kb.op("act", lambda e, a=a, ps=ps: e.copy(out=a[:], in_=ps[:, 0:256]), reads=[pk], writes=[("sgv", t % 4)])
                            kb.op("dve", lambda e, b=b, ps2=ps2: e.tensor_copy(out=b[:], in_=ps2[:, 0:256]), reads=[pk2], writes=[("sgo", t % 4)])
                            dma("sp", gv_d[t * 128:(t + 1) * 128, :], a[:], reads=[("sgv", t % 4)], stream="o1", n=12)
                            dma("sp", gout_d[t * 128:(t + 1) * 128, :], b[:], reads=[("sgo", t % 4)], stream="o1", n=12)
                        if not _tm & 2:
                            ps = aps[pi % 5]
                            pk = ("aps", pi % 5)
                            pi += 1
                            for kc in range(8):
                                mm(ps[:, :], hTg[:, kc, s * 128:(s + 1) * 128], win[:, kc, 2320:2832], kc == 0, kc == 7,
                                   reads=WINK + [("hT", g % 2)], writes=[pk])
                            m = smv[t % 4]
                            psv = ps[:].rearrange("p (h d) -> p h d", h=8)
                            if _tm & 4:
                                pass
                            elif t % 2:
                                kb.op("act", lambda e, m=m, psv=psv: e.copy(out=m[:, :, 0:64], in_=psv), reads=[pk], writes=[("smv", t % 4)])
                            else:
                                kb.op("dve", lambda e, m=m, psv=psv: e.tensor_copy(out=m[:, :, 0:64], in_=psv), reads=[pk], writes=[("smv", t % 4)])
                            if not _tm & 8:
                                dma("sp", mvp_d[t * 128:(t + 1) * 128, :], m[:].rearrange("p h d -> p (h d)"), reads=[("smv", t % 4)], stream="o1", n=12)
                if _os.environ.get("P1_TAILSTORE"):
                    dma("sp", lruT_d[0:128, 0:8], rst[:], reads=["p1rs"], stream="o1", n=12)
                end_phase()

        def phase2a_gen(l, ph):
            TB = 1024
            if True:
                cols = sbt(ph, [128, 2, 8], F32, "lcols")
                ccol = sbt(ph, [128, 2], F32, "ccol")
                wstage = sbt(ph, [128, 2, 2, 128], F32, "wstage")
                wbd = sbt(ph, [128, 2, 2, 128], BF16, "wbd")
                xin = [sbt(ph, [128, TB + 3], F32, "xin") for _ in range(2)]
                gin = [sbt(ph, [128, TB], F32, "gin") for _ in range(2)]
                xc = sbt(ph, [128, TB], F32, "xc")
                xcb = sbt(ph, [128, TB], BF16, "xcb")
                rr_ = sbt(ph, [128, TB], F32, "r")
                ii_ = sbt(ph, [128, TB], F32, "i")
                aa = sbt(ph, [128, TB], F32, "a")
                mmul = sbt(ph, [128, TB], F32, "mult")
                uu = sbt(ph, [128, TB], F32, "u")
                hh = [sbt(ph, [128, TB], F32, "h") for _ in range(2)]
                gt = sbt(ph, [128, TB], F32, "gt")
                gs = sbt(ph, [128, TB], F32, "gs")
                yb = [sbt(ph, [128, TB], BF16, "yb") for _ in range(2)]
                gps = [pst(ph, [128, 512], F32, "gps") for _ in range(2)]
                for h in range(2):
                    dma("sp", cols[:, h, :], lcols_d[l, h], writes=["lcols"])
                kb.op("pool", lambda e: e.memset(wstage[:], 0.0), writes=["wstage"])
                for ax, src in enumerate((lwa_d, lwx_d)):
                    for h in range(2):
                        for b in range(2):
                            dma("sp", wstage[b * 64:(b + 1) * 64, ax, h, b * 64:(b + 1) * 64], src[l, 2 * h + b],
                                reads=[], writes=["wstage"])
                kb.op("dve", lambda e: e.tensor_copy(out=wbd[:], in_=wstage[:]), reads=["wstage"], writes=["wbd"])
                kb.op("act", lambda e: e.activation(out=ccol[:], in_=cols[:, :, 7], func=AF.Exp, scale=-1.0), reads=["lcols"], writes=["ccol"])
                kb.op("act", lambda e: e.activation(out=ccol[:], in_=ccol[:], func=AF.Ln, bias=1.0), reads=["ccol"], writes=["ccol"])
                kb.op("dve", lambda e: e.tensor_scalar(out=ccol[:], in0=ccol[:], scalar1=-8.0, scalar2=None, op0=ALU.mult), reads=["ccol"], writes=["ccol"])

                nb = S // TB
                it = 0
                for h in range(2):
                    for b in range(nb):
                        t0 = b * TB
                        xi = xin[it % 2]
                        gi = gin[it % 2]
                        hcur = hh[it % 2]
                        hprev = hh[(it + 1) % 2]
                        ybs = yb[it % 2]
                        kx, kg, ky = ("xin", it % 2), ("gin", it % 2), ("yb", it % 2)
                        kh, khp = ("h", it % 2), ("h", (it + 1) % 2)
                        it += 1
                        if b == 0:
                            kb.op("pool", lambda e, xi=xi: e.memset(xi[:, 0:3], 0.0), writes=[kx])
                            dma("sp", xi[:, 3:], lruT_d[h * 128:(h + 1) * 128, 0:TB], writes=[kx], stream="x", n=3)
                        else:
                            dma("sp", xi[:], lruT_d[h * 128:(h + 1) * 128, t0 - 3:t0 + TB], writes=[kx], stream="x", n=3)
                        dma("sp", gi[:], lruT_d[256 + h * 128:256 + (h + 1) * 128, t0:t0 + TB], writes=[kg], stream="x", n=3)
                        yield
                        kb.op("dve", lambda e, xi=xi, h=h: e.tensor_scalar(out=xc[:], in0=xi[:, 3:TB + 3], scalar1=cols[:, h, 3:4], scalar2=cols[:, h, 4:5], op0=ALU.mult, op1=ALU.add),
                              reads=[kx, "lcols"], writes=["xc"])
                        for j in range(3):
                            kb.op("dve", lambda e, xi=xi, h=h, j=j: e.scalar_tensor_tensor(out=xc[:], in0=xi[:, j:TB + j], scalar=cols[:, h, j:j + 1], in1=xc[:], op0=ALU.mult, op1=ALU.add),
                                  reads=[kx, "lcols", "xc"], writes=["xc"])
                        yield
                        kb.op("pool", lambda e: e.tensor_copy(out=xcb[:], in_=xc[:]), reads=["xc"], writes=["xcb"])
                        for sblk in range(TB // 512):
                            cs = slice(sblk * 512, (sblk + 1) * 512)
                            pa, px = gps[0], gps[1]
                            ka, kx_ = ("gps", 0), ("gps", 1)
                            mm(pa[:], wbd[:, 0, h, :], xcb[:, cs], True, True, reads=["wbd", "xcb"], writes=[ka])
                            mm(px[:], wbd[:, 1, h, :], xcb[:, cs], True, True, reads=["wbd", "xcb"], writes=[kx_])
                            kb.op("act", lambda e, pa=pa, cs=cs, h=h: e.activation(out=rr_[:, cs], in_=pa[:], func=AF.Sigmoid, bias=cols[:, h, 5:6]),
                                  reads=[ka, "lcols"], writes=["r"])
                            kb.op("act", lambda e, px=px, cs=cs, h=h: e.activation(out=ii_[:, cs], in_=px[:], func=AF.Sigmoid, bias=cols[:, h, 6:7]),
                                  reads=[kx_, "lcols"], writes=["i"])
                        yield
                        kb.op("pool", lambda e, gi=gi: e.tensor_tensor(out=gt[:], in0=gi[:], in1=gi[:], op=ALU.mult), reads=[kg], writes=["gt"])
                        kb.op("pool", lambda e: e.tensor_scalar(out=gt[:], in0=gt[:], scalar1=0.044715, scalar2=1.0, op0=ALU.mult, op1=ALU.add), reads=["gt"], writes=["gt"])
                        kb.op("pool", lambda e, gi=gi: e.tensor_tensor(out=gt[:], in0=gt[:], in1=gi[:], op=ALU.mult), reads=["gt", kg], writes=["gt"])
                        kb.op("act", lambda e: e.activation(out=gs[:], in_=gt[:], func=AF.Sigmoid, scale=1.5957691216057308), reads=["gt"], writes=["gs"])
                        kb.op("pool", lambda e, gi=gi: e.tensor_tensor(out=gs[:], in0=gs[:], in1=gi[:], op=ALU.mult), reads=["gs", kg], writes=["gs"])
                        yield
                        kb.op("act", lambda e, h=h: e.activation(out=aa[:], in_=rr_[:], func=AF.Exp, scale=ccol[:, h:h + 1]), reads=["r", "ccol"], writes=["a"])
                        kb.op("pool", lambda e: e.tensor_tensor(out=mmul[:], in0=aa[:], in1=aa[:], op=ALU.mult), reads=["a"], writes=["mult"])
                        kb.op("act", lambda e: e.activation(out=mmul[:], in_=mmul[:], func=AF.Sqrt, scale=-1.0, bias=1.0), reads=["mult"], writes=["mult"])
                        yield
                        if b == 0:
                            kb.op("dve", lambda e: e.memset(mmul[:, 0:1], 1.0), reads=["mult"], writes=["mult"])
                        kb.op("dve", lambda e: e.tensor_tensor(out=uu[:], in0=ii_[:], in1=xc[:], op=ALU.mult), reads=["i", "xc"], writes=["u"])
                        kb.op("dve", lambda e: e.tensor_tensor(out=uu[:], in0=uu[:], in1=mmul[:], op=ALU.mult), reads=["u", "mult"], writes=["u"])
                        yield
                        if b == 0:
                            kb.op("dve", lambda e, hcur=hcur: e.tensor_tensor_scan(out=hcur[:], data0=aa[:], data1=uu[:], initial=0.0, op0=ALU.mult, op1=ALU.add),
                                  reads=["a", "u"], writes=[kh])
                        else:
                            kb.op("dve", lambda e, hcur=hcur, hprev=hprev: e.tensor_tensor_scan(out=hcur[:], data0=aa[:], data1=uu[:], initial=hprev[:, TB - 1:TB], op0=ALU.mult, op1=ALU.add),
                                  reads=["a", "u", khp], writes=[kh])
                        yield
                        kb.op("dve", lambda e, hcur=hcur, ybs=ybs: e.tensor_tensor(out=ybs[:], in0=hcur[:], in1=gs[:], op=ALU.mult), reads=[kh, "gs"], writes=[ky])
                        dma("sp", mixT_d[h * 128:(h + 1) * 128, t0:t0 + TB], ybs[:], reads=[ky], stream="o", n=4)
                        yield

        def phase2b_gen(l, ph):
            TB = 1024
            NCH = TB // 128
            if True:
                w2s = sbt(ph, [16, 128], F32, "w2s")
                w2b = sbt(ph, [16, 128], BF16, "w2b")
                negb = sbt(ph, [128, 1], F32, "negb")
                gn = sbt(ph, [128, 256], F32, "gn")
                tri = sbt(ph, [128, 128], F32, "tri")
                bmask = sbt(ph, [128, 256], F32, "bmask")
                hm = sbt(ph, [128, 4], F32, "hm")
                ones = sbt(ph, [128, 128], F32, "ones")
                glr = [sbt(ph, [16, TB], BF16, "glr") for _ in range(2)]
                qT = [sbt(ph, [128, TB], F32, "qT") for _ in range(2)]
                kT = [sbt(ph, [128, TB], F32, "kT") for _ in range(2)]
                vv = [sbt(ph, [128, NCH, 256], BF16, "vv") for _ in range(2)]
                go = [sbt(ph, [128, NCH, 256], F32, "go") for _ in range(2)]
                ee = sbt(ph, [128, TB], F32, "ee")
                cum = sbt(ph, [128, TB], F32, "cum")
                ex = sbt(ph, [128, TB], F32, "ex")
                dd = sbt(ph, [128, TB], F32, "dd")
                qd = sbt(ph, [128, TB], BF16, "qd")
                kdm = sbt(ph, [128, 4, TB], BF16, "kdm")
                kdec = sbt(ph, [128, TB], BF16, "kdec")
                dcol = sbt(ph, [128, NCH], F32, "dcol")
                kdtm = [sbt(ph, [128, 128], BF16, "kdtm") for _ in range(2)]
                am = [sbt(ph, [128, 4, 128], BF16, "am") for _ in range(2)]
                Sst = sbt(ph, [128, 256], F32, "Sst")
                Sbf = sbt(ph, [128, 256], BF16, "Sbf")
                kvm = sbt(ph, [128, 256], F32, "kvm")
                ob = sbt(ph, [128, NCH, 256], F32, "ob")
                osq = sbt(ph, [128, NCH, 256], F32, "osq")
                ssq = sbt(ph, [128, NCH * 4], F32, "gssq")
                rst = sbt(ph, [128, NCH * 4], F32, "grst")
                sg = sbt(ph, [128, NCH, 256], F32, "sg")
                yb = sbt(ph, [128, NCH, 256], BF16, "yb")
                yT = [sbt(ph, [128, 2, TB], BF16, "yT") for _ in range(2)]
                zps = [pst(ph, [128, 512], F32, "zps") for _ in range(1)]
                tps = pst(ph, [128, 1024], BF16, "gtps")
                aps_ = [pst(ph, [128, 512], F32, "gaps") for _ in range(1)]
                ops_ = [pst(ph, [128, 512], F32, "gops") for _ in range(2)]
                kvps = pst(ph, [128, 512], F32, "kvps")

                dma("sp", w2s[:], gw2_d[l], writes=["w2s"])
                kb.op("dve", lambda e: e.tensor_copy(out=w2b[:], in_=w2s[:]), reads=["w2s"], writes=["w2b"])
                dma("sp", negb[:], gb_d[l], writes=["negb"])
                kb.op("dve", lambda e: e.tensor_scalar(out=negb[:], in0=negb[:], scalar1=-1.0, scalar2=None, op0=ALU.mult), reads=["negb"], writes=["negb"])
                dma("sp", gn[:], gn_d[l:l + 1, :].partition_broadcast(128), writes=["gn"])
                dma("sp", tri[:], tri_d, writes=["tri"])
                dma("sp", bmask[:], bmask_d, writes=["bmask"])
                dma("sp", hm[:], hm_d, writes=["hm"])
                kb.op("pool", lambda e: e.memset(ones[:], 1.0), writes=["ones"])
                kb.op("pool", lambda e: e.memset(Sst[:], 0.0), writes=["Sst"])
                kb.op("pool", lambda e: e.memset(Sbf[:], 0.0), writes=["Sbf"])

                def load(b):
                    i = b % 2
                    t0 = b * TB
                    dma("sp", glr[i][:], glrT_d[:, t0:t0 + TB], writes=[("glr", i)], stream="x", n=3)
                    dma("sp", qT[i][:], gqT_d[:, t0:t0 + TB], writes=[("qT", i)], stream="x", n=3)
                    dma("sp", kT[i][:], gkT_d[:, t0:t0 + TB], writes=[("kT", i)], stream="x", n=3)
                    dma("sp", vv[i][:], gv_d[t0:t0 + TB, :].rearrange("(c p) f -> p c f", p=128), writes=[("vv", i)], stream="x", n=3)
                    dma("sp", go[i][:], gout_d[t0:t0 + TB, :].rearrange("(c p) f -> p c f", p=128), writes=[("go", i)], stream="x", n=3)

                nb = S // TB
                load(0)
                for b in range(nb):
                    if b + 1 < nb:
                        load(b + 1)
                    i = b % 2
                    t0 = b * TB
                    q_, k_, v_, g_, r_ = qT[i], kT[i], vv[i], go[i], glr[i]
                    kq, kk, kv, kg, kr = ("qT", i), ("kT", i), ("vv", i), ("go", i), ("glr", i)
                    for sblk in range(TB // 512):
                        cs = slice(sblk * 512, (sblk + 1) * 512)
                        zp = zps[0]
                        mm(zp[:], w2b[:], r_[:, cs], True, True, reads=["w2b", kr], writes=[("zps", 0)])
                        kb.op("act", lambda e, zp=zp, cs=cs: e.activation(out=ee[:, cs], in_=zp[:], func=AF.Exp, scale=-1.0, bias=negb[:]),
                              reads=[("zps", 0), "negb"], writes=["ee"])
                    yield
                    kb.op("act", lambda e: e.activation(out=ee[:], in_=ee[:], func=AF.Ln, bias=1.0), reads=["ee"], writes=["ee"])
                    for c in range(NCH):
                        cs = slice(c * 128, (c + 1) * 128)
                        kb.op("dve", lambda e, cs=cs: e.tensor_tensor_scan(out=cum[:, cs], data0=ones[:], data1=ee[:, cs], initial=0.0, op0=ALU.mult, op1=ALU.add),
                              reads=["ones", "ee"], writes=["cum"])
                    yield
                    kb.op("act", lambda e: e.activation(out=ex[:], in_=cum[:], func=AF.Exp, scale=-1.0 / 16.0), reads=["cum"], writes=["ex"])
                    kb.op("dve", lambda e, q_=q_: e.scalar_tensor_tensor(out=qd[:], in0=q_[:], scalar=32.0 ** -0.5, in1=ex[:], op0=ALU.mult, op1=ALU.mult),
                          reads=[kq, "ex"], writes=["qd"])
                    yield
                    kb.op("act", lambda e: e.activation(out=dcol[:], in_=cum[:].rearrange("p (c t) -> p c t", t=128)[:, :, 127], func=AF.Exp, scale=-1.0 / 16.0),
                          reads=["cum"], writes=["dcol"])
                    for c in range(NCH):
                        cs = slice(c * 128, (c + 1) * 128)
                        kb.op("pool", lambda e, cs=cs, c=c: e.tensor_scalar(out=dd[:, cs], in0=cum[:, cs], scalar1=cum[:, c * 128 + 127:c * 128 + 128], scalar2=None, op0=ALU.subtract),
                              reads=["cum"], writes=["dd"])
                    yield
                    kb.op("act", lambda e: e.activation(out=ex[:], in_=cum[:], func=AF.Exp, scale=1.0 / 16.0), reads=["cum", "qd"], writes=["ex"])
                    for hh_ in range(4):
                        kb.op("dve", lambda e, k_=k_, hh_=hh_: e.scalar_tensor_tensor(out=kdm[:, hh_, :], in0=k_[:], scalar=hm[:, hh_:hh_ + 1], in1=ex[:], op0=ALU.mult, op1=ALU.mult),
                              reads=[kk, "ex", "hm"], writes=["kdm"])
                    yield
                    kb.op("act", lambda e: e.activation(out=dd[:], in_=dd[:], func=AF.Exp, scale=1.0 / 16.0), reads=["dd"], writes=["dd"])
                    kb.op("pool", lambda e, k_=k_: e.tensor_tensor(out=kdec[:], in0=k_[:], in1=dd[:], op=ALU.mult), reads=[kk, "dd"], writes=["kdec"])
                    kb.op("act", lambda e, g_=g_: e.activation(out=sg[:], in_=g_[:], func=AF.Silu), reads=[kg], writes=["sg"])
                    for c in range(NCH):
                        cs = slice(c * 128, (c + 1) * 128)
                        j = c % 2
                        yield
                        tp(tps[:, j * 128:(j + 1) * 128], kdec[:, cs], ident[:], reads=["kdec", "ident"], writes=["gtps"])
                        kb.op("act", lambda e, j=j: e.copy(out=kdtm[j][:], in_=tps[:, j * 128:(j + 1) * 128]), reads=["gtps"], writes=[("kdtm", j)])
                        yield
                        ap_ = aps_[0]
                        for hh_ in range(4):
                            mm(ap_[:, hh_ * 128:(hh_ + 1) * 128], kdm[:, hh_, cs], qd[:, cs], True, True, reads=["kdm", "qd"], writes=[("gaps", 0)])
                        kb.op("dve", lambda e, ap_=ap_, j=j: e.tensor_tensor(out=am[j][:], in0=ap_[:].rearrange("p (h c) -> p h c", h=4),
                                                                             in1=tri[:].unsqueeze(1).broadcast_to([128, 4, 128]), op=ALU.mult),
                              reads=[("gaps", 0), "tri"], writes=[("am", j)])
                        yield
                        op_ = ops_[j]
                        mm(op_[:, 0:256], qd[:, cs], Sbf[:], True, True, reads=["qd", "Sbf"], writes=[("gops", j)])
                        for hh_ in range(4):
                            mm(op_[:, hh_ * 64:(hh_ + 1) * 64], am[j][:, hh_, :], v_[:, c, hh_ * 64:(hh_ + 1) * 64], False, True,
                               reads=[("am", j), kv], writes=[("gops", j)], skip_group_check=True)
                        kb.op("act", lambda e, op_=op_, c=c: e.copy(out=ob[:, c, :], in_=op_[:, 0:256]), reads=[("gops", j)], writes=["ob"])
                        yield
                        mm(kvps[:, 0:256], kdtm[j][:], v_[:, c, :], True, True, reads=[("kdtm", j), kv], writes=["kvps"])
                        kb.op("dve", lambda e: e.tensor_tensor(out=kvm[:], in0=kvps[:, 0:256], in1=bmask[:], op=ALU.mult), reads=["kvps", "bmask"], writes=["kvm"])
                        kb.op("dve", lambda e, c=c: e.scalar_tensor_tensor(out=Sst[:], in0=Sst[:], scalar=dcol[:, c:c + 1], in1=kvm[:], op0=ALU.mult, op1=ALU.add),
                              reads=["Sst", "dcol", "kvm"], writes=["Sst"])
                        kb.op("pool", lambda e: e.tensor_copy(out=Sbf[:], in_=Sst[:]), reads=["Sst"], writes=["Sbf"])
                    yield
                    kb.op("pool", lambda e: e.tensor_tensor(out=osq[:], in0=ob[:], in1=ob[:], op=ALU.mult), reads=["ob"], writes=["osq"])
                    kb.op("dve", lambda e: e.tensor_reduce(out=ssq[:], in_=osq[:].rearrange("p c (h v) -> p (c h) v", h=4), axis=AX.X, op=ALU.add),
                          reads=["osq"], writes=["p2bssq"])
                    rstd_from_ssq(ssq[:], rst[:], 64, "p2b")
                    kb.op("dve", lambda e: e.tensor_tensor(out=ob[:].rearrange("p c (h v) -> p (c h) v", h=4), in0=ob[:].rearrange("p c (h v) -> p (c h) v", h=4),
                                                           in1=rst[:].unsqueeze(2).broadcast_to([128, NCH * 4, 64]), op=ALU.mult),
                          reads=["ob", "p2brs"], writes=["ob"])
                    kb.op("pool", lambda e: e.tensor_tensor(out=sg[:], in0=sg[:], in1=gn[:].unsqueeze(1).broadcast_to([128, NCH, 256]), op=ALU.mult),
                          reads=["sg", "gn"], writes=["sg"])
                    kb.op("dve", lambda e: e.tensor_tensor(out=yb[:], in0=ob[:], in1=sg[:], op=ALU.mult), reads=["ob", "sg"], writes=["yb"])
                    yield
                    yTb = yT[b % 2]
                    for c in range(NCH):
                        for f in range(2):
                            jj = (c * 2 + f) % 4
                            tp(tps[:, jj * 128:(jj + 1) * 128], yb[:, c, f * 128:(f + 1) * 128], ident[:], reads=["yb", "ident"], writes=["gtps"])
                            kb.op("act", lambda e, jj=jj, c=c, f=f, yTb=yTb: e.copy(out=yTb[:, f, c * 128:(c + 1) * 128], in_=tps[:, jj * 128:(jj + 1) * 128]),
                                  reads=["gtps"], writes=[("yT", b % 2)])
                    dma("sp", mixT_d[256:512, t0:t0 + TB].rearrange("(f p) t -> p f t", p=128), yTb[:], reads=[("yT", b % 2)], stream="o", n=4)

        def phase2ab(l):
            with ExitStack() as ph:
                gens = [phase2a_gen(l, ph), phase2b_gen(l, ph)]
                while gens:
                    for g_ in list(gens):
                        try:
                            next(g_)
                        except StopIteration:
                            gens.remove(g_)
                end_phase()

        def phase2c(l):
            with ExitStack() as ph:
                kaug = sbt(ph, [128, 8, S], BF16, "kaug")
                vp = sbt(ph, [128, 32, 520], BF16, "vp")
                qaug = [sbt(ph, [128, 8, 512], BF16, "qaug") for _ in range(2)]
                km = sbt(ph, [64, 8, 16], F32, "km")
                kmb = sbt(ph, [64, 8, 16], BF16, "kmb")
                cm = sbt(ph, [128, 16, 16], F32, "cm")
                pm = sbt(ph, [128, 16, 16], F32, "pm")
                b31 = sbt(ph, [128, 8], F32, "b31")
                caus = sbt(ph, [128, 128], F32, "caus")
                tstage = sbt(ph, [128, 2, 8, 128], F32, "tstage")
                tdT = sbt(ph, [128, 8, 128], BF16, "tdT")
                toT = sbt(ph, [128, 8, 128], BF16, "toT")
                tomT = sbt(ph, [128, 8, 128], BF16, "tomT")
                zer = sbt(ph, [128, 260], BF16, "zer")
                gm = sbt(ph, [128, 4, 8, 16], F32, "gm")
                m8 = sbt(ph, [128, 4, 8, 8], F32, "m8")
                sel = sbt(ph, [128, 4, 8, 16], F32, "sel")
                mpad = [sbt(ph, [128, 4, 8, 80], BF16, "mpad") for _ in range(2)]
                pT = [sbt(ph, [128, 512], BF16, "pT") for _ in range(4)]
                rcp = sbt(ph, [128, 4], F32, "rcp")
                ymo = [sbt(ph, [128, 4, 512], BF16, "ymo") for _ in range(2)]
                ymT = [sbt(ph, [128, 4, 512], BF16, "ymT") for _ in range(2)]
                gps = pst(ph, [128, 512], F32, "mgps")
                mtps = [pst(ph, [128, 512], F32, "mtps") for _ in range(1)]
                mtps_b = [pst(ph, [128, 1024], BF16, "mtpsb") for _ in range(1)]
                sps = [pst(ph, [128, 512], F32, "sps") for _ in range(3)]
                accs_full = [pst(ph, [128, 512], F32, "acc") for _ in range(2)]
                accs = [a_[:, 0:260].rearrange("p (s d) -> p s d", s=4) for a_ in accs_full]

                for h in range(8):
                    dma("sp", kaug[0:64, h, :], mkT_d[h * 64:(h + 1) * 64, :], writes=["kaug"], stream="x", n=3)
                    dma("sp", kaug[64:80, h, :], e16_d, writes=["kaug"], stream="x", n=3)
                for c in range(4):
                    dma("sp", vp[:, c * 8:(c + 1) * 8, :], mvp_d[c * 1024:(c + 1) * 1024, :].rearrange("(c p) f -> p c f", p=128), writes=["vp"], stream="x", n=3)
                dma("sp", cm[:].rearrange("p a b -> p (a b)"), cm_d.partition_broadcast(128), writes=["cm"])
                dma("sp", pm[:].rearrange("p a b -> p (a b)"), pm_d.partition_broadcast(128), writes=["pm"])
                dma("sp", b31[:], rb31_d.partition_broadcast(128), writes=["b31"])
                dma("sp", caus[:], caus_d, writes=["caus"])
                dma("sp", tstage[:, 0], tdg_d, writes=["tstage"])
                dma("sp", tstage[:, 1], tof_d, writes=["tstage"])
                kb.op("dve", lambda e: e.tensor_tensor(out=tdT[:], in0=tstage[:, 0], in1=caus[:].unsqueeze(1).broadcast_to([128, 8, 128]), op=ALU.add),
                      reads=["tstage", "caus"], writes=["tdT"])
                kb.op("dve", lambda e: e.tensor_copy(out=toT[:], in_=tstage[:, 1]), reads=["tstage"], writes=["toT"])
                kb.op("dve", lambda e: e.tensor_tensor(out=tomT[:], in0=tstage[:, 1], in1=b31[:].unsqueeze(2).broadcast_to([128, 8, 128]), op=ALU.subtract),
                      reads=["tstage", "b31"], writes=["tomT"])
                kb.op("pool", lambda e: e.memset(zer[:], 0.0), writes=["zer"])
                for i in range(2):
                    kb.op("pool", lambda e, i=i: e.memset(mpad[i][:], 0.0), writes=[("mpad", i)])
                kb.op("dve", lambda e: e.tensor_reduce(out=km[:].rearrange("p h n -> p (h n)"), in_=kaug[0:64, :, :].rearrange("p h (n t) -> p (h n) t", t=256), axis=AX.X, op=ALU.add),
                      reads=["kaug"], writes=["km"])
                kb.op("dve", lambda e: e.tensor_scalar(out=kmb[:], in0=km[:], scalar1=1.0 / 256.0, scalar2=None, op0=ALU.mult), reads=["km"], writes=["kmb"])

                def loadq(G):
                    i = G % 2
                    dma("sp", qaug[i][0:64, :, :], mqT_d.rearrange("(h d) t -> d h t", d=64)[:, :, G * 512:(G + 1) * 512], writes=[("qaug", i)], stream="q", n=2)

                NG = S // 512
                loadq(0)
                si = 0
                ai = 0
                for G in range(NG):
                    if G + 1 < NG:
                        loadq(G + 1)
                    qi = G % 2
                    qa = qaug[qi]
                    kqa = ("qaug", qi)
                    mp = mpad[qi]
                    for s in range(4):
                        for h in range(8):
                            mm(gps[:, (s * 8 + h) * 16:(s * 8 + h + 1) * 16], qa[0:64, h, s * 128:(s + 1) * 128], kmb[:, h, :], True, True,
                               reads=[kqa, "kmb"], writes=["mgps"])
                    np0 = 2 * G
                    for a in range(2):
                        cmv = cm[:, np0 + a, :].unsqueeze(1).unsqueeze(1).broadcast_to([128, 2, 8, 16])
                        kb.op("dve", lambda e, cmv=cmv, a=a: e.tensor_tensor(out=gm[:, 2 * a:2 * a + 2], in0=gps[:].rearrange("p (s h n) -> p s h n", s=4, h=8)[:, 2 * a:2 * a + 2],
                                                                             in1=cmv, op=ALU.add),
                              reads=["mgps", "cm"], writes=["gm"])
                    for s in range(4):
                        for h in range(8):
                            kb.op("dve", lambda e, s=s, h=h: e.max(out=m8[:, s, h, :], in_=gm[:, s, h, :]), reads=["gm"], writes=["m8"])
                    kb.op("dve", lambda e: e.tensor_tensor(out=sel[:], in0=gm[:], in1=m8[:, :, :, 2:3].broadcast_to([128, 4, 8, 16]), op=ALU.is_ge),
                          reads=["gm", "m8"], writes=["sel"])
                    kb.op("dve", lambda e: e.tensor_scalar(out=sel[:], in0=sel[:], scalar1=-NEG, scalar2=NEG, op0=ALU.mult, op1=ALU.add), reads=["sel"], writes=["sel"])
                    kb.op("dve", lambda e: e.tensor_tensor(out=sel[:], in0=sel[:], in1=b31[:].unsqueeze(1).unsqueeze(3).broadcast_to([128, 4, 8, 16]), op=ALU.add),
                          reads=["sel", "b31"], writes=["sel"])
                    for a in range(2):
                        pmv = pm[:, np0 + a, :].unsqueeze(1).unsqueeze(1).broadcast_to([128, 2, 8, 16])
                        kb.op("dve", lambda e, pmv=pmv, mp=mp, a=a: e.tensor_tensor(out=mp[:, 2 * a:2 * a + 2, :, 64:80], in0=sel[:, 2 * a:2 * a + 2], in1=pmv, op=ALU.mult),
                              reads=["sel", "pm"], writes=[("mpad", qi)])
                    for h in range(8):
                        mt = mtps[0]
                        for s in range(4):
                            mm(mt[0:80, s * 128:(s + 1) * 128], mp[:, s, h, :], ident[:], True, True, reads=[("mpad", qi), "ident"], writes=[("mtps", 0)])
                        kb.op("act", lambda e, mt=mt, h=h, qa=qa: e.copy(out=qa[64:80, h, :], in_=mt[64:80, :]),
                              reads=[("mtps", 0)], writes=[kqa])
                    ym = ymo[G % 2]
                    nj = 4 * G + 4
                    DEPTH = 2

                    def stageA(h, j):
                        nonlocal si
                        acc = accs[h % 2]
                        ka = ("acc", h % 2)
                        if j == 0:
                            mm(accs_full[h % 2][:, 0:260], zer[:, 0:128], zer[:, :], True, True, reads=["zer"], writes=[ka])
                        r = j - 4 * G
                        c0 = max(r, 0) * 128
                        sp_ = sps[si % 3]
                        ks = ("sps", si % 3)
                        pt = pT[si % 4]
                        kp = ("pT", si % 4)
                        si += 1
                        mm(sp_[:, c0:512], kaug[0:80, h, j * 128:(j + 1) * 128], qa[0:80, h, c0:512], True, True,
                           reads=["kaug", kqa], writes=[ks])
                        if r == -1:
                            mm(sp_[:, 0:128], ident[:], tomT[:, h, :], False, True, reads=["ident", "tomT"], writes=[ks], skip_group_check=True)
                        if r >= 0:
                            mm(sp_[:, r * 128:(r + 1) * 128], ident[:], tdT[:, h, :], False, True, reads=["ident", "tdT"], writes=[ks], skip_group_check=True)
                            if r < 3:
                                tt = toT if r % 2 == 0 else tomT
                                mm(sp_[:, (r + 1) * 128:(r + 2) * 128], ident[:], tt[:, h, :], False, True, reads=["ident", "toT", "tomT"], writes=[ks], skip_group_check=True)
                        kb.op("act", lambda e, pt=pt, sp_=sp_, c0=c0: e.activation(out=pt[:, c0:512], in_=sp_[:, c0:512], func=AF.Exp),
                              reads=[ks], writes=[kp])
                        return (h, j, r, pt, kp, acc, ka)

                    def stageB(info):
                        h, j, r, pt, kp, acc, ka = info
                        for s in range(max(r, 0), 4):
                            mm(acc[:, s, :], pt[:, s * 128:(s + 1) * 128], vp[:, j, h * 65:(h + 1) * 65], False, True,
                               reads=[kp, "vp"], writes=[ka], skip_group_check=True)
                        if j == nj - 1:
                            kb.op("dve", lambda e, acc=acc: e.reciprocal(out=rcp[:], in_=acc[:, :, 64]), reads=[ka], writes=["rcp"])
                            kb.op("dve", lambda e, acc=acc, h=h, ym=ym: e.tensor_tensor(out=ym[:, :, h * 64:(h + 1) * 64], in0=acc[:, :, 0:64],
                                                                                  in1=rcp[:].unsqueeze(2).broadcast_to([128, 4, 64]), op=ALU.mult),
                                  reads=[ka, "rcp"], writes=[("ymo", G % 2)])

                    pend = []
                    for h in range(8):
                        for j in range(nj):
                            pend.append(stageA(h, j))
                            if len(pend) > DEPTH:
                                stageB(pend.pop(0))
                    while pend:
                        stageB(pend.pop(0))
                    yt = ymT[G % 2]
                    for s in range(4):
                        tpb = mtps_b[0]
                        for f in range(4):
                            tp(tpb[:, f * 128:(f + 1) * 128], ym[:, s, f * 128:(f + 1) * 128], ident[:], reads=[("ymo", G % 2), "ident"], writes=[("mtpsb", 0)])
                        kb.op("act", lambda e, tpb=tpb, yt=yt, s=s: e.copy(out=yt[:, :, s * 128:(s + 1) * 128], in_=tpb[:, 0:512].rearrange("p (f q) -> p f q", f=4)),
                              reads=[("mtpsb", 0)], writes=[("ymT", G % 2)])
                    dma("sp", mixT_d[512:1024, G * 512:(G + 1) * 512].rearrange("(f p) t -> p f t", p=128), yt[:], reads=[("ymT", G % 2)], stream="o", n=4)
                end_phase()

        def phase3(l, xin_d, xout_d):
            with ExitStack() as ph:
                wout = sbt(ph, [128, 8, D], BF16, "wout")
                wdn = sbt(ph, [128, NFC, D], BF16, "wdn")
                g3 = sbt(ph, [128, 3, D], F32, "g3")
                wgu = [sbt(ph, [128, 2, 8, 128], BF16, "wgu") for _ in range(8)]
                mixT = [sbt(ph, [128, 8, 512], BF16, "mixT") for _ in range(2)]
                xt = [sbt(ph, [128, D], F32, "xt3") for _ in range(2)]
                x1 = [sbt(ph, [128, D], F32, "x1") for _ in range(4)]
                tmp = [sbt(ph, [128, D], F32, "tmp3") for _ in range(2)]
                junk = sbt(ph, [128, D], BF16, "junk3")
                hb = [sbt(ph, [128, D], BF16, "hb3") for _ in range(4)]
                hT = sbt(ph, [128, 8, 512], BF16, "hT3")
                actT = sbt(ph, [128, NFC, 512], BF16, "actT")
                sgt = [sbt(ph, [128, 512], F32, "sgt") for _ in range(2)]
                ssq = sbt(ph, [128, 16], F32, "ssq3")
                rst = sbt(ph, [128, 16], F32, "rst3")
                xo = [sbt(ph, [128, D], F32, "xo") for _ in range(2)]
                ops_ = [pst(ph, [128, 2, 512], F32, "p3o") for _ in range(2)]
                tps = pst(ph, [128, D], BF16, "p3t")
                gus = [pst(ph, [128, 512], F32, "p3gu") for _ in range(3)]
                for kc in range(8):
                    dma("sp", wout[:, kc, :], woutb_d[l, kc * 128:(kc + 1) * 128, :], writes=["wout"], stream="w", n=4)
                for fc in range(NFC):
                    dma("sp", wdn[:, fc, :], wdb_d[l, fc * 128:(fc + 1) * 128, :], writes=["wdn"], stream="w", n=4)
                for i in range(3):
                    dma("sp", g3[:, i, :], norms_d[l, i + 1:i + 2, :].partition_broadcast(128), writes=["g3"])

                wi = [0]

                def loadw(fc):
                    i = wi[0] % 8
                    wi[0] += 1
                    dma("sp", wgu[i][:], wgub_d[l, fc], writes=[("wgu", i)], stream="wgu", n=8)
                    return i

                NG = S // 512
                sq = 0

                def loadg(G):
                    i = G % 2
                    dma("sp", mixT[i][:], mixT_d[:, G * 512:(G + 1) * 512].rearrange("(k p) t -> p k t", p=128), writes=[("mixT", i)], stream="m", n=2)

                def loadx(t):
                    dma("sp", xt[t % 2][:], xin_d[t * 128:(t + 1) * 128, :], writes=[("xt3", t % 2)], stream="x", n=3)

                loadg(0)
                loadx(0)
                wq = []
                PRE = 7
                for G in range(NG):
                    if G + 1 < NG:
                        loadg(G + 1)
                    mT = mixT[G % 2]
                    while len(wq) < PRE:
                        wq.append(loadw(len(wq)))
                    for s in range(4):
                        t = G * 4 + s
                        if t + 1 < S // 128:
                            loadx(t + 1)
                        xs = xt[t % 2]
                        x1s = x1[s]
                        op_ = ops_[s % 2]
                        ko = ("p3o", s % 2)
                        tm_ = tmp[s % 2]
                        kt = ("tmp3", s % 2)
                        for hf in range(2):
                            for kc in range(8):
                                mm(op_[:, hf, :], mT[:, kc, s * 128:(s + 1) * 128], wout[:, kc, hf * 512:(hf + 1) * 512], kc == 0, kc == 7,
                                   reads=[("mixT", G % 2), "wout"], writes=[ko])
                        c = sq % 16
                        sq += 1
                        kb.op("act", lambda e, c=c, op_=op_: e.activation(out=junk[:], in_=op_[:].rearrange("p a b -> p (a b)"), func=AF.Square, accum_out=ssq[:, c:c + 1]),
                              reads=[ko], writes=["junk3", "p3ssq"])
                        rstd_from_ssq(ssq[:, c:c + 1], rst[:, c:c + 1], D, "p3")
                        kb.op("dve", lambda e, c=c, op_=op_, tm_=tm_: e.scalar_tensor_tensor(out=tm_[:], in0=op_[:].rearrange("p a b -> p (a b)"), scalar=rst[:, c:c + 1], in1=g3[:, 0, :], op0=ALU.mult, op1=ALU.mult),
                              reads=[ko, "p3rs", "g3"], writes=[kt])
                        kb.op("pool", lambda e, xs=xs, x1s=x1s, tm_=tm_: e.tensor_tensor(out=x1s[:], in0=xs[:], in1=tm_[:], op=ALU.add),
                              reads=[("xt3", t % 2), kt], writes=[("x1", s)])
                        c2 = sq % 16
                        sq += 1
                        kb.op("act", lambda e, c2=c2, x1s=x1s: e.activation(out=junk[:], in_=x1s[:], func=AF.Square, accum_out=ssq[:, c2:c2 + 1]),
                              reads=[("x1", s)], writes=["junk3", "p3ssq"])
                        rstd_from_ssq(ssq[:, c2:c2 + 1], rst[:, c2:c2 + 1], D, "p3")
                        hbs = hb[s]
                        kb.op("dve", lambda e, c2=c2, x1s=x1s, hbs=hbs: e.scalar_tensor_tensor(out=hbs[:], in0=x1s[:], scalar=rst[:, c2:c2 + 1], in1=g3[:, 1, :], op0=ALU.mult, op1=ALU.mult),
                              reads=[("x1", s), "p3rs", "g3"], writes=[("hb3", s)])
                    for s in range(4):
                        hbs = hb[s]
                        for kc in range(8):
                            tp(tps[:, kc * 128:(kc + 1) * 128], hbs[:, kc * 128:(kc + 1) * 128], ident[:], reads=[("hb3", s), "ident"], writes=["p3t"])
                        kb.op("act", lambda e, s=s: e.copy(out=hT[:, :, s * 128:(s + 1) * 128], in_=tps[:].rearrange("p (k c) -> p k c", k=8)),
                              reads=["p3t"], writes=["hT3"])
                    for fc in range(NFC):
                        wslot = wq.pop(0)
                        nxt = fc + PRE
                        if nxt < NFC:
                            wq.append(loadw(nxt))
                        w_ = wgu[wslot]
                        gp, up = gus[(2 * fc) % 3], gus[(2 * fc + 1) % 3]
                        kgp, kup = ("p3gu", (2 * fc) % 3), ("p3gu", (2 * fc + 1) % 3)
                        for kc in range(8):
                            mm(gp[:], w_[:, 0, kc, :], hT[:, kc, :], kc == 0, kc == 7, reads=[("wgu", wslot), "hT3"], writes=[kgp])
                        for kc in range(8):
                            mm(up[:], w_[:, 1, kc, :], hT[:, kc, :], kc == 0, kc == 7, reads=[("wgu", wslot), "hT3"], writes=[kup])
                        sg_ = sgt[fc % 2]
                        kb.op("act", lambda e, sg_=sg_, gp=gp: e.activation(out=sg_[:], in_=gp[:], func=AF.Silu), reads=[kgp], writes=[("sgt", fc % 2)])
                        kb.op("dve", lambda e, sg_=sg_, up=up, fc=fc: e.tensor_tensor(out=actT[:, fc, :], in0=up[:], in1=sg_[:], op=ALU.mult),
                              reads=[kup, ("sgt", fc % 2)], writes=["actT"])
                    if G + 1 < NG:
                        while len(wq) < PRE:
                            wq.append(loadw(len(wq)))
                    for s in range(4):
                        t = G * 4 + s
                        op_ = ops_[s % 2]
                        ko = ("p3o", s % 2)
                        tm_ = tmp[s % 2]
                        kt = ("tmp3", s % 2)
                        for hf in range(2):
                            for fc in range(NFC):
                                mm(op_[:, hf, :], actT[:, fc, s * 128:(s + 1) * 128], wdn[:, fc, hf * 512:(hf + 1) * 512], fc == 0, fc == NFC - 1,
                                   reads=["actT", "wdn"], writes=[ko])
                        c = sq % 16
                        sq += 1
                        kb.op("act", lambda e, c=c, op_=op_: e.activation(out=junk[:], in_=op_[:].rearrange("p a b -> p (a b)"), func=AF.Square, accum_out=ssq[:, c:c + 1]),
                              reads=[ko], writes=["junk3", "p3ssq"])
                        rstd_from_ssq(ssq[:, c:c + 1], rst[:, c:c + 1], D, "p3")
                        kb.op("dve", lambda e, c=c, op_=op_, tm_=tm_: e.scalar_tensor_tensor(out=tm_[:], in0=op_[:].rearrange("p a b -> p (a b)"), scalar=rst[:, c:c + 1], in1=g3[:, 2, :], op0=ALU.mult, op1=ALU.mult),
                              reads=[ko, "p3rs", "g3"], writes=[kt])
                        xos = xo[t % 2]
                        kb.op("pool", lambda e, xos=xos, s=s, tm_=tm_: e.tensor_tensor(out=xos[:], in0=x1[s][:], in1=tm_[:], op=ALU.add),
                              reads=[("x1", s), kt], writes=[("xo", t % 2)])
                        dma("sp", xout_d[t * 128:(t + 1) * 128, :], xos[:], reads=[("xo", t % 2)], stream="o", n=4)
                end_phase()

        kb.barrier()
        import os as _os2
        if not _os2.environ.get("SKIP_P0"):
            phase0()
        done = stop_after == "p0"
        for l in range(L):
            if done:
                break
            xin = x_d if l == 0 else xs1_d
            xout = xs1_d if l == 0 else out_d
            for nm, fn in (("p1", lambda: phase1(l, xin)), ("p2b", lambda: phase2ab(l)),
                           ("p2c", lambda: phase2c(l)), ("p3", lambda: phase3(l, xin, xout))):
                fn()
                if stop_after == (l, nm):
                    done = True
                    break
            if done:
                break
        kb.barrier()
        kb.emit()

    return nc


def _host_inputs(inputs):
    f = lambda a: np.ascontiguousarray(np.asarray(a, dtype=np.float32))
    c = _consts()
    idx_diag, idx_off1 = _bias_idx()
    rel = f(inputs["rel_bias"])
    shared = {
        "norms": f(np.stack([inputs["pre_mix_norm"], inputs["post_mix_norm"], inputs["pre_ffn_norm"], inputs["post_ffn_norm"]], axis=1)),
        "w_in": f(inputs["w_in"]), "w_out": f(inputs["w_out"]),
        "w_ffn_gate": f(inputs["w_ffn_gate"]), "w_ffn_up": f(inputs["w_ffn_up"]), "w_ffn_down": f(inputs["w_ffn_down"]),
        "lru_wa": f(inputs["lru_wa"]), "lru_wx": f(inputs["lru_wx"]),
        "gla_gate_w2": f(inputs["gla_gate_w2"]),
        "gla_gate_b": f(np.asarray(inputs["gla_gate_b"]).reshape(L, 128, 1)),
        "gla_norm": f(inputs["gla_norm"]),
        "rb31": f(rel[31:32, :]),
        "tdg": f(np.transpose(rel[idx_diag], (0, 2, 1))),
        "tof": f(np.transpose(rel[idx_off1], (0, 2, 1))),
        "ident": c["ident"], "tri": c["tri"], "caus": c["caus"], "e16": c["e16"],
        "cm": c["cm"], "pm": c["pm"], "bmask": c["bmask"], "hm": c["hm"],
    }
    cw = np.transpose(np.asarray(inputs["lru_conv_w"], dtype=np.float32), (0, 2, 1))
    cols = np.concatenate([cw] + [np.asarray(inputs[k], dtype=np.float32)[:, :, None]
                                  for k in ("lru_conv_b", "lru_ba", "lru_bx", "lru_lambda")], axis=2)
    shared["lru_cols"] = f(cols.reshape(L, 2, 128, 8))
    x = np.asarray(inputs["x"], dtype=np.float32)
    return [dict(shared, x=np.ascontiguousarray(x[b])) for b in range(x.shape[0])]


_NC_CACHE = {}


def kernel(**inputs):
    in_maps = _host_inputs(inputs)
    if "nc" not in _NC_CACHE:
        _NC_CACHE["nc"] = build()
    nc = _NC_CACHE["nc"]
    n = len(in_maps)
    res = run_bass_kernel_spmd(nc, in_maps, core_ids=list(range(n)))
    return np.stack([np.asarray(r["out"], dtype=np.float32) for r in res.results], axis=0)
```

```python
from contextlib import ExitStack
import math
import numpy as np
import ml_dtypes
import concourse.bass as bass
import concourse.mybir as mybir
from concourse.bass_utils import run_bass_kernel_spmd

F32 = mybir.dt.float32
BF16 = mybir.dt.bfloat16
ALU = mybir.AluOpType
AF = mybir.ActivationFunctionType
AX = mybir.AxisListType

S = 4096
D = 1024
L = 2
DIN = 2832
DFF = 2816
NFC = DFF // 128
EPS = 1e-6
NEG = -30000.0
ENGS = ("pe", "act", "dve", "pool", "sp")


class KB:
    def __init__(self, nc, stack, sync_same=True):
        self.nc = nc
        self.stack = stack
        self.sync_same = sync_same
        self.ops = {e: [] for e in ENGS}
        self.sem = {}
        self.cnt = {}
        self.step = {}
        self.known = {e: {} for e in ENGS}
        self.lw = {}
        self.rd = {}
        for e in ENGS:
            self._dom(e, 1)

    def _dom(self, name, step):
        if name not in self.sem:
            self.sem[name] = self.stack.enter_context(self.nc.semaphore("s_" + name))
            self.cnt[name] = 0
            self.step[name] = step
        return name

    def op(self, eng, fn, reads=(), writes=(), dma=None):
        dom = eng if dma is None else self._dom("d_" + dma, 16)
        deps = {}

        def add(d):
            if d is not None and deps.get(d[0], 0) < d[1]:
                deps[d[0]] = d[1]

        for k in reads:
            add(self.lw.get(k))
        for k in writes:
            add(self.lw.get(k))
            for dm, c in self.rd.get(k, {}).items():
                add((dm, c))
        if dma is not None and self.cnt[dom] > 0:
            add((dom, self.cnt[dom]))
        kn = self.known[eng]
        for d, c in deps.items():
            if d == eng and (eng == "pe" or not self.sync_same):
                continue
            if kn.get(d, 0) >= c:
                continue
            self.ops[eng].append(("w", self.sem[d], c))
            kn[d] = c
        self.cnt[dom] += self.step[dom]
        me = (dom, self.cnt[dom])
        self.ops[eng].append(("o", fn, self.sem[dom], self.step[dom]))
        for k in writes:
            self.lw[k] = me
            self.rd[k] = {}
        for k in reads:
            r = self.rd.setdefault(k, {})
            if r.get(dom, 0) < me[1]:
                r[dom] = me[1]
        return me

    def barrier(self):
        for eng in ENGS:
            kn = self.known[eng]
            for dom, c in self.cnt.items():
                if c > 0 and dom != eng and kn.get(dom, 0) < c:
                    self.ops[eng].append(("w", self.sem[dom], c))
                    kn[dom] = c
        self.lw = {}
        self.rd = {}

    def emit(self):
        nc = self.nc
        ops = self.ops

        def run(lst, e):
            for it in lst:
                if it[0] == "w":
                    e.wait_ge(it[1], it[2])
                else:
                    it[1](e).then_inc(it[2], it[3])

        with nc.Block() as block:
            @block.tensor
            def _(e):
                run(ops["pe"], e)

            @block.scalar
            def _(e):
                run(ops["act"], e)

            @block.vector
            def _(e):
                run(ops["dve"], e)

            @block.gpsimd
            def _(e):
                run(ops["pool"], e)

            @block.sync
            def _(e):
                run(ops["sp"], e)
        self.ops = {e: [] for e in ENGS}


def _t5_bucket(n):
    n = np.maximum(n, 0)
    nf = np.maximum(n, 1).astype(np.float32)
    large = 16 + (np.log(nf / np.float32(16)) / np.float32(math.log(128 / 16)) * np.float32(16)).astype(np.int32)
    large = np.minimum(large, 31)
    return np.where(n < 16, n, large)


def _consts():
    c = {}
    c["ident"] = np.eye(128, dtype=np.float32).astype(ml_dtypes.bfloat16)
    e = np.arange(128)
    c["tri"] = (e[:, None] <= e[None, :]).astype(np.float32)
    c["caus"] = np.where(e[None, :] >= e[:, None], 0.0, NEG).astype(np.float32)
    keys = np.arange(S)
    c["e16"] = (keys[None, :] // 256 == np.arange(16)[:, None]).astype(np.float32).astype(ml_dtypes.bfloat16)
    npast = np.arange(16)[:, None]
    nn = np.arange(16)[None, :]
    c["cm"] = np.where(nn < npast, 0.0, -1e30).astype(np.float32).reshape(1, 256)
    c["pm"] = (nn < npast).astype(np.float32).reshape(1, 256)
    p = np.arange(128)[:, None]
    c["bmask"] = (p // 32 == (np.arange(256)[None, :] // 64)).astype(np.float32)
    c["hm"] = (p // 32 == np.arange(4)[None, :]).astype(np.float32)
    return c


def _bias_idx():
    k = np.arange(128)[:, None]
    q = np.arange(128)[None, :]
    idx_diag = _t5_bucket(q - k)
    idx_off1 = _t5_bucket(q + 128 - k)
    return idx_diag, idx_off1


def build(debug=False, stop_after=None):
    nc = bass.Bass("TRN2", target_bir_lowering=False)
    dr = lambda name, shape, dt, kind="Internal": nc.dram_tensor(name, list(shape), dt, kind=kind).ap()
    IN = "ExternalInput"
    x_d = dr("x", [S, D], F32, IN)
    norms_d = dr("norms", [L, 4, D], F32, IN)
    w_in_d = dr("w_in", [L, D, DIN], F32, IN)
    w_out_d = dr("w_out", [L, D, D], F32, IN)
    wg_d = dr("w_ffn_gate", [L, D, DFF], F32, IN)
    wu_d = dr("w_ffn_up", [L, D, DFF], F32, IN)
    wd_d = dr("w_ffn_down", [L, DFF, D], F32, IN)
    lcols_d = dr("lru_cols", [L, 2, 128, 8], F32, IN)
    lwa_d = dr("lru_wa", [L, 4, 64, 64], F32, IN)
    lwx_d = dr("lru_wx", [L, 4, 64, 64], F32, IN)
    gw2_d = dr("gla_gate_w2", [L, 16, 128], F32, IN)
    gb_d = dr("gla_gate_b", [L, 128, 1], F32, IN)
    gn_d = dr("gla_norm", [L, 256], F32, IN)
    rb31_d = dr("rb31", [1, 8], F32, IN)
    tdg_d = dr("tdg", [128, 8, 128], F32, IN)
    tof_d = dr("tof", [128, 8, 128], F32, IN)
    ident_d = dr("ident", [128, 128], BF16, IN)
    tri_d = dr("tri", [128, 128], F32, IN)
    caus_d = dr("caus", [128, 128], F32, IN)
    e16_d = dr("e16", [16, S], BF16, IN)
    cm_d = dr("cm", [1, 256], F32, IN)
    pm_d = dr("pm", [1, 256], F32, IN)
    bmask_d = dr("bmask", [128, 256], F32, IN)
    hm_d = dr("hm", [128, 4], F32, IN)
    out_d = dr("out", [S, D], F32, "ExternalOutput")

    dk = "ExternalOutput" if debug else "Internal"
    winb_d = dr("winb", [L, D, DIN], BF16)
    woutb_d = dr("woutb", [L, D, D], BF16)
    wgub_d = dr("wgub", [L, NFC, 128, 2, 8, 128], BF16)
    wdb_d = dr("wdb", [L, DFF, D], BF16)
    xs1_d = dr("xs1", [S, D], F32, dk)
    lruT_d = dr("lruT", [512, S], F32, dk)
    gqT_d = dr("gqT", [128, S], F32, dk)
    gkT_d = dr("gkT", [128, S], F32, dk)
    glrT_d = dr("glrT", [16, S], BF16, dk)
    mqT_d = dr("mqT", [512, S], BF16, dk)
    mkT_d = dr("mkT", [512, S], BF16, dk)
    gv_d = dr("gv", [S, 256], BF16, dk)
    gout_d = dr("gout", [S, 256], F32, dk)
    mvp_d = dr("mvp", [S, 520], BF16, dk)
    mixT_d = dr("mixT", [D, S], BF16, dk)

    with ExitStack() as st:
        kb = KB(nc, st)
        uid = [0]

        def sbt(ctx, shape, dt, name=None):
            uid[0] += 1
            return ctx.enter_context(nc.sbuf_tensor("%s_%d" % (name or "t", uid[0]), list(shape), dt))

        def pst(ctx, shape, dt, name=None):
            uid[0] += 1
            return ctx.enter_context(nc.psum_tensor("%s_%d" % (name or "p", uid[0]), list(shape), dt))

        rr = {}

        def dmaname(stream, n):
            i = rr.get(stream, 0)
            rr[stream] = i + 1
            return "%s%d" % (stream, i % n)

        def dma(eng, out, in_, reads=(), writes=(), stream="g", n=4):
            kb.op(eng, lambda e: e.dma_start(out=out, in_=in_), reads=reads, writes=writes, dma=dmaname(stream, n))

        def mm(out, lhsT, rhs, start, stop, reads, writes, **kw):
            kb.op("pe", lambda e: e.matmul(out, lhsT=lhsT, rhs=rhs, start=start, stop=stop, **kw),
                  reads=reads, writes=writes)

        def tp(out, in_, ident, reads, writes):
            kb.op("pe", lambda e: e.transpose(out, in_, ident), reads=reads, writes=writes)

        ident = sbt(st, [128, 128], BF16, "ident")
        dma("sp", ident[:], ident_d, writes=["ident"])

        def end_phase():
            kb.barrier()
            kb.emit()

        def phase0():
            for l in range(L):
                for kc in range(8):
                    r0 = kc * 128
                    for c0 in range(0, DIN, 944):
                        dma("pool", winb_d[l, r0:r0 + 128, c0:c0 + 944], w_in_d[l, r0:r0 + 128, c0:c0 + 944], writes=[("winb", l, kc, c0)], stream="cast", n=4)
                if l == 0:
                    continue
            for l in range(L):
                for kc in range(8):
                    r0 = kc * 128
                    dma("pool", woutb_d[l, r0:r0 + 128, :], w_out_d[l, r0:r0 + 128, :], stream="cast", n=4)
                for fc in range(NFC):
                    for gu, wsrc in enumerate((wg_d, wu_d)):
                        dma("pool", wgub_d[l, fc, :, gu, :, :],
                            wsrc[l].rearrange("(kc p) f -> p kc f", p=128)[:, :, fc * 128:(fc + 1) * 128],
                            stream="cast", n=4)
                    dma("pool", wdb_d[l, fc * 128:(fc + 1) * 128, :], wd_d[l, fc * 128:(fc + 1) * 128, :], stream="cast", n=4)
            kb.emit()

        def rstd_from_ssq(ssq, rstd, n, tag):
            kb.op("dve", lambda e: e.tensor_scalar(out=rstd, in0=ssq, scalar1=1.0 / n, scalar2=EPS, op0=ALU.mult, op1=ALU.add),
                  reads=[tag + "ssq"], writes=[tag + "rs"])
            kb.op("act", lambda e: e.sqrt(out=rstd, in_=rstd), reads=[tag + "rs"], writes=[tag + "rs"])
            kb.op("dve", lambda e: e.reciprocal(out=rstd, in_=rstd), reads=[tag + "rs"], writes=[tag + "rs"])

        def phase1(l, xin_d):
            with ExitStack() as ph:
                win = sbt(ph, [128, 8, DIN], BF16, "win")
                gpre = sbt(ph, [128, D], F32, "gpre")
                xt = [sbt(ph, [128, D], F32, "xt") for _ in range(8)]
                hb = [sbt(ph, [128, D], BF16, "hb") for _ in range(4)]
                junk = sbt(ph, [128, D], BF16, "junk")
                hT = [sbt(ph, [128, 8, 512], BF16, "hT") for _ in range(2)]
                ssq = sbt(ph, [128, 8], F32, "ssq")
                rst = sbt(ph, [128, 8], F32, "rst")
                sf = [sbt(ph, [128, 512], F32, "sf") for _ in range(6)]
                sbf = [sbt(ph, [128, 512], BF16, "sbf") for _ in range(6)]
                sgv = [sbt(ph, [128, 256], BF16, "sgv") for _ in range(4)]
                sgo = [sbt(ph, [128, 256], F32, "sgo") for _ in range(4)]
                smv = [sbt(ph, [128, 8, 65], BF16, "smv") for _ in range(4)]
                tps = [pst(ph, [128, D], BF16, "tps") for _ in range(2)]
                aps = [pst(ph, [128, 512], F32, "aps") for _ in range(5)]
                for kc in range(8):
                    dma("sp", win[:, kc, :], winb_d[l, kc * 128:(kc + 1) * 128, :], reads=[("winb", l, kc, c0_) for c0_ in range(0, DIN, 944)], writes=[("win", kc)], stream="w", n=8)
                dma("sp", gpre[:], norms_d[l, 0:1, :].partition_broadcast(128), writes=["gpre"])
                for i in range(4):
                    kb.op("pool", lambda e, i=i: e.memset(smv[i][:], 1.0), writes=[("smv", i)])

                WINK = [("win", kc_) for kc_ in range(8)]
                flist = [("lru", lruT_d, 0, 0, 128, F32), ("lru", lruT_d, 128, 128, 128, F32),
                         ("lru", lruT_d, 256, 256, 128, F32), ("lru", lruT_d, 384, 384, 128, F32),
                         ("gq", gqT_d, 0, 512, 128, F32), ("gk", gkT_d, 0, 640, 128, F32),
                         ("glr", glrT_d, 0, 1024, 16, BF16)]
                for i in range(4):
                    flist.append(("mq", mqT_d, i * 128, 1296 + i * 128, 128, BF16))
                for i in range(4):
                    flist.append(("mk", mkT_d, i * 128, 1808 + i * 128, 128, BF16))

                def load(t):
                    dma("sp", xt[t % 8][:], xin_d[t * 128:(t + 1) * 128, :], writes=[("xt", t % 8)], stream="x", n=4)

                NT = S // 128
                pi = 0
                ev = 0
                import os as _os
                _ng = int(_os.environ.get("P1_GROUPS", S // 512))
                _parts = int(_os.environ.get("P1_PARTS", 7))

                def chain(g):
                    for s in range(4):
                        t = g * 4 + s
                        xs = xt[t % 8]
                        hbs = hb[s]
                        c = t % 8
                        kb.op("act", lambda e, xs=xs, c=c: e.activation(out=junk[:], in_=xs[:], func=AF.Square, accum_out=ssq[:, c:c + 1]),
                              reads=[("xt", t % 8)], writes=["junk", "p1ssq"])
                        rstd_from_ssq(ssq[:, c:c + 1], rst[:, c:c + 1], D, "p1")
                        kb.op("dve", lambda e, xs=xs, hbs=hbs, c=c: e.scalar_tensor_tensor(out=hbs[:], in0=xs[:], scalar=rst[:, c:c + 1], in1=gpre[:], op0=ALU.mult, op1=ALU.mult),
                              reads=[("xt", t % 8), "p1rs", "gpre"], writes=[("hb", s)])

                def transp(g):
                    hTg_ = hT[g % 2]
                    for s in range(4):
                        t = g * 4 + s
                        hbs = hb[s]
                        tpp = tps[t % 2]
                        for kc in range(8):
                            tp(tpp[:, kc * 128:(kc + 1) * 128], hbs[:, kc * 128:(kc + 1) * 128], ident[:],
                               reads=[("hb", s), "ident"], writes=[("tps", t % 2)])
                        kb.op("act", lambda e, tpp=tpp, hTg_=hTg_, s=s: e.copy(out=hTg_[:, :, s * 128:(s + 1) * 128], in_=tpp[:].rearrange("p (k c) -> p k c", k=8)),
                              reads=[("tps", t % 2)], writes=[("hT", g % 2)])

                for t in range(8):
                    load(t)
                chain(0)
                transp(0)
                for g in range(_ng):
                    hTg = hT[g % 2]
                    if g + 1 < _ng:
                        chain(g + 1)
                    if g + 2 < _ng:
                        for s in range(4):
                            load((g + 2) * 4 + s)
                    for (nm, dst, drow, wcol, wid, dt) in (flist if _parts & 2 else []):
                        ps = aps[pi % 5]
                        pk = ("aps", pi % 5)
                        pi += 1
                        for kc in range(8):
                            mm(ps[0:wid, :], win[:, kc, wcol:wcol + wid], hTg[:, kc, :], kc == 0, kc == 7,
                               reads=WINK + [("hT", g % 2)], writes=[pk])
                        if dt == F32:
                            stg = sf[ev % 6]
                            sk = ("sf", ev % 6)
                        else:
                            stg = sbf[ev % 6]
                            sk = ("sbf", ev % 6)
                        eng = "act" if ev % 2 == 0 else "dve"
                        ev += 1
                        if nm == "mq":
                            if eng == "act":
                                kb.op("act", lambda e, stg=stg, ps=ps, wid=wid: e.mul(out=stg[0:wid, :], in_=ps[0:wid, :], mul=0.125), reads=[pk], writes=[sk])
                            else:
                                kb.op("dve", lambda e, stg=stg, ps=ps, wid=wid: e.tensor_scalar(out=stg[0:wid, :], in0=ps[0:wid, :], scalar1=0.125, scalar2=None, op0=ALU.mult), reads=[pk], writes=[sk])
                        else:
                            if eng == "act":
                                kb.op("act", lambda e, stg=stg, ps=ps, wid=wid: e.copy(out=stg[0:wid, :], in_=ps[0:wid, :]), reads=[pk], writes=[sk])
                            else:
                                kb.op("dve", lambda e, stg=stg, ps=ps, wid=wid: e.tensor_copy(out=stg[0:wid, :], in_=ps[0:wid, :]), reads=[pk], writes=[sk])
                        dma("sp", dst[drow:drow + wid, g * 512:(g + 1) * 512], stg[0:wid, :], reads=[sk], stream="o1", n=12)
                    if g + 1 < _ng:
                        transp(g + 1)
                    for s in (range(4) if _parts & 4 else []):
                        t = g * 4 + s
                        _tm = int(_os.environ.get("TM_SKIP", 0))
                        if not _tm & 1:
                            ps = aps[pi % 5]
                            pk = ("aps", pi % 5)
                            pi += 1
                            ps2 = aps[pi % 5]
                            pk2 = ("aps", pi % 5)
                            pi += 1
                            for kc in range(8):
                                mm(ps[:, 0:256], hTg[:, kc, s * 128:(s + 1) * 128], win[:, kc, 768:1024], kc == 0, kc == 7,
                                   reads=WINK + [("hT", g % 2)], writes=[pk])
                            for kc in range(8):
                                mm(ps2[:, 0:256], hTg[:, kc, s * 128:(s + 1) * 128], win[:, kc, 1040:1296], kc == 0, kc == 7,
                                   reads=WINK + [("hT", g % 2)], writes=[pk2])
                            a, b = sgv[t % 4], sgo[t % 4]
                            kb.op("act", lambda e, a=a, ps=ps: e.copy(out=a[:], in_=ps[:, 0:256]), reads=[pk], writes=[("sgv", t % 4)])
                            kb.op("dve", lambda e, b=b, ps2=ps2: e.tensor_copy(out=b[:], in_=ps2[:, 0:256]), reads=[pk2], writes=[("sgo", t % 4)])
                            dma("sp", gv_d[t * 128:(t + 1) * 128, :], a[:], reads=[("sgv", t % 4)], stream="o1", n=12)
                            dma("sp", gout_d[t * 128:(t + 1) * 128, :], b[:], reads=[("sgo", t % 4)], stream="o1", n=12)
                        if not _tm & 2:
                            ps = aps[pi % 5]
                            pk = ("aps", pi % 5)
                            pi += 1
                            for kc in range(8):
                                mm(ps[:, :], hTg[:, kc, s * 128:(s + 1) * 128], win[:, kc, 2320:2832], kc == 0, kc == 7,
                                   reads=WINK + [("hT", g % 2)], writes=[pk])
                            m = smv[t % 4]
                            psv = ps[:].rearrange("p (h d) -> p h d", h=8)
                            if _tm & 4:
                                pass
                            elif t % 2:
                                kb.op("act", lambda e, m=m, psv=psv: e.copy(out=m[:, :, 0:64], in_=psv), reads=[pk], writes=[("smv", t % 4)])
                            else:
                                kb.op("dve", lambda e, m=m, psv=psv: e.tensor_copy(out=m[:, :, 0:64], in_=psv), reads=[pk], writes=[("smv", t % 4)])
                            if not _tm & 8:
                                dma("sp", mvp_d[t * 128:(t + 1) * 128, :], m[:].rearrange("p h d -> p (h d)"), reads=[("smv", t % 4)], stream="o1", n=12)
                if _os.environ.get("P1_TAILSTORE"):
                    dma("sp", lruT_d[0:128, 0:8], rst[:], reads=["p1rs"], stream="o1", n=12)
                end_phase()

        def phase2a_gen(l, ph):
            TB = 1024
            if True:
                cols = sbt(ph, [128, 2, 8], F32, "lcols")
                ccol = sbt(ph, [128, 2], F32, "ccol")
                wstage = sbt(ph, [128, 2, 2, 128], F32, "wstage")
                wbd = sbt(ph, [128, 2, 2, 128], BF16, "wbd")
                xin = [sbt(ph, [128, TB + 3], F32, "xin") for _ in range(2)]
                gin = [sbt(ph, [128, TB], F32, "gin") for _ in range(2)]
                xc = sbt(ph, [128, TB], F32, "xc")
                xcb = sbt(ph, [128, TB], BF16, "xcb")
                rr_ = sbt(ph, [128, TB], F32, "r")
                ii_ = sbt(ph, [128, TB], F32, "i")
                aa = sbt(ph, [128, TB], F32, "a")
                mmul = sbt(ph, [128, TB], F32, "mult")
                uu = sbt(ph, [128, TB], F32, "u")
                hh = [sbt(ph, [128, TB], F32, "h") for _ in range(2)]
                gt = sbt(ph, [128, TB], F32, "gt")
                gs = sbt(ph, [128, TB], F32, "gs")
                yb = [sbt(ph, [128, TB], BF16, "yb") for _ in range(2)]
                gps = [pst(ph, [128, 512], F32, "gps") for _ in range(2)]
                for h in range(2):
                    dma("sp", cols[:, h, :], lcols_d[l, h], writes=["lcols"])
                kb.op("pool", lambda e: e.memset(wstage[:], 0.0), writes=["wstage"])
                for ax, src in enumerate((lwa_d, lwx_d)):
                    for h in range(2):
                        for b in range(2):
                            dma("sp", wstage[b * 64:(b + 1) * 64, ax, h, b * 64:(b + 1) * 64], src[l, 2 * h + b],
                                reads=[], writes=["wstage"])
                kb.op("dve", lambda e: e.tensor_copy(out=wbd[:], in_=wstage[:]), reads=["wstage"], writes=["wbd"])
                kb.op("act", lambda e: e.activation(out=ccol[:], in_=cols[:, :, 7], func=AF.Exp, scale=-1.0), reads=["lcols"], writes=["ccol"])
                kb.op("act", lambda e: e.activation(out=ccol[:], in_=ccol[:], func=AF.Ln, bias=1.0), reads=["ccol"], writes=["ccol"])
                kb.op("dve", lambda e: e.tensor_scalar(out=ccol[:], in0=ccol[:], scalar1=-8.0, scalar2=None, op0=ALU.mult), reads=["ccol"], writes=["ccol"])

                nb = S // TB
                it = 0
                for h in range(2):
                    for b in range(nb):
                        t0 = b * TB
                        xi = xin[it % 2]
                        gi = gin[it % 2]
                        hcur = hh[it % 2]
                        hprev = hh[(it + 1) % 2]
                        ybs = yb[it % 2]
                        kx, kg, ky = ("xin", it % 2), ("gin", it % 2), ("yb", it % 2)
                        kh, khp = ("h", it % 2), ("h", (it + 1) % 2)
                        it += 1
                        if b == 0:
                            kb.op("pool", lambda e, xi=xi: e.memset(xi[:, 0:3], 0.0), writes=[kx])
                            dma("sp", xi[:, 3:], lruT_d[h * 128:(h + 1) * 128, 0:TB], writes=[kx], stream="x", n=3)
                        else:
                            dma("sp", xi[:], lruT_d[h * 128:(h + 1) * 128, t0 - 3:t0 + TB], writes=[kx], stream="x", n=3)
                        dma("sp", gi[:], lruT_d[256 + h * 128:256 + (h + 1) * 128, t0:t0 + TB], writes=[kg], stream="x", n=3)
                        yield
                        kb.op("dve", lambda e, xi=xi, h=h: e.tensor_scalar(out=xc[:], in0=xi[:, 3:TB + 3], scalar1=cols[:, h, 3:4], scalar2=cols[:, h, 4:5], op0=ALU.mult, op1=ALU.add),
                              reads=[kx, "lcols"], writes=["xc"])
                        for j in range(3):
                            kb.op("dve", lambda e, xi=xi, h=h, j=j: e.scalar_tensor_tensor(out=xc[:], in0=xi[:, j:TB + j], scalar=cols[:, h, j:j + 1], in1=xc[:], op0=ALU.mult, op1=ALU.add),
                                  reads=[kx, "lcols", "xc"], writes=["xc"])
                        yield
                        kb.op("pool", lambda e: e.tensor_copy(out=xcb[:], in_=xc[:]), reads=["xc"], writes=["xcb"])
                        for sblk in range(TB // 512):
                            cs = slice(sblk * 512, (sblk + 1) * 512)
                            pa, px = gps[0], gps[1]
                            ka, kx_ = ("gps", 0), ("gps", 1)
                            mm(pa[:], wbd[:, 0, h, :], xcb[:, cs], True, True, reads=["wbd", "xcb"], writes=[ka])
                            mm(px[:], wbd[:, 1, h, :], xcb[:, cs], True, True, reads=["wbd", "xcb"], writes=[kx_])
                            kb.op("act", lambda e, pa=pa, cs=cs, h=h: e.activation(out=rr_[:, cs], in_=pa[:], func=AF.Sigmoid, bias=cols[:, h, 5:6]),
                                  reads=[ka, "lcols"], writes=["r"])
                            kb.op("act", lambda e, px=px, cs=cs, h=h: e.activation(out=ii_[:, cs], in_=px[:], func=AF.Sigmoid, bias=cols[:, h, 6:7]),
                                  reads=[kx_, "lcols"], writes=["i"])
                        yield
                        kb.op("pool", lambda e, gi=gi: e.tensor_tensor(out=gt[:], in0=gi[:], in1=gi[:], op=ALU.mult), reads=[kg], writes=["gt"])
                        kb.op("pool", lambda e: e.tensor_scalar(out=gt[:], in0=gt[:], scalar1=0.044715, scalar2=1.0, op0=ALU.mult, op1=ALU.add), reads=["gt"], writes=["gt"])
                        kb.op("pool", lambda e, gi=gi: e.tensor_tensor(out=gt[:], in0=gt[:], in1=gi[:], op=ALU.mult), reads=["gt", kg], writes=["gt"])
                        kb.op("act", lambda e: e.activation(out=gs[:], in_=gt[:], func=AF.Sigmoid, scale=1.5957691216057308), reads=["gt"], writes=["gs"])
                        kb.op("pool", lambda e, gi=gi: e.tensor_tensor(out=gs[:], in0=gs[:], in1=gi[:], op=ALU.mult), reads=["gs", kg], writes=["gs"])
                        yield
                        kb.op("act", lambda e, h=h: e.activation(out=aa[:], in_=rr_[:], func=AF.Exp, scale=ccol[:, h:h + 1]), reads=["r", "ccol"], writes=["a"])
                        kb.op("pool", lambda e: e.tensor_tensor(out=mmul[:], in0=aa[:], in1=aa[:], op=ALU.mult), reads=["a"], writes=["mult"])
                        kb.op("act", lambda e: e.activation(out=mmul[:], in_=mmul[:], func=AF.Sqrt, scale=-1.0, bias=1.0), reads=["mult"], writes=["mult"])
                        yield
                        if b == 0:
                            kb.op("dve", lambda e: e.memset(mmul[:, 0:1], 1.0), reads=["mult"], writes=["mult"])
                        kb.op("dve", lambda e: e.tensor_tensor(out=uu[:], in0=ii_[:], in1=xc[:], op=ALU.mult), reads=["i", "xc"], writes=["u"])
                        kb.op("dve", lambda e: e.tensor_tensor(out=uu[:], in0=uu[:], in1=mmul[:], op=ALU.mult), reads=["u", "mult"], writes=["u"])
                        yield
                        if b == 0:
                            kb.op("dve", lambda e, hcur=hcur: e.tensor_tensor_scan(out=hcur[:], data0=aa[:], data1=uu[:], initial=0.0, op0=ALU.mult, op1=ALU.add),
                                  reads=["a", "u"], writes=[kh])
                        else:
                            kb.op("dve", lambda e, hcur=hcur, hprev=hprev: e.tensor_tensor_scan(out=hcur[:], data0=aa[:], data1=uu[:], initial=hprev[:, TB - 1:TB], op0=ALU.mult, op1=ALU.add),
                                  reads=["a", "u", khp], writes=[kh])
                        yield
                        kb.op("dve", lambda e, hcur=hcur, ybs=ybs: e.tensor_tensor(out=ybs[:], in0=hcur[:], in1=gs[:], op=ALU.mult), reads=[kh, "gs"], writes=[ky])
                        dma("sp", mixT_d[h * 128:(h + 1) * 128, t0:t0 + TB], ybs[:], reads=[ky], stream="o", n=4)
                        yield

        def phase2b_gen(l, ph):
            TB = 1024
            NCH = TB // 128
            if True:
                w2s = sbt(ph, [16, 128], F32, "w2s")
                w2b = sbt(ph, [16, 128], BF16, "w2b")
                negb = sbt(ph, [128, 1], F32, "negb")
                gn = sbt(ph, [128, 256], F32, "gn")
                tri = sbt(ph, [128, 128], F32, "tri")
                bmask = sbt(ph, [128, 256], F32, "bmask")
                hm = sbt(ph, [128, 4], F32, "hm")
                ones = sbt(ph, [128, 128], F32, "ones")
                glr = [sbt(ph, [16, TB], BF16, "glr") for _ in range(2)]
                qT = [sbt(ph, [128, TB], F32, "qT") for _ in range(2)]
                kT = [sbt(ph, [128, TB], F32, "kT") for _ in range(2)]
                vv = [sbt(ph, [128, NCH, 256], BF16, "vv") for _ in range(2)]
                go = [sbt(ph, [128, NCH, 256], F32, "go") for _ in range(2)]
                ee = sbt(ph, [128, TB], F32, "ee")
                cum = sbt(ph, [128, TB], F32, "cum")
                ex = sbt(ph, [128, TB], F32, "ex")
                dd = sbt(ph, [128, TB], F32, "dd")
                qd = sbt(ph, [128, TB], BF16, "qd")
                kdm = sbt(ph, [128, 4, TB], BF16, "kdm")
                kdec = sbt(ph, [128, TB], BF16, "kdec")
                dcol = sbt(ph, [128, NCH], F32, "dcol")
                kdtm = [sbt(ph, [128, 128], BF16, "kdtm") for _ in range(2)]
                am = [sbt(ph, [128, 4, 128], BF16, "am") for _ in range(2)]
                Sst = sbt(ph, [128, 256], F32, "Sst")
                Sbf = sbt(ph, [128, 256], BF16, "Sbf")
                kvm = sbt(ph, [128, 256], F32, "kvm")
                ob = sbt(ph, [128, NCH, 256], F32, "ob")
                osq = sbt(ph, [128, NCH, 256], F32, "osq")
                ssq = sbt(ph, [128, NCH * 4], F32, "gssq")
                rst = sbt(ph, [128, NCH * 4], F32, "grst")
                sg = sbt(ph, [128, NCH, 256], F32, "sg")
                yb = sbt(ph, [128, NCH, 256], BF16, "yb")
                yT = [sbt(ph, [128, 2, TB], BF16, "yT") for _ in range(2)]
                zps = [pst(ph, [128, 512], F32, "zps") for _ in range(1)]
                tps = pst(ph, [128, 1024], BF16, "gtps")
                aps_ = [pst(ph, [128, 512], F32, "gaps") for _ in range(1)]
                ops_ = [pst(ph, [128, 512], F32, "gops") for _ in range(2)]
                kvps = pst(ph, [128, 512], F32, "kvps")

                dma("sp", w2s[:], gw2_d[l], writes=["w2s"])
                kb.op("dve", lambda e: e.tensor_copy(out=w2b[:], in_=w2s[:]), reads=["w2s"], writes=["w2b"])
                dma("sp", negb[:], gb_d[l], writes=["negb"])
                kb.op("dve", lambda e: e.tensor_scalar(out=negb[:], in0=negb[:], scalar1=-1.0, scalar2=None, op0=ALU.mult), reads=["negb"], writes=["negb"])
                dma("sp", gn[:], gn_d[l:l + 1, :].partition_broadcast(128), writes=["gn"])
                dma("sp", tri[:], tri_d, writes=["tri"])
                dma("sp", bmask[:], bmask_d, writes=["bmask"])
                dma("sp", hm[:], hm_d, writes=["hm"])
                kb.op("pool", lambda e: e.memset(ones[:], 1.0), writes=["ones"])
                kb.op("pool", lambda e: e.memset(Sst[:], 0.0), writes=["Sst"])
                kb.op("pool", lambda e: e.memset(Sbf[:], 0.0), writes=["Sbf"])

                def load(b):
                    i = b % 2
                    t0 = b * TB
                    dma("sp", glr[i][:], glrT_d[:, t0:t0 + TB], writes=[("glr", i)], stream="x", n=3)
                    dma("sp", qT[i][:], gqT_d[:, t0:t0 + TB], writes=[("qT", i)], stream="x", n=3)
                    dma("sp", kT[i][:], gkT_d[:, t0:t0 + TB], writes=[("kT", i)], stream="x", n=3)
                    dma("sp", vv[i][:], gv_d[t0:t0 + TB, :].rearrange("(c p) f -> p c f", p=128), writes=[("vv", i)], stream="x", n=3)
                    dma("sp", go[i][:], gout_d[t0:t0 + TB, :].rearrange("(c p) f -> p c f", p=128), writes=[("go", i)], stream="x", n=3)

                nb = S // TB
                load(0)
                for b in range(nb):
                    if b + 1 < nb:
                        load(b + 1)
                    i = b % 2
                    t0 = b * TB
                    q_, k_, v_, g_, r_ = qT[i], kT[i], vv[i], go[i], glr[i]
                    kq, kk, kv, kg, kr = ("qT", i), ("kT", i), ("vv", i), ("go", i), ("glr", i)
                    for sblk in range(TB // 512):
                        cs = slice(sblk * 512, (sblk + 1) * 512)
                        zp = zps[0]
                        mm(zp[:], w2b[:], r_[:, cs], True, True, reads=["w2b", kr], writes=[("zps", 0)])
                        kb.op("act", lambda e, zp=zp, cs=cs: e.activation(out=ee[:, cs], in_=zp[:], func=AF.Exp, scale=-1.0, bias=negb[:]),
                              reads=[("zps", 0), "negb"], writes=["ee"])
                    yield
                    kb.op("act", lambda e: e.activation(out=ee[:], in_=ee[:], func=AF.Ln, bias=1.0), reads=["ee"], writes=["ee"])
                    for c in range(NCH):
                        cs = slice(c * 128, (c + 1) * 128)
                        kb.op("dve", lambda e, cs=cs: e.tensor_tensor_scan(out=cum[:, cs], data0=ones[:], data1=ee[:, cs], initial=0.0, op0=ALU.mult, op1=ALU.add),
                              reads=["ones", "ee"], writes=["cum"])
                    yield
                    kb.op("act", lambda e: e.activation(out=ex[:], in_=cum[:], func=AF.Exp, scale=-1.0 / 16.0), reads=["cum"], writes=["ex"])
                    kb.op("dve", lambda e, q_=q_: e.scalar_tensor_tensor(out=qd[:], in0=q_[:], scalar=32.0 ** -0.5, in1=ex[:], op0=ALU.mult, op1=ALU.mult),
                          reads=[kq, "ex"], writes=["qd"])
                    yield
                    kb.op("act", lambda e: e.activation(out=dcol[:], in_=cum[:].rearrange("p (c t) -> p c t", t=128)[:, :, 127], func=AF.Exp, scale=-1.0 / 16.0),
                          reads=["cum"], writes=["dcol"])
                    for c in range(NCH):
                        cs = slice(c * 128, (c + 1) * 128)
                        kb.op("pool", lambda e, cs=cs, c=c: e.tensor_scalar(out=dd[:, cs], in0=cum[:, cs], scalar1=cum[:, c * 128 + 127:c * 128 + 128], scalar2=None, op0=ALU.subtract),
                              reads=["cum"], writes=["dd"])
                    yield
                    kb.op("act", lambda e: e.activation(out=ex[:], in_=cum[:], func=AF.Exp, scale=1.0 / 16.0), reads=["cum", "qd"], writes=["ex"])
                    for hh_ in range(4):
                        kb.op("dve", lambda e, k_=k_, hh_=hh_: e.scalar_tensor_tensor(out=kdm[:, hh_, :], in0=k_[:], scalar=hm[:, hh_:hh_ + 1], in1=ex[:], op0=ALU.mult, op1=ALU.mult),
                              reads=[kk, "ex", "hm"], writes=["kdm"])
                    yield
                    kb.op("act", lambda e: e.activation(out=dd[:], in_=dd[:], func=AF.Exp, scale=1.0 / 16.0), reads=["dd"], writes=["dd"])
                    kb.op("pool", lambda e, k_=k_: e.tensor_tensor(out=kdec[:], in0=k_[:], in1=dd[:], op=ALU.mult), reads=[kk, "dd"], writes=["kdec"])
                    kb.op("act", lambda e, g_=g_: e.activation(out=sg[:], in_=g_[:], func=AF.Silu), reads=[kg], writes=["sg"])
                    for c in range(NCH):
                        cs = slice(c * 128, (c + 1) * 128)
                        j = c % 2
                        yield
                        tp(tps[:, j * 128:(j + 1) * 128], kdec[:, cs], ident[:], reads=["kdec", "ident"], writes=["gtps"])
                        kb.op("act", lambda e, j=j: e.copy(out=kdtm[j][:], in_=tps[:, j * 128:(j + 1) * 128]), reads=["gtps"], writes=[("kdtm", j)])
                        yield
                        ap_ = aps_[0]
                        for hh_ in range(4):
                            mm(ap_[:, hh_ * 128:(hh_ + 1) * 128], kdm[:, hh_, cs], qd[:, cs], True, True, reads=["kdm", "qd"], writes=[("gaps", 0)])
                        kb.op("dve", lambda e, ap_=ap_, j=j: e.tensor_tensor(out=am[j][:], in0=ap_[:].rearrange("p (h c) -> p h c", h=4),
                                                                             in1=tri[:].unsqueeze(1).broadcast_to([128, 4, 128]), op=ALU.mult),
                              reads=[("gaps", 0), "tri"], writes=[("am", j)])
                        yield
                        op_ = ops_[j]
                        mm(op_[:, 0:256], qd[:, cs], Sbf[:], True, True, reads=["qd", "Sbf"], writes=[("gops", j)])
                        for hh_ in range(4):
                            mm(op_[:, hh_ * 64:(hh_ + 1) * 64], am[j][:, hh_, :], v_[:, c, hh_ * 64:(hh_ + 1) * 64], False, True,
                               reads=[("am", j), kv], writes=[("gops", j)], skip_group_check=True)
                        kb.op("act", lambda e, op_=op_, c=c: e.copy(out=ob[:, c, :], in_=op_[:, 0:256]), reads=[("gops", j)], writes=["ob"])
                        yield
                        mm(kvps[:, 0:256], kdtm[j][:], v_[:, c, :], True, True, reads=[("kdtm", j), kv], writes=["kvps"])
                        kb.op("dve", lambda e: e.tensor_tensor(out=kvm[:], in0=kvps[:, 0:256], in1=bmask[:], op=ALU.mult), reads=["kvps", "bmask"], writes=["kvm"])
                        kb.op("dve", lambda e, c=c: e.scalar_tensor_tensor(out=Sst[:], in0=Sst[:], scalar=dcol[:, c:c + 1], in1=kvm[:], op0=ALU.mult, op1=ALU.add),
                              reads=["Sst", "dcol", "kvm"], writes=["Sst"])
                        kb.op("pool", lambda e: e.tensor_copy(out=Sbf[:], in_=Sst[:]), reads=["Sst"], writes=["Sbf"])
                    yield
                    kb.op("pool", lambda e: e.tensor_tensor(out=osq[:], in0=ob[:], in1=ob[:], op=ALU.mult), reads=["ob"], writes=["osq"])
                    kb.op("dve", lambda e: e.tensor_reduce(out=ssq[:], in_=osq[:].rearrange("p c (h v) -> p (c h) v", h=4), axis=AX.X, op=ALU.add),
                          reads=["osq"], writes=["p2bssq"])
                    rstd_from_ssq(ssq[:], rst[:], 64, "p2b")
                    kb.op("dve", lambda e: e.tensor_tensor(out=ob[:].rearrange("p c (h v) -> p (c h) v", h=4), in0=ob[:].rearrange("p c (h v) -> p (c h) v", h=4),
                                                           in1=rst[:].unsqueeze(2).broadcast_to([128, NCH * 4, 64]), op=ALU.mult),
                          reads=["ob", "p2brs"], writes=["ob"])
                    kb.op("pool", lambda e: e.tensor_tensor(out=sg[:], in0=sg[:], in1=gn[:].unsqueeze(1).broadcast_to([128, NCH, 256]), op=ALU.mult),
                          reads=["sg", "gn"], writes=["sg"])
                    kb.op("dve", lambda e: e.tensor_tensor(out=yb[:], in0=ob[:], in1=sg[:], op=ALU.mult), reads=["ob", "sg"], writes=["yb"])
                    yield
                    yTb = yT[b % 2]
                    for c in range(NCH):
                        for f in range(2):
                            jj = (c * 2 + f) % 4
                            tp(tps[:, jj * 128:(jj + 1) * 128], yb[:, c, f * 128:(f + 1) * 128], ident[:], reads=["yb", "ident"], writes=["gtps"])
                            kb.op("act", lambda e, jj=jj, c=c, f=f, yTb=yTb: e.copy(out=yTb[:, f, c * 128:(c + 1) * 128], in_=tps[:, jj * 128:(jj + 1) * 128]),
                                  reads=["gtps"], writes=[("yT", b % 2)])
                    dma("sp", mixT_d[256:512, t0:t0 + TB].rearrange("(f p) t -> p f t", p=128), yTb[:], reads=[("yT", b % 2)], stream="o", n=4)

        def phase2ab(l):
            with ExitStack() as ph:
                gens = [phase2a_gen(l, ph), phase2b_gen(l, ph)]
                while gens:
                    for g_ in list(gens):
                        try:
                            next(g_)
                        except StopIteration:
                            gens.remove(g_)
                end_phase()

        def phase2c(l):
            with ExitStack() as ph:
                kaug = sbt(ph, [128, 8, S], BF16, "kaug")
                vp = sbt(ph, [128, 32, 520], BF16, "vp")
                qaug = [sbt(ph, [128, 8, 512], BF16, "qaug") for _ in range(2)]
                km = sbt(ph, [64, 8, 16], F32, "km")
                kmb = sbt(ph, [64, 8, 16], BF16, "kmb")
                cm = sbt(ph, [128, 16, 16], F32, "cm")
                pm = sbt(ph, [128, 16, 16], F32, "pm")
                b31 = sbt(ph, [128, 8], F32, "b31")
                caus = sbt(ph, [128, 128], F32, "caus")
                tstage = sbt(ph, [128, 2, 8, 128], F32, "tstage")
                tdT = sbt(ph, [128, 8, 128], BF16, "tdT")
                toT = sbt(ph, [128, 8, 128], BF16, "toT")
                tomT = sbt(ph, [128, 8, 128], BF16, "tomT")
                zer = sbt(ph, [128, 260], BF16, "zer")
                gm = sbt(ph, [128, 4, 8, 16], F32, "gm")
                m8 = sbt(ph, [128, 4, 8, 8], F32, "m8")
                sel = sbt(ph, [128, 4, 8, 16], F32, "sel")
                mpad = [sbt(ph, [128, 4, 8, 80], BF16, "mpad") for _ in range(2)]
                pT = [sbt(ph, [128, 512], BF16, "pT") for _ in range(4)]
                rcp = sbt(ph, [128, 4], F32, "rcp")
                ymo = [sbt(ph, [128, 4, 512], BF16, "ymo") for _ in range(2)]
                ymT = [sbt(ph, [128, 4, 512], BF16, "ymT") for _ in range(2)]
                gps = pst(ph, [128, 512], F32, "mgps")
                mtps = [pst(ph, [128, 512], F32, "mtps") for _ in range(1)]
                mtps_b = [pst(ph, [128, 1024], BF16, "mtpsb") for _ in range(1)]
                sps = [pst(ph, [128, 512], F32, "sps") for _ in range(3)]
                accs_full = [pst(ph, [128, 512], F32, "acc") for _ in range(2)]
                accs = [a_[:, 0:260].rearrange("p (s d) -> p s d", s=4) for a_ in accs_full]

                for h in range(8):
                    dma("sp", kaug[0:64, h, :], mkT_d[h * 64:(h + 1) * 64, :], writes=["kaug"], stream="x", n=3)
                    dma("sp", kaug[64:80, h, :], e16_d, writes=["kaug"], stream="x", n=3)
                for c in range(4):
                    dma("sp", vp[:, c * 8:(c + 1) * 8, :], mvp_d[c * 1024:(c + 1) * 1024, :].rearrange("(c p) f -> p c f", p=128), writes=["vp"], stream="x", n=3)
                dma("sp", cm[:].rearrange("p a b -> p (a b)"), cm_d.partition_broadcast(128), writes=["cm"])
                dma("sp", pm[:].rearrange("p a b -> p (a b)"), pm_d.partition_broadcast(128), writes=["pm"])
                dma("sp", b31[:], rb31_d.partition_broadcast(128), writes=["b31"])
                dma("sp", caus[:], caus_d, writes=["caus"])
                dma("sp", tstage[:, 0], tdg_d, writes=["tstage"])
                dma("sp", tstage[:, 1], tof_d, writes=["tstage"])
                kb.op("dve", lambda e: e.tensor_tensor(out=tdT[:], in0=tstage[:, 0], in1=caus[:].unsqueeze(1).broadcast_to([128, 8, 128]), op=ALU.add),
                      reads=["tstage", "caus"], writes=["tdT"])
                kb.op("dve", lambda e: e.tensor_copy(out=toT[:], in_=tstage[:, 1]), reads=["tstage"], writes=["toT"])
                kb.op("dve", lambda e: e.tensor_tensor(out=tomT[:], in0=tstage[:, 1], in1=b31[:].unsqueeze(2).broadcast_to([128, 8, 128]), op=ALU.subtract),
                      reads=["tstage", "b31"], writes=["tomT"])
                kb.op("pool", lambda e: e.memset(zer[:], 0.0), writes=["zer"])
                for i in range(2):
                    kb.op("pool", lambda e, i=i: e.memset(mpad[i][:], 0.0), writes=[("mpad", i)])
                kb.op("dve", lambda e: e.tensor_reduce(out=km[:].rearrange("p h n -> p (h n)"), in_=kaug[0:64, :, :].rearrange("p h (n t) -> p (h n) t", t=256), axis=AX.X, op=ALU.add),
                      reads=["kaug"], writes=["km"])
                kb.op("dve", lambda e: e.tensor_scalar(out=kmb[:], in0=km[:], scalar1=1.0 / 256.0, scalar2=None, op0=ALU.mult), reads=["km"], writes=["kmb"])

                def loadq(G):
                    i = G % 2
                    dma("sp", qaug[i][0:64, :, :], mqT_d.rearrange("(h d) t -> d h t", d=64)[:, :, G * 512:(G + 1) * 512], writes=[("qaug", i)], stream="q", n=2)

                NG = S // 512
                loadq(0)
                si = 0
                ai = 0
                for G in range(NG):
                    if G + 1 < NG:
                        loadq(G + 1)
                    qi = G % 2
                    qa = qaug[qi]
                    kqa = ("qaug", qi)
                    mp = mpad[qi]
                    for s in range(4):
                        for h in range(8):
                            mm(gps[:, (s * 8 + h) * 16:(s * 8 + h + 1) * 16], qa[0:64, h, s * 128:(s + 1) * 128], kmb[:, h, :], True, True,
                               reads=[kqa, "kmb"], writes=["mgps"])
                    np0 = 2 * G
                    for a in range(2):
                        cmv = cm[:, np0 + a, :].unsqueeze(1).unsqueeze(1).broadcast_to([128, 2, 8, 16])
                        kb.op("dve", lambda e, cmv=cmv, a=a: e.tensor_tensor(out=gm[:, 2 * a:2 * a + 2], in0=gps[:].rearrange("p (s h n) -> p s h n", s=4, h=8)[:, 2 * a:2 * a + 2],
                                                                             in1=cmv, op=ALU.add),
                              reads=["mgps", "cm"], writes=["gm"])
                    for s in range(4):
                        for h in range(8):
                            kb.op("dve", lambda e, s=s, h=h: e.max(out=m8[:, s, h, :], in_=gm[:, s, h, :]), reads=["gm"], writes=["m8"])
                    kb.op("dve", lambda e: e.tensor_tensor(out=sel[:], in0=gm[:], in1=m8[:, :, :, 2:3].broadcast_to([128, 4, 8, 16]), op=ALU.is_ge),
                          reads=["gm", "m8"], writes=["sel"])
                    kb.op("dve", lambda e: e.tensor_scalar(out=sel[:], in0=sel[:], scalar1=-NEG, scalar2=NEG, op0=ALU.mult, op1=ALU.add), reads=["sel"], writes=["sel"])
                    kb.op("dve", lambda e: e.tensor_tensor(out=sel[:], in0=sel[:], in1=b31[:].unsqueeze(1).unsqueeze(3).broadcast_to([128, 4, 8, 16]), op=ALU.add),
                          reads=["sel", "b31"], writes=["sel"])
                    for a in range(2):
                        pmv = pm[:, np0 + a, :].unsqueeze(1).unsqueeze(1).broadcast_to([128, 2, 8, 16])
                        kb.op("dve", lambda e, pmv=pmv, mp=mp, a=a: e.tensor_tensor(out=mp[:, 2 * a:2 * a + 2, :, 64:80], in0=sel[:, 2 * a:2 * a + 2], in1=pmv, op=ALU.mult),
                              reads=["sel", "pm"], writes=[("mpad", qi)])
                    for h in range(8):
                        mt = mtps[0]
                        for s in range(4):
                            mm(mt[0:80, s * 128:(s + 1) * 128], mp[:, s, h, :], ident[:], True, True, reads=[("mpad", qi), "ident"], writes=[("mtps", 0)])
                        kb.op("act", lambda e, mt=mt, h=h, qa=qa: e.copy(out=qa[64:80, h, :], in_=mt[64:80, :]),
                              reads=[("mtps", 0)], writes=[kqa])
                    ym = ymo[G % 2]
                    nj = 4 * G + 4
                    DEPTH = 2

                    def stageA(h, j):
                        nonlocal si
                        acc = accs[h % 2]
                        ka = ("acc", h % 2)
                        if j == 0:
                            mm(accs_full[h % 2][:, 0:260], zer[:, 0:128], zer[:, :], True, True, reads=["zer"], writes=[ka])
                        r = j - 4 * G
                        c0 = max(r, 0) * 128
                        sp_ = sps[si % 3]
                        ks = ("sps", si % 3)
                        pt = pT[si % 4]
                        kp = ("pT", si % 4)
                        si += 1
                        mm(sp_[:, c0:512], kaug[0:80, h, j * 128:(j + 1) * 128], qa[0:80, h, c0:512], True, True,
                           reads=["kaug", kqa], writes=[ks])
                        if r == -1:
                            mm(sp_[:, 0:128], ident[:], tomT[:, h, :], False, True, reads=["ident", "tomT"], writes=[ks], skip_group_check=True)
                        if r >= 0:
                            mm(sp_[:, r * 128:(r + 1) * 128], ident[:], tdT[:, h, :], False, True, reads=["ident", "tdT"], writes=[ks], skip_group_check=True)
                            if r < 3:
                                tt = toT if r % 2 == 0 else tomT
                                mm(sp_[:, (r + 1) * 128:(r + 2) * 128], ident[:], tt[:, h, :], False, True, reads=["ident", "toT", "tomT"], writes=[ks], skip_group_check=True)
                        kb.op("act", lambda e, pt=pt, sp_=sp_, c0=c0: e.activation(out=pt[:, c0:512], in_=sp_[:, c0:512], func=AF.Exp),
                              reads=[ks], writes=[kp])
                        return (h, j, r, pt, kp, acc, ka)

                    def stageB(info):
                        h, j, r, pt, kp, acc, ka = info
                        for s in range(max(r, 0), 4):
                            mm(acc[:, s, :], pt[:, s * 128:(s + 1) * 128], vp[:, j, h * 65:(h + 1) * 65], False, True,
                               reads=[kp, "vp"], writes=[ka], skip_group_check=True)
                        if j == nj - 1:
                            kb.op("dve", lambda e, acc=acc: e.reciprocal(out=rcp[:], in_=acc[:, :, 64]), reads=[ka], writes=["rcp"])
                            kb.op("dve", lambda e, acc=acc, h=h, ym=ym: e.tensor_tensor(out=ym[:, :, h * 64:(h + 1) * 64], in0=acc[:, :, 0:64],
                                                                                  in1=rcp[:].unsqueeze(2).broadcast_to([128, 4, 64]), op=ALU.mult),
                                  reads=[ka, "rcp"], writes=[("ymo", G % 2)])

                    pend = []
                    for h in range(8):
                        for j in range(nj):
                            pend.append(stageA(h, j))
                            if len(pend) > DEPTH:
                                stageB(pend.pop(0))
                    while pend:
                        stageB(pend.pop(0))
                    yt = ymT[G % 2]
                    for s in range(4):
                        tpb = mtps_b[0]
                        for f in range(4):
                            tp(tpb[:, f * 128:(f + 1) * 128], ym[:, s, f * 128:(f + 1) * 128], ident[:], reads=[("ymo", G % 2), "ident"], writes=[("mtpsb", 0)])
                        kb.op("act", lambda e, tpb=tpb, yt=yt, s=s: e.copy(out=yt[:, :, s * 128:(s + 1) * 128], in_=tpb[:, 0:512].rearrange("p (f q) -> p f q", f=4)),
                              reads=[("mtpsb", 0)], writes=[("ymT", G % 2)])
                    dma("sp", mixT_d[512:1024, G * 512:(G + 1) * 512].rearrange("(f p) t -> p f t", p=128), yt[:], reads=[("ymT", G % 2)], stream="o", n=4)
                end_phase()

        def phase3(l, xin_d, xout_d):
            with ExitStack() as ph:
                wout = sbt(ph, [128, 8, D], BF16, "wout")
                wdn = sbt(ph, [128, NFC, D], BF16, "wdn")
                g3 = sbt(ph, [128, 3, D], F32, "g3")
                wgu = [sbt(ph, [128, 2, 8, 128], BF16, "wgu") for _ in range(4)]
                mixT = [sbt(ph, [128, 8, 512], BF16, "mixT") for _ in range(2)]
                xt = [sbt(ph, [128, D], F32, "xt3") for _ in range(2)]
                x1 = [sbt(ph, [128, D], F32, "x1") for _ in range(8)]
                tmp = [sbt(ph, [128, D], F32, "tmp3") for _ in range(2)]
                junk = sbt(ph, [128, D], BF16, "junk3")
                hb = [sbt(ph, [128, D], BF16, "hb3") for _ in range(4)]
                hT = sbt(ph, [128, 8, 512], BF16, "hT3")
                actT = sbt(ph, [128, NFC, 512], BF16, "actT")
                sgt = [sbt(ph, [128, 512], F32, "sgt") for _ in range(2)]
                ssq = sbt(ph, [128, 16], F32, "ssq3")
                rst = sbt(ph, [128, 16], F32, "rst3")
                xo = [sbt(ph, [128, D], F32, "xo") for _ in range(2)]
                ops_ = [pst(ph, [128, 2, 512], F32, "p3o") for _ in range(2)]
                tps = pst(ph, [128, D], BF16, "p3t")
                gus = [pst(ph, [128, 512], F32, "p3gu") for _ in range(3)]
                for kc in range(8):
                    dma("sp", wout[:, kc, :], woutb_d[l, kc * 128:(kc + 1) * 128, :], writes=["wout"], stream="w", n=4)
                for fc in range(NFC):
                    dma("sp", wdn[:, fc, :], wdb_d[l, fc * 128:(fc + 1) * 128, :], writes=["wdn"], stream="w", n=4)
                for i in range(3):
                    dma("sp", g3[:, i, :], norms_d[l, i + 1:i + 2, :].partition_broadcast(128), writes=["g3"])

                wi = [0]

                def loadw(fc):
                    i = wi[0] % 4
                    wi[0] += 1
                    dma("sp", wgu[i][:], wgub_d[l, fc], writes=[("wgu", i)], stream="wgu", n=4)
                    return i

                NG = S // 512
                sq = 0

                def loadg(G):
                    i = G % 2
                    dma("sp", mixT[i][:], mixT_d[:, G * 512:(G + 1) * 512].rearrange("(k p) t -> p k t", p=128), writes=[("mixT", i)], stream="m", n=2)

                def loadx(t):
                    dma("sp", xt[t % 2][:], xin_d[t * 128:(t + 1) * 128, :], writes=[("xt3", t % 2)], stream="x", n=3)

                loadg(0)
                loadx(0)
                wq = []
                PRE = 3
                def p3_front(G):
                    nonlocal sq
                    mT = mixT[G % 2]
                    for s in range(4):
                        t = G * 4 + s
                        if t + 1 < S // 128:
                            loadx(t + 1)
                        xs = xt[t % 2]
                        x1s = x1[(G % 2) * 4 + s]
                        op_ = ops_[s % 2]
                        ko = ("p3o", s % 2)
                        tm_ = tmp[s % 2]
                        kt = ("tmp3", s % 2)
                        for hf in range(2):
                            for kc in range(8):
                                mm(op_[:, hf, :], mT[:, kc, s * 128:(s + 1) * 128], wout[:, kc, hf * 512:(hf + 1) * 512], kc == 0, kc == 7,
                                   reads=[("mixT", G % 2), "wout"], writes=[ko])
                        c = sq % 16
                        sq += 1
                        kb.op("act", lambda e, c=c, op_=op_: e.activation(out=junk[:], in_=op_[:].rearrange("p a b -> p (a b)"), func=AF.Square, accum_out=ssq[:, c:c + 1]),
                              reads=[ko], writes=["junk3", "p3ssq"])
                        rstd_from_ssq(ssq[:, c:c + 1], rst[:, c:c + 1], D, "p3")
                        kb.op("dve", lambda e, c=c, op_=op_, tm_=tm_: e.scalar_tensor_tensor(out=tm_[:], in0=op_[:].rearrange("p a b -> p (a b)"), scalar=rst[:, c:c + 1], in1=g3[:, 0, :], op0=ALU.mult, op1=ALU.mult),
                              reads=[ko, "p3rs", "g3"], writes=[kt])
                        kb.op("pool", lambda e, xs=xs, x1s=x1s, tm_=tm_: e.tensor_tensor(out=x1s[:], in0=xs[:], in1=tm_[:], op=ALU.add),
                              reads=[("xt3", t % 2), kt], writes=[("x1", (G % 2) * 4 + s)])
                        c2 = sq % 16
                        sq += 1
                        kb.op("act", lambda e, c2=c2, x1s=x1s: e.activation(out=junk[:], in_=x1s[:], func=AF.Square, accum_out=ssq[:, c2:c2 + 1]),
                              reads=[("x1", (G % 2) * 4 + s)], writes=["junk3", "p3ssq"])
                        rstd_from_ssq(ssq[:, c2:c2 + 1], rst[:, c2:c2 + 1], D, "p3")
                        hbs = hb[s]
                        kb.op("dve", lambda e, c2=c2, x1s=x1s, hbs=hbs: e.scalar_tensor_tensor(out=hbs[:], in0=x1s[:], scalar=rst[:, c2:c2 + 1], in1=g3[:, 1, :], op0=ALU.mult, op1=ALU.mult),
                              reads=[("x1", (G % 2) * 4 + s), "p3rs", "g3"], writes=[("hb3", s)])

                def p3_trans(G):
                    for s in range(4):
                        hbs = hb[s]
                        for kc in range(8):
                            tp(tps[:, kc * 128:(kc + 1) * 128], hbs[:, kc * 128:(kc + 1) * 128], ident[:], reads=[("hb3", s), "ident"], writes=["p3t"])
                        kb.op("act", lambda e, s=s: e.copy(out=hT[:, :, s * 128:(s + 1) * 128], in_=tps[:].rearrange("p (k c) -> p k c", k=8)),
                              reads=["p3t"], writes=["hT3"])

                def p3_gateup(G):
                    for fc in range(NFC):
                        wslot = wq.pop(0)
                        nxt = fc + PRE
                        if nxt < NFC:
                            wq.append(loadw(nxt))
                        w_ = wgu[wslot]
                        gp, up = gus[(2 * fc) % 3], gus[(2 * fc + 1) % 3]
                        kgp, kup = ("p3gu", (2 * fc) % 3), ("p3gu", (2 * fc + 1) % 3)
                        for kc in range(8):
                            mm(gp[:], w_[:, 0, kc, :], hT[:, kc, :], kc == 0, kc == 7, reads=[("wgu", wslot), "hT3"], writes=[kgp])
                        for kc in range(8):
                            mm(up[:], w_[:, 1, kc, :], hT[:, kc, :], kc == 0, kc == 7, reads=[("wgu", wslot), "hT3"], writes=[kup])
                        sg_ = sgt[fc % 2]
                        kb.op("act", lambda e, sg_=sg_, gp=gp: e.activation(out=sg_[:], in_=gp[:], func=AF.Silu), reads=[kgp], writes=[("sgt", fc % 2)])
                        kb.op("dve", lambda e, sg_=sg_, up=up, fc=fc: e.tensor_tensor(out=actT[:, fc, :], in0=up[:], in1=sg_[:], op=ALU.mult),
                              reads=[kup, ("sgt", fc % 2)], writes=["actT"])

                def p3_down(G):
                    nonlocal sq
                    for s in range(4):
                        t = G * 4 + s
                        op_ = ops_[s % 2]
                        ko = ("p3o", s % 2)
                        tm_ = tmp[s % 2]
                        kt = ("tmp3", s % 2)
                        for hf in range(2):
                            for fc in range(NFC):
                                mm(op_[:, hf, :], actT[:, fc, s * 128:(s + 1) * 128], wdn[:, fc, hf * 512:(hf + 1) * 512], fc == 0, fc == NFC - 1,
                                   reads=["actT", "wdn"], writes=[ko])
                        c = sq % 16
                        sq += 1
                        kb.op("act", lambda e, c=c, op_=op_: e.activation(out=junk[:], in_=op_[:].rearrange("p a b -> p (a b)"), func=AF.Square, accum_out=ssq[:, c:c + 1]),
                              reads=[ko], writes=["junk3", "p3ssq"])
                        rstd_from_ssq(ssq[:, c:c + 1], rst[:, c:c + 1], D, "p3")
                        kb.op("dve", lambda e, c=c, op_=op_, tm_=tm_: e.scalar_tensor_tensor(out=tm_[:], in0=op_[:].rearrange("p a b -> p (a b)"), scalar=rst[:, c:c + 1], in1=g3[:, 2, :], op0=ALU.mult, op1=ALU.mult),
                              reads=[ko, "p3rs", "g3"], writes=[kt])
                        xos = xo[t % 2]
                        kb.op("pool", lambda e, xos=xos, s=s, tm_=tm_: e.tensor_tensor(out=xos[:], in0=x1[(G % 2) * 4 + s][:], in1=tm_[:], op=ALU.add),
                              reads=[("x1", (G % 2) * 4 + s), kt], writes=[("xo", t % 2)])
                        dma("sp", xout_d[t * 128:(t + 1) * 128, :], xos[:], reads=[("xo", t % 2)], stream="o", n=4)

                for G in range(NG):
                    if G + 1 < NG:
                        loadg(G + 1)
                    while len(wq) < PRE:
                        wq.append(loadw(len(wq)))
                    p3_front(G)
                    if G > 0:
                        p3_down(G - 1)
                    p3_trans(G)
                    p3_gateup(G)
                p3_down(NG - 1)
                end_phase()

        kb.barrier()
        import os as _os2
        if not _os2.environ.get("SKIP_P0"):
            phase0()
        done = stop_after == "p0"
        for l in range(L):
            if done:
                break
            xin = x_d if l == 0 else xs1_d
            xout = xs1_d if l == 0 else out_d
            for nm, fn in (("p1", lambda: phase1(l, xin)), ("p2b", lambda: phase2ab(l)),
                           ("p2c", lambda: phase2c(l)), ("p3", lambda: phase3(l, xin, xout))):
                fn()
                if stop_after == (l, nm):
                    done = True
                    break
            if done:
                break
        kb.barrier()
        kb.emit()

    return nc


def _host_inputs(inputs):
    f = lambda a: np.ascontiguousarray(np.asarray(a, dtype=np.float32))
    c = _consts()
    idx_diag, idx_off1 = _bias_idx()
    rel = f(inputs["rel_bias"])
    shared = {
        "norms": f(np.stack([inputs["pre_mix_norm"], inputs["post_mix_norm"], inputs["pre_ffn_norm"], inputs["post_ffn_norm"]], axis=1)),
        "w_in": f(inputs["w_in"]), "w_out": f(inputs["w_out"]),
        "w_ffn_gate": f(inputs["w_ffn_gate"]), "w_ffn_up": f(inputs["w_ffn_up"]), "w_ffn_down": f(inputs["w_ffn_down"]),
        "lru_wa": f(inputs["lru_wa"]), "lru_wx": f(inputs["lru_wx"]),
        "gla_gate_w2": f(inputs["gla_gate_w2"]),
        "gla_gate_b": f(np.asarray(inputs["gla_gate_b"]).reshape(L, 128, 1)),
        "gla_norm": f(inputs["gla_norm"]),
        "rb31": f(rel[31:32, :]),
        "tdg": f(np.transpose(rel[idx_diag], (0, 2, 1))),
        "tof": f(np.transpose(rel[idx_off1], (0, 2, 1))),
        "ident": c["ident"], "tri": c["tri"], "caus": c["caus"], "e16": c["e16"],
        "cm": c["cm"], "pm": c["pm"], "bmask": c["bmask"], "hm": c["hm"],
    }
    cw = np.transpose(np.asarray(inputs["lru_conv_w"], dtype=np.float32), (0, 2, 1))
    cols = np.concatenate([cw] + [np.asarray(inputs[k], dtype=np.float32)[:, :, None]
                                  for k in ("lru_conv_b", "lru_ba", "lru_bx", "lru_lambda")], axis=2)
    shared["lru_cols"] = f(cols.reshape(L, 2, 128, 8))
    x = np.asarray(inputs["x"], dtype=np.float32)
    return [dict(shared, x=np.ascontiguousarray(x[b])) for b in range(x.shape[0])]


_NC_CACHE = {}


def kernel(**inputs):
    in_maps = _host_inputs(inputs)
    if "nc" not in _NC_CACHE:
        _NC_CACHE["nc"] = build()
    nc = _NC_CACHE["nc"]
    n = len(in_maps)
    res = run_bass_kernel_spmd(nc, in_maps, core_ids=list(range(n)))
    return np.stack([np.asarray(r["out"], dtype=np.float32) for r in res.results], axis=0)
```

```python
from contextlib import ExitStack
import math
import numpy as np
import ml_dtypes
import concourse.bass as bass
import concourse.mybir as mybir
from concourse.bass_utils import run_bass_kernel_spmd

F32 = mybir.dt.float32
BF16 = mybir.dt.bfloat16
ALU = mybir.AluOpType
AF = mybir.ActivationFunctionType
AX = mybir.AxisListType

S = 4096
D = 1024
L = 2
DIN = 2832
DFF = 2816
NFC = DFF // 128
EPS = 1e-6
NEG = -30000.0
ENGS = ("pe", "act", "dve", "pool", "sp")


class KB:
    def __init__(self, nc, stack, sync_same=True):
        self.nc = nc
        self.stack = stack
        self.sync_same = sync_same
        self.ops = {e: [] for e in ENGS}
        self.sem = {}
        self.cnt = {}
        self.step = {}
        self.known = {e: {} for e in ENGS}
        self.lw = {}
        self.rd = {}
        for e in ENGS:
            self._dom(e, 1)

    def _dom(self, name, step):
        if name not in self.sem:
            self.sem[name] = self.stack.enter_context(self.nc.semaphore("s_" + name))
            self.cnt[name] = 0
            self.step[name] = step
        return name

    def op(self, eng, fn, reads=(), writes=(), dma=None):
        dom = eng if dma is None else self._dom("d_" + dma, 16)
        deps = {}

        def add(d):
            if d is not None and deps.get(d[0], 0) < d[1]:
                deps[d[0]] = d[1]

        for k in reads:
            add(self.lw.get(k))
        for k in writes:
            add(self.lw.get(k))
            for dm, c in self.rd.get(k, {}).items():
                add((dm, c))
        if dma is not None and self.cnt[dom] > 0:
            add((dom, self.cnt[dom]))
        kn = self.known[eng]
        for d, c in deps.items():
            if d == eng and (eng == "pe" or not self.sync_same):
                continue
            if kn.get(d, 0) >= c:
                continue
            self.ops[eng].append(("w", self.sem[d], c))
            kn[d] = c
        self.cnt[dom] += self.step[dom]
        me = (dom, self.cnt[dom])
        self.ops[eng].append(("o", fn, self.sem[dom], self.step[dom]))
        for k in writes:
            self.lw[k] = me
            self.rd[k] = {}
        for k in reads:
            r = self.rd.setdefault(k, {})
            if r.get(dom, 0) < me[1]:
                r[dom] = me[1]
        return me

    def barrier(self):
        for eng in ENGS:
            kn = self.known[eng]
            for dom, c in self.cnt.items():
                if c > 0 and dom != eng and kn.get(dom, 0) < c:
                    self.ops[eng].append(("w", self.sem[dom], c))
                    kn[dom] = c
        self.lw = {}
        self.rd = {}

    def emit(self):
        nc = self.nc
        ops = self.ops

        def run(lst, e):
            for it in lst:
                if it[0] == "w":
                    e.wait_ge(it[1], it[2])
                else:
                    it[1](e).then_inc(it[2], it[3])

        with nc.Block() as block:
            @block.tensor
            def _(e):
                run(ops["pe"], e)

            @block.scalar
            def _(e):
                run(ops["act"], e)

            @block.vector
            def _(e):
                run(ops["dve"], e)

            @block.gpsimd
            def _(e):
                run(ops["pool"], e)

            @block.sync
            def _(e):
                run(ops["sp"], e)
        self.ops = {e: [] for e in ENGS}


def _t5_bucket(n):
    n = np.maximum(n, 0)
    nf = np.maximum(n, 1).astype(np.float32)
    large = 16 + (np.log(nf / np.float32(16)) / np.float32(math.log(128 / 16)) * np.float32(16)).astype(np.int32)
    large = np.minimum(large, 31)
    return np.where(n < 16, n, large)


def _consts():
    c = {}
    c["ident"] = np.eye(128, dtype=np.float32).astype(ml_dtypes.bfloat16)
    e = np.arange(128)
    c["tri"] = (e[:, None] <= e[None, :]).astype(np.float32)
    c["caus"] = np.where(e[None, :] >= e[:, None], 0.0, NEG).astype(np.float32)
    keys = np.arange(S)
    c["e16"] = (keys[None, :] // 256 == np.arange(16)[:, None]).astype(np.float32).astype(ml_dtypes.bfloat16)
    npast = np.arange(16)[:, None]
    nn = np.arange(16)[None, :]
    c["cm"] = np.where(nn < npast, 0.0, -1e30).astype(np.float32).reshape(1, 256)
    c["pm"] = (nn < npast).astype(np.float32).reshape(1, 256)
    p = np.arange(128)[:, None]
    c["bmask"] = (p // 32 == (np.arange(256)[None, :] // 64)).astype(np.float32)
    c["hm"] = (p // 32 == np.arange(4)[None, :]).astype(np.float32)
    return c


def _bias_idx():
    k = np.arange(128)[:, None]
    q = np.arange(128)[None, :]
    idx_diag = _t5_bucket(q - k)
    idx_off1 = _t5_bucket(q + 128 - k)
    return idx_diag, idx_off1


def build(debug=False, stop_after=None):
    nc = bass.Bass("TRN2", target_bir_lowering=False)
    dr = lambda name, shape, dt, kind="Internal": nc.dram_tensor(name, list(shape), dt, kind=kind).ap()
    IN = "ExternalInput"
    x_d = dr("x", [S, D], F32, IN)
    norms_d = dr("norms", [L, 4, D], F32, IN)
    w_in_d = dr("w_in", [L, D, DIN], F32, IN)
    w_out_d = dr("w_out", [L, D, D], F32, IN)
    wg_d = dr("w_ffn_gate", [L, D, DFF], F32, IN)
    wu_d = dr("w_ffn_up", [L, D, DFF], F32, IN)
    wd_d = dr("w_ffn_down", [L, DFF, D], F32, IN)
    lcols_d = dr("lru_cols", [L, 2, 128, 8], F32, IN)
    lwa_d = dr("lru_wa", [L, 4, 64, 64], F32, IN)
    lwx_d = dr("lru_wx", [L, 4, 64, 64], F32, IN)
    gw2_d = dr("gla_gate_w2", [L, 16, 128], F32, IN)
    gb_d = dr("gla_gate_b", [L, 128, 1], F32, IN)
    gn_d = dr("gla_norm", [L, 256], F32, IN)
    rb31_d = dr("rb31", [1, 8], F32, IN)
    tdg_d = dr("tdg", [128, 8, 128], F32, IN)
    tof_d = dr("tof", [128, 8, 128], F32, IN)
    ident_d = dr("ident", [128, 128], BF16, IN)
    tri_d = dr("tri", [128, 128], F32, IN)
    caus_d = dr("caus", [128, 128], F32, IN)
    e16_d = dr("e16", [16, S], BF16, IN)
    cm_d = dr("cm", [1, 256], F32, IN)
    pm_d = dr("pm", [1, 256], F32, IN)
    bmask_d = dr("bmask", [128, 256], F32, IN)
    hm_d = dr("hm", [128, 4], F32, IN)
    out_d = dr("out", [S, D], F32, "ExternalOutput")

    dk = "ExternalOutput" if debug else "Internal"
    winb_d = dr("winb", [L, D, DIN], BF16)
    woutb_d = dr("woutb", [L, D, D], BF16)
    wgub_d = dr("wgub", [L, NFC, 128, 2, 8, 128], BF16)
    wdb_d = dr("wdb", [L, DFF, D], BF16)
    xs1_d = dr("xs1", [S, D], F32, dk)
    lruT_d = dr("lruT", [512, S], F32, dk)
    gqT_d = dr("gqT", [128, S], F32, dk)
    gkT_d = dr("gkT", [128, S], F32, dk)
    glrT_d = dr("glrT", [16, S], BF16, dk)
    mqT_d = dr("mqT", [512, S], BF16, dk)
    mkT_d = dr("mkT", [512, S], BF16, dk)
    gv_d = dr("gv", [S, 256], BF16, dk)
    gout_d = dr("gout", [S, 256], F32, dk)
    mvp_d = dr("mvp", [S, 520], BF16, dk)
    mixT_d = dr("mixT", [D, S], BF16, dk)

    with ExitStack() as st:
        kb = KB(nc, st)
        uid = [0]

        def sbt(ctx, shape, dt, name=None):
            uid[0] += 1
            return ctx.enter_context(nc.sbuf_tensor("%s_%d" % (name or "t", uid[0]), list(shape), dt))

        def pst(ctx, shape, dt, name=None):
            uid[0] += 1
            return ctx.enter_context(nc.psum_tensor("%s_%d" % (name or "p", uid[0]), list(shape), dt))

        rr = {}

        def dmaname(stream, n):
            i = rr.get(stream, 0)
            rr[stream] = i + 1
            return "%s%d" % (stream, i % n)

        def dma(eng, out, in_, reads=(), writes=(), stream="g", n=4):
            kb.op(eng, lambda e: e.dma_start(out=out, in_=in_), reads=reads, writes=writes, dma=dmaname(stream, n))

        def mm(out, lhsT, rhs, start, stop, reads, writes, **kw):
            kb.op("pe", lambda e: e.matmul(out, lhsT=lhsT, rhs=rhs, start=start, stop=stop, **kw),
                  reads=reads, writes=writes)

        def tp(out, in_, ident, reads, writes):
            kb.op("pe", lambda e: e.transpose(out, in_, ident), reads=reads, writes=writes)

        ident = sbt(st, [128, 128], BF16, "ident")
        dma("sp", ident[:], ident_d, writes=["ident"])

        def end_phase():
            kb.barrier()
            kb.emit()

        def phase0():
            for l in range(L):
                for kc in range(8):
                    r0 = kc * 128
                    for c0 in range(0, DIN, 944):
                        dma("pool", winb_d[l, r0:r0 + 128, c0:c0 + 944], w_in_d[l, r0:r0 + 128, c0:c0 + 944], writes=[("winb", l, kc, c0)], stream="cast", n=4)
                if l == 0:
                    continue
            for l in range(L):
                for kc in range(8):
                    r0 = kc * 128
                    dma("pool", woutb_d[l, r0:r0 + 128, :], w_out_d[l, r0:r0 + 128, :], stream="cast", n=4)
                for fc in range(NFC):
                    for gu, wsrc in enumerate((wg_d, wu_d)):
                        dma("pool", wgub_d[l, fc, :, gu, :, :],
                            wsrc[l].rearrange("(kc p) f -> p kc f", p=128)[:, :, fc * 128:(fc + 1) * 128],
                            stream="cast", n=4)
                    dma("pool", wdb_d[l, fc * 128:(fc + 1) * 128, :], wd_d[l, fc * 128:(fc + 1) * 128, :], stream="cast", n=4)
            kb.emit()

        def rstd_from_ssq(ssq, rstd, n, tag):
            kb.op("dve", lambda e: e.tensor_scalar(out=rstd, in0=ssq, scalar1=1.0 / n, scalar2=EPS, op0=ALU.mult, op1=ALU.add),
                  reads=[tag + "ssq"], writes=[tag + "rs"])
            kb.op("act", lambda e: e.sqrt(out=rstd, in_=rstd), reads=[tag + "rs"], writes=[tag + "rs"])
            kb.op("dve", lambda e: e.reciprocal(out=rstd, in_=rstd), reads=[tag + "rs"], writes=[tag + "rs"])

        def phase1(l, xin_d):
            with ExitStack() as ph:
                win = sbt(ph, [128, 8, DIN], BF16, "win")
                gpre = sbt(ph, [128, D], F32, "gpre")
                xt = [sbt(ph, [128, D], F32, "xt") for _ in range(8)]
                hb = [sbt(ph, [128, D], BF16, "hb") for _ in range(4)]
                junk = sbt(ph, [128, D], BF16, "junk")
                hT = [sbt(ph, [128, 8, 512], BF16, "hT") for _ in range(2)]
                ssq = sbt(ph, [128, 8], F32, "ssq")
                rst = sbt(ph, [128, 8], F32, "rst")
                sf = [sbt(ph, [128, 512], F32, "sf") for _ in range(6)]
                sbf = [sbt(ph, [128, 512], BF16, "sbf") for _ in range(6)]
                sgv = [sbt(ph, [128, 256], BF16, "sgv") for _ in range(4)]
                sgo = [sbt(ph, [128, 256], F32, "sgo") for _ in range(4)]
                smv = [sbt(ph, [128, 8, 65], BF16, "smv") for _ in range(4)]
                tps = [pst(ph, [128, D], BF16, "tps") for _ in range(2)]
                aps = [pst(ph, [128, 512], F32, "aps") for _ in range(5)]
                for kc in range(8):
                    dma("sp", win[:, kc, :], winb_d[l, kc * 128:(kc + 1) * 128, :], reads=[("winb", l, kc, c0_) for c0_ in range(0, DIN, 944)], writes=[("win", kc)], stream="w", n=8)
                dma("sp", gpre[:], norms_d[l, 0:1, :].partition_broadcast(128), writes=["gpre"])
                for i in range(4):
                    kb.op("pool", lambda e, i=i: e.memset(smv[i][:], 1.0), writes=[("smv", i)])

                WINK = [("win", kc_) for kc_ in range(8)]
                flist = [("lru", lruT_d, 0, 0, 128, F32), ("lru", lruT_d, 128, 128, 128, F32),
                         ("lru", lruT_d, 256, 256, 128, F32), ("lru", lruT_d, 384, 384, 128, F32),
                         ("gq", gqT_d, 0, 512, 128, F32), ("gk", gkT_d, 0, 640, 128, F32),
                         ("glr", glrT_d, 0, 1024, 16, BF16)]
                for i in range(4):
                    flist.append(("mq", mqT_d, i * 128, 1296 + i * 128, 128, BF16))
                for i in range(4):
                    flist.append(("mk", mkT_d, i * 128, 1808 + i * 128, 128, BF16))

                def load(t):
                    dma("sp", xt[t % 8][:], xin_d[t * 128:(t + 1) * 128, :], writes=[("xt", t % 8)], stream="x", n=4)

                NT = S // 128
                pi = 0
                ev = 0
                import os as _os
                _ng = int(_os.environ.get("P1_GROUPS", S // 512))
                _parts = int(_os.environ.get("P1_PARTS", 7))

                def chain(g):
                    for s in range(4):
                        t = g * 4 + s
                        xs = xt[t % 8]
                        hbs = hb[s]
                        c = t % 8
                        kb.op("act", lambda e, xs=xs, c=c: e.activation(out=junk[:], in_=xs[:], func=AF.Square, accum_out=ssq[:, c:c + 1]),
                              reads=[("xt", t % 8)], writes=["junk", "p1ssq"])
                        rstd_from_ssq(ssq[:, c:c + 1], rst[:, c:c + 1], D, "p1")
                        kb.op("dve", lambda e, xs=xs, hbs=hbs, c=c: e.scalar_tensor_tensor(out=hbs[:], in0=xs[:], scalar=rst[:, c:c + 1], in1=gpre[:], op0=ALU.mult, op1=ALU.mult),
                              reads=[("xt", t % 8), "p1rs", "gpre"], writes=[("hb", s)])

                def transp(g):
                    hTg_ = hT[g % 2]
                    for s in range(4):
                        t = g * 4 + s
                        hbs = hb[s]
                        tpp = tps[t % 2]
                        for kc in range(8):
                            tp(tpp[:, kc * 128:(kc + 1) * 128], hbs[:, kc * 128:(kc + 1) * 128], ident[:],
                               reads=[("hb", s), "ident"], writes=[("tps", t % 2)])
                        kb.op("act", lambda e, tpp=tpp, hTg_=hTg_, s=s: e.copy(out=hTg_[:, :, s * 128:(s + 1) * 128], in_=tpp[:].rearrange("p (k c) -> p k c", k=8)),
                              reads=[("tps", t % 2)], writes=[("hT", g % 2)])

                for t in range(8):
                    load(t)
                chain(0)
                transp(0)
                for g in range(_ng):
                    hTg = hT[g % 2]
                    if g + 1 < _ng:
                        chain(g + 1)
                    if g + 2 < _ng:
                        for s in range(4):
                            load((g + 2) * 4 + s)
                    for (nm, dst, drow, wcol, wid, dt) in (flist if _parts & 2 else []):
                        ps = aps[pi % 5]
                        pk = ("aps", pi % 5)
                        pi += 1
                        for kc in range(8):
                            mm(ps[0:wid, :], win[:, kc, wcol:wcol + wid], hTg[:, kc, :], kc == 0, kc == 7,
                               reads=WINK + [("hT", g % 2)], writes=[pk])
                        if dt == F32:
                            stg = sf[ev % 6]
                            sk = ("sf", ev % 6)
                        else:
                            stg = sbf[ev % 6]
                            sk = ("sbf", ev % 6)
                        eng = "act" if ev % 2 == 0 else "dve"
                        ev += 1
                        if nm == "mq":
                            if eng == "act":
                                kb.op("act", lambda e, stg=stg, ps=ps, wid=wid: e.mul(out=stg[0:wid, :], in_=ps[0:wid, :], mul=0.125), reads=[pk], writes=[sk])
                            else:
                                kb.op("dve", lambda e, stg=stg, ps=ps, wid=wid: e.tensor_scalar(out=stg[0:wid, :], in0=ps[0:wid, :], scalar1=0.125, scalar2=None, op0=ALU.mult), reads=[pk], writes=[sk])
                        else:
                            if eng == "act":
                                kb.op("act", lambda e, stg=stg, ps=ps, wid=wid: e.copy(out=stg[0:wid, :], in_=ps[0:wid, :]), reads=[pk], writes=[sk])
                            else:
                                kb.op("dve", lambda e, stg=stg, ps=ps, wid=wid: e.tensor_copy(out=stg[0:wid, :], in_=ps[0:wid, :]), reads=[pk], writes=[sk])
                        dma("sp", dst[drow:drow + wid, g * 512:(g + 1) * 512], stg[0:wid, :], reads=[sk], stream="o1", n=12)
                    if g + 1 < _ng:
                        transp(g + 1)
                    for s in (range(4) if _parts & 4 else []):
                        t = g * 4 + s
                        _tm = int(_os.environ.get("TM_SKIP", 0))
                        if not _tm & 1:
                            ps = aps[pi % 5]
                            pk = ("aps", pi % 5)
                            pi += 1
                            ps2 = aps[pi % 5]
                            pk2 = ("aps", pi % 5)
                            pi += 1
                            for kc in range(8):
                                mm(ps[:, 0:256], hTg[:, kc, s * 128:(s + 1) * 128], win[:, kc, 768:1024], kc == 0, kc == 7,
                                   reads=WINK + [("hT", g % 2)], writes=[pk])
                            for kc in range(8):
                                mm(ps2[:, 0:256], hTg[:, kc, s * 128:(s + 1) * 128], win[:, kc, 1040:1296], kc == 0, kc == 7,
                                   reads=WINK + [("hT", g % 2)], writes=[pk2])
                            a, b = sgv[t % 4], sgo[t % 4]
                            kb.op("act", lambda e, a=a, ps=ps: e.copy(out=a[:], in_=ps[:, 0:256]), reads=[pk], writes=[("sgv", t % 4)])
                            kb.op("dve", lambda e, b=b, ps2=ps2: e.tensor_copy(out=b[:], in_=ps2[:, 0:256]), reads=[pk2], writes=[("sgo", t % 4)])
                            dma("sp", gv_d[t * 128:(t + 1) * 128, :], a[:], reads=[("sgv", t % 4)], stream="o1", n=12)
                            dma("sp", gout_d[t * 128:(t + 1) * 128, :], b[:], reads=[("sgo", t % 4)], stream="o1", n=12)
                        if not _tm & 2:
                            ps = aps[pi % 5]
                            pk = ("aps", pi % 5)
                            pi += 1
                            for kc in range(8):
                                mm(ps[:, :], hTg[:, kc, s * 128:(s + 1) * 128], win[:, kc, 2320:2832], kc == 0, kc == 7,
                                   reads=WINK + [("hT", g % 2)], writes=[pk])
                            m = smv[t % 4]
                            psv = ps[:].rearrange("p (h d) -> p h d", h=8)
                            if _tm & 4:
                                pass
                            elif t % 2:
                                kb.op("act", lambda e, m=m, psv=psv: e.copy(out=m[:, :, 0:64], in_=psv), reads=[pk], writes=[("smv", t % 4)])
                            else:
                                kb.op("dve", lambda e, m=m, psv=psv: e.tensor_copy(out=m[:, :, 0:64], in_=psv), reads=[pk], writes=[("smv", t % 4)])
                            if not _tm & 8:
                                dma("sp", mvp_d[t * 128:(t + 1) * 128, :], m[:].rearrange("p h d -> p (h d)"), reads=[("smv", t % 4)], stream="o1", n=12)
                if _os.environ.get("P1_TAILSTORE"):
                    dma("sp", lruT_d[0:128, 0:8], rst[:], reads=["p1rs"], stream="o1", n=12)
                end_phase()

        def phase2a_gen(l, ph):
            TB = 1024
            if True:
                cols = sbt(ph, [128, 2, 8], F32, "lcols")
                ccol = sbt(ph, [128, 2], F32, "ccol")
                wstage = sbt(ph, [128, 2, 2, 128], F32, "wstage")
                wbd = sbt(ph, [128, 2, 2, 128], BF16, "wbd")
                xin = [sbt(ph, [128, TB + 3], F32, "xin") for _ in range(2)]
                gin = [sbt(ph, [128, TB], F32, "gin") for _ in range(2)]
                xc = sbt(ph, [128, TB], F32, "xc")
                xcb = sbt(ph, [128, TB], BF16, "xcb")
                rr_ = sbt(ph, [128, TB], F32, "r")
                ii_ = sbt(ph, [128, TB], F32, "i")
                aa = sbt(ph, [128, TB], F32, "a")
                mmul = sbt(ph, [128, TB], F32, "mult")
                uu = sbt(ph, [128, TB], F32, "u")
                hh = [sbt(ph, [128, TB], F32, "h") for _ in range(2)]
                gt = sbt(ph, [128, TB], F32, "gt")
                gs = sbt(ph, [128, TB], F32, "gs")
                yb = [sbt(ph, [128, TB], BF16, "yb") for _ in range(2)]
                gps = [pst(ph, [128, 512], F32, "gps") for _ in range(2)]
                for h in range(2):
                    dma("sp", cols[:, h, :], lcols_d[l, h], writes=["lcols"])
                kb.op("pool", lambda e: e.memset(wstage[:], 0.0), writes=["wstage"])
                for ax, src in enumerate((lwa_d, lwx_d)):
                    for h in range(2):
                        for b in range(2):
                            dma("sp", wstage[b * 64:(b + 1) * 64, ax, h, b * 64:(b + 1) * 64], src[l, 2 * h + b],
                                reads=[], writes=["wstage"])
                kb.op("dve", lambda e: e.tensor_copy(out=wbd[:], in_=wstage[:]), reads=["wstage"], writes=["wbd"])
                kb.op("act", lambda e: e.activation(out=ccol[:], in_=cols[:, :, 7], func=AF.Exp, scale=-1.0), reads=["lcols"], writes=["ccol"])
                kb.op("act", lambda e: e.activation(out=ccol[:], in_=ccol[:], func=AF.Ln, bias=1.0), reads=["ccol"], writes=["ccol"])
                kb.op("dve", lambda e: e.tensor_scalar(out=ccol[:], in0=ccol[:], scalar1=-8.0, scalar2=None, op0=ALU.mult), reads=["ccol"], writes=["ccol"])

                nb = S // TB
                it = 0
                for h in range(2):
                    for b in range(nb):
                        t0 = b * TB
                        xi = xin[it % 2]
                        gi = gin[it % 2]
                        hcur = hh[it % 2]
                        hprev = hh[(it + 1) % 2]
                        ybs = yb[it % 2]
                        kx, kg, ky = ("xin", it % 2), ("gin", it % 2), ("yb", it % 2)
                        kh, khp = ("h", it % 2), ("h", (it + 1) % 2)
                        it += 1
                        if b == 0:
                            kb.op("pool", lambda e, xi=xi: e.memset(xi[:, 0:3], 0.0), writes=[kx])
                            dma("sp", xi[:, 3:], lruT_d[h * 128:(h + 1) * 128, 0:TB], writes=[kx], stream="x", n=3)
                        else:
                            dma("sp", xi[:], lruT_d[h * 128:(h + 1) * 128, t0 - 3:t0 + TB], writes=[kx], stream="x", n=3)
                        dma("sp", gi[:], lruT_d[256 + h * 128:256 + (h + 1) * 128, t0:t0 + TB], writes=[kg], stream="x", n=3)
                        yield
                        kb.op("dve", lambda e, xi=xi, h=h: e.tensor_scalar(out=xc[:], in0=xi[:, 3:TB + 3], scalar1=cols[:, h, 3:4], scalar2=cols[:, h, 4:5], op0=ALU.mult, op1=ALU.add),
                              reads=[kx, "lcols"], writes=["xc"])
                        for j in range(3):
                            kb.op("dve", lambda e, xi=xi, h=h, j=j: e.scalar_tensor_tensor(out=xc[:], in0=xi[:, j:TB + j], scalar=cols[:, h, j:j + 1], in1=xc[:], op0=ALU.mult, op1=ALU.add),
                                  reads=[kx, "lcols", "xc"], writes=["xc"])
                        yield
                        kb.op("pool", lambda e: e.tensor_copy(out=xcb[:], in_=xc[:]), reads=["xc"], writes=["xcb"])
                        for sblk in range(TB // 512):
                            cs = slice(sblk * 512, (sblk + 1) * 512)
                            pa, px = gps[0], gps[1]
                            ka, kx_ = ("gps", 0), ("gps", 1)
                            mm(pa[:], wbd[:, 0, h, :], xcb[:, cs], True, True, reads=["wbd", "xcb"], writes=[ka])
                            mm(px[:], wbd[:, 1, h, :], xcb[:, cs], True, True, reads=["wbd", "xcb"], writes=[kx_])
                            kb.op("act", lambda e, pa=pa, cs=cs, h=h: e.activation(out=rr_[:, cs], in_=pa[:], func=AF.Sigmoid, bias=cols[:, h, 5:6]),
                                  reads=[ka, "lcols"], writes=["r"])
                            kb.op("act", lambda e, px=px, cs=cs, h=h: e.activation(out=ii_[:, cs], in_=px[:], func=AF.Sigmoid, bias=cols[:, h, 6:7]),
                                  reads=[kx_, "lcols"], writes=["i"])
                        yield
                        kb.op("pool", lambda e, gi=gi: e.tensor_tensor(out=gt[:], in0=gi[:], in1=gi[:], op=ALU.mult), reads=[kg], writes=["gt"])
                        kb.op("pool", lambda e: e.tensor_scalar(out=gt[:], in0=gt[:], scalar1=0.044715, scalar2=1.0, op0=ALU.mult, op1=ALU.add), reads=["gt"], writes=["gt"])
                        kb.op("pool", lambda e, gi=gi: e.tensor_tensor(out=gt[:], in0=gt[:], in1=gi[:], op=ALU.mult), reads=["gt", kg], writes=["gt"])
                        kb.op("act", lambda e: e.activation(out=gs[:], in_=gt[:], func=AF.Sigmoid, scale=1.5957691216057308), reads=["gt"], writes=["gs"])
                        kb.op("pool", lambda e, gi=gi: e.tensor_tensor(out=gs[:], in0=gs[:], in1=gi[:], op=ALU.mult), reads=["gs", kg], writes=["gs"])
                        yield
                        kb.op("act", lambda e, h=h: e.activation(out=aa[:], in_=rr_[:], func=AF.Exp, scale=ccol[:, h:h + 1]), reads=["r", "ccol"], writes=["a"])
                        kb.op("pool", lambda e: e.tensor_tensor(out=mmul[:], in0=aa[:], in1=aa[:], op=ALU.mult), reads=["a"], writes=["mult"])
                        kb.op("act", lambda e: e.activation(out=mmul[:], in_=mmul[:], func=AF.Sqrt, scale=-1.0, bias=1.0), reads=["mult"], writes=["mult"])
                        yield
                        if b == 0:
                            kb.op("dve", lambda e: e.memset(mmul[:, 0:1], 1.0), reads=["mult"], writes=["mult"])
                        kb.op("dve", lambda e: e.tensor_tensor(out=uu[:], in0=ii_[:], in1=xc[:], op=ALU.mult), reads=["i", "xc"], writes=["u"])
                        kb.op("dve", lambda e: e.tensor_tensor(out=uu[:], in0=uu[:], in1=mmul[:], op=ALU.mult), reads=["u", "mult"], writes=["u"])
                        yield
                        if b == 0:
                            kb.op("dve", lambda e, hcur=hcur: e.tensor_tensor_scan(out=hcur[:], data0=aa[:], data1=uu[:], initial=0.0, op0=ALU.mult, op1=ALU.add),
                                  reads=["a", "u"], writes=[kh])
                        else:
                            kb.op("dve", lambda e, hcur=hcur, hprev=hprev: e.tensor_tensor_scan(out=hcur[:], data0=aa[:], data1=uu[:], initial=hprev[:, TB - 1:TB], op0=ALU.mult, op1=ALU.add),
                                  reads=["a", "u", khp], writes=[kh])
                        yield
                        kb.op("dve", lambda e, hcur=hcur, ybs=ybs: e.tensor_tensor(out=ybs[:], in0=hcur[:], in1=gs[:], op=ALU.mult), reads=[kh, "gs"], writes=[ky])
                        dma("sp", mixT_d[h * 128:(h + 1) * 128, t0:t0 + TB], ybs[:], reads=[ky], stream="o", n=4)
                        yield

        def phase2b_gen(l, ph):
            TB = 1024
            NCH = TB // 128
            if True:
                w2s = sbt(ph, [16, 128], F32, "w2s")
                w2b = sbt(ph, [16, 128], BF16, "w2b")
                negb = sbt(ph, [128, 1], F32, "negb")
                gn = sbt(ph, [128, 256], F32, "gn")
                tri = sbt(ph, [128, 128], F32, "tri")
                bmask = sbt(ph, [128, 256], F32, "bmask")
                hm = sbt(ph, [128, 4], F32, "hm")
                ones = sbt(ph, [128, 128], F32, "ones")
                glr = [sbt(ph, [16, TB], BF16, "glr") for _ in range(2)]
                qT = [sbt(ph, [128, TB], F32, "qT") for _ in range(2)]
                kT = [sbt(ph, [128, TB], F32, "kT") for _ in range(2)]
                vv = [sbt(ph, [128, NCH, 256], BF16, "vv") for _ in range(2)]
                go = [sbt(ph, [128, NCH, 256], F32, "go") for _ in range(2)]
                ee = sbt(ph, [128, TB], F32, "ee")
                cum = sbt(ph, [128, TB], F32, "cum")
                ex = sbt(ph, [128, TB], F32, "ex")
                dd = sbt(ph, [128, TB], F32, "dd")
                qd = sbt(ph, [128, TB], BF16, "qd")
                kdm = sbt(ph, [128, 4, TB], BF16, "kdm")
                kdec = sbt(ph, [128, TB], BF16, "kdec")
                dcol = sbt(ph, [128, NCH], F32, "dcol")
                kdtm = [sbt(ph, [128, 128], BF16, "kdtm") for _ in range(2)]
                am = [sbt(ph, [128, 4, 128], BF16, "am") for _ in range(2)]
                Sst = sbt(ph, [128, 256], F32, "Sst")
                Sbf = sbt(ph, [128, 256], BF16, "Sbf")
                kvm = sbt(ph, [128, 256], F32, "kvm")
                ob = sbt(ph, [128, NCH, 256], F32, "ob")
                osq = sbt(ph, [128, NCH, 256], F32, "osq")
                ssq = sbt(ph, [128, NCH * 4], F32, "gssq")
                rst = sbt(ph, [128, NCH * 4], F32, "grst")
                sg = sbt(ph, [128, NCH, 256], F32, "sg")
                yb = sbt(ph, [128, NCH, 256], BF16, "yb")
                yT = [sbt(ph, [128, 2, TB], BF16, "yT") for _ in range(2)]
                zps = [pst(ph, [128, 512], F32, "zps") for _ in range(1)]
                tps = pst(ph, [128, 1024], BF16, "gtps")
                aps_ = [pst(ph, [128, 512], F32, "gaps") for _ in range(1)]
                ops_ = [pst(ph, [128, 512], F32, "gops") for _ in range(2)]
                kvps = pst(ph, [128, 512], F32, "kvps")

                dma("sp", w2s[:], gw2_d[l], writes=["w2s"])
                kb.op("dve", lambda e: e.tensor_copy(out=w2b[:], in_=w2s[:]), reads=["w2s"], writes=["w2b"])
                dma("sp", negb[:], gb_d[l], writes=["negb"])
                kb.op("dve", lambda e: e.tensor_scalar(out=negb[:], in0=negb[:], scalar1=-1.0, scalar2=None, op0=ALU.mult), reads=["negb"], writes=["negb"])
                dma("sp", gn[:], gn_d[l:l + 1, :].partition_broadcast(128), writes=["gn"])
                dma("sp", tri[:], tri_d, writes=["tri"])
                dma("sp", bmask[:], bmask_d, writes=["bmask"])
                dma("sp", hm[:], hm_d, writes=["hm"])
                kb.op("pool", lambda e: e.memset(ones[:], 1.0), writes=["ones"])
                kb.op("pool", lambda e: e.memset(Sst[:], 0.0), writes=["Sst"])
                kb.op("pool", lambda e: e.memset(Sbf[:], 0.0), writes=["Sbf"])

                def load(b):
                    i = b % 2
                    t0 = b * TB
                    dma("sp", glr[i][:], glrT_d[:, t0:t0 + TB], writes=[("glr", i)], stream="x", n=3)
                    dma("sp", qT[i][:], gqT_d[:, t0:t0 + TB], writes=[("qT", i)], stream="x", n=3)
                    dma("sp", kT[i][:], gkT_d[:, t0:t0 + TB], writes=[("kT", i)], stream="x", n=3)
                    dma("sp", vv[i][:], gv_d[t0:t0 + TB, :].rearrange("(c p) f -> p c f", p=128), writes=[("vv", i)], stream="x", n=3)
                    dma("sp", go[i][:], gout_d[t0:t0 + TB, :].rearrange("(c p) f -> p c f", p=128), writes=[("go", i)], stream="x", n=3)

                nb = S // TB
                load(0)
                for b in range(nb):
                    if b + 1 < nb:
                        load(b + 1)
                    i = b % 2
                    t0 = b * TB
                    q_, k_, v_, g_, r_ = qT[i], kT[i], vv[i], go[i], glr[i]
                    kq, kk, kv, kg, kr = ("qT", i), ("kT", i), ("vv", i), ("go", i), ("glr", i)
                    for sblk in range(TB // 512):
                        cs = slice(sblk * 512, (sblk + 1) * 512)
                        zp = zps[0]
                        mm(zp[:], w2b[:], r_[:, cs], True, True, reads=["w2b", kr], writes=[("zps", 0)])
                        kb.op("act", lambda e, zp=zp, cs=cs: e.activation(out=ee[:, cs], in_=zp[:], func=AF.Exp, scale=-1.0, bias=negb[:]),
                              reads=[("zps", 0), "negb"], writes=["ee"])
                    yield
                    kb.op("act", lambda e: e.activation(out=ee[:], in_=ee[:], func=AF.Ln, bias=1.0), reads=["ee"], writes=["ee"])
                    for c in range(NCH):
                        cs = slice(c * 128, (c + 1) * 128)
                        kb.op("dve", lambda e, cs=cs: e.tensor_tensor_scan(out=cum[:, cs], data0=ones[:], data1=ee[:, cs], initial=0.0, op0=ALU.mult, op1=ALU.add),
                              reads=["ones", "ee"], writes=["cum"])
                    yield
                    kb.op("act", lambda e: e.activation(out=ex[:], in_=cum[:], func=AF.Exp, scale=-1.0 / 16.0), reads=["cum"], writes=["ex"])
                    kb.op("dve", lambda e, q_=q_: e.scalar_tensor_tensor(out=qd[:], in0=q_[:], scalar=32.0 ** -0.5, in1=ex[:], op0=ALU.mult, op1=ALU.mult),
                          reads=[kq, "ex"], writes=["qd"])
                    yield
                    kb.op("act", lambda e: e.activation(out=dcol[:], in_=cum[:].rearrange("p (c t) -> p c t", t=128)[:, :, 127], func=AF.Exp, scale=-1.0 / 16.0),
                          reads=["cum"], writes=["dcol"])
                    for c in range(NCH):
                        cs = slice(c * 128, (c + 1) * 128)
                        kb.op("pool", lambda e, cs=cs, c=c: e.tensor_scalar(out=dd[:, cs], in0=cum[:, cs], scalar1=cum[:, c * 128 + 127:c * 128 + 128], scalar2=None, op0=ALU.subtract),
                              reads=["cum"], writes=["dd"])
                    yield
                    kb.op("act", lambda e: e.activation(out=ex[:], in_=cum[:], func=AF.Exp, scale=1.0 / 16.0), reads=["cum", "qd"], writes=["ex"])
                    for hh_ in range(4):
                        kb.op("dve", lambda e, k_=k_, hh_=hh_: e.scalar_tensor_tensor(out=kdm[:, hh_, :], in0=k_[:], scalar=hm[:, hh_:hh_ + 1], in1=ex[:], op0=ALU.mult, op1=ALU.mult),
                              reads=[kk, "ex", "hm"], writes=["kdm"])
                    yield
                    kb.op("act", lambda e: e.activation(out=dd[:], in_=dd[:], func=AF.Exp, scale=1.0 / 16.0), reads=["dd"], writes=["dd"])
                    kb.op("pool", lambda e, k_=k_: e.tensor_tensor(out=kdec[:], in0=k_[:], in1=dd[:], op=ALU.mult), reads=[kk, "dd"], writes=["kdec"])
                    kb.op("act", lambda e, g_=g_: e.activation(out=sg[:], in_=g_[:], func=AF.Silu), reads=[kg], writes=["sg"])
                    for c in range(NCH):
                        cs = slice(c * 128, (c + 1) * 128)
                        j = c % 2
                        yield
                        tp(tps[:, j * 128:(j + 1) * 128], kdec[:, cs], ident[:], reads=["kdec", "ident"], writes=["gtps"])
                        kb.op("act", lambda e, j=j: e.copy(out=kdtm[j][:], in_=tps[:, j * 128:(j + 1) * 128]), reads=["gtps"], writes=[("kdtm", j)])
                        yield
                        ap_ = aps_[0]
                        for hh_ in range(4):
                            mm(ap_[:, hh_ * 128:(hh_ + 1) * 128], kdm[:, hh_, cs], qd[:, cs], True, True, reads=["kdm", "qd"], writes=[("gaps", 0)])
                        kb.op("dve", lambda e, ap_=ap_, j=j: e.tensor_tensor(out=am[j][:], in0=ap_[:].rearrange("p (h c) -> p h c", h=4),
                                                                             in1=tri[:].unsqueeze(1).broadcast_to([128, 4, 128]), op=ALU.mult),
                              reads=[("gaps", 0), "tri"], writes=[("am", j)])
                        yield
                        op_ = ops_[j]
                        mm(op_[:, 0:256], qd[:, cs], Sbf[:], True, True, reads=["qd", "Sbf"], writes=[("gops", j)])
                        for hh_ in range(4):
                            mm(op_[:, hh_ * 64:(hh_ + 1) * 64], am[j][:, hh_, :], v_[:, c, hh_ * 64:(hh_ + 1) * 64], False, True,
                               reads=[("am", j), kv], writes=[("gops", j)], skip_group_check=True)
                        kb.op("act", lambda e, op_=op_, c=c: e.copy(out=ob[:, c, :], in_=op_[:, 0:256]), reads=[("gops", j)], writes=["ob"])
                        yield
                        mm(kvps[:, 0:256], kdtm[j][:], v_[:, c, :], True, True, reads=[("kdtm", j), kv], writes=["kvps"])
                        kb.op("dve", lambda e: e.tensor_tensor(out=kvm[:], in0=kvps[:, 0:256], in1=bmask[:], op=ALU.mult), reads=["kvps", "bmask"], writes=["kvm"])
                        kb.op("dve", lambda e, c=c: e.scalar_tensor_tensor(out=Sst[:], in0=Sst[:], scalar=dcol[:, c:c + 1], in1=kvm[:], op0=ALU.mult, op1=ALU.add),
                              reads=["Sst", "dcol", "kvm"], writes=["Sst"])
                        kb.op("pool", lambda e: e.tensor_copy(out=Sbf[:], in_=Sst[:]), reads=["Sst"], writes=["Sbf"])
                    yield
                    kb.op("pool", lambda e: e.tensor_tensor(out=osq[:], in0=ob[:], in1=ob[:], op=ALU.mult), reads=["ob"], writes=["osq"])
                    kb.op("dve", lambda e: e.tensor_reduce(out=ssq[:], in_=osq[:].rearrange("p c (h v) -> p (c h) v", h=4), axis=AX.X, op=ALU.add),
                          reads=["osq"], writes=["p2bssq"])
                    rstd_from_ssq(ssq[:], rst[:], 64, "p2b")
                    kb.op("dve", lambda e: e.tensor_tensor(out=ob[:].rearrange("p c (h v) -> p (c h) v", h=4), in0=ob[:].rearrange("p c (h v) -> p (c h) v", h=4),
                                                           in1=rst[:].unsqueeze(2).broadcast_to([128, NCH * 4, 64]), op=ALU.mult),
                          reads=["ob", "p2brs"], writes=["ob"])
                    kb.op("pool", lambda e: e.tensor_tensor(out=sg[:], in0=sg[:], in1=gn[:].unsqueeze(1).broadcast_to([128, NCH, 256]), op=ALU.mult),
                          reads=["sg", "gn"], writes=["sg"])
                    kb.op("dve", lambda e: e.tensor_tensor(out=yb[:], in0=ob[:], in1=sg[:], op=ALU.mult), reads=["ob", "sg"], writes=["yb"])
                    yield
                    yTb = yT[b % 2]
                    for c in range(NCH):
                        for f in range(2):
                            jj = (c * 2 + f) % 4
                            tp(tps[:, jj * 128:(jj + 1) * 128], yb[:, c, f * 128:(f + 1) * 128], ident[:], reads=["yb", "ident"], writes=["gtps"])
                            kb.op("act", lambda e, jj=jj, c=c, f=f, yTb=yTb: e.copy(out=yTb[:, f, c * 128:(c + 1) * 128], in_=tps[:, jj * 128:(jj + 1) * 128]),
                                  reads=["gtps"], writes=[("yT", b % 2)])
                    dma("sp", mixT_d[256:512, t0:t0 + TB].rearrange("(f p) t -> p f t", p=128), yTb[:], reads=[("yT", b % 2)], stream="o", n=4)

        def phase2ab(l):
            with ExitStack() as ph:
                gens = [phase2a_gen(l, ph), phase2b_gen(l, ph)]
                while gens:
                    for g_ in list(gens):
                        try:
                            next(g_)
                        except StopIteration:
                            gens.remove(g_)
                end_phase()

        def phase2c(l):
            with ExitStack() as ph:
                kaug = sbt(ph, [128, 8, S], BF16, "kaug")
                vp = sbt(ph, [128, 32, 520], BF16, "vp")
                qaug = [sbt(ph, [128, 8, 512], BF16, "qaug") for _ in range(3)]
                km = sbt(ph, [64, 8, 16], F32, "km")
                kmb = sbt(ph, [64, 8, 16], BF16, "kmb")
                cm = sbt(ph, [128, 16, 16], F32, "cm")
                pm = sbt(ph, [128, 16, 16], F32, "pm")
                b31 = sbt(ph, [128, 8], F32, "b31")
                caus = sbt(ph, [128, 128], F32, "caus")
                tstage = sbt(ph, [128, 2, 8, 128], F32, "tstage")
                tdT = sbt(ph, [128, 8, 128], BF16, "tdT")
                toT = sbt(ph, [128, 8, 128], BF16, "toT")
                tomT = sbt(ph, [128, 8, 128], BF16, "tomT")
                zer = sbt(ph, [128, 260], BF16, "zer")
                gm = sbt(ph, [128, 4, 8, 16], F32, "gm")
                m8 = sbt(ph, [128, 4, 8, 8], F32, "m8")
                sel = sbt(ph, [128, 4, 8, 16], F32, "sel")
                mpad = [sbt(ph, [128, 4, 8, 80], BF16, "mpad") for _ in range(2)]
                pT = [sbt(ph, [128, 512], BF16, "pT") for _ in range(4)]
                rcp = sbt(ph, [128, 4], F32, "rcp")
                ymo = [sbt(ph, [128, 4, 512], BF16, "ymo") for _ in range(2)]
                ymT = [sbt(ph, [128, 4, 512], BF16, "ymT") for _ in range(2)]
                gps = pst(ph, [128, 512], F32, "mgps")
                mtps = [pst(ph, [128, 512], F32, "mtps") for _ in range(1)]
                mtps_b = [pst(ph, [128, 1024], BF16, "mtpsb") for _ in range(1)]
                sps = [pst(ph, [128, 512], F32, "sps") for _ in range(3)]
                accs_full = [pst(ph, [128, 512], F32, "acc") for _ in range(2)]
                accs = [a_[:, 0:260].rearrange("p (s d) -> p s d", s=4) for a_ in accs_full]

                for h in range(8):
                    dma("sp", kaug[0:64, h, :], mkT_d[h * 64:(h + 1) * 64, :], writes=["kaug"], stream="x", n=3)
                    dma("sp", kaug[64:80, h, :], e16_d, writes=["kaug"], stream="x", n=3)
                for c in range(4):
                    dma("sp", vp[:, c * 8:(c + 1) * 8, :], mvp_d[c * 1024:(c + 1) * 1024, :].rearrange("(c p) f -> p c f", p=128), writes=["vp"], stream="x", n=3)
                dma("sp", cm[:].rearrange("p a b -> p (a b)"), cm_d.partition_broadcast(128), writes=["cm"])
                dma("sp", pm[:].rearrange("p a b -> p (a b)"), pm_d.partition_broadcast(128), writes=["pm"])
                dma("sp", b31[:], rb31_d.partition_broadcast(128), writes=["b31"])
                dma("sp", caus[:], caus_d, writes=["caus"])
                dma("sp", tstage[:, 0], tdg_d, writes=["tstage"])
                dma("sp", tstage[:, 1], tof_d, writes=["tstage"])
                kb.op("dve", lambda e: e.tensor_tensor(out=tdT[:], in0=tstage[:, 0], in1=caus[:].unsqueeze(1).broadcast_to([128, 8, 128]), op=ALU.add),
                      reads=["tstage", "caus"], writes=["tdT"])
                kb.op("dve", lambda e: e.tensor_copy(out=toT[:], in_=tstage[:, 1]), reads=["tstage"], writes=["toT"])
                kb.op("dve", lambda e: e.tensor_tensor(out=tomT[:], in0=tstage[:, 1], in1=b31[:].unsqueeze(2).broadcast_to([128, 8, 128]), op=ALU.subtract),
                      reads=["tstage", "b31"], writes=["tomT"])
                kb.op("pool", lambda e: e.memset(zer[:], 0.0), writes=["zer"])
                for i in range(2):
                    kb.op("pool", lambda e, i=i: e.memset(mpad[i][:], 0.0), writes=[("mpad", i)])
                kb.op("dve", lambda e: e.tensor_reduce(out=km[:].rearrange("p h n -> p (h n)"), in_=kaug[0:64, :, :].rearrange("p h (n t) -> p (h n) t", t=256), axis=AX.X, op=ALU.add),
                      reads=["kaug"], writes=["km"])
                kb.op("dve", lambda e: e.tensor_scalar(out=kmb[:], in0=km[:], scalar1=1.0 / 256.0, scalar2=None, op0=ALU.mult), reads=["km"], writes=["kmb"])

                def loadq(G):
                    i = G % 3
                    dma("sp", qaug[i][0:64, :, :], mqT_d.rearrange("(h d) t -> d h t", d=64)[:, :, G * 512:(G + 1) * 512], writes=[("qaug", i)], stream="q", n=3)

                NG = S // 512
                si = 0
                ai = 0

                def pre1(G):
                    qi = G % 3
                    qa = qaug[qi]
                    kqa = ("qaug", qi)
                    mp = mpad[G % 2]
                    for s in range(4):
                        for h in range(8):
                            mm(gps[:, (s * 8 + h) * 16:(s * 8 + h + 1) * 16], qa[0:64, h, s * 128:(s + 1) * 128], kmb[:, h, :], True, True,
                               reads=[kqa, "kmb"], writes=["mgps"])
                    np0 = 2 * G
                    for a in range(2):
                        cmv = cm[:, np0 + a, :].unsqueeze(1).unsqueeze(1).broadcast_to([128, 2, 8, 16])
                        kb.op("dve", lambda e, cmv=cmv, a=a: e.tensor_tensor(out=gm[:, 2 * a:2 * a + 2], in0=gps[:].rearrange("p (s h n) -> p s h n", s=4, h=8)[:, 2 * a:2 * a + 2],
                                                                             in1=cmv, op=ALU.add),
                              reads=["mgps", "cm"], writes=["gm"])
                    for s in range(4):
                        for h in range(8):
                            kb.op("dve", lambda e, s=s, h=h: e.max(out=m8[:, s, h, :], in_=gm[:, s, h, :]), reads=["gm"], writes=["m8"])
                    kb.op("dve", lambda e: e.tensor_tensor(out=sel[:], in0=gm[:], in1=m8[:, :, :, 2:3].broadcast_to([128, 4, 8, 16]), op=ALU.is_ge),
                          reads=["gm", "m8"], writes=["sel"])
                    kb.op("dve", lambda e: e.tensor_scalar(out=sel[:], in0=sel[:], scalar1=-NEG, scalar2=NEG, op0=ALU.mult, op1=ALU.add), reads=["sel"], writes=["sel"])
                    kb.op("dve", lambda e: e.tensor_tensor(out=sel[:], in0=sel[:], in1=b31[:].unsqueeze(1).unsqueeze(3).broadcast_to([128, 4, 8, 16]), op=ALU.add),
                          reads=["sel", "b31"], writes=["sel"])
                    for a in range(2):
                        pmv = pm[:, np0 + a, :].unsqueeze(1).unsqueeze(1).broadcast_to([128, 2, 8, 16])
                        kb.op("dve", lambda e, pmv=pmv, mp=mp, a=a: e.tensor_tensor(out=mp[:, 2 * a:2 * a + 2, :, 64:80], in0=sel[:, 2 * a:2 * a + 2], in1=pmv, op=ALU.mult),
                              reads=["sel", "pm"], writes=[("mpad", G % 2)])

                def pre2(G):
                    qi = G % 3
                    qa = qaug[qi]
                    kqa = ("qaug", qi)
                    mp = mpad[G % 2]
                    for h in range(8):
                        mt = mtps[0]
                        for s in range(4):
                            mm(mt[0:80, s * 128:(s + 1) * 128], mp[:, s, h, :], ident[:], True, True, reads=[("mpad", G % 2), "ident"], writes=[("mtps", 0)])
                        kb.op("act", lambda e, mt=mt, h=h, qa=qa: e.copy(out=qa[64:80, h, :], in_=mt[64:80, :]),
                              reads=[("mtps", 0)], writes=[kqa])

                loadq(0)
                if NG > 1:
                    loadq(1)
                pre1(0)
                pre2(0)
                for G in range(NG):
                    if G + 2 < NG:
                        loadq(G + 2)
                    if G + 1 < NG:
                        pre1(G + 1)
                    qi = G % 3
                    qa = qaug[qi]
                    kqa = ("qaug", qi)
                    ym = ymo[G % 2]
                    nj = 4 * G + 4
                    DEPTH = 2

                    def stageA(h, j):
                        nonlocal si
                        acc = accs[h % 2]
                        ka = ("acc", h % 2)
                        if j == 0:
                            mm(accs_full[h % 2][:, 0:260], zer[:, 0:128], zer[:, :], True, True, reads=["zer"], writes=[ka])
                        r = j - 4 * G
                        c0 = max(r, 0) * 128
                        sp_ = sps[si % 3]
                        ks = ("sps", si % 3)
                        pt = pT[si % 4]
                        kp = ("pT", si % 4)
                        si += 1
                        mm(sp_[:, c0:512], kaug[0:80, h, j * 128:(j + 1) * 128], qa[0:80, h, c0:512], True, True,
                           reads=["kaug", kqa], writes=[ks])
                        if r == -1:
                            mm(sp_[:, 0:128], ident[:], tomT[:, h, :], False, True, reads=["ident", "tomT"], writes=[ks], skip_group_check=True)
                        if r >= 0:
                            mm(sp_[:, r * 128:(r + 1) * 128], ident[:], tdT[:, h, :], False, True, reads=["ident", "tdT"], writes=[ks], skip_group_check=True)
                            if r < 3:
                                tt = toT if r % 2 == 0 else tomT
                                mm(sp_[:, (r + 1) * 128:(r + 2) * 128], ident[:], tt[:, h, :], False, True, reads=["ident", "toT", "tomT"], writes=[ks], skip_group_check=True)
                        kb.op("act", lambda e, pt=pt, sp_=sp_, c0=c0: e.activation(out=pt[:, c0:512], in_=sp_[:, c0:512], func=AF.Exp),
                              reads=[ks], writes=[kp])
                        return (h, j, r, pt, kp, acc, ka)

                    def stageB(info):
                        h, j, r, pt, kp, acc, ka = info
                        for s in range(max(r, 0), 4):
                            mm(acc[:, s, :], pt[:, s * 128:(s + 1) * 128], vp[:, j, h * 65:(h + 1) * 65], False, True,
                               reads=[kp, "vp"], writes=[ka], skip_group_check=True)
                        if j == nj - 1:
                            kb.op("dve", lambda e, acc=acc: e.reciprocal(out=rcp[:], in_=acc[:, :, 64]), reads=[ka], writes=["rcp"])
                            kb.op("dve", lambda e, acc=acc, h=h, ym=ym: e.tensor_tensor(out=ym[:, :, h * 64:(h + 1) * 64], in0=acc[:, :, 0:64],
                                                                                  in1=rcp[:].unsqueeze(2).broadcast_to([128, 4, 64]), op=ALU.mult),
                                  reads=[ka, "rcp"], writes=[("ymo", G % 2)])

                    pend = []
                    for h in range(8):
                        if h == 4 and G + 1 < NG:
                            pre2(G + 1)
                        for j in range(nj):
                            pend.append(stageA(h, j))
                            if len(pend) > DEPTH:
                                stageB(pend.pop(0))
                    while pend:
                        stageB(pend.pop(0))
                    yt = ymT[G % 2]
                    for s in range(4):
                        tpb = mtps_b[0]
                        for f in range(4):
                            tp(tpb[:, f * 128:(f + 1) * 128], ym[:, s, f * 128:(f + 1) * 128], ident[:], reads=[("ymo", G % 2), "ident"], writes=[("mtpsb", 0)])
                        kb.op("act", lambda e, tpb=tpb, yt=yt, s=s: e.copy(out=yt[:, :, s * 128:(s + 1) * 128], in_=tpb[:, 0:512].rearrange("p (f q) -> p f q", f=4)),
                              reads=[("mtpsb", 0)], writes=[("ymT", G % 2)])
                    dma("sp", mixT_d[512:1024, G * 512:(G + 1) * 512].rearrange("(f p) t -> p f t", p=128), yt[:], reads=[("ymT", G % 2)], stream="o", n=4)
                end_phase()

        def phase3(l, xin_d, xout_d):
            with ExitStack() as ph:
                wout = sbt(ph, [128, 8, D], BF16, "wout")
                wdn = sbt(ph, [128, NFC, D], BF16, "wdn")
                g3 = sbt(ph, [128, 3, D], F32, "g3")
                wgu = [sbt(ph, [128, 2, 8, 128], BF16, "wgu") for _ in range(4)]
                mixT = [sbt(ph, [128, 8, 512], BF16, "mixT") for _ in range(2)]
                xt = [sbt(ph, [128, D], F32, "xt3") for _ in range(2)]
                x1 = [sbt(ph, [128, D], F32, "x1") for _ in range(8)]
                tmp = [sbt(ph, [128, D], F32, "tmp3") for _ in range(2)]
                junk = sbt(ph, [128, D], BF16, "junk3")
                hb = [sbt(ph, [128, D], BF16, "hb3") for _ in range(4)]
                hT = sbt(ph, [128, 8, 512], BF16, "hT3")
                actT = sbt(ph, [128, NFC, 512], BF16, "actT")
                sgt = [sbt(ph, [128, 512], F32, "sgt") for _ in range(2)]
                ssq = sbt(ph, [128, 16], F32, "ssq3")
                rst = sbt(ph, [128, 16], F32, "rst3")
                xo = [sbt(ph, [128, D], F32, "xo") for _ in range(2)]
                ops_ = [pst(ph, [128, 2, 512], F32, "p3o") for _ in range(2)]
                tps = pst(ph, [128, D], BF16, "p3t")
                gus = [pst(ph, [128, 512], F32, "p3gu") for _ in range(3)]
                for kc in range(8):
                    dma("sp", wout[:, kc, :], woutb_d[l, kc * 128:(kc + 1) * 128, :], writes=["wout"], stream="w", n=4)
                for fc in range(NFC):
                    dma("sp", wdn[:, fc, :], wdb_d[l, fc * 128:(fc + 1) * 128, :], writes=["wdn"], stream="w", n=4)
                for i in range(3):
                    dma("sp", g3[:, i, :], norms_d[l, i + 1:i + 2, :].partition_broadcast(128), writes=["g3"])

                wi = [0]

                def loadw(fc):
                    i = wi[0] % 4
                    wi[0] += 1
                    dma("sp", wgu[i][:], wgub_d[l, fc], writes=[("wgu", i)], stream="wgu", n=4)
                    return i

                NG = S // 512
                sq = 0

                def loadg(G):
                    i = G % 2
                    dma("sp", mixT[i][:], mixT_d[:, G * 512:(G + 1) * 512].rearrange("(k p) t -> p k t", p=128), writes=[("mixT", i)], stream="m", n=2)

                def loadx(t):
                    dma("sp", xt[t % 2][:], xin_d[t * 128:(t + 1) * 128, :], writes=[("xt3", t % 2)], stream="x", n=3)

                loadg(0)
                loadx(0)
                wq = []
                PRE = 3
                def p3_front(G):
                    nonlocal sq
                    mT = mixT[G % 2]
                    for s in range(4):
                        t = G * 4 + s
                        if t + 1 < S // 128:
                            loadx(t + 1)
                        xs = xt[t % 2]
                        x1s = x1[(G % 2) * 4 + s]
                        op_ = ops_[s % 2]
                        ko = ("p3o", s % 2)
                        tm_ = tmp[s % 2]
                        kt = ("tmp3", s % 2)
                        for hf in range(2):
                            for kc in range(8):
                                mm(op_[:, hf, :], mT[:, kc, s * 128:(s + 1) * 128], wout[:, kc, hf * 512:(hf + 1) * 512], kc == 0, kc == 7,
                                   reads=[("mixT", G % 2), "wout"], writes=[ko])
                        c = sq % 16
                        sq += 1
                        kb.op("act", lambda e, c=c, op_=op_: e.activation(out=junk[:], in_=op_[:].rearrange("p a b -> p (a b)"), func=AF.Square, accum_out=ssq[:, c:c + 1]),
                              reads=[ko], writes=["junk3", "p3ssq"])
                        rstd_from_ssq(ssq[:, c:c + 1], rst[:, c:c + 1], D, "p3")
                        kb.op("dve", lambda e, c=c, op_=op_, tm_=tm_: e.scalar_tensor_tensor(out=tm_[:], in0=op_[:].rearrange("p a b -> p (a b)"), scalar=rst[:, c:c + 1], in1=g3[:, 0, :], op0=ALU.mult, op1=ALU.mult),
                              reads=[ko, "p3rs", "g3"], writes=[kt])
                        kb.op("pool", lambda e, xs=xs, x1s=x1s, tm_=tm_: e.tensor_tensor(out=x1s[:], in0=xs[:], in1=tm_[:], op=ALU.add),
                              reads=[("xt3", t % 2), kt], writes=[("x1", (G % 2) * 4 + s)])
                        c2 = sq % 16
                        sq += 1
                        kb.op("act", lambda e, c2=c2, x1s=x1s: e.activation(out=junk[:], in_=x1s[:], func=AF.Square, accum_out=ssq[:, c2:c2 + 1]),
                              reads=[("x1", (G % 2) * 4 + s)], writes=["junk3", "p3ssq"])
                        rstd_from_ssq(ssq[:, c2:c2 + 1], rst[:, c2:c2 + 1], D, "p3")
                        hbs = hb[s]
                        kb.op("dve", lambda e, c2=c2, x1s=x1s, hbs=hbs: e.scalar_tensor_tensor(out=hbs[:], in0=x1s[:], scalar=rst[:, c2:c2 + 1], in1=g3[:, 1, :], op0=ALU.mult, op1=ALU.mult),
                              reads=[("x1", (G % 2) * 4 + s), "p3rs", "g3"], writes=[("hb3", s)])

                def p3_trans(G):
                    for s in range(4):
                        hbs = hb[s]
                        for kc in range(8):
                            tp(tps[:, kc * 128:(kc + 1) * 128], hbs[:, kc * 128:(kc + 1) * 128], ident[:], reads=[("hb3", s), "ident"], writes=["p3t"])
                        kb.op("act", lambda e, s=s: e.copy(out=hT[:, :, s * 128:(s + 1) * 128], in_=tps[:].rearrange("p (k c) -> p k c", k=8)),
                              reads=["p3t"], writes=["hT3"])

                def p3_gateup(G):
                    for fc in range(NFC):
                        wslot = wq.pop(0)
                        nxt = fc + PRE
                        if nxt < NFC:
                            wq.append(loadw(nxt))
                        w_ = wgu[wslot]
                        gp, up = gus[(2 * fc) % 3], gus[(2 * fc + 1) % 3]
                        kgp, kup = ("p3gu", (2 * fc) % 3), ("p3gu", (2 * fc + 1) % 3)
                        for kc in range(8):
                            mm(gp[:], w_[:, 0, kc, :], hT[:, kc, :], kc == 0, kc == 7, reads=[("wgu", wslot), "hT3"], writes=[kgp])
                        for kc in range(8):
                            mm(up[:], w_[:, 1, kc, :], hT[:, kc, :], kc == 0, kc == 7, reads=[("wgu", wslot), "hT3"], writes=[kup])
                        sg_ = sgt[fc % 2]
                        kb.op("act", lambda e, sg_=sg_, gp=gp: e.activation(out=sg_[:], in_=gp[:], func=AF.Silu), reads=[kgp], writes=[("sgt", fc % 2)])
                        kb.op("dve", lambda e, sg_=sg_, up=up, fc=fc: e.tensor_tensor(out=actT[:, fc, :], in0=up[:], in1=sg_[:], op=ALU.mult),
                              reads=[kup, ("sgt", fc % 2)], writes=["actT"])

                def p3_down(G):
                    nonlocal sq
                    for s in range(4):
                        t = G * 4 + s
                        op_ = ops_[s % 2]
                        ko = ("p3o", s % 2)
                        tm_ = tmp[s % 2]
                        kt = ("tmp3", s % 2)
                        for hf in range(2):
                            for fc in range(NFC):
                                mm(op_[:, hf, :], actT[:, fc, s * 128:(s + 1) * 128], wdn[:, fc, hf * 512:(hf + 1) * 512], fc == 0, fc == NFC - 1,
                                   reads=["actT", "wdn"], writes=[ko])
                        c = sq % 16
                        sq += 1
                        kb.op("act", lambda e, c=c, op_=op_: e.activation(out=junk[:], in_=op_[:].rearrange("p a b -> p (a b)"), func=AF.Square, accum_out=ssq[:, c:c + 1]),
                              reads=[ko], writes=["junk3", "p3ssq"])
                        rstd_from_ssq(ssq[:, c:c + 1], rst[:, c:c + 1], D, "p3")
                        kb.op("dve", lambda e, c=c, op_=op_, tm_=tm_: e.scalar_tensor_tensor(out=tm_[:], in0=op_[:].rearrange("p a b -> p (a b)"), scalar=rst[:, c:c + 1], in1=g3[:, 2, :], op0=ALU.mult, op1=ALU.mult),
                              reads=[ko, "p3rs", "g3"], writes=[kt])
                        xos = xo[t % 2]
                        kb.op("pool", lambda e, xos=xos, s=s, tm_=tm_: e.tensor_tensor(out=xos[:], in0=x1[(G % 2) * 4 + s][:], in1=tm_[:], op=ALU.add),
                              reads=[("x1", (G % 2) * 4 + s), kt], writes=[("xo", t % 2)])
                        dma("sp", xout_d[t * 128:(t + 1) * 128, :], xos[:], reads=[("xo", t % 2)], stream="o", n=4)

                for G in range(NG):
                    if G + 1 < NG:
                        loadg(G + 1)
                    while len(wq) < PRE:
                        wq.append(loadw(len(wq)))
                    p3_front(G)
                    if G > 0:
                        p3_down(G - 1)
                    p3_trans(G)
                    p3_gateup(G)
                p3_down(NG - 1)
                end_phase()

        kb.barrier()
        import os as _os2
        if not _os2.environ.get("SKIP_P0"):
            phase0()
        done = stop_after == "p0"
        for l in range(L):
            if done:
                break
            xin = x_d if l == 0 else xs1_d
            xout = xs1_d if l == 0 else out_d
            for nm, fn in (("p1", lambda: phase1(l, xin)), ("p2b", lambda: phase2ab(l)),
                           ("p2c", lambda: phase2c(l)), ("p3", lambda: phase3(l, xin, xout))):
                fn()
                if stop_after == (l, nm):
                    done = True
                    break
            if done:
                break
        kb.barrier()
        kb.emit()

    return nc


def _host_inputs(inputs):
    f = lambda a: np.ascontiguousarray(np.asarray(a, dtype=np.float32))
    c = _consts()
    idx_diag, idx_off1 = _bias_idx()
    rel = f(inputs["rel_bias"])
    shared = {
        "norms": f(np.stack([inputs["pre_mix_norm"], inputs["post_mix_norm"], inputs["pre_ffn_norm"], inputs["post_ffn_norm"]], axis=1)),
        "w_in": f(inputs["w_in"]), "w_out": f(inputs["w_out"]),
        "w_ffn_gate": f(inputs["w_ffn_gate"]), "w_ffn_up": f(inputs["w_ffn_up"]), "w_ffn_down": f(inputs["w_ffn_down"]),
        "lru_wa": f(inputs["lru_wa"]), "lru_wx": f(inputs["lru_wx"]),
        "gla_gate_w2": f(inputs["gla_gate_w2"]),
        "gla_gate_b": f(np.asarray(inputs["gla_gate_b"]).reshape(L, 128, 1)),
        "gla_norm": f(inputs["gla_norm"]),
        "rb31": f(rel[31:32, :]),
        "tdg": f(np.transpose(rel[idx_diag], (0, 2, 1))),
        "tof": f(np.transpose(rel[idx_off1], (0, 2, 1))),
        "ident": c["ident"], "tri": c["tri"], "caus": c["caus"], "e16": c["e16"],
        "cm": c["cm"], "pm": c["pm"], "bmask": c["bmask"], "hm": c["hm"],
    }
    cw = np.transpose(np.asarray(inputs["lru_conv_w"], dtype=np.float32), (0, 2, 1))
    cols = np.concatenate([cw] + [np.asarray(inputs[k], dtype=np.float32)[:, :, None]
                                  for k in ("lru_conv_b", "lru_ba", "lru_bx", "lru_lambda")], axis=2)
    shared["lru_cols"] = f(cols.reshape(L, 2, 128, 8))
    x = np.asarray(inputs["x"], dtype=np.float32)
    return [dict(shared, x=np.ascontiguousarray(x[b])) for b in range(x.shape[0])]


_NC_CACHE = {}


def kernel(**inputs):
    in_maps = _host_inputs(inputs)
    if "nc" not in _NC_CACHE:
        _NC_CACHE["nc"] = build()
    nc = _NC_CACHE["nc"]
    n = len(in_maps)
    res = run_bass_kernel_spmd(nc, in_maps, core_ids=list(range(n)))
    return np.stack([np.asarray(r["out"], dtype=np.float32) for r in res.results], axis=0)
```

```python
from contextlib import ExitStack
import math
import numpy as np
import ml_dtypes
import concourse.bass as bass
import concourse.mybir as mybir
from concourse.bass_utils import run_bass_kernel_spmd

F32 = mybir.dt.float32
BF16 = mybir.dt.bfloat16
ALU = mybir.AluOpType
AF = mybir.ActivationFunctionType
AX = mybir.AxisListType

S = 4096
D = 1024
L = 2
DIN = 2832
DFF = 2816
NFC = DFF // 128
EPS = 1e-6
NEG = -30000.0
ENGS = ("pe", "act", "dve", "pool", "sp")


class KB:
    def __init__(self, nc, stack, sync_same=True):
        self.nc = nc
        self.stack = stack
        self.sync_same = sync_same
        self.ops = {e: [] for e in ENGS}
        self.sem = {}
        self.cnt = {}
        self.step = {}
        self.known = {e: {} for e in ENGS}
        self.lw = {}
        self.rd = {}
        for e in ENGS:
            self._dom(e, 1)

    def _dom(self, name, step):
        if name not in self.sem:
            self.sem[name] = self.stack.enter_context(self.nc.semaphore("s_" + name))
            self.cnt[name] = 0
            self.step[name] = step
        return name

    def op(self, eng, fn, reads=(), writes=(), dma=None):
        dom = eng if dma is None else self._dom("d_" + dma, 16)
        deps = {}

        def add(d):
            if d is not None and deps.get(d[0], 0) < d[1]:
                deps[d[0]] = d[1]

        for k in reads:
            add(self.lw.get(k))
        for k in writes:
            add(self.lw.get(k))
            for dm, c in self.rd.get(k, {}).items():
                add((dm, c))
        if dma is not None and self.cnt[dom] > 0:
            add((dom, self.cnt[dom]))
        kn = self.known[eng]
        for d, c in deps.items():
            if d == eng and (eng == "pe" or not self.sync_same):
                continue
            if kn.get(d, 0) >= c:
                continue
            self.ops[eng].append(("w", self.sem[d], c))
            kn[d] = c
        self.cnt[dom] += self.step[dom]
        me = (dom, self.cnt[dom])
        self.ops[eng].append(("o", fn, self.sem[dom], self.step[dom]))
        for k in writes:
            self.lw[k] = me
            self.rd[k] = {}
        for k in reads:
            r = self.rd.setdefault(k, {})
            if r.get(dom, 0) < me[1]:
                r[dom] = me[1]
        return me

    def barrier(self):
        for eng in ENGS:
            kn = self.known[eng]
            for dom, c in self.cnt.items():
                if c > 0 and dom != eng and kn.get(dom, 0) < c:
                    self.ops[eng].append(("w", self.sem[dom], c))
                    kn[dom] = c
        self.lw = {}
        self.rd = {}

    def emit(self):
        nc = self.nc
        ops = self.ops

        def run(lst, e):
            for it in lst:
                if it[0] == "w":
                    e.wait_ge(it[1], it[2])
                else:
                    it[1](e).then_inc(it[2], it[3])

        with nc.Block() as block:
            @block.tensor
            def _(e):
                run(ops["pe"], e)

            @block.scalar
            def _(e):
                run(ops["act"], e)

            @block.vector
            def _(e):
                run(ops["dve"], e)

            @block.gpsimd
            def _(e):
                run(ops["pool"], e)

            @block.sync
            def _(e):
                run(ops["sp"], e)
        self.ops = {e: [] for e in ENGS}


def _t5_bucket(n):
    n = np.maximum(n, 0)
    nf = np.maximum(n, 1).astype(np.float32)
    large = 16 + (np.log(nf / np.float32(16)) / np.float32(math.log(128 / 16)) * np.float32(16)).astype(np.int32)
    large = np.minimum(large, 31)
    return np.where(n < 16, n, large)


def _consts():
    c = {}
    c["ident"] = np.eye(128, dtype=np.float32).astype(ml_dtypes.bfloat16)
    e = np.arange(128)
    c["tri"] = (e[:, None] <= e[None, :]).astype(np.float32)
    c["caus"] = np.where(e[None, :] >= e[:, None], 0.0, NEG).astype(np.float32)
    keys = np.arange(S)
    c["e16"] = (keys[None, :] // 256 == np.arange(16)[:, None]).astype(np.float32).astype(ml_dtypes.bfloat16)
    npast = np.arange(16)[:, None]
    nn = np.arange(16)[None, :]
    c["cm"] = np.where(nn < npast, 0.0, -1e30).astype(np.float32).reshape(1, 256)
    c["pm"] = (nn < npast).astype(np.float32).reshape(1, 256)
    p = np.arange(128)[:, None]
    c["bmask"] = (p // 32 == (np.arange(256)[None, :] // 64)).astype(np.float32)
    c["hm"] = (p // 32 == np.arange(4)[None, :]).astype(np.float32)
    return c


def _bias_idx():
    k = np.arange(128)[:, None]
    q = np.arange(128)[None, :]
    idx_diag = _t5_bucket(q - k)
    idx_off1 = _t5_bucket(q + 128 - k)
    return idx_diag, idx_off1


def build(debug=False, stop_after=None):
    nc = bass.Bass("TRN2", target_bir_lowering=False)
    dr = lambda name, shape, dt, kind="Internal": nc.dram_tensor(name, list(shape), dt, kind=kind).ap()
    IN = "ExternalInput"
    x_d = dr("x", [S, D], F32, IN)
    norms_d = dr("norms", [L, 4, D], F32, IN)
    w_in_d = dr("w_in", [L, D, DIN], F32, IN)
    w_out_d = dr("w_out", [L, D, D], F32, IN)
    wg_d = dr("w_ffn_gate", [L, D, DFF], F32, IN)
    wu_d = dr("w_ffn_up", [L, D, DFF], F32, IN)
    wd_d = dr("w_ffn_down", [L, DFF, D], F32, IN)
    lcols_d = dr("lru_cols", [L, 2, 128, 8], F32, IN)
    lwa_d = dr("lru_wa", [L, 4, 64, 64], F32, IN)
    lwx_d = dr("lru_wx", [L, 4, 64, 64], F32, IN)
    gw2_d = dr("gla_gate_w2", [L, 16, 128], F32, IN)
    gb_d = dr("gla_gate_b", [L, 128, 1], F32, IN)
    gn_d = dr("gla_norm", [L, 256], F32, IN)
    rb31_d = dr("rb31", [1, 8], F32, IN)
    tdg_d = dr("tdg", [128, 8, 128], F32, IN)
    tof_d = dr("tof", [128, 8, 128], F32, IN)
    ident_d = dr("ident", [128, 128], BF16, IN)
    tri_d = dr("tri", [128, 128], F32, IN)
    caus_d = dr("caus", [128, 128], F32, IN)
    e16_d = dr("e16", [16, S], BF16, IN)
    cm_d = dr("cm", [1, 256], F32, IN)
    pm_d = dr("pm", [1, 256], F32, IN)
    bmask_d = dr("bmask", [128, 256], F32, IN)
    hm_d = dr("hm", [128, 4], F32, IN)
    out_d = dr("out", [S, D], F32, "ExternalOutput")

    dk = "ExternalOutput" if debug else "Internal"
    winb_d = dr("winb", [L, D, DIN], BF16)
    woutb_d = dr("woutb", [L, D, D], BF16)
    wgub_d = dr("wgub", [L, NFC, 128, 2, 8, 128], BF16)
    wdb_d = dr("wdb", [L, DFF, D], BF16)
    xs1_d = dr("xs1", [S, D], F32, dk)
    lruT_d = dr("lruT", [512, S], F32, dk)
    gqT_d = dr("gqT", [128, S], F32, dk)
    gkT_d = dr("gkT", [128, S], F32, dk)
    glrT_d = dr("glrT", [16, S], BF16, dk)
    mqT_d = dr("mqT", [512, S], BF16, dk)
    mkT_d = dr("mkT", [512, S], BF16, dk)
    gv_d = dr("gv", [S, 256], BF16, dk)
    gout_d = dr("gout", [S, 256], F32, dk)
    mvp_d = dr("mvp", [S, 520], BF16, dk)
    mixT_d = dr("mixT", [D, S], BF16, dk)

    with ExitStack() as st:
        kb = KB(nc, st)
        uid = [0]

        def sbt(ctx, shape, dt, name=None):
            uid[0] += 1
            return ctx.enter_context(nc.sbuf_tensor("%s_%d" % (name or "t", uid[0]), list(shape), dt))

        def pst(ctx, shape, dt, name=None):
            uid[0] += 1
            return ctx.enter_context(nc.psum_tensor("%s_%d" % (name or "p", uid[0]), list(shape), dt))

        rr = {}

        def dmaname(stream, n):
            i = rr.get(stream, 0)
            rr[stream] = i + 1
            return "%s%d" % (stream, i % n)

        def dma(eng, out, in_, reads=(), writes=(), stream="g", n=4):
            kb.op(eng, lambda e: e.dma_start(out=out, in_=in_), reads=reads, writes=writes, dma=dmaname(stream, n))

        def mm(out, lhsT, rhs, start, stop, reads, writes, **kw):
            kb.op("pe", lambda e: e.matmul(out, lhsT=lhsT, rhs=rhs, start=start, stop=stop, **kw),
                  reads=reads, writes=writes)

        def tp(out, in_, ident, reads, writes):
            kb.op("pe", lambda e: e.transpose(out, in_, ident), reads=reads, writes=writes)

        ident = sbt(st, [128, 128], BF16, "ident")
        dma("sp", ident[:], ident_d, writes=["ident"])

        def end_phase():
            kb.barrier()
            kb.emit()

        def phase0():
            for l in range(L):
                for c0 in range(0, DIN, 944):
                    for kc in range(8):
                        r0 = kc * 128
                        dma("pool", winb_d[l, r0:r0 + 128, c0:c0 + 944], w_in_d[l, r0:r0 + 128, c0:c0 + 944], writes=[("winb", l, kc, c0)], stream="cast", n=4)
                if l == 0:
                    continue
            for l in range(L):
                for kc in range(8):
                    r0 = kc * 128
                    dma("pool", woutb_d[l, r0:r0 + 128, :], w_out_d[l, r0:r0 + 128, :], stream="cast", n=4)
                for fc in range(NFC):
                    for gu, wsrc in enumerate((wg_d, wu_d)):
                        dma("pool", wgub_d[l, fc, :, gu, :, :],
                            wsrc[l].rearrange("(kc p) f -> p kc f", p=128)[:, :, fc * 128:(fc + 1) * 128],
                            stream="cast", n=4)
                    dma("pool", wdb_d[l, fc * 128:(fc + 1) * 128, :], wd_d[l, fc * 128:(fc + 1) * 128, :], stream="cast", n=4)
            kb.emit()

        def rstd_from_ssq(ssq, rstd, n, tag):
            kb.op("dve", lambda e: e.tensor_scalar(out=rstd, in0=ssq, scalar1=1.0 / n, scalar2=EPS, op0=ALU.mult, op1=ALU.add),
                  reads=[tag + "ssq"], writes=[tag + "rs"])
            kb.op("act", lambda e: e.sqrt(out=rstd, in_=rstd), reads=[tag + "rs"], writes=[tag + "rs"])
            kb.op("dve", lambda e: e.reciprocal(out=rstd, in_=rstd), reads=[tag + "rs"], writes=[tag + "rs"])

        def phase1(l, xin_d):
            with ExitStack() as ph:
                win = sbt(ph, [128, 8, DIN], BF16, "win")
                gpre = sbt(ph, [128, D], F32, "gpre")
                xt = [sbt(ph, [128, D], F32, "xt") for _ in range(8)]
                hb = [sbt(ph, [128, D], BF16, "hb") for _ in range(4)]
                junk = sbt(ph, [128, D], BF16, "junk")
                hT = [sbt(ph, [128, 8, 512], BF16, "hT") for _ in range(2)]
                ssq = sbt(ph, [128, 8], F32, "ssq")
                rst = sbt(ph, [128, 8], F32, "rst")
                sf = [sbt(ph, [128, 512], F32, "sf") for _ in range(6)]
                sbf = [sbt(ph, [128, 512], BF16, "sbf") for _ in range(6)]
                sgv = [sbt(ph, [128, 256], BF16, "sgv") for _ in range(4)]
                sgo = [sbt(ph, [128, 256], F32, "sgo") for _ in range(4)]
                smv = [sbt(ph, [128, 8, 65], BF16, "smv") for _ in range(4)]
                tps = [pst(ph, [128, D], BF16, "tps") for _ in range(2)]
                aps = [pst(ph, [128, 512], F32, "aps") for _ in range(5)]
                for c0_ in range(0, DIN, 944):
                    for kc in range(8):
                        dma("sp", win[:, kc, c0_:c0_ + 944], winb_d[l, kc * 128:(kc + 1) * 128, c0_:c0_ + 944], reads=[("winb", l, kc, c0_)], writes=[("win", kc, c0_)], stream="w", n=8)

                def wink(c_lo, c_hi):
                    return [("win", kc_, cb_) for kc_ in range(8) for cb_ in range(0, DIN, 944) if cb_ < c_hi and cb_ + 944 > c_lo]

                dma("sp", gpre[:], norms_d[l, 0:1, :].partition_broadcast(128), writes=["gpre"])
                for i in range(4):
                    kb.op("pool", lambda e, i=i: e.memset(smv[i][:], 1.0), writes=[("smv", i)])

                flist = [("lru", lruT_d, 0, 0, 128, F32), ("lru", lruT_d, 128, 128, 128, F32),
                         ("lru", lruT_d, 256, 256, 128, F32), ("lru", lruT_d, 384, 384, 128, F32),
                         ("gq", gqT_d, 0, 512, 128, F32), ("gk", gkT_d, 0, 640, 128, F32),
                         ("glr", glrT_d, 0, 1024, 16, BF16)]
                for i in range(4):
                    flist.append(("mq", mqT_d, i * 128, 1296 + i * 128, 128, BF16))
                for i in range(4):
                    flist.append(("mk", mkT_d, i * 128, 1808 + i * 128, 128, BF16))

                def load(t):
                    dma("sp", xt[t % 8][:], xin_d[t * 128:(t + 1) * 128, :], writes=[("xt", t % 8)], stream="x", n=4)

                NT = S // 128
                pi = 0
                ev = 0
                import os as _os
                _ng = int(_os.environ.get("P1_GROUPS", S // 512))
                _parts = int(_os.environ.get("P1_PARTS", 7))

                def chain(g):
                    for s in range(4):
                        t = g * 4 + s
                        xs = xt[t % 8]
                        hbs = hb[s]
                        c = t % 8
                        kb.op("act", lambda e, xs=xs, c=c: e.activation(out=junk[:], in_=xs[:], func=AF.Square, accum_out=ssq[:, c:c + 1]),
                              reads=[("xt", t % 8)], writes=["junk", "p1ssq"])
                        rstd_from_ssq(ssq[:, c:c + 1], rst[:, c:c + 1], D, "p1")
                        kb.op("dve", lambda e, xs=xs, hbs=hbs, c=c: e.scalar_tensor_tensor(out=hbs[:], in0=xs[:], scalar=rst[:, c:c + 1], in1=gpre[:], op0=ALU.mult, op1=ALU.mult),
                              reads=[("xt", t % 8), "p1rs", "gpre"], writes=[("hb", s)])

                def transp(g):
                    hTg_ = hT[g % 2]
                    for s in range(4):
                        t = g * 4 + s
                        hbs = hb[s]
                        tpp = tps[t % 2]
                        for kc in range(8):
                            tp(tpp[:, kc * 128:(kc + 1) * 128], hbs[:, kc * 128:(kc + 1) * 128], ident[:],
                               reads=[("hb", s), "ident"], writes=[("tps", t % 2)])
                        kb.op("act", lambda e, tpp=tpp, hTg_=hTg_, s=s: e.copy(out=hTg_[:, :, s * 128:(s + 1) * 128], in_=tpp[:].rearrange("p (k c) -> p k c", k=8)),
                              reads=[("tps", t % 2)], writes=[("hT", g % 2)])

                for t in range(8):
                    load(t)
                chain(0)
                transp(0)
                for g in range(_ng):
                    hTg = hT[g % 2]
                    if g + 1 < _ng:
                        chain(g + 1)
                    if g + 2 < _ng:
                        for s in range(4):
                            load((g + 2) * 4 + s)
                    for (nm, dst, drow, wcol, wid, dt) in (flist if _parts & 2 else []):
                        ps = aps[pi % 5]
                        pk = ("aps", pi % 5)
                        pi += 1
                        for kc in range(8):
                            mm(ps[0:wid, :], win[:, kc, wcol:wcol + wid], hTg[:, kc, :], kc == 0, kc == 7,
                               reads=wink(wcol, wcol + wid) + [("hT", g % 2)], writes=[pk])
                        if dt == F32:
                            stg = sf[ev % 6]
                            sk = ("sf", ev % 6)
                        else:
                            stg = sbf[ev % 6]
                            sk = ("sbf", ev % 6)
                        eng = "act" if ev % 2 == 0 else "dve"
                        ev += 1
                        if nm == "mq":
                            if eng == "act":
                                kb.op("act", lambda e, stg=stg, ps=ps, wid=wid: e.mul(out=stg[0:wid, :], in_=ps[0:wid, :], mul=0.125), reads=[pk], writes=[sk])
                            else:
                                kb.op("dve", lambda e, stg=stg, ps=ps, wid=wid: e.tensor_scalar(out=stg[0:wid, :], in0=ps[0:wid, :], scalar1=0.125, scalar2=None, op0=ALU.mult), reads=[pk], writes=[sk])
                        else:
                            if eng == "act":
                                kb.op("act", lambda e, stg=stg, ps=ps, wid=wid: e.copy(out=stg[0:wid, :], in_=ps[0:wid, :]), reads=[pk], writes=[sk])
                            else:
                                kb.op("dve", lambda e, stg=stg, ps=ps, wid=wid: e.tensor_copy(out=stg[0:wid, :], in_=ps[0:wid, :]), reads=[pk], writes=[sk])
                        dma("sp", dst[drow:drow + wid, g * 512:(g + 1) * 512], stg[0:wid, :], reads=[sk], stream="o1", n=12)
                    if g + 1 < _ng:
                        transp(g + 1)
                    for s in (range(4) if _parts & 4 else []):
                        t = g * 4 + s
                        _tm = int(_os.environ.get("TM_SKIP", 0))
                        if not _tm & 1:
                            ps = aps[pi % 5]
                            pk = ("aps", pi % 5)
                            pi += 1
                            ps2 = aps[pi % 5]
                            pk2 = ("aps", pi % 5)
                            pi += 1
                            for kc in range(8):
                                mm(ps[:, 0:256], hTg[:, kc, s * 128:(s + 1) * 128], win[:, kc, 768:1024], kc == 0, kc == 7,
                                   reads=wink(768, 1024) + [("hT", g % 2)], writes=[pk])
                            for kc in range(8):
                                mm(ps2[:, 0:256], hTg[:, kc, s * 128:(s + 1) * 128], win[:, kc, 1040:1296], kc == 0, kc == 7,
                                   reads=wink(1040, 1296) + [("hT", g % 2)], writes=[pk2])
                            a, b = sgv[t % 4], sgo[t % 4]
                            kb.op("act", lambda e, a=a, ps=ps: e.copy(out=a[:], in_=ps[:, 0:256]), reads=[pk], writes=[("sgv", t % 4)])
                            kb.op("dve", lambda e, b=b, ps2=ps2: e.tensor_copy(out=b[:], in_=ps2[:, 0:256]), reads=[pk2], writes=[("sgo", t % 4)])
                            dma("sp", gv_d[t * 128:(t + 1) * 128, :], a[:], reads=[("sgv", t % 4)], stream="o1", n=12)
                            dma("sp", gout_d[t * 128:(t + 1) * 128, :], b[:], reads=[("sgo", t % 4)], stream="o1", n=12)
                        if not _tm & 2:
                            ps = aps[pi % 5]
                            pk = ("aps", pi % 5)
                            pi += 1
                            for kc in range(8):
                                mm(ps[:, :], hTg[:, kc, s * 128:(s + 1) * 128], win[:, kc, 2320:2832], kc == 0, kc == 7,
                                   reads=wink(2320, 2832) + [("hT", g % 2)], writes=[pk])
                            m = smv[t % 4]
                            psv = ps[:].rearrange("p (h d) -> p h d", h=8)
                            if _tm & 4:
                                pass
                            elif t % 2:
                                kb.op("act", lambda e, m=m, psv=psv: e.copy(out=m[:, :, 0:64], in_=psv), reads=[pk], writes=[("smv", t % 4)])
                            else:
                                kb.op("dve", lambda e, m=m, psv=psv: e.tensor_copy(out=m[:, :, 0:64], in_=psv), reads=[pk], writes=[("smv", t % 4)])
                            if not _tm & 8:
                                dma("sp", mvp_d[t * 128:(t + 1) * 128, :], m[:].rearrange("p h d -> p (h d)"), reads=[("smv", t % 4)], stream="o1", n=12)
                if _os.environ.get("P1_TAILSTORE"):
                    dma("sp", lruT_d[0:128, 0:8], rst[:], reads=["p1rs"], stream="o1", n=12)
                end_phase()

        def phase2a_gen(l, ph):
            TB = 1024
            if True:
                cols = sbt(ph, [128, 2, 8], F32, "lcols")
                ccol = sbt(ph, [128, 2], F32, "ccol")
                wstage = sbt(ph, [128, 2, 2, 128], F32, "wstage")
                wbd = sbt(ph, [128, 2, 2, 128], BF16, "wbd")
                xin = [sbt(ph, [128, TB + 3], F32, "xin") for _ in range(2)]
                gin = [sbt(ph, [128, TB], F32, "gin") for _ in range(2)]
                xc = sbt(ph, [128, TB], F32, "xc")
                xcb = sbt(ph, [128, TB], BF16, "xcb")
                rr_ = sbt(ph, [128, TB], F32, "r")
                ii_ = sbt(ph, [128, TB], F32, "i")
                aa = sbt(ph, [128, TB], F32, "a")
                mmul = sbt(ph, [128, TB], F32, "mult")
                uu = sbt(ph, [128, TB], F32, "u")
                hh = [sbt(ph, [128, TB], F32, "h") for _ in range(2)]
                gt = sbt(ph, [128, TB], F32, "gt")
                gs = sbt(ph, [128, TB], F32, "gs")
                yb = [sbt(ph, [128, TB], BF16, "yb") for _ in range(2)]
                gps = [pst(ph, [128, 512], F32, "gps") for _ in range(2)]
                for h in range(2):
                    dma("sp", cols[:, h, :], lcols_d[l, h], writes=["lcols"])
                kb.op("pool", lambda e: e.memset(wstage[:], 0.0), writes=["wstage"])
                for ax, src in enumerate((lwa_d, lwx_d)):
                    for h in range(2):
                        for b in range(2):
                            dma("sp", wstage[b * 64:(b + 1) * 64, ax, h, b * 64:(b + 1) * 64], src[l, 2 * h + b],
                                reads=[], writes=["wstage"])
                kb.op("dve", lambda e: e.tensor_copy(out=wbd[:], in_=wstage[:]), reads=["wstage"], writes=["wbd"])
                kb.op("act", lambda e: e.activation(out=ccol[:], in_=cols[:, :, 7], func=AF.Exp, scale=-1.0), reads=["lcols"], writes=["ccol"])
                kb.op("act", lambda e: e.activation(out=ccol[:], in_=ccol[:], func=AF.Ln, bias=1.0), reads=["ccol"], writes=["ccol"])
                kb.op("dve", lambda e: e.tensor_scalar(out=ccol[:], in0=ccol[:], scalar1=-8.0, scalar2=None, op0=ALU.mult), reads=["ccol"], writes=["ccol"])

                nb = S // TB
                it = 0
                for h in range(2):
                    for b in range(nb):
                        t0 = b * TB
                        xi = xin[it % 2]
                        gi = gin[it % 2]
                        hcur = hh[it % 2]
                        hprev = hh[(it + 1) % 2]
                        ybs = yb[it % 2]
                        kx, kg, ky = ("xin", it % 2), ("gin", it % 2), ("yb", it % 2)
                        kh, khp = ("h", it % 2), ("h", (it + 1) % 2)
                        it += 1
                        if b == 0:
                            kb.op("pool", lambda e, xi=xi: e.memset(xi[:, 0:3], 0.0), writes=[kx])
                            dma("sp", xi[:, 3:], lruT_d[h * 128:(h + 1) * 128, 0:TB], writes=[kx], stream="x", n=3)
                        else:
                            dma("sp", xi[:], lruT_d[h * 128:(h + 1) * 128, t0 - 3:t0 + TB], writes=[kx], stream="x", n=3)
                        dma("sp", gi[:], lruT_d[256 + h * 128:256 + (h + 1) * 128, t0:t0 + TB], writes=[kg], stream="x", n=3)
                        yield
                        kb.op("dve", lambda e, xi=xi, h=h: e.tensor_scalar(out=xc[:], in0=xi[:, 3:TB + 3], scalar1=cols[:, h, 3:4], scalar2=cols[:, h, 4:5], op0=ALU.mult, op1=ALU.add),
                              reads=[kx, "lcols"], writes=["xc"])
                        for j in range(3):
                            kb.op("dve", lambda e, xi=xi, h=h, j=j: e.scalar_tensor_tensor(out=xc[:], in0=xi[:, j:TB + j], scalar=cols[:, h, j:j + 1], in1=xc[:], op0=ALU.mult, op1=ALU.add),
                                  reads=[kx, "lcols", "xc"], writes=["xc"])
                        yield
                        kb.op("pool", lambda e: e.tensor_copy(out=xcb[:], in_=xc[:]), reads=["xc"], writes=["xcb"])
                        for sblk in range(TB // 512):
                            cs = slice(sblk * 512, (sblk + 1) * 512)
                            pa, px = gps[0], gps[1]
                            ka, kx_ = ("gps", 0), ("gps", 1)
                            mm(pa[:], wbd[:, 0, h, :], xcb[:, cs], True, True, reads=["wbd", "xcb"], writes=[ka])
                            mm(px[:], wbd[:, 1, h, :], xcb[:, cs], True, True, reads=["wbd", "xcb"], writes=[kx_])
                            kb.op("act", lambda e, pa=pa, cs=cs, h=h: e.activation(out=rr_[:, cs], in_=pa[:], func=AF.Sigmoid, bias=cols[:, h, 5:6]),
                                  reads=[ka, "lcols"], writes=["r"])
                            kb.op("act", lambda e, px=px, cs=cs, h=h: e.activation(out=ii_[:, cs], in_=px[:], func=AF.Sigmoid, bias=cols[:, h, 6:7]),
                                  reads=[kx_, "lcols"], writes=["i"])
                        yield
                        kb.op("pool", lambda e, gi=gi: e.tensor_tensor(out=gt[:], in0=gi[:], in1=gi[:], op=ALU.mult), reads=[kg], writes=["gt"])
                        kb.op("pool", lambda e: e.tensor_scalar(out=gt[:], in0=gt[:], scalar1=0.044715, scalar2=1.0, op0=ALU.mult, op1=ALU.add), reads=["gt"], writes=["gt"])
                        kb.op("pool", lambda e, gi=gi: e.tensor_tensor(out=gt[:], in0=gt[:], in1=gi[:], op=ALU.mult), reads=["gt", kg], writes=["gt"])
                        kb.op("act", lambda e: e.activation(out=gs[:], in_=gt[:], func=AF.Sigmoid, scale=1.5957691216057308), reads=["gt"], writes=["gs"])
                        kb.op("pool", lambda e, gi=gi: e.tensor_tensor(out=gs[:], in0=gs[:], in1=gi[:], op=ALU.mult), reads=["gs", kg], writes=["gs"])
                        yield
                        kb.op("act", lambda e, h=h: e.activation(out=aa[:], in_=rr_[:], func=AF.Exp, scale=ccol[:, h:h + 1]), reads=["r", "ccol"], writes=["a"])
                        kb.op("pool", lambda e: e.tensor_tensor(out=mmul[:], in0=aa[:], in1=aa[:], op=ALU.mult), reads=["a"], writes=["mult"])
                        kb.op("act", lambda e: e.activation(out=mmul[:], in_=mmul[:], func=AF.Sqrt, scale=-1.0, bias=1.0), reads=["mult"], writes=["mult"])
                        yield
                        if b == 0:
                            kb.op("dve", lambda e: e.memset(mmul[:, 0:1], 1.0), reads=["mult"], writes=["mult"])
                        kb.op("dve", lambda e: e.tensor_tensor(out=uu[:], in0=ii_[:], in1=xc[:], op=ALU.mult), reads=["i", "xc"], writes=["u"])
                        kb.op("dve", lambda e: e.tensor_tensor(out=uu[:], in0=uu[:], in1=mmul[:], op=ALU.mult), reads=["u", "mult"], writes=["u"])
                        yield
                        if b == 0:
                            kb.op("dve", lambda e, hcur=hcur: e.tensor_tensor_scan(out=hcur[:], data0=aa[:], data1=uu[:], initial=0.0, op0=ALU.mult, op1=ALU.add),
                                  reads=["a", "u"], writes=[kh])
                        else:
                            kb.op("dve", lambda e, hcur=hcur, hprev=hprev: e.tensor_tensor_scan(out=hcur[:], data0=aa[:], data1=uu[:], initial=hprev[:, TB - 1:TB], op0=ALU.mult, op1=ALU.add),
                                  reads=["a", "u", khp], writes=[kh])
                        yield
                        kb.op("dve", lambda e, hcur=hcur, ybs=ybs: e.tensor_tensor(out=ybs[:], in0=hcur[:], in1=gs[:], op=ALU.mult), reads=[kh, "gs"], writes=[ky])
                        dma("sp", mixT_d[h * 128:(h + 1) * 128, t0:t0 + TB], ybs[:], reads=[ky], stream="o", n=4)
                        yield

        def phase2b_gen(l, ph):
            TB = 1024
            NCH = TB // 128
            if True:
                w2s = sbt(ph, [16, 128], F32, "w2s")
                w2b = sbt(ph, [16, 128], BF16, "w2b")
                negb = sbt(ph, [128, 1], F32, "negb")
                gn = sbt(ph, [128, 256], F32, "gn")
                tri = sbt(ph, [128, 128], F32, "tri")
                bmask = sbt(ph, [128, 256], F32, "bmask")
                hm = sbt(ph, [128, 4], F32, "hm")
                ones = sbt(ph, [128, 128], F32, "ones")
                glr = [sbt(ph, [16, TB], BF16, "glr") for _ in range(2)]
                qT = [sbt(ph, [128, TB], F32, "qT") for _ in range(2)]
                kT = [sbt(ph, [128, TB], F32, "kT") for _ in range(2)]
                vv = [sbt(ph, [128, NCH, 256], BF16, "vv") for _ in range(2)]
                go = [sbt(ph, [128, NCH, 256], F32, "go") for _ in range(2)]
                ee = sbt(ph, [128, TB], F32, "ee")
                cum = sbt(ph, [128, TB], F32, "cum")
                ex = sbt(ph, [128, TB], F32, "ex")
                dd = sbt(ph, [128, TB], F32, "dd")
                qd = sbt(ph, [128, TB], BF16, "qd")
                kdm = sbt(ph, [128, 4, TB], BF16, "kdm")
                kdec = sbt(ph, [128, TB], BF16, "kdec")
                dcol = sbt(ph, [128, NCH], F32, "dcol")
                kdtm = [sbt(ph, [128, 128], BF16, "kdtm") for _ in range(2)]
                am = [sbt(ph, [128, 4, 128], BF16, "am") for _ in range(2)]
                Sst = sbt(ph, [128, 256], F32, "Sst")
                Sbf = sbt(ph, [128, 256], BF16, "Sbf")
                kvm = sbt(ph, [128, 256], F32, "kvm")
                ob = sbt(ph, [128, NCH, 256], F32, "ob")
                osq = sbt(ph, [128, NCH, 256], F32, "osq")
                ssq = sbt(ph, [128, NCH * 4], F32, "gssq")
                rst = sbt(ph, [128, NCH * 4], F32, "grst")
                sg = sbt(ph, [128, NCH, 256], F32, "sg")
                yb = sbt(ph, [128, NCH, 256], BF16, "yb")
                yT = [sbt(ph, [128, 2, TB], BF16, "yT") for _ in range(2)]
                zps = [pst(ph, [128, 512], F32, "zps") for _ in range(1)]
                tps = pst(ph, [128, 1024], BF16, "gtps")
                aps_ = [pst(ph, [128, 512], F32, "gaps") for _ in range(1)]
                ops_ = [pst(ph, [128, 512], F32, "gops") for _ in range(2)]
                kvps = pst(ph, [128, 512], F32, "kvps")

                dma("sp", w2s[:], gw2_d[l], writes=["w2s"])
                kb.op("dve", lambda e: e.tensor_copy(out=w2b[:], in_=w2s[:]), reads=["w2s"], writes=["w2b"])
                dma("sp", negb[:], gb_d[l], writes=["negb"])
                kb.op("dve", lambda e: e.tensor_scalar(out=negb[:], in0=negb[:], scalar1=-1.0, scalar2=None, op0=ALU.mult), reads=["negb"], writes=["negb"])
                dma("sp", gn[:], gn_d[l:l + 1, :].partition_broadcast(128), writes=["gn"])
                dma("sp", tri[:], tri_d, writes=["tri"])
                dma("sp", bmask[:], bmask_d, writes=["bmask"])
                dma("sp", hm[:], hm_d, writes=["hm"])
                kb.op("pool", lambda e: e.memset(ones[:], 1.0), writes=["ones"])
                kb.op("pool", lambda e: e.memset(Sst[:], 0.0), writes=["Sst"])
                kb.op("pool", lambda e: e.memset(Sbf[:], 0.0), writes=["Sbf"])

                def load(b):
                    i = b % 2
                    t0 = b * TB
                    dma("sp", glr[i][:], glrT_d[:, t0:t0 + TB], writes=[("glr", i)], stream="x", n=3)
                    dma("sp", qT[i][:], gqT_d[:, t0:t0 + TB], writes=[("qT", i)], stream="x", n=3)
                    dma("sp", kT[i][:], gkT_d[:, t0:t0 + TB], writes=[("kT", i)], stream="x", n=3)
                    dma("sp", vv[i][:], gv_d[t0:t0 + TB, :].rearrange("(c p) f -> p c f", p=128), writes=[("vv", i)], stream="x", n=3)
                    dma("sp", go[i][:], gout_d[t0:t0 + TB, :].rearrange("(c p) f -> p c f", p=128), writes=[("go", i)], stream="x", n=3)

                nb = S // TB
                load(0)
                for b in range(nb):
                    if b + 1 < nb:
                        load(b + 1)
                    i = b % 2
                    t0 = b * TB
                    q_, k_, v_, g_, r_ = qT[i], kT[i], vv[i], go[i], glr[i]
                    kq, kk, kv, kg, kr = ("qT", i), ("kT", i), ("vv", i), ("go", i), ("glr", i)
                    for sblk in range(TB // 512):
                        cs = slice(sblk * 512, (sblk + 1) * 512)
                        zp = zps[0]
                        mm(zp[:], w2b[:], r_[:, cs], True, True, reads=["w2b", kr], writes=[("zps", 0)])
                        kb.op("act", lambda e, zp=zp, cs=cs: e.activation(out=ee[:, cs], in_=zp[:], func=AF.Exp, scale=-1.0, bias=negb[:]),
                              reads=[("zps", 0), "negb"], writes=["ee"])
                    yield
                    kb.op("act", lambda e: e.activation(out=ee[:], in_=ee[:], func=AF.Ln, bias=1.0), reads=["ee"], writes=["ee"])
                    for c in range(NCH):
                        cs = slice(c * 128, (c + 1) * 128)
                        kb.op("dve", lambda e, cs=cs: e.tensor_tensor_scan(out=cum[:, cs], data0=ones[:], data1=ee[:, cs], initial=0.0, op0=ALU.mult, op1=ALU.add),
                              reads=["ones", "ee"], writes=["cum"])
                    yield
                    kb.op("act", lambda e: e.activation(out=ex[:], in_=cum[:], func=AF.Exp, scale=-1.0 / 16.0), reads=["cum"], writes=["ex"])
                    kb.op("dve", lambda e, q_=q_: e.scalar_tensor_tensor(out=qd[:], in0=q_[:], scalar=32.0 ** -0.5, in1=ex[:], op0=ALU.mult, op1=ALU.mult),
                          reads=[kq, "ex"], writes=["qd"])
                    yield
                    kb.op("act", lambda e: e.activation(out=dcol[:], in_=cum[:].rearrange("p (c t) -> p c t", t=128)[:, :, 127], func=AF.Exp, scale=-1.0 / 16.0),
                          reads=["cum"], writes=["dcol"])
                    for c in range(NCH):
                        cs = slice(c * 128, (c + 1) * 128)
                        kb.op("pool", lambda e, cs=cs, c=c: e.tensor_scalar(out=dd[:, cs], in0=cum[:, cs], scalar1=cum[:, c * 128 + 127:c * 128 + 128], scalar2=None, op0=ALU.subtract),
                              reads=["cum"], writes=["dd"])
                    yield
                    kb.op("act", lambda e: e.activation(out=ex[:], in_=cum[:], func=AF.Exp, scale=1.0 / 16.0), reads=["cum", "qd"], writes=["ex"])
                    for hh_ in range(4):
                        kb.op("dve", lambda e, k_=k_, hh_=hh_: e.scalar_tensor_tensor(out=kdm[:, hh_, :], in0=k_[:], scalar=hm[:, hh_:hh_ + 1], in1=ex[:], op0=ALU.mult, op1=ALU.mult),
                              reads=[kk, "ex", "hm"], writes=["kdm"])
                    yield
                    kb.op("act", lambda e: e.activation(out=dd[:], in_=dd[:], func=AF.Exp, scale=1.0 / 16.0), reads=["dd"], writes=["dd"])
                    kb.op("pool", lambda e, k_=k_: e.tensor_tensor(out=kdec[:], in0=k_[:], in1=dd[:], op=ALU.mult), reads=[kk, "dd"], writes=["kdec"])
                    kb.op("act", lambda e, g_=g_: e.activation(out=sg[:], in_=g_[:], func=AF.Silu), reads=[kg], writes=["sg"])
                    def gla_s1(c):
                        cs = slice(c * 128, (c + 1) * 128)
                        j = c % 2
                        tp(tps[:, j * 128:(j + 1) * 128], kdec[:, cs], ident[:], reads=["kdec", "ident"], writes=["gtps"])
                        kb.op("act", lambda e, j=j: e.copy(out=kdtm[j][:], in_=tps[:, j * 128:(j + 1) * 128]), reads=["gtps"], writes=[("kdtm", j)])
                        ap_ = aps_[0]
                        for hh_ in range(4):
                            mm(ap_[:, hh_ * 128:(hh_ + 1) * 128], kdm[:, hh_, cs], qd[:, cs], True, True, reads=["kdm", "qd"], writes=[("gaps", 0)])
                        kb.op("dve", lambda e, ap_=ap_, j=j: e.tensor_tensor(out=am[j][:], in0=ap_[:].rearrange("p (h c) -> p h c", h=4),
                                                                             in1=tri[:].unsqueeze(1).broadcast_to([128, 4, 128]), op=ALU.mult),
                              reads=[("gaps", 0), "tri"], writes=[("am", j)])

                    def gla_s2(c):
                        cs = slice(c * 128, (c + 1) * 128)
                        j = c % 2
                        op_ = ops_[j]
                        mm(op_[:, 0:256], qd[:, cs], Sbf[:], True, True, reads=["qd", "Sbf"], writes=[("gops", j)])
                        for hh_ in range(4):
                            mm(op_[:, hh_ * 64:(hh_ + 1) * 64], am[j][:, hh_, :], v_[:, c, hh_ * 64:(hh_ + 1) * 64], False, True,
                               reads=[("am", j), kv], writes=[("gops", j)], skip_group_check=True)
                        kb.op("act", lambda e, op_=op_, c=c: e.copy(out=ob[:, c, :], in_=op_[:, 0:256]), reads=[("gops", j)], writes=["ob"])
                        mm(kvps[:, 0:256], kdtm[j][:], v_[:, c, :], True, True, reads=[("kdtm", j), kv], writes=["kvps"])
                        kb.op("dve", lambda e: e.tensor_tensor(out=kvm[:], in0=kvps[:, 0:256], in1=bmask[:], op=ALU.mult), reads=["kvps", "bmask"], writes=["kvm"])
                        kb.op("dve", lambda e, c=c: e.scalar_tensor_tensor(out=Sst[:], in0=Sst[:], scalar=dcol[:, c:c + 1], in1=kvm[:], op0=ALU.mult, op1=ALU.add),
                              reads=["Sst", "dcol", "kvm"], writes=["Sst"])
                        kb.op("pool", lambda e: e.tensor_copy(out=Sbf[:], in_=Sst[:]), reads=["Sst"], writes=["Sbf"])

                    gla_s1(0)
                    yield
                    for c in range(NCH):
                        if c + 1 < NCH:
                            gla_s1(c + 1)
                            yield
                        gla_s2(c)
                        yield
                    yield
                    kb.op("pool", lambda e: e.tensor_tensor(out=osq[:], in0=ob[:], in1=ob[:], op=ALU.mult), reads=["ob"], writes=["osq"])
                    kb.op("dve", lambda e: e.tensor_reduce(out=ssq[:], in_=osq[:].rearrange("p c (h v) -> p (c h) v", h=4), axis=AX.X, op=ALU.add),
                          reads=["osq"], writes=["p2bssq"])
                    rstd_from_ssq(ssq[:], rst[:], 64, "p2b")
                    kb.op("dve", lambda e: e.tensor_tensor(out=ob[:].rearrange("p c (h v) -> p (c h) v", h=4), in0=ob[:].rearrange("p c (h v) -> p (c h) v", h=4),
                                                           in1=rst[:].unsqueeze(2).broadcast_to([128, NCH * 4, 64]), op=ALU.mult),
                          reads=["ob", "p2brs"], writes=["ob"])
                    kb.op("pool", lambda e: e.tensor_tensor(out=sg[:], in0=sg[:], in1=gn[:].unsqueeze(1).broadcast_to([128, NCH, 256]), op=ALU.mult),
                          reads=["sg", "gn"], writes=["sg"])
                    kb.op("dve", lambda e: e.tensor_tensor(out=yb[:], in0=ob[:], in1=sg[:], op=ALU.mult), reads=["ob", "sg"], writes=["yb"])
                    yield
                    yTb = yT[b % 2]
                    for c in range(NCH):
                        for f in range(2):
                            jj = (c * 2 + f) % 4
                            tp(tps[:, jj * 128:(jj + 1) * 128], yb[:, c, f * 128:(f + 1) * 128], ident[:], reads=["yb", "ident"], writes=["gtps"])
                            kb.op("act", lambda e, jj=jj, c=c, f=f, yTb=yTb: e.copy(out=yTb[:, f, c * 128:(c + 1) * 128], in_=tps[:, jj * 128:(jj + 1) * 128]),
                                  reads=["gtps"], writes=[("yT", b % 2)])
                    dma("sp", mixT_d[256:512, t0:t0 + TB].rearrange("(f p) t -> p f t", p=128), yTb[:], reads=[("yT", b % 2)], stream="o", n=4)

        def phase2ab(l):
            with ExitStack() as ph:
                gens = [phase2a_gen(l, ph), phase2b_gen(l, ph)]
                while gens:
                    for g_ in list(gens):
                        try:
                            next(g_)
                        except StopIteration:
                            gens.remove(g_)
                end_phase()

        def phase2c(l):
            with ExitStack() as ph:
                kaug = sbt(ph, [128, 8, S], BF16, "kaug")
                vp = sbt(ph, [128, 32, 520], BF16, "vp")
                qaug = [sbt(ph, [128, 8, 512], BF16, "qaug") for _ in range(3)]
                km = sbt(ph, [64, 8, 16], F32, "km")
                kmb = sbt(ph, [64, 8, 16], BF16, "kmb")
                cm = sbt(ph, [128, 16, 16], F32, "cm")
                pm = sbt(ph, [128, 16, 16], F32, "pm")
                b31 = sbt(ph, [128, 8], F32, "b31")
                caus = sbt(ph, [128, 128], F32, "caus")
                tstage = sbt(ph, [128, 2, 8, 128], F32, "tstage")
                tdT = sbt(ph, [128, 8, 128], BF16, "tdT")
                toT = sbt(ph, [128, 8, 128], BF16, "toT")
                tomT = sbt(ph, [128, 8, 128], BF16, "tomT")
                zer = sbt(ph, [128, 260], BF16, "zer")
                gm = sbt(ph, [128, 4, 8, 16], F32, "gm")
                m8 = sbt(ph, [128, 4, 8, 8], F32, "m8")
                sel = sbt(ph, [128, 4, 8, 16], F32, "sel")
                mpad = [sbt(ph, [128, 4, 8, 80], BF16, "mpad") for _ in range(2)]
                pT = [sbt(ph, [128, 512], BF16, "pT") for _ in range(4)]
                rcp = sbt(ph, [128, 4], F32, "rcp")
                ymo = [sbt(ph, [128, 4, 512], BF16, "ymo") for _ in range(2)]
                ymT = [sbt(ph, [128, 4, 512], BF16, "ymT") for _ in range(2)]
                gps = pst(ph, [128, 512], F32, "mgps")
                mtps = [pst(ph, [128, 512], F32, "mtps") for _ in range(1)]
                mtps_b = [pst(ph, [128, 1024], BF16, "mtpsb") for _ in range(1)]
                sps = [pst(ph, [128, 512], F32, "sps") for _ in range(3)]
                accs_full = [pst(ph, [128, 512], F32, "acc") for _ in range(2)]
                accs = [a_[:, 0:260].rearrange("p (s d) -> p s d", s=4) for a_ in accs_full]

                for h in range(8):
                    dma("sp", kaug[0:64, h, :], mkT_d[h * 64:(h + 1) * 64, :], writes=["kaug"], stream="x", n=3)
                    dma("sp", kaug[64:80, h, :], e16_d, writes=["kaug"], stream="x", n=3)
                for c in range(4):
                    dma("sp", vp[:, c * 8:(c + 1) * 8, :], mvp_d[c * 1024:(c + 1) * 1024, :].rearrange("(c p) f -> p c f", p=128), writes=["vp"], stream="x", n=3)
                dma("sp", cm[:].rearrange("p a b -> p (a b)"), cm_d.partition_broadcast(128), writes=["cm"])
                dma("sp", pm[:].rearrange("p a b -> p (a b)"), pm_d.partition_broadcast(128), writes=["pm"])
                dma("sp", b31[:], rb31_d.partition_broadcast(128), writes=["b31"])
                dma("sp", caus[:], caus_d, writes=["caus"])
                dma("sp", tstage[:, 0], tdg_d, writes=["tstage"])
                dma("sp", tstage[:, 1], tof_d, writes=["tstage"])
                kb.op("dve", lambda e: e.tensor_tensor(out=tdT[:], in0=tstage[:, 0], in1=caus[:].unsqueeze(1).broadcast_to([128, 8, 128]), op=ALU.add),
                      reads=["tstage", "caus"], writes=["tdT"])
                kb.op("dve", lambda e: e.tensor_copy(out=toT[:], in_=tstage[:, 1]), reads=["tstage"], writes=["toT"])
                kb.op("dve", lambda e: e.tensor_tensor(out=tomT[:], in0=tstage[:, 1], in1=b31[:].unsqueeze(2).broadcast_to([128, 8, 128]), op=ALU.subtract),
                      reads=["tstage", "b31"], writes=["tomT"])
                kb.op("pool", lambda e: e.memset(zer[:], 0.0), writes=["zer"])
                for i in range(2):
                    kb.op("pool", lambda e, i=i: e.memset(mpad[i][:], 0.0), writes=[("mpad", i)])
                kb.op("dve", lambda e: e.tensor_reduce(out=km[:].rearrange("p h n -> p (h n)"), in_=kaug[0:64, :, :].rearrange("p h (n t) -> p (h n) t", t=256), axis=AX.X, op=ALU.add),
                      reads=["kaug"], writes=["km"])
                kb.op("dve", lambda e: e.tensor_scalar(out=kmb[:], in0=km[:], scalar1=1.0 / 256.0, scalar2=None, op0=ALU.mult), reads=["km"], writes=["kmb"])

                def loadq(G):
                    i = G % 3
                    dma("sp", qaug[i][0:64, :, :], mqT_d.rearrange("(h d) t -> d h t", d=64)[:, :, G * 512:(G + 1) * 512], writes=[("qaug", i)], stream="q", n=3)

                NG = S // 512
                si = 0
                ai = 0

                def pre1(G):
                    qi = G % 3
                    qa = qaug[qi]
                    kqa = ("qaug", qi)
                    mp = mpad[G % 2]
                    for s in range(4):
                        for h in range(8):
                            mm(gps[:, (s * 8 + h) * 16:(s * 8 + h + 1) * 16], qa[0:64, h, s * 128:(s + 1) * 128], kmb[:, h, :], True, True,
                               reads=[kqa, "kmb"], writes=["mgps"])
                    np0 = 2 * G
                    for a in range(2):
                        cmv = cm[:, np0 + a, :].unsqueeze(1).unsqueeze(1).broadcast_to([128, 2, 8, 16])
                        kb.op("dve", lambda e, cmv=cmv, a=a: e.tensor_tensor(out=gm[:, 2 * a:2 * a + 2], in0=gps[:].rearrange("p (s h n) -> p s h n", s=4, h=8)[:, 2 * a:2 * a + 2],
                                                                             in1=cmv, op=ALU.add),
                              reads=["mgps", "cm"], writes=["gm"])
                    for s in range(4):
                        for h in range(8):
                            kb.op("dve", lambda e, s=s, h=h: e.max(out=m8[:, s, h, :], in_=gm[:, s, h, :]), reads=["gm"], writes=["m8"])
                    kb.op("dve", lambda e: e.tensor_tensor(out=sel[:], in0=gm[:], in1=m8[:, :, :, 2:3].broadcast_to([128, 4, 8, 16]), op=ALU.is_ge),
                          reads=["gm", "m8"], writes=["sel"])
                    kb.op("dve", lambda e: e.tensor_scalar(out=sel[:], in0=sel[:], scalar1=-NEG, scalar2=NEG, op0=ALU.mult, op1=ALU.add), reads=["sel"], writes=["sel"])
                    kb.op("dve", lambda e: e.tensor_tensor(out=sel[:], in0=sel[:], in1=b31[:].unsqueeze(1).unsqueeze(3).broadcast_to([128, 4, 8, 16]), op=ALU.add),
                          reads=["sel", "b31"], writes=["sel"])
                    for a in range(2):
                        pmv = pm[:, np0 + a, :].unsqueeze(1).unsqueeze(1).broadcast_to([128, 2, 8, 16])
                        kb.op("dve", lambda e, pmv=pmv, mp=mp, a=a: e.tensor_tensor(out=mp[:, 2 * a:2 * a + 2, :, 64:80], in0=sel[:, 2 * a:2 * a + 2], in1=pmv, op=ALU.mult),
                              reads=["sel", "pm"], writes=[("mpad", G % 2)])

                def pre2(G):
                    qi = G % 3
                    qa = qaug[qi]
                    kqa = ("qaug", qi)
                    mp = mpad[G % 2]
                    for h in range(8):
                        mt = mtps[0]
                        for s in range(4):
                            mm(mt[0:80, s * 128:(s + 1) * 128], mp[:, s, h, :], ident[:], True, True, reads=[("mpad", G % 2), "ident"], writes=[("mtps", 0)])
                        kb.op("act", lambda e, mt=mt, h=h, qa=qa: e.copy(out=qa[64:80, h, :], in_=mt[64:80, :]),
                              reads=[("mtps", 0)], writes=[kqa])

                loadq(0)
                if NG > 1:
                    loadq(1)
                pre1(0)
                pre2(0)
                for G in range(NG):
                    if G + 2 < NG:
                        loadq(G + 2)
                    if G + 1 < NG:
                        pre1(G + 1)
                    qi = G % 3
                    qa = qaug[qi]
                    kqa = ("qaug", qi)
                    ym = ymo[G % 2]
                    nj = 4 * G + 4
                    DEPTH = 2

                    def stageA(h, j):
                        nonlocal si
                        acc = accs[h % 2]
                        ka = ("acc", h % 2)
                        if j == 0:
                            mm(accs_full[h % 2][:, 0:260], zer[:, 0:128], zer[:, :], True, True, reads=["zer"], writes=[ka])
                        r = j - 4 * G
                        c0 = max(r, 0) * 128
                        sp_ = sps[si % 3]
                        ks = ("sps", si % 3)
                        pt = pT[si % 4]
                        kp = ("pT", si % 4)
                        si += 1
                        mm(sp_[:, c0:512], kaug[0:80, h, j * 128:(j + 1) * 128], qa[0:80, h, c0:512], True, True,
                           reads=["kaug", kqa], writes=[ks])
                        if r == -1:
                            mm(sp_[:, 0:128], ident[:], tomT[:, h, :], False, True, reads=["ident", "tomT"], writes=[ks], skip_group_check=True)
                        if r >= 0:
                            mm(sp_[:, r * 128:(r + 1) * 128], ident[:], tdT[:, h, :], False, True, reads=["ident", "tdT"], writes=[ks], skip_group_check=True)
                            if r < 3:
                                tt = toT if r % 2 == 0 else tomT
                                mm(sp_[:, (r + 1) * 128:(r + 2) * 128], ident[:], tt[:, h, :], False, True, reads=["ident", "toT", "tomT"], writes=[ks], skip_group_check=True)
                        kb.op("act", lambda e, pt=pt, sp_=sp_, c0=c0: e.activation(out=pt[:, c0:512], in_=sp_[:, c0:512], func=AF.Exp),
                              reads=[ks], writes=[kp])
                        return (h, j, r, pt, kp, acc, ka)

                    def stageB(info):
                        h, j, r, pt, kp, acc, ka = info
                        for s in range(max(r, 0), 4):
                            mm(acc[:, s, :], pt[:, s * 128:(s + 1) * 128], vp[:, j, h * 65:(h + 1) * 65], False, True,
                               reads=[kp, "vp"], writes=[ka], skip_group_check=True)
                        if j == nj - 1:
                            kb.op("dve", lambda e, acc=acc: e.reciprocal(out=rcp[:], in_=acc[:, :, 64]), reads=[ka], writes=["rcp"])
                            kb.op("dve", lambda e, acc=acc, h=h, ym=ym: e.tensor_tensor(out=ym[:, :, h * 64:(h + 1) * 64], in0=acc[:, :, 0:64],
                                                                                  in1=rcp[:].unsqueeze(2).broadcast_to([128, 4, 64]), op=ALU.mult),
                                  reads=[ka, "rcp"], writes=[("ymo", G % 2)])

                    pend = []
                    for h in range(8):
                        if h == 4 and G + 1 < NG:
                            pre2(G + 1)
                        for j in range(nj):
                            pend.append(stageA(h, j))
                            if len(pend) > DEPTH:
                                stageB(pend.pop(0))
                    while pend:
                        stageB(pend.pop(0))
                    yt = ymT[G % 2]
                    for s in range(4):
                        tpb = mtps_b[0]
                        for f in range(4):
                            tp(tpb[:, f * 128:(f + 1) * 128], ym[:, s, f * 128:(f + 1) * 128], ident[:], reads=[("ymo", G % 2), "ident"], writes=[("mtpsb", 0)])
                        kb.op("act", lambda e, tpb=tpb, yt=yt, s=s: e.copy(out=yt[:, :, s * 128:(s + 1) * 128], in_=tpb[:, 0:512].rearrange("p (f q) -> p f q", f=4)),
                              reads=[("mtpsb", 0)], writes=[("ymT", G % 2)])
                    dma("sp", mixT_d[512:1024, G * 512:(G + 1) * 512].rearrange("(f p) t -> p f t", p=128), yt[:], reads=[("ymT", G % 2)], stream="o", n=4)
                end_phase()

        def phase3(l, xin_d, xout_d):
            with ExitStack() as ph:
                wout = sbt(ph, [128, 8, D], BF16, "wout")
                wdn = sbt(ph, [128, NFC, D], BF16, "wdn")
                g3 = sbt(ph, [128, 3, D], F32, "g3")
                wgu = [sbt(ph, [128, 2, 8, 128], BF16, "wgu") for _ in range(4)]
                mixT = [sbt(ph, [128, 8, 512], BF16, "mixT") for _ in range(2)]
                xt = [sbt(ph, [128, D], F32, "xt3") for _ in range(2)]
                x1 = [sbt(ph, [128, D], F32, "x1") for _ in range(8)]
                tmp = [sbt(ph, [128, D], F32, "tmp3") for _ in range(2)]
                junk = sbt(ph, [128, D], BF16, "junk3")
                hb = [sbt(ph, [128, D], BF16, "hb3") for _ in range(4)]
                hT = sbt(ph, [128, 8, 512], BF16, "hT3")
                actT = sbt(ph, [128, NFC, 512], BF16, "actT")
                sgt = [sbt(ph, [128, 512], F32, "sgt") for _ in range(2)]
                ssq = sbt(ph, [128, 16], F32, "ssq3")
                rst = sbt(ph, [128, 16], F32, "rst3")
                xo = [sbt(ph, [128, D], F32, "xo") for _ in range(2)]
                ops_ = [pst(ph, [128, 2, 512], F32, "p3o") for _ in range(2)]
                tps = pst(ph, [128, D], BF16, "p3t")
                gus = [pst(ph, [128, 512], F32, "p3gu") for _ in range(3)]
                for kc in range(8):
                    dma("sp", wout[:, kc, :], woutb_d[l, kc * 128:(kc + 1) * 128, :], writes=["wout"], stream="w", n=4)
                for fc in range(NFC):
                    dma("sp", wdn[:, fc, :], wdb_d[l, fc * 128:(fc + 1) * 128, :], writes=["wdn"], stream="w", n=4)
                for i in range(3):
                    dma("sp", g3[:, i, :], norms_d[l, i + 1:i + 2, :].partition_broadcast(128), writes=["g3"])

                wi = [0]

                def loadw(fc):
                    i = wi[0] % 4
                    wi[0] += 1
                    dma("sp", wgu[i][:], wgub_d[l, fc], writes=[("wgu", i)], stream="wgu", n=4)
                    return i

                NG = S // 512
                sq = 0

                def loadg(G):
                    i = G % 2
                    dma("sp", mixT[i][:], mixT_d[:, G * 512:(G + 1) * 512].rearrange("(k p) t -> p k t", p=128), writes=[("mixT", i)], stream="m", n=2)

                def loadx(t):
                    dma("sp", xt[t % 2][:], xin_d[t * 128:(t + 1) * 128, :], writes=[("xt3", t % 2)], stream="x", n=3)

                loadg(0)
                loadx(0)
                wq = []
                PRE = 3
                def p3_front(G):
                    nonlocal sq
                    mT = mixT[G % 2]
                    for s in range(4):
                        t = G * 4 + s
                        if t + 1 < S // 128:
                            loadx(t + 1)
                        xs = xt[t % 2]
                        x1s = x1[(G % 2) * 4 + s]
                        op_ = ops_[s % 2]
                        ko = ("p3o", s % 2)
                        tm_ = tmp[s % 2]
                        kt = ("tmp3", s % 2)
                        for hf in range(2):
                            for kc in range(8):
                                mm(op_[:, hf, :], mT[:, kc, s * 128:(s + 1) * 128], wout[:, kc, hf * 512:(hf + 1) * 512], kc == 0, kc == 7,
                                   reads=[("mixT", G % 2), "wout"], writes=[ko])
                        c = sq % 16
                        sq += 1
                        kb.op("act", lambda e, c=c, op_=op_: e.activation(out=junk[:], in_=op_[:].rearrange("p a b -> p (a b)"), func=AF.Square, accum_out=ssq[:, c:c + 1]),
                              reads=[ko], writes=["junk3", "p3ssq"])
                        rstd_from_ssq(ssq[:, c:c + 1], rst[:, c:c + 1], D, "p3")
                        kb.op("dve", lambda e, c=c, op_=op_, tm_=tm_: e.scalar_tensor_tensor(out=tm_[:], in0=op_[:].rearrange("p a b -> p (a b)"), scalar=rst[:, c:c + 1], in1=g3[:, 0, :], op0=ALU.mult, op1=ALU.mult),
                              reads=[ko, "p3rs", "g3"], writes=[kt])
                        kb.op("pool", lambda e, xs=xs, x1s=x1s, tm_=tm_: e.tensor_tensor(out=x1s[:], in0=xs[:], in1=tm_[:], op=ALU.add),
                              reads=[("xt3", t % 2), kt], writes=[("x1", (G % 2) * 4 + s)])
                        c2 = sq % 16
                        sq += 1
                        kb.op("act", lambda e, c2=c2, x1s=x1s: e.activation(out=junk[:], in_=x1s[:], func=AF.Square, accum_out=ssq[:, c2:c2 + 1]),
                              reads=[("x1", (G % 2) * 4 + s)], writes=["junk3", "p3ssq"])
                        rstd_from_ssq(ssq[:, c2:c2 + 1], rst[:, c2:c2 + 1], D, "p3")
                        hbs = hb[s]
                        kb.op("dve", lambda e, c2=c2, x1s=x1s, hbs=hbs: e.scalar_tensor_tensor(out=hbs[:], in0=x1s[:], scalar=rst[:, c2:c2 + 1], in1=g3[:, 1, :], op0=ALU.mult, op1=ALU.mult),
                              reads=[("x1", (G % 2) * 4 + s), "p3rs", "g3"], writes=[("hb3", s)])

                def p3_trans(G):
                    for s in range(4):
                        hbs = hb[s]
                        for kc in range(8):
                            tp(tps[:, kc * 128:(kc + 1) * 128], hbs[:, kc * 128:(kc + 1) * 128], ident[:], reads=[("hb3", s), "ident"], writes=["p3t"])
                        kb.op("act", lambda e, s=s: e.copy(out=hT[:, :, s * 128:(s + 1) * 128], in_=tps[:].rearrange("p (k c) -> p k c", k=8)),
                              reads=["p3t"], writes=["hT3"])

                def p3_gateup(G):
                    for fc in range(NFC):
                        wslot = wq.pop(0)
                        nxt = fc + PRE
                        if nxt < NFC:
                            wq.append(loadw(nxt))
                        w_ = wgu[wslot]
                        gp, up = gus[(2 * fc) % 3], gus[(2 * fc + 1) % 3]
                        kgp, kup = ("p3gu", (2 * fc) % 3), ("p3gu", (2 * fc + 1) % 3)
                        for kc in range(8):
                            mm(gp[:], w_[:, 0, kc, :], hT[:, kc, :], kc == 0, kc == 7, reads=[("wgu", wslot), "hT3"], writes=[kgp])
                        for kc in range(8):
                            mm(up[:], w_[:, 1, kc, :], hT[:, kc, :], kc == 0, kc == 7, reads=[("wgu", wslot), "hT3"], writes=[kup])
                        sg_ = sgt[fc % 2]
                        kb.op("act", lambda e, sg_=sg_, gp=gp: e.activation(out=sg_[:], in_=gp[:], func=AF.Silu), reads=[kgp], writes=[("sgt", fc % 2)])
                        kb.op("dve", lambda e, sg_=sg_, up=up, fc=fc: e.tensor_tensor(out=actT[:, fc, :], in0=up[:], in1=sg_[:], op=ALU.mult),
                              reads=[kup, ("sgt", fc % 2)], writes=["actT"])

                def p3_down(G):
                    nonlocal sq
                    for s in range(4):
                        t = G * 4 + s
                        op_ = ops_[s % 2]
                        ko = ("p3o", s % 2)
                        tm_ = tmp[s % 2]
                        kt = ("tmp3", s % 2)
                        for hf in range(2):
                            for fc in range(NFC):
                                mm(op_[:, hf, :], actT[:, fc, s * 128:(s + 1) * 128], wdn[:, fc, hf * 512:(hf + 1) * 512], fc == 0, fc == NFC - 1,
                                   reads=["actT", "wdn"], writes=[ko])
                        c = sq % 16
                        sq += 1
                        kb.op("act", lambda e, c=c, op_=op_: e.activation(out=junk[:], in_=op_[:].rearrange("p a b -> p (a b)"), func=AF.Square, accum_out=ssq[:, c:c + 1]),
                              reads=[ko], writes=["junk3", "p3ssq"])
                        rstd_from_ssq(ssq[:, c:c + 1], rst[:, c:c + 1], D, "p3")
                        kb.op("dve", lambda e, c=c, op_=op_, tm_=tm_: e.scalar_tensor_tensor(out=tm_[:], in0=op_[:].rearrange("p a b -> p (a b)"), scalar=rst[:, c:c + 1], in1=g3[:, 2, :], op0=ALU.mult, op1=ALU.mult),
                              reads=[ko, "p3rs", "g3"], writes=[kt])
                        xos = xo[t % 2]
                        kb.op("pool", lambda e, xos=xos, s=s, tm_=tm_: e.tensor_tensor(out=xos[:], in0=x1[(G % 2) * 4 + s][:], in1=tm_[:], op=ALU.add),
                              reads=[("x1", (G % 2) * 4 + s), kt], writes=[("xo", t % 2)])
                        dma("sp", xout_d[t * 128:(t + 1) * 128, :], xos[:], reads=[("xo", t % 2)], stream="o", n=4)

                for G in range(NG):
                    if G + 1 < NG:
                        loadg(G + 1)
                    while len(wq) < PRE:
                        wq.append(loadw(len(wq)))
                    p3_front(G)
                    if G > 0:
                        p3_down(G - 1)
                    p3_trans(G)
                    p3_gateup(G)
                p3_down(NG - 1)
                end_phase()

        kb.barrier()
        import os as _os2
        if not _os2.environ.get("SKIP_P0"):
            phase0()
        done = stop_after == "p0"
        for l in range(L):
            if done:
                break
            xin = x_d if l == 0 else xs1_d
            xout = xs1_d if l == 0 else out_d
            for nm, fn in (("p1", lambda: phase1(l, xin)), ("p2b", lambda: phase2ab(l)),
                           ("p2c", lambda: phase2c(l)), ("p3", lambda: phase3(l, xin, xout))):
                fn()
                if stop_after == (l, nm):
                    done = True
                    break
            if done:
                break
        kb.barrier()
        kb.emit()

    return nc


def _host_inputs(inputs):
    f = lambda a: np.ascontiguousarray(np.asarray(a, dtype=np.float32))
    c = _consts()
    idx_diag, idx_off1 = _bias_idx()
    rel = f(inputs["rel_bias"])
    shared = {
        "norms": f(np.stack([inputs["pre_mix_norm"], inputs["post_mix_norm"], inputs["pre_ffn_norm"], inputs["post_ffn_norm"]], axis=1)),
        "w_in": f(inputs["w_in"]), "w_out": f(inputs["w_out"]),
        "w_ffn_gate": f(inputs["w_ffn_gate"]), "w_ffn_up": f(inputs["w_ffn_up"]), "w_ffn_down": f(inputs["w_ffn_down"]),
        "lru_wa": f(inputs["lru_wa"]), "lru_wx": f(inputs["lru_wx"]),
        "gla_gate_w2": f(inputs["gla_gate_w2"]),
        "gla_gate_b": f(np.asarray(inputs["gla_gate_b"]).reshape(L, 128, 1)),
        "gla_norm": f(inputs["gla_norm"]),
        "rb31": f(rel[31:32, :]),
        "tdg": f(np.transpose(rel[idx_diag], (0, 2, 1))),
        "tof": f(np.transpose(rel[idx_off1], (0, 2, 1))),
        "ident": c["ident"], "tri": c["tri"], "caus": c["caus"], "e16": c["e16"],
        "cm": c["cm"], "pm": c["pm"], "bmask": c["bmask"], "hm": c["hm"],
    }
    cw = np.transpose(np.asarray(inputs["lru_conv_w"], dtype=np.float32), (0, 2, 1))
    cols = np.concatenate([cw] + [np.asarray(inputs[k], dtype=np.float32)[:, :, None]
                                  for k in ("lru_conv_b", "lru_ba", "lru_bx", "lru_lambda")], axis=2)
    shared["lru_cols"] = f(cols.reshape(L, 2, 128, 8))
    x = np.asarray(inputs["x"], dtype=np.float32)
    return [dict(shared, x=np.ascontiguousarray(x[b])) for b in range(x.shape[0])]


_NC_CACHE = {}


def kernel(**inputs):
    in_maps = _host_inputs(inputs)
    if "nc" not in _NC_CACHE:
        _NC_CACHE["nc"] = build()
    nc = _NC_CACHE["nc"]
    n = len(in_maps)
    res = run_bass_kernel_spmd(nc, in_maps, core_ids=list(range(n)))
    return np.stack([np.asarray(r["out"], dtype=np.float32) for r in res.results], axis=0)
```

```python
from contextlib import ExitStack
import math
import numpy as np
import ml_dtypes
import concourse.bass as bass
import concourse.mybir as mybir
from concourse.bass_utils import run_bass_kernel_spmd

F32 = mybir.dt.float32
BF16 = mybir.dt.bfloat16
ALU = mybir.AluOpType
AF = mybir.ActivationFunctionType
AX = mybir.AxisListType

S = 4096
D = 1024
L = 2
DIN = 2832
DFF = 2816
NFC = DFF // 128
EPS = 1e-6
NEG = -30000.0
ENGS = ("pe", "act", "dve", "pool", "sp")


class KB:
    def __init__(self, nc, stack, sync_same=True):
        self.nc = nc
        self.stack = stack
        self.sync_same = sync_same
        self.ops = {e: [] for e in ENGS}
        self.sem = {}
        self.cnt = {}
        self.step = {}
        self.known = {e: {} for e in ENGS}
        self.lw = {}
        self.rd = {}
        for e in ENGS:
            self._dom(e, 1)

    def _dom(self, name, step):
        if name not in self.sem:
            self.sem[name] = self.stack.enter_context(self.nc.semaphore("s_" + name))
            self.cnt[name] = 0
            self.step[name] = step
        return name

    def op(self, eng, fn, reads=(), writes=(), dma=None):
        dom = eng if dma is None else self._dom("d_" + dma, 16)
        deps = {}

        def add(d):
            if d is not None and deps.get(d[0], 0) < d[1]:
                deps[d[0]] = d[1]

        for k in reads:
            add(self.lw.get(k))
        for k in writes:
            add(self.lw.get(k))
            for dm, c in self.rd.get(k, {}).items():
                add((dm, c))
        if dma is not None and self.cnt[dom] > 0:
            add((dom, self.cnt[dom]))
        kn = self.known[eng]
        for d, c in deps.items():
            if d == eng and (eng == "pe" or not self.sync_same):
                continue
            if kn.get(d, 0) >= c:
                continue
            self.ops[eng].append(("w", self.sem[d], c))
            kn[d] = c
        self.cnt[dom] += self.step[dom]
        me = (dom, self.cnt[dom])
        self.ops[eng].append(("o", fn, self.sem[dom], self.step[dom]))
        for k in writes:
            self.lw[k] = me
            self.rd[k] = {}
        for k in reads:
            r = self.rd.setdefault(k, {})
            if r.get(dom, 0) < me[1]:
                r[dom] = me[1]
        return me

    def barrier(self):
        for eng in ENGS:
            kn = self.known[eng]
            for dom, c in self.cnt.items():
                if c > 0 and dom != eng and kn.get(dom, 0) < c:
                    self.ops[eng].append(("w", self.sem[dom], c))
                    kn[dom] = c
        self.lw = {}
        self.rd = {}

    def emit(self):
        nc = self.nc
        ops = self.ops

        def run(lst, e):
            for it in lst:
                if it[0] == "w":
                    e.wait_ge(it[1], it[2])
                else:
                    it[1](e).then_inc(it[2], it[3])

        with nc.Block() as block:
            @block.tensor
            def _(e):
                run(ops["pe"], e)

            @block.scalar
            def _(e):
                run(ops["act"], e)

            @block.vector
            def _(e):
                run(ops["dve"], e)

            @block.gpsimd
            def _(e):
                run(ops["pool"], e)

            @block.sync
            def _(e):
                run(ops["sp"], e)
        self.ops = {e: [] for e in ENGS}


def _t5_bucket(n):
    n = np.maximum(n, 0)
    nf = np.maximum(n, 1).astype(np.float32)
    large = 16 + (np.log(nf / np.float32(16)) / np.float32(math.log(128 / 16)) * np.float32(16)).astype(np.int32)
    large = np.minimum(large, 31)
    return np.where(n < 16, n, large)


def _consts():
    c = {}
    c["ident"] = np.eye(128, dtype=np.float32).astype(ml_dtypes.bfloat16)
    e = np.arange(128)
    c["tri"] = (e[:, None] <= e[None, :]).astype(np.float32)
    c["caus"] = np.where(e[None, :] >= e[:, None], 0.0, NEG).astype(np.float32)
    keys = np.arange(S)
    c["e16"] = (keys[None, :] // 256 == np.arange(16)[:, None]).astype(np.float32).astype(ml_dtypes.bfloat16)
    npast = np.arange(16)[:, None]
    nn = np.arange(16)[None, :]
    c["cm"] = np.where(nn < npast, 0.0, -1e30).astype(np.float32).reshape(1, 256)
    c["pm"] = (nn < npast).astype(np.float32).reshape(1, 256)
    p = np.arange(128)[:, None]
    c["bmask"] = (p // 32 == (np.arange(256)[None, :] // 64)).astype(np.float32)
    c["hm"] = (p // 32 == np.arange(4)[None, :]).astype(np.float32)
    return c


def _bias_idx():
    k = np.arange(128)[:, None]
    q = np.arange(128)[None, :]
    idx_diag = _t5_bucket(q - k)
    idx_off1 = _t5_bucket(q + 128 - k)
    return idx_diag, idx_off1


def build(debug=False, stop_after=None):
    nc = bass.Bass("TRN2", target_bir_lowering=False)
    dr = lambda name, shape, dt, kind="Internal": nc.dram_tensor(name, list(shape), dt, kind=kind).ap()
    IN = "ExternalInput"
    x_d = dr("x", [S, D], F32, IN)
    norms_d = dr("norms", [L, 4, D], F32, IN)
    w_in_d = dr("w_in", [L, D, DIN], F32, IN)
    w_out_d = dr("w_out", [L, D, D], F32, IN)
    wg_d = dr("w_ffn_gate", [L, D, DFF], F32, IN)
    wu_d = dr("w_ffn_up", [L, D, DFF], F32, IN)
    wd_d = dr("w_ffn_down", [L, DFF, D], F32, IN)
    lcols_d = dr("lru_cols", [L, 2, 128, 8], F32, IN)
    lwa_d = dr("lru_wa", [L, 4, 64, 64], F32, IN)
    lwx_d = dr("lru_wx", [L, 4, 64, 64], F32, IN)
    gw2_d = dr("gla_gate_w2", [L, 16, 128], F32, IN)
    gb_d = dr("gla_gate_b", [L, 128, 1], F32, IN)
    gn_d = dr("gla_norm", [L, 256], F32, IN)
    rb31_d = dr("rb31", [1, 8], F32, IN)
    tdg_d = dr("tdg", [128, 8, 128], F32, IN)
    tof_d = dr("tof", [128, 8, 128], F32, IN)
    ident_d = dr("ident", [128, 128], BF16, IN)
    tri_d = dr("tri", [128, 128], F32, IN)
    caus_d = dr("caus", [128, 128], F32, IN)
    e16_d = dr("e16", [16, S], BF16, IN)
    cm_d = dr("cm", [1, 256], F32, IN)
    pm_d = dr("pm", [1, 256], F32, IN)
    bmask_d = dr("bmask", [128, 256], F32, IN)
    hm_d = dr("hm", [128, 4], F32, IN)
    out_d = dr("out", [S, D], F32, "ExternalOutput")

    dk = "ExternalOutput" if debug else "Internal"
    winb_d = dr("winb", [L, D, DIN], BF16)
    woutb_d = dr("woutb", [L, D, D], BF16)
    wgub_d = dr("wgub", [L, NFC, 128, 2, 8, 128], BF16)
    wdb_d = dr("wdb", [L, DFF, D], BF16)
    xs1_d = dr("xs1", [S, D], F32, dk)
    lruT_d = dr("lruT", [512, S], F32, dk)
    gqT_d = dr("gqT", [128, S], F32, dk)
    gkT_d = dr("gkT", [128, S], F32, dk)
    glrT_d = dr("glrT", [16, S], BF16, dk)
    mqT_d = dr("mqT", [512, S], BF16, dk)
    mkT_d = dr("mkT", [512, S], BF16, dk)
    gv_d = dr("gv", [S, 256], BF16, dk)
    gout_d = dr("gout", [S, 256], F32, dk)
    mvp_d = dr("mvp", [S, 520], BF16, dk)
    mixT_d = dr("mixT", [D, S], BF16, dk)

    with ExitStack() as st:
        kb = KB(nc, st)
        uid = [0]

        def sbt(ctx, shape, dt, name=None):
            uid[0] += 1
            return ctx.enter_context(nc.sbuf_tensor("%s_%d" % (name or "t", uid[0]), list(shape), dt))

        def pst(ctx, shape, dt, name=None):
            uid[0] += 1
            return ctx.enter_context(nc.psum_tensor("%s_%d" % (name or "p", uid[0]), list(shape), dt))

        rr = {}

        def dmaname(stream, n):
            i = rr.get(stream, 0)
            rr[stream] = i + 1
            return "%s%d" % (stream, i % n)

        def dma(eng, out, in_, reads=(), writes=(), stream="g", n=4):
            kb.op(eng, lambda e: e.dma_start(out=out, in_=in_), reads=reads, writes=writes, dma=dmaname(stream, n))

        def mm(out, lhsT, rhs, start, stop, reads, writes, **kw):
            kb.op("pe", lambda e: e.matmul(out, lhsT=lhsT, rhs=rhs, start=start, stop=stop, **kw),
                  reads=reads, writes=writes)

        def tp(out, in_, ident, reads, writes):
            kb.op("pe", lambda e: e.transpose(out, in_, ident), reads=reads, writes=writes)

        ident = sbt(st, [128, 128], BF16, "ident")
        dma("sp", ident[:], ident_d, writes=["ident"])

        def end_phase():
            kb.barrier()
            kb.emit()

        def phase0():
            for l in range(L):
                for c0 in range(0, DIN, 944):
                    for kc in range(8):
                        r0 = kc * 128
                        dma("pool", winb_d[l, r0:r0 + 128, c0:c0 + 944], w_in_d[l, r0:r0 + 128, c0:c0 + 944], writes=[("winb", l, kc, c0)], stream="cast", n=4)
                if l == 0:
                    continue
            for l in range(L):
                for kc in range(8):
                    r0 = kc * 128
                    dma("pool", woutb_d[l, r0:r0 + 128, :], w_out_d[l, r0:r0 + 128, :], stream="cast", n=4)
                for fc in range(NFC):
                    for gu, wsrc in enumerate((wg_d, wu_d)):
                        dma("pool", wgub_d[l, fc, :, gu, :, :],
                            wsrc[l].rearrange("(kc p) f -> p kc f", p=128)[:, :, fc * 128:(fc + 1) * 128],
                            stream="cast", n=4)
                    dma("pool", wdb_d[l, fc * 128:(fc + 1) * 128, :], wd_d[l, fc * 128:(fc + 1) * 128, :], stream="cast", n=4)

        def rstd_from_ssq(ssq, rstd, n, tag):
            kb.op("dve", lambda e: e.tensor_scalar(out=rstd, in0=ssq, scalar1=1.0 / n, scalar2=EPS, op0=ALU.mult, op1=ALU.add),
                  reads=[tag + "ssq"], writes=[tag + "rs"])
            kb.op("act", lambda e: e.sqrt(out=rstd, in_=rstd), reads=[tag + "rs"], writes=[tag + "rs"])
            kb.op("dve", lambda e: e.reciprocal(out=rstd, in_=rstd), reads=[tag + "rs"], writes=[tag + "rs"])

        def phase1(l, xin_d):
            with ExitStack() as ph:
                win = sbt(ph, [128, 8, DIN], BF16, "win")
                gpre = sbt(ph, [128, D], F32, "gpre")
                xt = [sbt(ph, [128, D], F32, "xt") for _ in range(8)]
                hb = [sbt(ph, [128, D], BF16, "hb") for _ in range(4)]
                junk = sbt(ph, [128, D], BF16, "junk")
                hT = [sbt(ph, [128, 8, 512], BF16, "hT") for _ in range(2)]
                ssq = sbt(ph, [128, 8], F32, "ssq")
                rst = sbt(ph, [128, 8], F32, "rst")
                sf = [sbt(ph, [128, 512], F32, "sf") for _ in range(6)]
                sbf = [sbt(ph, [128, 512], BF16, "sbf") for _ in range(6)]
                sgv = [sbt(ph, [128, 256], BF16, "sgv") for _ in range(4)]
                sgo = [sbt(ph, [128, 256], F32, "sgo") for _ in range(4)]
                smv = [sbt(ph, [128, 8, 65], BF16, "smv") for _ in range(4)]
                tps = [pst(ph, [128, D], BF16, "tps") for _ in range(2)]
                aps = [pst(ph, [128, 512], F32, "aps") for _ in range(5)]
                for c0_ in range(0, DIN, 944):
                    for kc in range(8):
                        dma("sp", win[:, kc, c0_:c0_ + 944], winb_d[l, kc * 128:(kc + 1) * 128, c0_:c0_ + 944], reads=[("winb", l, kc, c0_)], writes=[("win", kc, c0_)], stream="w", n=8)

                def wink(c_lo, c_hi):
                    return [("win", kc_, cb_) for kc_ in range(8) for cb_ in range(0, DIN, 944) if cb_ < c_hi and cb_ + 944 > c_lo]

                dma("sp", gpre[:], norms_d[l, 0:1, :].partition_broadcast(128), writes=["gpre"])
                for i in range(4):
                    kb.op("dve", lambda e, i=i: e.memset(smv[i][:], 1.0), writes=[("smv", i)])

                flist = [("lru", lruT_d, 0, 0, 128, F32), ("lru", lruT_d, 128, 128, 128, F32),
                         ("lru", lruT_d, 256, 256, 128, F32), ("lru", lruT_d, 384, 384, 128, F32),
                         ("gq", gqT_d, 0, 512, 128, F32), ("gk", gkT_d, 0, 640, 128, F32),
                         ("glr", glrT_d, 0, 1024, 16, BF16)]
                for i in range(4):
                    flist.append(("mq", mqT_d, i * 128, 1296 + i * 128, 128, BF16))
                for i in range(4):
                    flist.append(("mk", mkT_d, i * 128, 1808 + i * 128, 128, BF16))

                def load(t):
                    dma("sp", xt[t % 8][:], xin_d[t * 128:(t + 1) * 128, :], writes=[("xt", t % 8)], stream="x", n=4)

                NT = S // 128
                pi = 0
                ev = 0
                import os as _os
                _ng = int(_os.environ.get("P1_GROUPS", S // 512))
                _parts = int(_os.environ.get("P1_PARTS", 7))

                def chain(g):
                    for s in range(4):
                        t = g * 4 + s
                        xs = xt[t % 8]
                        hbs = hb[s]
                        c = t % 8
                        kb.op("act", lambda e, xs=xs, c=c: e.activation(out=junk[:], in_=xs[:], func=AF.Square, accum_out=ssq[:, c:c + 1]),
                              reads=[("xt", t % 8)], writes=["junk", "p1ssq"])
                        rstd_from_ssq(ssq[:, c:c + 1], rst[:, c:c + 1], D, "p1")
                        kb.op("dve", lambda e, xs=xs, hbs=hbs, c=c: e.scalar_tensor_tensor(out=hbs[:], in0=xs[:], scalar=rst[:, c:c + 1], in1=gpre[:], op0=ALU.mult, op1=ALU.mult),
                              reads=[("xt", t % 8), "p1rs", "gpre"], writes=[("hb", s)])

                def transp(g):
                    hTg_ = hT[g % 2]
                    for s in range(4):
                        t = g * 4 + s
                        hbs = hb[s]
                        tpp = tps[t % 2]
                        for kc in range(8):
                            tp(tpp[:, kc * 128:(kc + 1) * 128], hbs[:, kc * 128:(kc + 1) * 128], ident[:],
                               reads=[("hb", s), "ident"], writes=[("tps", t % 2)])
                        kb.op("act", lambda e, tpp=tpp, hTg_=hTg_, s=s: e.copy(out=hTg_[:, :, s * 128:(s + 1) * 128], in_=tpp[:].rearrange("p (k c) -> p k c", k=8)),
                              reads=[("tps", t % 2)], writes=[("hT", g % 2)])

                for t in range(8):
                    load(t)
                chain(0)
                transp(0)
                for g in range(_ng):
                    hTg = hT[g % 2]
                    if g + 1 < _ng:
                        chain(g + 1)
                    if g + 2 < _ng:
                        for s in range(4):
                            load((g + 2) * 4 + s)
                    for (nm, dst, drow, wcol, wid, dt) in (flist if _parts & 2 else []):
                        ps = aps[pi % 5]
                        pk = ("aps", pi % 5)
                        pi += 1
                        for kc in range(8):
                            mm(ps[0:wid, :], win[:, kc, wcol:wcol + wid], hTg[:, kc, :], kc == 0, kc == 7,
                               reads=wink(wcol, wcol + wid) + [("hT", g % 2)], writes=[pk])
                        if dt == F32:
                            stg = sf[ev % 6]
                            sk = ("sf", ev % 6)
                        else:
                            stg = sbf[ev % 6]
                            sk = ("sbf", ev % 6)
                        eng = "act" if ev % 2 == 0 else "dve"
                        ev += 1
                        if nm == "mq":
                            if eng == "act":
                                kb.op("act", lambda e, stg=stg, ps=ps, wid=wid: e.mul(out=stg[0:wid, :], in_=ps[0:wid, :], mul=0.125), reads=[pk], writes=[sk])
                            else:
                                kb.op("dve", lambda e, stg=stg, ps=ps, wid=wid: e.tensor_scalar(out=stg[0:wid, :], in0=ps[0:wid, :], scalar1=0.125, scalar2=None, op0=ALU.mult), reads=[pk], writes=[sk])
                        else:
                            if eng == "act":
                                kb.op("act", lambda e, stg=stg, ps=ps, wid=wid: e.copy(out=stg[0:wid, :], in_=ps[0:wid, :]), reads=[pk], writes=[sk])
                            else:
                                kb.op("dve", lambda e, stg=stg, ps=ps, wid=wid: e.tensor_copy(out=stg[0:wid, :], in_=ps[0:wid, :]), reads=[pk], writes=[sk])
                        dma("sp", dst[drow:drow + wid, g * 512:(g + 1) * 512], stg[0:wid, :], reads=[sk], stream="o1", n=12)
                    if g + 1 < _ng:
                        transp(g + 1)
                    for s in (range(4) if _parts & 4 else []):
                        t = g * 4 + s
                        _tm = int(_os.environ.get("TM_SKIP", 0))
                        if not _tm & 1:
                            ps = aps[pi % 5]
                            pk = ("aps", pi % 5)
                            pi += 1
                            ps2 = aps[pi % 5]
                            pk2 = ("aps", pi % 5)
                            pi += 1
                            for kc in range(8):
                                mm(ps[:, 0:256], hTg[:, kc, s * 128:(s + 1) * 128], win[:, kc, 768:1024], kc == 0, kc == 7,
                                   reads=wink(768, 1024) + [("hT", g % 2)], writes=[pk])
                            for kc in range(8):
                                mm(ps2[:, 0:256], hTg[:, kc, s * 128:(s + 1) * 128], win[:, kc, 1040:1296], kc == 0, kc == 7,
                                   reads=wink(1040, 1296) + [("hT", g % 2)], writes=[pk2])
                            a, b = sgv[t % 4], sgo[t % 4]
                            kb.op("act", lambda e, a=a, ps=ps: e.copy(out=a[:], in_=ps[:, 0:256]), reads=[pk], writes=[("sgv", t % 4)])
                            kb.op("dve", lambda e, b=b, ps2=ps2: e.tensor_copy(out=b[:], in_=ps2[:, 0:256]), reads=[pk2], writes=[("sgo", t % 4)])
                            dma("sp", gv_d[t * 128:(t + 1) * 128, :], a[:], reads=[("sgv", t % 4)], stream="o1", n=12)
                            dma("sp", gout_d[t * 128:(t + 1) * 128, :], b[:], reads=[("sgo", t % 4)], stream="o1", n=12)
                        if not _tm & 2:
                            ps = aps[pi % 5]
                            pk = ("aps", pi % 5)
                            pi += 1
                            for kc in range(8):
                                mm(ps[:, :], hTg[:, kc, s * 128:(s + 1) * 128], win[:, kc, 2320:2832], kc == 0, kc == 7,
                                   reads=wink(2320, 2832) + [("hT", g % 2)], writes=[pk])
                            m = smv[t % 4]
                            psv = ps[:].rearrange("p (h d) -> p h d", h=8)
                            if _tm & 4:
                                pass
                            elif t % 2:
                                kb.op("act", lambda e, m=m, psv=psv: e.copy(out=m[:, :, 0:64], in_=psv), reads=[pk], writes=[("smv", t % 4)])
                            else:
                                kb.op("dve", lambda e, m=m, psv=psv: e.tensor_copy(out=m[:, :, 0:64], in_=psv), reads=[pk], writes=[("smv", t % 4)])
                            if not _tm & 8:
                                dma("sp", mvp_d[t * 128:(t + 1) * 128, :], m[:].rearrange("p h d -> p (h d)"), reads=[("smv", t % 4)], stream="o1", n=12)
                if _os.environ.get("P1_TAILSTORE"):
                    dma("sp", lruT_d[0:128, 0:8], rst[:], reads=["p1rs"], stream="o1", n=12)
                end_phase()

        def phase2a_gen(l, ph):
            TB = 1024
            if True:
                cols = sbt(ph, [128, 2, 8], F32, "lcols")
                ccol = sbt(ph, [128, 2], F32, "ccol")
                wstage = sbt(ph, [128, 2, 2, 128], F32, "wstage")
                wbd = sbt(ph, [128, 2, 2, 128], BF16, "wbd")
                xin = [sbt(ph, [128, TB + 3], F32, "xin") for _ in range(2)]
                gin = [sbt(ph, [128, TB], F32, "gin") for _ in range(2)]
                xc = sbt(ph, [128, TB], F32, "xc")
                xcb = sbt(ph, [128, TB], BF16, "xcb")
                rr_ = sbt(ph, [128, TB], F32, "r")
                ii_ = sbt(ph, [128, TB], F32, "i")
                aa = sbt(ph, [128, TB], F32, "a")
                mmul = sbt(ph, [128, TB], F32, "mult")
                uu = sbt(ph, [128, TB], F32, "u")
                hh = [sbt(ph, [128, TB], F32, "h") for _ in range(2)]
                gt = sbt(ph, [128, TB], F32, "gt")
                gs = sbt(ph, [128, TB], F32, "gs")
                yb = [sbt(ph, [128, TB], BF16, "yb") for _ in range(2)]
                gps = [pst(ph, [128, 512], F32, "gps") for _ in range(2)]
                for h in range(2):
                    dma("sp", cols[:, h, :], lcols_d[l, h], writes=["lcols"])
                kb.op("pool", lambda e: e.memset(wstage[:], 0.0), writes=["wstage"])
                for ax, src in enumerate((lwa_d, lwx_d)):
                    for h in range(2):
                        for b in range(2):
                            dma("sp", wstage[b * 64:(b + 1) * 64, ax, h, b * 64:(b + 1) * 64], src[l, 2 * h + b],
                                reads=[], writes=["wstage"])
                kb.op("dve", lambda e: e.tensor_copy(out=wbd[:], in_=wstage[:]), reads=["wstage"], writes=["wbd"])
                kb.op("act", lambda e: e.activation(out=ccol[:], in_=cols[:, :, 7], func=AF.Exp, scale=-1.0), reads=["lcols"], writes=["ccol"])
                kb.op("act", lambda e: e.activation(out=ccol[:], in_=ccol[:], func=AF.Ln, bias=1.0), reads=["ccol"], writes=["ccol"])
                kb.op("dve", lambda e: e.tensor_scalar(out=ccol[:], in0=ccol[:], scalar1=-8.0, scalar2=None, op0=ALU.mult), reads=["ccol"], writes=["ccol"])

                nb = S // TB
                it = 0
                for h in range(2):
                    for b in range(nb):
                        t0 = b * TB
                        xi = xin[it % 2]
                        gi = gin[it % 2]
                        hcur = hh[it % 2]
                        hprev = hh[(it + 1) % 2]
                        ybs = yb[it % 2]
                        kx, kg, ky = ("xin", it % 2), ("gin", it % 2), ("yb", it % 2)
                        kh, khp = ("h", it % 2), ("h", (it + 1) % 2)
                        it += 1
                        if b == 0:
                            kb.op("pool", lambda e, xi=xi: e.memset(xi[:, 0:3], 0.0), writes=[kx])
                            dma("sp", xi[:, 3:], lruT_d[h * 128:(h + 1) * 128, 0:TB], writes=[kx], stream="x", n=3)
                        else:
                            dma("sp", xi[:], lruT_d[h * 128:(h + 1) * 128, t0 - 3:t0 + TB], writes=[kx], stream="x", n=3)
                        dma("sp", gi[:], lruT_d[256 + h * 128:256 + (h + 1) * 128, t0:t0 + TB], writes=[kg], stream="x", n=3)
                        yield
                        kb.op("dve", lambda e, xi=xi, h=h: e.tensor_scalar(out=xc[:], in0=xi[:, 3:TB + 3], scalar1=cols[:, h, 3:4], scalar2=cols[:, h, 4:5], op0=ALU.mult, op1=ALU.add),
                              reads=[kx, "lcols"], writes=["xc"])
                        for j in range(3):
                            kb.op("dve", lambda e, xi=xi, h=h, j=j: e.scalar_tensor_tensor(out=xc[:], in0=xi[:, j:TB + j], scalar=cols[:, h, j:j + 1], in1=xc[:], op0=ALU.mult, op1=ALU.add),
                                  reads=[kx, "lcols", "xc"], writes=["xc"])
                        yield
                        kb.op("pool", lambda e: e.tensor_copy(out=xcb[:], in_=xc[:]), reads=["xc"], writes=["xcb"])
                        for sblk in range(TB // 512):
                            cs = slice(sblk * 512, (sblk + 1) * 512)
                            pa, px = gps[0], gps[1]
                            ka, kx_ = ("gps", 0), ("gps", 1)
                            mm(pa[:], wbd[:, 0, h, :], xcb[:, cs], True, True, reads=["wbd", "xcb"], writes=[ka])
                            mm(px[:], wbd[:, 1, h, :], xcb[:, cs], True, True, reads=["wbd", "xcb"], writes=[kx_])
                            kb.op("act", lambda e, pa=pa, cs=cs, h=h: e.activation(out=rr_[:, cs], in_=pa[:], func=AF.Sigmoid, bias=cols[:, h, 5:6]),
                                  reads=[ka, "lcols"], writes=["r"])
                            kb.op("act", lambda e, px=px, cs=cs, h=h: e.activation(out=ii_[:, cs], in_=px[:], func=AF.Sigmoid, bias=cols[:, h, 6:7]),
                                  reads=[kx_, "lcols"], writes=["i"])
                        yield
                        kb.op("pool", lambda e, gi=gi: e.tensor_tensor(out=gt[:], in0=gi[:], in1=gi[:], op=ALU.mult), reads=[kg], writes=["gt"])
                        kb.op("pool", lambda e: e.tensor_scalar(out=gt[:], in0=gt[:], scalar1=0.044715, scalar2=1.0, op0=ALU.mult, op1=ALU.add), reads=["gt"], writes=["gt"])
                        kb.op("pool", lambda e, gi=gi: e.tensor_tensor(out=gt[:], in0=gt[:], in1=gi[:], op=ALU.mult), reads=["gt", kg], writes=["gt"])
                        kb.op("act", lambda e: e.activation(out=gs[:], in_=gt[:], func=AF.Sigmoid, scale=1.5957691216057308), reads=["gt"], writes=["gs"])
                        kb.op("pool", lambda e, gi=gi: e.tensor_tensor(out=gs[:], in0=gs[:], in1=gi[:], op=ALU.mult), reads=["gs", kg], writes=["gs"])
                        yield
                        kb.op("act", lambda e, h=h: e.activation(out=aa[:], in_=rr_[:], func=AF.Exp, scale=ccol[:, h:h + 1]), reads=["r", "ccol"], writes=["a"])
                        kb.op("pool", lambda e: e.tensor_tensor(out=mmul[:], in0=aa[:], in1=aa[:], op=ALU.mult), reads=["a"], writes=["mult"])
                        kb.op("act", lambda e: e.activation(out=mmul[:], in_=mmul[:], func=AF.Sqrt, scale=-1.0, bias=1.0), reads=["mult"], writes=["mult"])
                        yield
                        if b == 0:
                            kb.op("dve", lambda e: e.memset(mmul[:, 0:1], 1.0), reads=["mult"], writes=["mult"])
                        kb.op("dve", lambda e: e.tensor_tensor(out=uu[:], in0=ii_[:], in1=xc[:], op=ALU.mult), reads=["i", "xc"], writes=["u"])
                        kb.op("dve", lambda e: e.tensor_tensor(out=uu[:], in0=uu[:], in1=mmul[:], op=ALU.mult), reads=["u", "mult"], writes=["u"])
                        yield
                        if b == 0:
                            kb.op("dve", lambda e, hcur=hcur: e.tensor_tensor_scan(out=hcur[:], data0=aa[:], data1=uu[:], initial=0.0, op0=ALU.mult, op1=ALU.add),
                                  reads=["a", "u"], writes=[kh])
                        else:
                            kb.op("dve", lambda e, hcur=hcur, hprev=hprev: e.tensor_tensor_scan(out=hcur[:], data0=aa[:], data1=uu[:], initial=hprev[:, TB - 1:TB], op0=ALU.mult, op1=ALU.add),
                                  reads=["a", "u", khp], writes=[kh])
                        yield
                        kb.op("dve", lambda e, hcur=hcur, ybs=ybs: e.tensor_tensor(out=ybs[:], in0=hcur[:], in1=gs[:], op=ALU.mult), reads=[kh, "gs"], writes=[ky])
                        dma("sp", mixT_d[h * 128:(h + 1) * 128, t0:t0 + TB], ybs[:], reads=[ky], stream="o", n=4)
                        yield

        def phase2b_gen(l, ph):
            TB = 1024
            NCH = TB // 128
            if True:
                w2s = sbt(ph, [16, 128], F32, "w2s")
                w2b = sbt(ph, [16, 128], BF16, "w2b")
                negb = sbt(ph, [128, 1], F32, "negb")
                gn = sbt(ph, [128, 256], F32, "gn")
                tri = sbt(ph, [128, 128], F32, "tri")
                bmask = sbt(ph, [128, 256], F32, "bmask")
                hm = sbt(ph, [128, 4], F32, "hm")
                ones = sbt(ph, [128, 128], F32, "ones")
                glr = [sbt(ph, [16, TB], BF16, "glr") for _ in range(2)]
                qT = [sbt(ph, [128, TB], F32, "qT") for _ in range(2)]
                kT = [sbt(ph, [128, TB], F32, "kT") for _ in range(2)]
                vv = [sbt(ph, [128, NCH, 256], BF16, "vv") for _ in range(2)]
                go = [sbt(ph, [128, NCH, 256], F32, "go") for _ in range(2)]
                ee = sbt(ph, [128, TB], F32, "ee")
                cum = sbt(ph, [128, TB], F32, "cum")
                ex = sbt(ph, [128, TB], F32, "ex")
                dd = sbt(ph, [128, TB], F32, "dd")
                qd = sbt(ph, [128, TB], BF16, "qd")
                kdm = sbt(ph, [128, 4, TB], BF16, "kdm")
                kdec = sbt(ph, [128, TB], BF16, "kdec")
                dcol = sbt(ph, [128, NCH], F32, "dcol")
                kdtm = [sbt(ph, [128, 128], BF16, "kdtm") for _ in range(2)]
                am = [sbt(ph, [128, 4, 128], BF16, "am") for _ in range(2)]
                Sst = sbt(ph, [128, 256], F32, "Sst")
                Sbf = sbt(ph, [128, 256], BF16, "Sbf")
                kvm = sbt(ph, [128, 256], F32, "kvm")
                ob = sbt(ph, [128, NCH, 256], F32, "ob")
                osq = sbt(ph, [128, NCH, 256], F32, "osq")
                ssq = sbt(ph, [128, NCH * 4], F32, "gssq")
                rst = sbt(ph, [128, NCH * 4], F32, "grst")
                sg = sbt(ph, [128, NCH, 256], F32, "sg")
                yb = sbt(ph, [128, NCH, 256], BF16, "yb")
                yT = [sbt(ph, [128, 2, TB], BF16, "yT") for _ in range(2)]
                zps = [pst(ph, [128, 512], F32, "zps") for _ in range(1)]
                tps = pst(ph, [128, 1024], BF16, "gtps")
                aps_ = [pst(ph, [128, 512], F32, "gaps") for _ in range(1)]
                ops_ = [pst(ph, [128, 512], F32, "gops") for _ in range(2)]
                kvps = pst(ph, [128, 512], F32, "kvps")

                dma("sp", w2s[:], gw2_d[l], writes=["w2s"])
                kb.op("dve", lambda e: e.tensor_copy(out=w2b[:], in_=w2s[:]), reads=["w2s"], writes=["w2b"])
                dma("sp", negb[:], gb_d[l], writes=["negb"])
                kb.op("dve", lambda e: e.tensor_scalar(out=negb[:], in0=negb[:], scalar1=-1.0, scalar2=None, op0=ALU.mult), reads=["negb"], writes=["negb"])
                dma("sp", gn[:], gn_d[l:l + 1, :].partition_broadcast(128), writes=["gn"])
                dma("sp", tri[:], tri_d, writes=["tri"])
                dma("sp", bmask[:], bmask_d, writes=["bmask"])
                dma("sp", hm[:], hm_d, writes=["hm"])
                kb.op("pool", lambda e: e.memset(ones[:], 1.0), writes=["ones"])
                kb.op("pool", lambda e: e.memset(Sst[:], 0.0), writes=["Sst"])
                kb.op("pool", lambda e: e.memset(Sbf[:], 0.0), writes=["Sbf"])

                def load(b):
                    i = b % 2
                    t0 = b * TB
                    dma("sp", glr[i][:], glrT_d[:, t0:t0 + TB], writes=[("glr", i)], stream="x", n=3)
                    dma("sp", qT[i][:], gqT_d[:, t0:t0 + TB], writes=[("qT", i)], stream="x", n=3)
                    dma("sp", kT[i][:], gkT_d[:, t0:t0 + TB], writes=[("kT", i)], stream="x", n=3)
                    dma("sp", vv[i][:], gv_d[t0:t0 + TB, :].rearrange("(c p) f -> p c f", p=128), writes=[("vv", i)], stream="x", n=3)
                    dma("sp", go[i][:], gout_d[t0:t0 + TB, :].rearrange("(c p) f -> p c f", p=128), writes=[("go", i)], stream="x", n=3)

                nb = S // TB
                load(0)
                for b in range(nb):
                    if b + 1 < nb:
                        load(b + 1)
                    i = b % 2
                    t0 = b * TB
                    q_, k_, v_, g_, r_ = qT[i], kT[i], vv[i], go[i], glr[i]
                    kq, kk, kv, kg, kr = ("qT", i), ("kT", i), ("vv", i), ("go", i), ("glr", i)
                    for sblk in range(TB // 512):
                        cs = slice(sblk * 512, (sblk + 1) * 512)
                        zp = zps[0]
                        mm(zp[:], w2b[:], r_[:, cs], True, True, reads=["w2b", kr], writes=[("zps", 0)])
                        kb.op("act", lambda e, zp=zp, cs=cs: e.activation(out=ee[:, cs], in_=zp[:], func=AF.Exp, scale=-1.0, bias=negb[:]),
                              reads=[("zps", 0), "negb"], writes=["ee"])
                    yield
                    kb.op("act", lambda e: e.activation(out=ee[:], in_=ee[:], func=AF.Ln, bias=1.0), reads=["ee"], writes=["ee"])
                    for c in range(NCH):
                        cs = slice(c * 128, (c + 1) * 128)
                        kb.op("dve", lambda e, cs=cs: e.tensor_tensor_scan(out=cum[:, cs], data0=ones[:], data1=ee[:, cs], initial=0.0, op0=ALU.mult, op1=ALU.add),
                              reads=["ones", "ee"], writes=["cum"])
                    yield
                    kb.op("act", lambda e: e.activation(out=ex[:], in_=cum[:], func=AF.Exp, scale=-1.0 / 16.0), reads=["cum"], writes=["ex"])
                    kb.op("dve", lambda e, q_=q_: e.scalar_tensor_tensor(out=qd[:], in0=q_[:], scalar=32.0 ** -0.5, in1=ex[:], op0=ALU.mult, op1=ALU.mult),
                          reads=[kq, "ex"], writes=["qd"])
                    yield
                    kb.op("act", lambda e: e.activation(out=dcol[:], in_=cum[:].rearrange("p (c t) -> p c t", t=128)[:, :, 127], func=AF.Exp, scale=-1.0 / 16.0),
                          reads=["cum"], writes=["dcol"])
                    for c in range(NCH):
                        cs = slice(c * 128, (c + 1) * 128)
                        kb.op("pool", lambda e, cs=cs, c=c: e.tensor_scalar(out=dd[:, cs], in0=cum[:, cs], scalar1=cum[:, c * 128 + 127:c * 128 + 128], scalar2=None, op0=ALU.subtract),
                              reads=["cum"], writes=["dd"])
                    yield
                    kb.op("act", lambda e: e.activation(out=ex[:], in_=cum[:], func=AF.Exp, scale=1.0 / 16.0), reads=["cum", "qd"], writes=["ex"])
                    for hh_ in range(4):
                        kb.op("dve", lambda e, k_=k_, hh_=hh_: e.scalar_tensor_tensor(out=kdm[:, hh_, :], in0=k_[:], scalar=hm[:, hh_:hh_ + 1], in1=ex[:], op0=ALU.mult, op1=ALU.mult),
                              reads=[kk, "ex", "hm"], writes=["kdm"])
                    yield
                    kb.op("act", lambda e: e.activation(out=dd[:], in_=dd[:], func=AF.Exp, scale=1.0 / 16.0), reads=["dd"], writes=["dd"])
                    kb.op("pool", lambda e, k_=k_: e.tensor_tensor(out=kdec[:], in0=k_[:], in1=dd[:], op=ALU.mult), reads=[kk, "dd"], writes=["kdec"])
                    kb.op("act", lambda e, g_=g_: e.activation(out=sg[:], in_=g_[:], func=AF.Silu), reads=[kg], writes=["sg"])
                    def gla_s1(c):
                        cs = slice(c * 128, (c + 1) * 128)
                        j = c % 2
                        tp(tps[:, j * 128:(j + 1) * 128], kdec[:, cs], ident[:], reads=["kdec", "ident"], writes=["gtps"])
                        kb.op("act", lambda e, j=j: e.copy(out=kdtm[j][:], in_=tps[:, j * 128:(j + 1) * 128]), reads=["gtps"], writes=[("kdtm", j)])
                        ap_ = aps_[0]
                        for hh_ in range(4):
                            mm(ap_[:, hh_ * 128:(hh_ + 1) * 128], kdm[:, hh_, cs], qd[:, cs], True, True, reads=["kdm", "qd"], writes=[("gaps", 0)])
                        kb.op("dve", lambda e, ap_=ap_, j=j: e.tensor_tensor(out=am[j][:], in0=ap_[:].rearrange("p (h c) -> p h c", h=4),
                                                                             in1=tri[:].unsqueeze(1).broadcast_to([128, 4, 128]), op=ALU.mult),
                              reads=[("gaps", 0), "tri"], writes=[("am", j)])

                    def gla_s2(c):
                        cs = slice(c * 128, (c + 1) * 128)
                        j = c % 2
                        op_ = ops_[j]
                        mm(op_[:, 0:256], qd[:, cs], Sbf[:], True, True, reads=["qd", "Sbf"], writes=[("gops", j)])
                        for hh_ in range(4):
                            mm(op_[:, hh_ * 64:(hh_ + 1) * 64], am[j][:, hh_, :], v_[:, c, hh_ * 64:(hh_ + 1) * 64], False, True,
                               reads=[("am", j), kv], writes=[("gops", j)], skip_group_check=True)
                        kb.op("act", lambda e, op_=op_, c=c: e.copy(out=ob[:, c, :], in_=op_[:, 0:256]), reads=[("gops", j)], writes=["ob"])
                        mm(kvps[:, 0:256], kdtm[j][:], v_[:, c, :], True, True, reads=[("kdtm", j), kv], writes=["kvps"])
                        kb.op("dve", lambda e: e.tensor_tensor(out=kvm[:], in0=kvps[:, 0:256], in1=bmask[:], op=ALU.mult), reads=["kvps", "bmask"], writes=["kvm"])
                        kb.op("dve", lambda e, c=c: e.scalar_tensor_tensor(out=Sst[:], in0=Sst[:], scalar=dcol[:, c:c + 1], in1=kvm[:], op0=ALU.mult, op1=ALU.add),
                              reads=["Sst", "dcol", "kvm"], writes=["Sst"])
                        kb.op("pool", lambda e: e.tensor_copy(out=Sbf[:], in_=Sst[:]), reads=["Sst"], writes=["Sbf"])

                    gla_s1(0)
                    yield
                    for c in range(NCH):
                        if c + 1 < NCH:
                            gla_s1(c + 1)
                            yield
                        gla_s2(c)
                        yield
                    yield
                    kb.op("pool", lambda e: e.tensor_tensor(out=osq[:], in0=ob[:], in1=ob[:], op=ALU.mult), reads=["ob"], writes=["osq"])
                    kb.op("dve", lambda e: e.tensor_reduce(out=ssq[:], in_=osq[:].rearrange("p c (h v) -> p (c h) v", h=4), axis=AX.X, op=ALU.add),
                          reads=["osq"], writes=["p2bssq"])
                    rstd_from_ssq(ssq[:], rst[:], 64, "p2b")
                    kb.op("dve", lambda e: e.tensor_tensor(out=ob[:].rearrange("p c (h v) -> p (c h) v", h=4), in0=ob[:].rearrange("p c (h v) -> p (c h) v", h=4),
                                                           in1=rst[:].unsqueeze(2).broadcast_to([128, NCH * 4, 64]), op=ALU.mult),
                          reads=["ob", "p2brs"], writes=["ob"])
                    kb.op("pool", lambda e: e.tensor_tensor(out=sg[:], in0=sg[:], in1=gn[:].unsqueeze(1).broadcast_to([128, NCH, 256]), op=ALU.mult),
                          reads=["sg", "gn"], writes=["sg"])
                    kb.op("dve", lambda e: e.tensor_tensor(out=yb[:], in0=ob[:], in1=sg[:], op=ALU.mult), reads=["ob", "sg"], writes=["yb"])
                    yield
                    yTb = yT[b % 2]
                    for c in range(NCH):
                        for f in range(2):
                            jj = (c * 2 + f) % 4
                            tp(tps[:, jj * 128:(jj + 1) * 128], yb[:, c, f * 128:(f + 1) * 128], ident[:], reads=["yb", "ident"], writes=["gtps"])
                            kb.op("act", lambda e, jj=jj, c=c, f=f, yTb=yTb: e.copy(out=yTb[:, f, c * 128:(c + 1) * 128], in_=tps[:, jj * 128:(jj + 1) * 128]),
                                  reads=["gtps"], writes=[("yT", b % 2)])
                    dma("sp", mixT_d[256:512, t0:t0 + TB].rearrange("(f p) t -> p f t", p=128), yTb[:], reads=[("yT", b % 2)], stream="o", n=4)

        def phase2ab(l):
            with ExitStack() as ph:
                gens = [phase2a_gen(l, ph), phase2b_gen(l, ph)]
                while gens:
                    for g_ in list(gens):
                        try:
                            next(g_)
                        except StopIteration:
                            gens.remove(g_)
                end_phase()

        def phase2c(l):
            with ExitStack() as ph:
                kaug = sbt(ph, [128, 8, S], BF16, "kaug")
                vp = sbt(ph, [128, 32, 520], BF16, "vp")
                qaug = [sbt(ph, [128, 8, 512], BF16, "qaug") for _ in range(3)]
                km = sbt(ph, [64, 8, 16], F32, "km")
                kmb = sbt(ph, [64, 8, 16], BF16, "kmb")
                cm = sbt(ph, [128, 16, 16], F32, "cm")
                pm = sbt(ph, [128, 16, 16], F32, "pm")
                b31 = sbt(ph, [128, 8], F32, "b31")
                caus = sbt(ph, [128, 128], F32, "caus")
                tstage = sbt(ph, [128, 2, 8, 128], F32, "tstage")
                tdT = sbt(ph, [128, 8, 128], BF16, "tdT")
                toT = sbt(ph, [128, 8, 128], BF16, "toT")
                tomT = sbt(ph, [128, 8, 128], BF16, "tomT")
                zer = sbt(ph, [128, 260], BF16, "zer")
                gm = sbt(ph, [128, 4, 8, 16], F32, "gm")
                m8 = sbt(ph, [128, 4, 8, 8], F32, "m8")
                sel = sbt(ph, [128, 4, 8, 16], F32, "sel")
                mpad = [sbt(ph, [128, 4, 8, 80], BF16, "mpad") for _ in range(2)]
                pT = [sbt(ph, [128, 512], BF16, "pT") for _ in range(4)]
                rcp = sbt(ph, [128, 4], F32, "rcp")
                ymo = [sbt(ph, [128, 4, 512], BF16, "ymo") for _ in range(2)]
                ymT = [sbt(ph, [128, 4, 512], BF16, "ymT") for _ in range(2)]
                gps = pst(ph, [128, 512], F32, "mgps")
                mtps = [pst(ph, [128, 512], F32, "mtps") for _ in range(1)]
                mtps_b = [pst(ph, [128, 1024], BF16, "mtpsb") for _ in range(1)]
                sps = [pst(ph, [128, 512], F32, "sps") for _ in range(3)]
                accs_full = [pst(ph, [128, 512], F32, "acc") for _ in range(2)]
                accs = [a_[:, 0:260].rearrange("p (s d) -> p s d", s=4) for a_ in accs_full]

                for h in range(8):
                    dma("sp", kaug[0:64, h, :], mkT_d[h * 64:(h + 1) * 64, :], writes=["kaug"], stream="x", n=3)
                    dma("sp", kaug[64:80, h, :], e16_d, writes=["kaug"], stream="x", n=3)
                for c in range(4):
                    dma("sp", vp[:, c * 8:(c + 1) * 8, :], mvp_d[c * 1024:(c + 1) * 1024, :].rearrange("(c p) f -> p c f", p=128), writes=["vp"], stream="x", n=3)
                dma("sp", cm[:].rearrange("p a b -> p (a b)"), cm_d.partition_broadcast(128), writes=["cm"])
                dma("sp", pm[:].rearrange("p a b -> p (a b)"), pm_d.partition_broadcast(128), writes=["pm"])
                dma("sp", b31[:], rb31_d.partition_broadcast(128), writes=["b31"])
                dma("sp", caus[:], caus_d, writes=["caus"])
                dma("sp", tstage[:, 0], tdg_d, writes=["tstage"])
                dma("sp", tstage[:, 1], tof_d, writes=["tstage"])
                kb.op("dve", lambda e: e.tensor_tensor(out=tdT[:], in0=tstage[:, 0], in1=caus[:].unsqueeze(1).broadcast_to([128, 8, 128]), op=ALU.add),
                      reads=["tstage", "caus"], writes=["tdT"])
                kb.op("dve", lambda e: e.tensor_copy(out=toT[:], in_=tstage[:, 1]), reads=["tstage"], writes=["toT"])
                kb.op("dve", lambda e: e.tensor_tensor(out=tomT[:], in0=tstage[:, 1], in1=b31[:].unsqueeze(2).broadcast_to([128, 8, 128]), op=ALU.subtract),
                      reads=["tstage", "b31"], writes=["tomT"])
                kb.op("pool", lambda e: e.memset(zer[:], 0.0), writes=["zer"])
                for i in range(2):
                    kb.op("pool", lambda e, i=i: e.memset(mpad[i][:], 0.0), writes=[("mpad", i)])
                kb.op("dve", lambda e: e.tensor_reduce(out=km[:].rearrange("p h n -> p (h n)"), in_=kaug[0:64, :, :].rearrange("p h (n t) -> p (h n) t", t=256), axis=AX.X, op=ALU.add),
                      reads=["kaug"], writes=["km"])
                kb.op("dve", lambda e: e.tensor_scalar(out=kmb[:], in0=km[:], scalar1=1.0 / 256.0, scalar2=None, op0=ALU.mult), reads=["km"], writes=["kmb"])

                def loadq(G):
                    i = G % 3
                    dma("sp", qaug[i][0:64, :, :], mqT_d.rearrange("(h d) t -> d h t", d=64)[:, :, G * 512:(G + 1) * 512], writes=[("qaug", i)], stream="q", n=3)

                NG = S // 512
                si = 0
                ai = 0

                def pre1(G):
                    qi = G % 3
                    qa = qaug[qi]
                    kqa = ("qaug", qi)
                    mp = mpad[G % 2]
                    for s in range(4):
                        for h in range(8):
                            mm(gps[:, (s * 8 + h) * 16:(s * 8 + h + 1) * 16], qa[0:64, h, s * 128:(s + 1) * 128], kmb[:, h, :], True, True,
                               reads=[kqa, "kmb"], writes=["mgps"])
                    np0 = 2 * G
                    for a in range(2):
                        cmv = cm[:, np0 + a, :].unsqueeze(1).unsqueeze(1).broadcast_to([128, 2, 8, 16])
                        kb.op("dve", lambda e, cmv=cmv, a=a: e.tensor_tensor(out=gm[:, 2 * a:2 * a + 2], in0=gps[:].rearrange("p (s h n) -> p s h n", s=4, h=8)[:, 2 * a:2 * a + 2],
                                                                             in1=cmv, op=ALU.add),
                              reads=["mgps", "cm"], writes=["gm"])
                    for s in range(4):
                        for h in range(8):
                            kb.op("dve", lambda e, s=s, h=h: e.max(out=m8[:, s, h, :], in_=gm[:, s, h, :]), reads=["gm"], writes=["m8"])
                    kb.op("dve", lambda e: e.tensor_tensor(out=sel[:], in0=gm[:], in1=m8[:, :, :, 2:3].broadcast_to([128, 4, 8, 16]), op=ALU.is_ge),
                          reads=["gm", "m8"], writes=["sel"])
                    kb.op("dve", lambda e: e.tensor_scalar(out=sel[:], in0=sel[:], scalar1=-NEG, scalar2=NEG, op0=ALU.mult, op1=ALU.add), reads=["sel"], writes=["sel"])
                    kb.op("dve", lambda e: e.tensor_tensor(out=sel[:], in0=sel[:], in1=b31[:].unsqueeze(1).unsqueeze(3).broadcast_to([128, 4, 8, 16]), op=ALU.add),
                          reads=["sel", "b31"], writes=["sel"])
                    for a in range(2):
                        pmv = pm[:, np0 + a, :].unsqueeze(1).unsqueeze(1).broadcast_to([128, 2, 8, 16])
                        kb.op("dve", lambda e, pmv=pmv, mp=mp, a=a: e.tensor_tensor(out=mp[:, 2 * a:2 * a + 2, :, 64:80], in0=sel[:, 2 * a:2 * a + 2], in1=pmv, op=ALU.mult),
                              reads=["sel", "pm"], writes=[("mpad", G % 2)])

                def pre2(G):
                    qi = G % 3
                    qa = qaug[qi]
                    kqa = ("qaug", qi)
                    mp = mpad[G % 2]
                    for h in range(8):
                        mt = mtps[0]
                        for s in range(4):
                            mm(mt[0:80, s * 128:(s + 1) * 128], mp[:, s, h, :], ident[:], True, True, reads=[("mpad", G % 2), "ident"], writes=[("mtps", 0)])
                        kb.op("act", lambda e, mt=mt, h=h, qa=qa: e.copy(out=qa[64:80, h, :], in_=mt[64:80, :]),
                              reads=[("mtps", 0)], writes=[kqa])

                loadq(0)
                if NG > 1:
                    loadq(1)
                pre1(0)
                pre2(0)
                for G in range(NG):
                    if G + 2 < NG:
                        loadq(G + 2)
                    if G + 1 < NG:
                        pre1(G + 1)
                    qi = G % 3
                    qa = qaug[qi]
                    kqa = ("qaug", qi)
                    ym = ymo[G % 2]
                    nj = 4 * G + 4
                    DEPTH = 2

                    def stageA(h, j):
                        nonlocal si
                        acc = accs[h % 2]
                        ka = ("acc", h % 2)
                        if j == 0:
                            mm(accs_full[h % 2][:, 0:260], zer[:, 0:128], zer[:, :], True, True, reads=["zer"], writes=[ka])
                        r = j - 4 * G
                        c0 = max(r, 0) * 128
                        sp_ = sps[si % 3]
                        ks = ("sps", si % 3)
                        pt = pT[si % 4]
                        kp = ("pT", si % 4)
                        si += 1
                        mm(sp_[:, c0:512], kaug[0:80, h, j * 128:(j + 1) * 128], qa[0:80, h, c0:512], True, True,
                           reads=["kaug", kqa], writes=[ks])
                        if r == -1:
                            mm(sp_[:, 0:128], ident[:], tomT[:, h, :], False, True, reads=["ident", "tomT"], writes=[ks], skip_group_check=True)
                        if r >= 0:
                            mm(sp_[:, r * 128:(r + 1) * 128], ident[:], tdT[:, h, :], False, True, reads=["ident", "tdT"], writes=[ks], skip_group_check=True)
                            if r < 3:
                                tt = toT if r % 2 == 0 else tomT
                                mm(sp_[:, (r + 1) * 128:(r + 2) * 128], ident[:], tt[:, h, :], False, True, reads=["ident", "toT", "tomT"], writes=[ks], skip_group_check=True)
                        kb.op("act", lambda e, pt=pt, sp_=sp_, c0=c0: e.activation(out=pt[:, c0:512], in_=sp_[:, c0:512], func=AF.Exp),
                              reads=[ks], writes=[kp])
                        return (h, j, r, pt, kp, acc, ka)

                    def stageB(info):
                        h, j, r, pt, kp, acc, ka = info
                        for s in range(max(r, 0), 4):
                            mm(acc[:, s, :], pt[:, s * 128:(s + 1) * 128], vp[:, j, h * 65:(h + 1) * 65], False, True,
                               reads=[kp, "vp"], writes=[ka], skip_group_check=True)
                        if j == nj - 1:
                            kb.op("dve", lambda e, acc=acc: e.reciprocal(out=rcp[:], in_=acc[:, :, 64]), reads=[ka], writes=["rcp"])
                            kb.op("dve", lambda e, acc=acc, h=h, ym=ym: e.tensor_tensor(out=ym[:, :, h * 64:(h + 1) * 64], in0=acc[:, :, 0:64],
                                                                                  in1=rcp[:].unsqueeze(2).broadcast_to([128, 4, 64]), op=ALU.mult),
                                  reads=[ka, "rcp"], writes=[("ymo", G % 2)])

                    pend = []
                    for h in range(8):
                        if h == 4 and G + 1 < NG:
                            pre2(G + 1)
                        for j in range(nj):
                            pend.append(stageA(h, j))
                            if len(pend) > DEPTH:
                                stageB(pend.pop(0))
                    while pend:
                        stageB(pend.pop(0))
                    yt = ymT[G % 2]
                    for s in range(4):
                        tpb = mtps_b[0]
                        for f in range(4):
                            tp(tpb[:, f * 128:(f + 1) * 128], ym[:, s, f * 128:(f + 1) * 128], ident[:], reads=[("ymo", G % 2), "ident"], writes=[("mtpsb", 0)])
                        kb.op("act", lambda e, tpb=tpb, yt=yt, s=s: e.copy(out=yt[:, :, s * 128:(s + 1) * 128], in_=tpb[:, 0:512].rearrange("p (f q) -> p f q", f=4)),
                              reads=[("mtpsb", 0)], writes=[("ymT", G % 2)])
                    dma("sp", mixT_d[512:1024, G * 512:(G + 1) * 512].rearrange("(f p) t -> p f t", p=128), yt[:], reads=[("ymT", G % 2)], stream="o", n=4)
                end_phase()

        def phase3(l, xin_d, xout_d):
            with ExitStack() as ph:
                wout = sbt(ph, [128, 8, D], BF16, "wout")
                wdn = sbt(ph, [128, NFC, D], BF16, "wdn")
                g3 = sbt(ph, [128, 3, D], F32, "g3")
                wgu = [sbt(ph, [128, 2, 8, 128], BF16, "wgu") for _ in range(4)]
                mixT = [sbt(ph, [128, 8, 512], BF16, "mixT") for _ in range(2)]
                xt = [sbt(ph, [128, D], F32, "xt3") for _ in range(2)]
                x1 = [sbt(ph, [128, D], F32, "x1") for _ in range(8)]
                tmp = [sbt(ph, [128, D], F32, "tmp3") for _ in range(2)]
                junk = sbt(ph, [128, D], BF16, "junk3")
                hb = [sbt(ph, [128, D], BF16, "hb3") for _ in range(4)]
                hT = sbt(ph, [128, 8, 512], BF16, "hT3")
                actT = sbt(ph, [128, NFC, 512], BF16, "actT")
                sgt = [sbt(ph, [128, 512], F32, "sgt") for _ in range(2)]
                ssq = sbt(ph, [128, 16], F32, "ssq3")
                rst = sbt(ph, [128, 16], F32, "rst3")
                xo = [sbt(ph, [128, D], F32, "xo") for _ in range(2)]
                ops_ = [pst(ph, [128, 2, 512], F32, "p3o") for _ in range(2)]
                tps = pst(ph, [128, D], BF16, "p3t")
                gus = [pst(ph, [128, 512], F32, "p3gu") for _ in range(3)]
                for kc in range(8):
                    dma("sp", wout[:, kc, :], woutb_d[l, kc * 128:(kc + 1) * 128, :], writes=["wout"], stream="w", n=4)
                for fc in range(NFC):
                    dma("sp", wdn[:, fc, :], wdb_d[l, fc * 128:(fc + 1) * 128, :], writes=["wdn"], stream="w", n=4)
                for i in range(3):
                    dma("sp", g3[:, i, :], norms_d[l, i + 1:i + 2, :].partition_broadcast(128), writes=["g3"])

                wi = [0]

                def loadw(fc):
                    i = wi[0] % 4
                    wi[0] += 1
                    dma("sp", wgu[i][:], wgub_d[l, fc], writes=[("wgu", i)], stream="wgu", n=4)
                    return i

                NG = S // 512
                sq = 0

                def loadg(G):
                    i = G % 2
                    dma("sp", mixT[i][:], mixT_d[:, G * 512:(G + 1) * 512].rearrange("(k p) t -> p k t", p=128), writes=[("mixT", i)], stream="m", n=2)

                def loadx(t):
                    dma("sp", xt[t % 2][:], xin_d[t * 128:(t + 1) * 128, :], writes=[("xt3", t % 2)], stream="x", n=3)

                loadg(0)
                loadx(0)
                wq = []
                PRE = 3
                def p3_front(G):
                    nonlocal sq
                    mT = mixT[G % 2]
                    for s in range(4):
                        t = G * 4 + s
                        if t + 1 < S // 128:
                            loadx(t + 1)
                        xs = xt[t % 2]
                        x1s = x1[(G % 2) * 4 + s]
                        op_ = ops_[s % 2]
                        ko = ("p3o", s % 2)
                        tm_ = tmp[s % 2]
                        kt = ("tmp3", s % 2)
                        for hf in range(2):
                            for kc in range(8):
                                mm(op_[:, hf, :], mT[:, kc, s * 128:(s + 1) * 128], wout[:, kc, hf * 512:(hf + 1) * 512], kc == 0, kc == 7,
                                   reads=[("mixT", G % 2), "wout"], writes=[ko])
                        c = sq % 16
                        sq += 1
                        kb.op("act", lambda e, c=c, op_=op_: e.activation(out=junk[:], in_=op_[:].rearrange("p a b -> p (a b)"), func=AF.Square, accum_out=ssq[:, c:c + 1]),
                              reads=[ko], writes=["junk3", "p3ssq"])
                        rstd_from_ssq(ssq[:, c:c + 1], rst[:, c:c + 1], D, "p3")
                        kb.op("dve", lambda e, c=c, op_=op_, tm_=tm_: e.scalar_tensor_tensor(out=tm_[:], in0=op_[:].rearrange("p a b -> p (a b)"), scalar=rst[:, c:c + 1], in1=g3[:, 0, :], op0=ALU.mult, op1=ALU.mult),
                              reads=[ko, "p3rs", "g3"], writes=[kt])
                        kb.op("pool", lambda e, xs=xs, x1s=x1s, tm_=tm_: e.tensor_tensor(out=x1s[:], in0=xs[:], in1=tm_[:], op=ALU.add),
                              reads=[("xt3", t % 2), kt], writes=[("x1", (G % 2) * 4 + s)])
                        c2 = sq % 16
                        sq += 1
                        kb.op("act", lambda e, c2=c2, x1s=x1s: e.activation(out=junk[:], in_=x1s[:], func=AF.Square, accum_out=ssq[:, c2:c2 + 1]),
                              reads=[("x1", (G % 2) * 4 + s)], writes=["junk3", "p3ssq"])
                        rstd_from_ssq(ssq[:, c2:c2 + 1], rst[:, c2:c2 + 1], D, "p3")
                        hbs = hb[s]
                        kb.op("dve", lambda e, c2=c2, x1s=x1s, hbs=hbs: e.scalar_tensor_tensor(out=hbs[:], in0=x1s[:], scalar=rst[:, c2:c2 + 1], in1=g3[:, 1, :], op0=ALU.mult, op1=ALU.mult),
                              reads=[("x1", (G % 2) * 4 + s), "p3rs", "g3"], writes=[("hb3", s)])

                def p3_trans(G):
                    for s in range(4):
                        hbs = hb[s]
                        for kc in range(8):
                            tp(tps[:, kc * 128:(kc + 1) * 128], hbs[:, kc * 128:(kc + 1) * 128], ident[:], reads=[("hb3", s), "ident"], writes=["p3t"])
                        kb.op("act", lambda e, s=s: e.copy(out=hT[:, :, s * 128:(s + 1) * 128], in_=tps[:].rearrange("p (k c) -> p k c", k=8)),
                              reads=["p3t"], writes=["hT3"])

                def p3_gateup(G):
                    for fc in range(NFC):
                        wslot = wq.pop(0)
                        nxt = fc + PRE
                        if nxt < NFC:
                            wq.append(loadw(nxt))
                        w_ = wgu[wslot]
                        gp, up = gus[(2 * fc) % 3], gus[(2 * fc + 1) % 3]
                        kgp, kup = ("p3gu", (2 * fc) % 3), ("p3gu", (2 * fc + 1) % 3)
                        for kc in range(8):
                            mm(gp[:], w_[:, 0, kc, :], hT[:, kc, :], kc == 0, kc == 7, reads=[("wgu", wslot), "hT3"], writes=[kgp])
                        for kc in range(8):
                            mm(up[:], w_[:, 1, kc, :], hT[:, kc, :], kc == 0, kc == 7, reads=[("wgu", wslot), "hT3"], writes=[kup])
                        sg_ = sgt[fc % 2]
                        kb.op("act", lambda e, sg_=sg_, gp=gp: e.activation(out=sg_[:], in_=gp[:], func=AF.Silu), reads=[kgp], writes=[("sgt", fc % 2)])
                        kb.op("dve", lambda e, sg_=sg_, up=up, fc=fc: e.tensor_tensor(out=actT[:, fc, :], in0=up[:], in1=sg_[:], op=ALU.mult),
                              reads=[kup, ("sgt", fc % 2)], writes=["actT"])

                def p3_down(G):
                    nonlocal sq
                    for s in range(4):
                        t = G * 4 + s
                        op_ = ops_[s % 2]
                        ko = ("p3o", s % 2)
                        tm_ = tmp[s % 2]
                        kt = ("tmp3", s % 2)
                        for hf in range(2):
                            for fc in range(NFC):
                                mm(op_[:, hf, :], actT[:, fc, s * 128:(s + 1) * 128], wdn[:, fc, hf * 512:(hf + 1) * 512], fc == 0, fc == NFC - 1,
                                   reads=["actT", "wdn"], writes=[ko])
                        c = sq % 16
                        sq += 1
                        kb.op("act", lambda e, c=c, op_=op_: e.activation(out=junk[:], in_=op_[:].rearrange("p a b -> p (a b)"), func=AF.Square, accum_out=ssq[:, c:c + 1]),
                              reads=[ko], writes=["junk3", "p3ssq"])
                        rstd_from_ssq(ssq[:, c:c + 1], rst[:, c:c + 1], D, "p3")
                        kb.op("dve", lambda e, c=c, op_=op_, tm_=tm_: e.scalar_tensor_tensor(out=tm_[:], in0=op_[:].rearrange("p a b -> p (a b)"), scalar=rst[:, c:c + 1], in1=g3[:, 2, :], op0=ALU.mult, op1=ALU.mult),
                              reads=[ko, "p3rs", "g3"], writes=[kt])
                        xos = xo[t % 2]
                        kb.op("pool", lambda e, xos=xos, s=s, tm_=tm_: e.tensor_tensor(out=xos[:], in0=x1[(G % 2) * 4 + s][:], in1=tm_[:], op=ALU.add),
                              reads=[("x1", (G % 2) * 4 + s), kt], writes=[("xo", t % 2)])
                        dma("sp", xout_d[t * 128:(t + 1) * 128, :], xos[:], reads=[("xo", t % 2)], stream="o", n=4)

                for G in range(NG):
                    if G + 1 < NG:
                        loadg(G + 1)
                    while len(wq) < PRE:
                        wq.append(loadw(len(wq)))
                    p3_front(G)
                    if G > 0:
                        p3_down(G - 1)
                    p3_trans(G)
                    p3_gateup(G)
                p3_down(NG - 1)
                end_phase()

        kb.barrier()
        import os as _os2
        if not _os2.environ.get("SKIP_P0"):
            phase0()
        done = stop_after == "p0"
        for l in range(L):
            if done:
                break
            xin = x_d if l == 0 else xs1_d
            xout = xs1_d if l == 0 else out_d
            for nm, fn in (("p1", lambda: phase1(l, xin)), ("p2b", lambda: phase2ab(l)),
                           ("p2c", lambda: phase2c(l)), ("p3", lambda: phase3(l, xin, xout))):
                fn()
                if stop_after == (l, nm):
                    done = True
                    break
            if done:
                break
        kb.barrier()
        kb.emit()

    return nc


def _host_inputs(inputs):
    f = lambda a: np.ascontiguousarray(np.asarray(a, dtype=np.float32))
    c = _consts()
    idx_diag, idx_off1 = _bias_idx()
    rel = f(inputs["rel_bias"])
    shared = {
        "norms": f(np.stack([inputs["pre_mix_norm"], inputs["post_mix_norm"], inputs["pre_ffn_norm"], inputs["post_ffn_norm"]], axis=1)),
        "w_in": f(inputs["w_in"]), "w_out": f(inputs["w_out"]),
        "w_ffn_gate": f(inputs["w_ffn_gate"]), "w_ffn_up": f(inputs["w_ffn_up"]), "w_ffn_down": f(inputs["w_ffn_down"]),
        "lru_wa": f(inputs["lru_wa"]), "lru_wx": f(inputs["lru_wx"]),
        "gla_gate_w2": f(inputs["gla_gate_w2"]),
        "gla_gate_b": f(np.asarray(inputs["gla_gate_b"]).reshape(L, 128, 1)),
        "gla_norm": f(inputs["gla_norm"]),
        "rb31": f(rel[31:32, :]),
        "tdg": f(np.transpose(rel[idx_diag], (0, 2, 1))),
        "tof": f(np.transpose(rel[idx_off1], (0, 2, 1))),
        "ident": c["ident"], "tri": c["tri"], "caus": c["caus"], "e16": c["e16"],
        "cm": c["cm"], "pm": c["pm"], "bmask": c["bmask"], "hm": c["hm"],
    }
    cw = np.transpose(np.asarray(inputs["lru_conv_w"], dtype=np.float32), (0, 2, 1))
    cols = np.concatenate([cw] + [np.asarray(inputs[k], dtype=np.float32)[:, :, None]
                                  for k in ("lru_conv_b", "lru_ba", "lru_bx", "lru_lambda")], axis=2)
    shared["lru_cols"] = f(cols.reshape(L, 2, 128, 8))
    x = np.asarray(inputs["x"], dtype=np.float32)
    return [dict(shared, x=np.ascontiguousarray(x[b])) for b in range(x.shape[0])]


_NC_CACHE = {}


def kernel(**inputs):
    in_maps = _host_inputs(inputs)
    if "nc" not in _NC_CACHE:
        _NC_CACHE["nc"] = build()
    nc = _NC_CACHE["nc"]
    n = len(in_maps)
    res = run_bass_kernel_spmd(nc, in_maps, core_ids=list(range(n)))
    return np.stack([np.asarray(r["out"], dtype=np.float32) for r in res.results], axis=0)
```

```python
from contextlib import ExitStack
import math
import numpy as np
import ml_dtypes
import concourse.bass as bass
import concourse.mybir as mybir
from concourse.bass_utils import run_bass_kernel_spmd

F32 = mybir.dt.float32
BF16 = mybir.dt.bfloat16
ALU = mybir.AluOpType
AF = mybir.ActivationFunctionType
AX = mybir.AxisListType

S = 4096
D = 1024
L = 2
DIN = 2832
DFF = 2816
NFC = DFF // 128
EPS = 1e-6
NEG = -30000.0
ENGS = ("pe", "act", "dve", "pool", "sp")


class KB:
    def __init__(self, nc, stack, sync_same=True):
        self.nc = nc
        self.stack = stack
        self.sync_same = sync_same
        self.ops = {e: [] for e in ENGS}
        self.sem = {}
        self.cnt = {}
        self.step = {}
        self.known = {e: {} for e in ENGS}
        self.lw = {}
        self.rd = {}
        for e in ENGS:
            self._dom(e, 1)

    def _dom(self, name, step):
        if name not in self.sem:
            self.sem[name] = self.stack.enter_context(self.nc.semaphore("s_" + name))
            self.cnt[name] = 0
            self.step[name] = step
        return name

    def op(self, eng, fn, reads=(), writes=(), dma=None):
        dom = eng if dma is None else self._dom("d_" + dma, 16)
        deps = {}

        def add(d):
            if d is not None and deps.get(d[0], 0) < d[1]:
                deps[d[0]] = d[1]

        for k in reads:
            add(self.lw.get(k))
        for k in writes:
            add(self.lw.get(k))
            for dm, c in self.rd.get(k, {}).items():
                add((dm, c))
        if dma is not None and self.cnt[dom] > 0:
            add((dom, self.cnt[dom]))
        kn = self.known[eng]
        for d, c in deps.items():
            if d == eng and (eng == "pe" or not self.sync_same):
                continue
            if kn.get(d, 0) >= c:
                continue
            self.ops[eng].append(("w", self.sem[d], c))
            kn[d] = c
        self.cnt[dom] += self.step[dom]
        me = (dom, self.cnt[dom])
        self.ops[eng].append(("o", fn, self.sem[dom], self.step[dom]))
        for k in writes:
            self.lw[k] = me
            self.rd[k] = {}
        for k in reads:
            r = self.rd.setdefault(k, {})
            if r.get(dom, 0) < me[1]:
                r[dom] = me[1]
        return me

    def barrier(self):
        for eng in ENGS:
            kn = self.known[eng]
            for dom, c in self.cnt.items():
                if c > 0 and dom != eng and kn.get(dom, 0) < c:
                    self.ops[eng].append(("w", self.sem[dom], c))
                    kn[dom] = c
        self.lw = {}
        self.rd = {}

    def emit(self):
        nc = self.nc
        ops = self.ops

        def run(lst, e):
            for it in lst:
                if it[0] == "w":
                    e.wait_ge(it[1], it[2])
                else:
                    it[1](e).then_inc(it[2], it[3])

        with nc.Block() as block:
            @block.tensor
            def _(e):
                run(ops["pe"], e)

            @block.scalar
            def _(e):
                run(ops["act"], e)

            @block.vector
            def _(e):
                run(ops["dve"], e)

            @block.gpsimd
            def _(e):
                run(ops["pool"], e)

            @block.sync
            def _(e):
                run(ops["sp"], e)
        self.ops = {e: [] for e in ENGS}


def _t5_bucket(n):
    n = np.maximum(n, 0)
    nf = np.maximum(n, 1).astype(np.float32)
    large = 16 + (np.log(nf / np.float32(16)) / np.float32(math.log(128 / 16)) * np.float32(16)).astype(np.int32)
    large = np.minimum(large, 31)
    return np.where(n < 16, n, large)


def _consts():
    c = {}
    c["ident"] = np.eye(128, dtype=np.float32).astype(ml_dtypes.bfloat16)
    e = np.arange(128)
    c["tri"] = (e[:, None] <= e[None, :]).astype(np.float32)
    c["caus"] = np.where(e[None, :] >= e[:, None], 0.0, NEG).astype(np.float32)
    keys = np.arange(S)
    c["e16"] = (keys[None, :] // 256 == np.arange(16)[:, None]).astype(np.float32).astype(ml_dtypes.bfloat16)
    npast = np.arange(16)[:, None]
    nn = np.arange(16)[None, :]
    c["cm"] = np.where(nn < npast, 0.0, -1e30).astype(np.float32).reshape(1, 256)
    c["pm"] = (nn < npast).astype(np.float32).reshape(1, 256)
    p = np.arange(128)[:, None]
    c["bmask"] = (p // 32 == (np.arange(256)[None, :] // 64)).astype(np.float32)
    c["hm"] = (p // 32 == np.arange(4)[None, :]).astype(np.float32)
    return c


def _bias_idx():
    k = np.arange(128)[:, None]
    q = np.arange(128)[None, :]
    idx_diag = _t5_bucket(q - k)
    idx_off1 = _t5_bucket(q + 128 - k)
    return idx_diag, idx_off1


def build(debug=False, stop_after=None):
    nc = bass.Bass("TRN2", target_bir_lowering=False)
    dr = lambda name, shape, dt, kind="Internal": nc.dram_tensor(name, list(shape), dt, kind=kind).ap()
    IN = "ExternalInput"
    x_d = dr("x", [S, D], F32, IN)
    norms_d = dr("norms", [L, 4, D], F32, IN)
    w_in_d = dr("w_in", [L, D, DIN], F32, IN)
    w_out_d = dr("w_out", [L, D, D], F32, IN)
    wg_d = dr("w_ffn_gate", [L, D, DFF], F32, IN)
    wu_d = dr("w_ffn_up", [L, D, DFF], F32, IN)
    wd_d = dr("w_ffn_down", [L, DFF, D], F32, IN)
    lcols_d = dr("lru_cols", [L, 2, 128, 8], F32, IN)
    lwa_d = dr("lru_wa", [L, 4, 64, 64], F32, IN)
    lwx_d = dr("lru_wx", [L, 4, 64, 64], F32, IN)
    gw2_d = dr("gla_gate_w2", [L, 16, 128], F32, IN)
    gb_d = dr("gla_gate_b", [L, 128, 1], F32, IN)
    gn_d = dr("gla_norm", [L, 256], F32, IN)
    rb31_d = dr("rb31", [1, 8], F32, IN)
    tdg_d = dr("tdg", [128, 8, 128], F32, IN)
    tof_d = dr("tof", [128, 8, 128], F32, IN)
    ident_d = dr("ident", [128, 128], BF16, IN)
    tri_d = dr("tri", [128, 128], F32, IN)
    caus_d = dr("caus", [128, 128], F32, IN)
    e16_d = dr("e16", [16, S], BF16, IN)
    cm_d = dr("cm", [1, 256], F32, IN)
    pm_d = dr("pm", [1, 256], F32, IN)
    bmask_d = dr("bmask", [128, 256], F32, IN)
    hm_d = dr("hm", [128, 4], F32, IN)
    out_d = dr("out", [S, D], F32, "ExternalOutput")

    dk = "ExternalOutput" if debug else "Internal"
    winb_d = dr("winb", [L, D, DIN], BF16)
    woutb_d = dr("woutb", [L, D, D], BF16)
    wgub_d = dr("wgub", [L, NFC, 128, 2, 8, 128], BF16)
    wdb_d = dr("wdb", [L, DFF, D], BF16)
    xs1_d = dr("xs1", [S, D], F32, dk)
    lruT_d = dr("lruT", [512, S], F32, dk)
    gqT_d = dr("gqT", [128, S], F32, dk)
    gkT_d = dr("gkT", [128, S], F32, dk)
    glrT_d = dr("glrT", [16, S], BF16, dk)
    mqT_d = dr("mqT", [512, S], BF16, dk)
    mkT_d = dr("mkT", [512, S], BF16, dk)
    gv_d = dr("gv", [S, 256], BF16, dk)
    gout_d = dr("gout", [S, 256], F32, dk)
    mvp_d = dr("mvp", [S, 520], BF16, dk)
    mixT_d = dr("mixT", [D, S], BF16, dk)

    with ExitStack() as st:
        kb = KB(nc, st)
        uid = [0]

        def sbt(ctx, shape, dt, name=None):
            uid[0] += 1
            return ctx.enter_context(nc.sbuf_tensor("%s_%d" % (name or "t", uid[0]), list(shape), dt))

        def pst(ctx, shape, dt, name=None):
            uid[0] += 1
            return ctx.enter_context(nc.psum_tensor("%s_%d" % (name or "p", uid[0]), list(shape), dt))

        rr = {}

        def dmaname(stream, n):
            i = rr.get(stream, 0)
            rr[stream] = i + 1
            return "%s%d" % (stream, i % n)

        def dma(eng, out, in_, reads=(), writes=(), stream="g", n=4):
            kb.op(eng, lambda e: e.dma_start(out=out, in_=in_), reads=reads, writes=writes, dma=dmaname(stream, n))

        def mm(out, lhsT, rhs, start, stop, reads, writes, **kw):
            kb.op("pe", lambda e: e.matmul(out, lhsT=lhsT, rhs=rhs, start=start, stop=stop, **kw),
                  reads=reads, writes=writes)

        def tp(out, in_, ident, reads, writes):
            kb.op("pe", lambda e: e.transpose(out, in_, ident), reads=reads, writes=writes)

        ident = sbt(st, [128, 128], BF16, "ident")
        dma("sp", ident[:], ident_d, writes=["ident"])

        def end_phase():
            kb.barrier()
            kb.emit()

        def phase0():
            l = 0
            for c0 in range(0, DIN, 944):
                for kc in range(8):
                    r0 = kc * 128
                    dma("pool", winb_d[l, r0:r0 + 128, c0:c0 + 944], w_in_d[l, r0:r0 + 128, c0:c0 + 944], writes=[("winb", l, kc, c0)], stream="cast", n=4)

        def casts_rest():
            for l in range(L):
                for kc in range(8):
                    r0 = kc * 128
                    dma("pool", woutb_d[l, r0:r0 + 128, :], w_out_d[l, r0:r0 + 128, :], stream="cast", n=4)
                for fc in range(NFC):
                    for gu, wsrc in enumerate((wg_d, wu_d)):
                        dma("pool", wgub_d[l, fc, :, gu, :, :],
                            wsrc[l].rearrange("(kc p) f -> p kc f", p=128)[:, :, fc * 128:(fc + 1) * 128],
                            stream="cast", n=4)
                    dma("pool", wdb_d[l, fc * 128:(fc + 1) * 128, :], wd_d[l, fc * 128:(fc + 1) * 128, :], stream="cast", n=4)
            l = 1
            for c0 in range(0, DIN, 944):
                for kc in range(8):
                    r0 = kc * 128
                    dma("pool", winb_d[l, r0:r0 + 128, c0:c0 + 944], w_in_d[l, r0:r0 + 128, c0:c0 + 944], stream="cast", n=4)

        def rstd_from_ssq(ssq, rstd, n, tag):
            kb.op("dve", lambda e: e.tensor_scalar(out=rstd, in0=ssq, scalar1=1.0 / n, scalar2=EPS, op0=ALU.mult, op1=ALU.add),
                  reads=[tag + "ssq"], writes=[tag + "rs"])
            kb.op("act", lambda e: e.sqrt(out=rstd, in_=rstd), reads=[tag + "rs"], writes=[tag + "rs"])
            kb.op("dve", lambda e: e.reciprocal(out=rstd, in_=rstd), reads=[tag + "rs"], writes=[tag + "rs"])

        def phase1(l, xin_d):
            with ExitStack() as ph:
                win = sbt(ph, [128, 8, DIN], BF16, "win")
                gpre = sbt(ph, [128, D], F32, "gpre")
                xt = [sbt(ph, [128, D], F32, "xt") for _ in range(8)]
                hb = [sbt(ph, [128, D], BF16, "hb") for _ in range(4)]
                junk = sbt(ph, [128, D], BF16, "junk")
                hT = [sbt(ph, [128, 8, 512], BF16, "hT") for _ in range(2)]
                ssq = sbt(ph, [128, 8], F32, "ssq")
                rst = sbt(ph, [128, 8], F32, "rst")
                sf = [sbt(ph, [128, 512], F32, "sf") for _ in range(6)]
                sbf = [sbt(ph, [128, 512], BF16, "sbf") for _ in range(6)]
                sgv = [sbt(ph, [128, 256], BF16, "sgv") for _ in range(4)]
                sgo = [sbt(ph, [128, 256], F32, "sgo") for _ in range(4)]
                smv = [sbt(ph, [128, 8, 65], BF16, "smv") for _ in range(4)]
                tps = [pst(ph, [128, D], BF16, "tps") for _ in range(2)]
                aps = [pst(ph, [128, 512], F32, "aps") for _ in range(5)]
                for c0_ in range(0, DIN, 944):
                    for kc in range(8):
                        dma("sp", win[:, kc, c0_:c0_ + 944], winb_d[l, kc * 128:(kc + 1) * 128, c0_:c0_ + 944], reads=[("winb", l, kc, c0_)], writes=[("win", kc, c0_)], stream="w", n=8)

                def wink(c_lo, c_hi):
                    return [("win", kc_, cb_) for kc_ in range(8) for cb_ in range(0, DIN, 944) if cb_ < c_hi and cb_ + 944 > c_lo]

                dma("sp", gpre[:], norms_d[l, 0:1, :].partition_broadcast(128), writes=["gpre"])
                for i in range(4):
                    kb.op("dve", lambda e, i=i: e.memset(smv[i][:], 1.0), writes=[("smv", i)])

                flist = [("lru", lruT_d, 0, 0, 128, F32), ("lru", lruT_d, 128, 128, 128, F32),
                         ("lru", lruT_d, 256, 256, 128, F32), ("lru", lruT_d, 384, 384, 128, F32),
                         ("gq", gqT_d, 0, 512, 128, F32), ("gk", gkT_d, 0, 640, 128, F32),
                         ("glr", glrT_d, 0, 1024, 16, BF16)]
                for i in range(4):
                    flist.append(("mq", mqT_d, i * 128, 1296 + i * 128, 128, BF16))
                for i in range(4):
                    flist.append(("mk", mkT_d, i * 128, 1808 + i * 128, 128, BF16))

                def load(t):
                    dma("sp", xt[t % 8][:], xin_d[t * 128:(t + 1) * 128, :], writes=[("xt", t % 8)], stream="x", n=4)

                NT = S // 128
                pi = 0
                ev = 0
                import os as _os
                _ng = int(_os.environ.get("P1_GROUPS", S // 512))
                _parts = int(_os.environ.get("P1_PARTS", 7))

                def chain(g):
                    for s in range(4):
                        t = g * 4 + s
                        xs = xt[t % 8]
                        hbs = hb[s]
                        c = t % 8
                        kb.op("act", lambda e, xs=xs, c=c: e.activation(out=junk[:], in_=xs[:], func=AF.Square, accum_out=ssq[:, c:c + 1]),
                              reads=[("xt", t % 8)], writes=["junk", "p1ssq"])
                        rstd_from_ssq(ssq[:, c:c + 1], rst[:, c:c + 1], D, "p1")
                        kb.op("dve", lambda e, xs=xs, hbs=hbs, c=c: e.scalar_tensor_tensor(out=hbs[:], in0=xs[:], scalar=rst[:, c:c + 1], in1=gpre[:], op0=ALU.mult, op1=ALU.mult),
                              reads=[("xt", t % 8), "p1rs", "gpre"], writes=[("hb", s)])

                def transp(g):
                    hTg_ = hT[g % 2]
                    for s in range(4):
                        t = g * 4 + s
                        hbs = hb[s]
                        tpp = tps[t % 2]
                        for kc in range(8):
                            tp(tpp[:, kc * 128:(kc + 1) * 128], hbs[:, kc * 128:(kc + 1) * 128], ident[:],
                               reads=[("hb", s), "ident"], writes=[("tps", t % 2)])
                        kb.op("act", lambda e, tpp=tpp, hTg_=hTg_, s=s: e.copy(out=hTg_[:, :, s * 128:(s + 1) * 128], in_=tpp[:].rearrange("p (k c) -> p k c", k=8)),
                              reads=[("tps", t % 2)], writes=[("hT", g % 2)])

                for t in range(8):
                    load(t)
                chain(0)
                transp(0)
                for g in range(_ng):
                    hTg = hT[g % 2]
                    if g + 1 < _ng:
                        chain(g + 1)
                    if g + 2 < _ng:
                        for s in range(4):
                            load((g + 2) * 4 + s)
                    for (nm, dst, drow, wcol, wid, dt) in (flist if _parts & 2 else []):
                        ps = aps[pi % 5]
                        pk = ("aps", pi % 5)
                        pi += 1
                        for kc in range(8):
                            mm(ps[0:wid, :], win[:, kc, wcol:wcol + wid], hTg[:, kc, :], kc == 0, kc == 7,
                               reads=wink(wcol, wcol + wid) + [("hT", g % 2)], writes=[pk])
                        if dt == F32:
                            stg = sf[ev % 6]
                            sk = ("sf", ev % 6)
                        else:
                            stg = sbf[ev % 6]
                            sk = ("sbf", ev % 6)
                        eng = "act" if ev % 2 == 0 else "dve"
                        ev += 1
                        if nm == "mq":
                            if eng == "act":
                                kb.op("act", lambda e, stg=stg, ps=ps, wid=wid: e.mul(out=stg[0:wid, :], in_=ps[0:wid, :], mul=0.125), reads=[pk], writes=[sk])
                            else:
                                kb.op("dve", lambda e, stg=stg, ps=ps, wid=wid: e.tensor_scalar(out=stg[0:wid, :], in0=ps[0:wid, :], scalar1=0.125, scalar2=None, op0=ALU.mult), reads=[pk], writes=[sk])
                        else:
                            if eng == "act":
                                kb.op("act", lambda e, stg=stg, ps=ps, wid=wid: e.copy(out=stg[0:wid, :], in_=ps[0:wid, :]), reads=[pk], writes=[sk])
                            else:
                                kb.op("dve", lambda e, stg=stg, ps=ps, wid=wid: e.tensor_copy(out=stg[0:wid, :], in_=ps[0:wid, :]), reads=[pk], writes=[sk])
                        dma("sp", dst[drow:drow + wid, g * 512:(g + 1) * 512], stg[0:wid, :], reads=[sk], stream="o1", n=12)
                    if g + 1 < _ng:
                        transp(g + 1)
                    for s in (range(4) if _parts & 4 else []):
                        t = g * 4 + s
                        _tm = int(_os.environ.get("TM_SKIP", 0))
                        if not _tm & 1:
                            ps = aps[pi % 5]
                            pk = ("aps", pi % 5)
                            pi += 1
                            ps2 = aps[pi % 5]
                            pk2 = ("aps", pi % 5)
                            pi += 1
                            for kc in range(8):
                                mm(ps[:, 0:256], hTg[:, kc, s * 128:(s + 1) * 128], win[:, kc, 768:1024], kc == 0, kc == 7,
                                   reads=wink(768, 1024) + [("hT", g % 2)], writes=[pk])
                            for kc in range(8):
                                mm(ps2[:, 0:256], hTg[:, kc, s * 128:(s + 1) * 128], win[:, kc, 1040:1296], kc == 0, kc == 7,
                                   reads=wink(1040, 1296) + [("hT", g % 2)], writes=[pk2])
                            a, b = sgv[t % 4], sgo[t % 4]
                            kb.op("act", lambda e, a=a, ps=ps: e.copy(out=a[:], in_=ps[:, 0:256]), reads=[pk], writes=[("sgv", t % 4)])
                            kb.op("dve", lambda e, b=b, ps2=ps2: e.tensor_copy(out=b[:], in_=ps2[:, 0:256]), reads=[pk2], writes=[("sgo", t % 4)])
                            dma("sp", gv_d[t * 128:(t + 1) * 128, :], a[:], reads=[("sgv", t % 4)], stream="o1", n=12)
                            dma("sp", gout_d[t * 128:(t + 1) * 128, :], b[:], reads=[("sgo", t % 4)], stream="o1", n=12)
                        if not _tm & 2:
                            ps = aps[pi % 5]
                            pk = ("aps", pi % 5)
                            pi += 1
                            for kc in range(8):
                                mm(ps[:, :], hTg[:, kc, s * 128:(s + 1) * 128], win[:, kc, 2320:2832], kc == 0, kc == 7,
                                   reads=wink(2320, 2832) + [("hT", g % 2)], writes=[pk])
                            m = smv[t % 4]
                            psv = ps[:].rearrange("p (h d) -> p h d", h=8)
                            if _tm & 4:
                                pass
                            elif t % 2:
                                kb.op("act", lambda e, m=m, psv=psv: e.copy(out=m[:, :, 0:64], in_=psv), reads=[pk], writes=[("smv", t % 4)])
                            else:
                                kb.op("dve", lambda e, m=m, psv=psv: e.tensor_copy(out=m[:, :, 0:64], in_=psv), reads=[pk], writes=[("smv", t % 4)])
                            if not _tm & 8:
                                dma("sp", mvp_d[t * 128:(t + 1) * 128, :], m[:].rearrange("p h d -> p (h d)"), reads=[("smv", t % 4)], stream="o1", n=12)
                if _os.environ.get("P1_TAILSTORE"):
                    dma("sp", lruT_d[0:128, 0:8], rst[:], reads=["p1rs"], stream="o1", n=12)
                end_phase()

        def phase2a_gen(l, ph):
            TB = 1024
            if True:
                cols = sbt(ph, [128, 2, 8], F32, "lcols")
                ccol = sbt(ph, [128, 2], F32, "ccol")
                wstage = sbt(ph, [128, 2, 2, 128], F32, "wstage")
                wbd = sbt(ph, [128, 2, 2, 128], BF16, "wbd")
                xin = [sbt(ph, [128, TB + 3], F32, "xin") for _ in range(2)]
                gin = [sbt(ph, [128, TB], F32, "gin") for _ in range(2)]
                xc = sbt(ph, [128, TB], F32, "xc")
                xcb = sbt(ph, [128, TB], BF16, "xcb")
                rr_ = sbt(ph, [128, TB], F32, "r")
                ii_ = sbt(ph, [128, TB], F32, "i")
                aa = sbt(ph, [128, TB], F32, "a")
                mmul = sbt(ph, [128, TB], F32, "mult")
                uu = sbt(ph, [128, TB], F32, "u")
                hh = [sbt(ph, [128, TB], F32, "h") for _ in range(2)]
                gt = sbt(ph, [128, TB], F32, "gt")
                gs = sbt(ph, [128, TB], F32, "gs")
                yb = [sbt(ph, [128, TB], BF16, "yb") for _ in range(2)]
                gps = [pst(ph, [128, 512], F32, "gps") for _ in range(2)]
                for h in range(2):
                    dma("sp", cols[:, h, :], lcols_d[l, h], writes=["lcols"])
                kb.op("pool", lambda e: e.memset(wstage[:], 0.0), writes=["wstage"])
                for ax, src in enumerate((lwa_d, lwx_d)):
                    for h in range(2):
                        for b in range(2):
                            dma("sp", wstage[b * 64:(b + 1) * 64, ax, h, b * 64:(b + 1) * 64], src[l, 2 * h + b],
                                reads=[], writes=["wstage"])
                kb.op("dve", lambda e: e.tensor_copy(out=wbd[:], in_=wstage[:]), reads=["wstage"], writes=["wbd"])
                kb.op("act", lambda e: e.activation(out=ccol[:], in_=cols[:, :, 7], func=AF.Exp, scale=-1.0), reads=["lcols"], writes=["ccol"])
                kb.op("act", lambda e: e.activation(out=ccol[:], in_=ccol[:], func=AF.Ln, bias=1.0), reads=["ccol"], writes=["ccol"])
                kb.op("dve", lambda e: e.tensor_scalar(out=ccol[:], in0=ccol[:], scalar1=-8.0, scalar2=None, op0=ALU.mult), reads=["ccol"], writes=["ccol"])

                nb = S // TB
                it = 0
                for h in range(2):
                    for b in range(nb):
                        t0 = b * TB
                        xi = xin[it % 2]
                        gi = gin[it % 2]
                        hcur = hh[it % 2]
                        hprev = hh[(it + 1) % 2]
                        ybs = yb[it % 2]
                        kx, kg, ky = ("xin", it % 2), ("gin", it % 2), ("yb", it % 2)
                        kh, khp = ("h", it % 2), ("h", (it + 1) % 2)
                        it += 1
                        if b == 0:
                            kb.op("pool", lambda e, xi=xi: e.memset(xi[:, 0:3], 0.0), writes=[kx])
                            dma("sp", xi[:, 3:], lruT_d[h * 128:(h + 1) * 128, 0:TB], writes=[kx], stream="x", n=3)
                        else:
                            dma("sp", xi[:], lruT_d[h * 128:(h + 1) * 128, t0 - 3:t0 + TB], writes=[kx], stream="x", n=3)
                        dma("sp", gi[:], lruT_d[256 + h * 128:256 + (h + 1) * 128, t0:t0 + TB], writes=[kg], stream="x", n=3)
                        yield
                        kb.op("dve", lambda e, xi=xi, h=h: e.tensor_scalar(out=xc[:], in0=xi[:, 3:TB + 3], scalar1=cols[:, h, 3:4], scalar2=cols[:, h, 4:5], op0=ALU.mult, op1=ALU.add),
                              reads=[kx, "lcols"], writes=["xc"])
                        for j in range(3):
                            kb.op("dve", lambda e, xi=xi, h=h, j=j: e.scalar_tensor_tensor(out=xc[:], in0=xi[:, j:TB + j], scalar=cols[:, h, j:j + 1], in1=xc[:], op0=ALU.mult, op1=ALU.add),
                                  reads=[kx, "lcols", "xc"], writes=["xc"])
                        yield
                        kb.op("pool", lambda e: e.tensor_copy(out=xcb[:], in_=xc[:]), reads=["xc"], writes=["xcb"])
                        for sblk in range(TB // 512):
                            cs = slice(sblk * 512, (sblk + 1) * 512)
                            pa, px = gps[0], gps[1]
                            ka, kx_ = ("gps", 0), ("gps", 1)
                            mm(pa[:], wbd[:, 0, h, :], xcb[:, cs], True, True, reads=["wbd", "xcb"], writes=[ka])
                            mm(px[:], wbd[:, 1, h, :], xcb[:, cs], True, True, reads=["wbd", "xcb"], writes=[kx_])
                            kb.op("act", lambda e, pa=pa, cs=cs, h=h: e.activation(out=rr_[:, cs], in_=pa[:], func=AF.Sigmoid, bias=cols[:, h, 5:6]),
                                  reads=[ka, "lcols"], writes=["r"])
                            kb.op("act", lambda e, px=px, cs=cs, h=h: e.activation(out=ii_[:, cs], in_=px[:], func=AF.Sigmoid, bias=cols[:, h, 6:7]),
                                  reads=[kx_, "lcols"], writes=["i"])
                        yield
                        kb.op("pool", lambda e, gi=gi: e.tensor_tensor(out=gt[:], in0=gi[:], in1=gi[:], op=ALU.mult), reads=[kg], writes=["gt"])
                        kb.op("pool", lambda e: e.tensor_scalar(out=gt[:], in0=gt[:], scalar1=0.044715, scalar2=1.0, op0=ALU.mult, op1=ALU.add), reads=["gt"], writes=["gt"])
                        kb.op("pool", lambda e, gi=gi: e.tensor_tensor(out=gt[:], in0=gt[:], in1=gi[:], op=ALU.mult), reads=["gt", kg], writes=["gt"])
                        kb.op("act", lambda e: e.activation(out=gs[:], in_=gt[:], func=AF.Sigmoid, scale=1.5957691216057308), reads=["gt"], writes=["gs"])
                        kb.op("pool", lambda e, gi=gi: e.tensor_tensor(out=gs[:], in0=gs[:], in1=gi[:], op=ALU.mult), reads=["gs", kg], writes=["gs"])
                        yield
                        kb.op("act", lambda e, h=h: e.activation(out=aa[:], in_=rr_[:], func=AF.Exp, scale=ccol[:, h:h + 1]), reads=["r", "ccol"], writes=["a"])
                        kb.op("pool", lambda e: e.tensor_tensor(out=mmul[:], in0=aa[:], in1=aa[:], op=ALU.mult), reads=["a"], writes=["mult"])
                        kb.op("act", lambda e: e.activation(out=mmul[:], in_=mmul[:], func=AF.Sqrt, scale=-1.0, bias=1.0), reads=["mult"], writes=["mult"])
                        yield
                        if b == 0:
                            kb.op("dve", lambda e: e.memset(mmul[:, 0:1], 1.0), reads=["mult"], writes=["mult"])
                        kb.op("dve", lambda e: e.tensor_tensor(out=uu[:], in0=ii_[:], in1=xc[:], op=ALU.mult), reads=["i", "xc"], writes=["u"])
                        kb.op("dve", lambda e: e.tensor_tensor(out=uu[:], in0=uu[:], in1=mmul[:], op=ALU.mult), reads=["u", "mult"], writes=["u"])
                        yield
                        if b == 0:
                            kb.op("dve", lambda e, hcur=hcur: e.tensor_tensor_scan(out=hcur[:], data0=aa[:], data1=uu[:], initial=0.0, op0=ALU.mult, op1=ALU.add),
                                  reads=["a", "u"], writes=[kh])
                        else:
                            kb.op("dve", lambda e, hcur=hcur, hprev=hprev: e.tensor_tensor_scan(out=hcur[:], data0=aa[:], data1=uu[:], initial=hprev[:, TB - 1:TB], op0=ALU.mult, op1=ALU.add),
                                  reads=["a", "u", khp], writes=[kh])
                        yield
                        kb.op("dve", lambda e, hcur=hcur, ybs=ybs: e.tensor_tensor(out=ybs[:], in0=hcur[:], in1=gs[:], op=ALU.mult), reads=[kh, "gs"], writes=[ky])
                        dma("sp", mixT_d[h * 128:(h + 1) * 128, t0:t0 + TB], ybs[:], reads=[ky], stream="o", n=4)
                        yield

        def phase2b_gen(l, ph):
            TB = 1024
            NCH = TB // 128
            if True:
                w2s = sbt(ph, [16, 128], F32, "w2s")
                w2b = sbt(ph, [16, 128], BF16, "w2b")
                negb = sbt(ph, [128, 1], F32, "negb")
                gn = sbt(ph, [128, 256], F32, "gn")
                tri = sbt(ph, [128, 128], F32, "tri")
                bmask = sbt(ph, [128, 256], F32, "bmask")
                hm = sbt(ph, [128, 4], F32, "hm")
                ones = sbt(ph, [128, 128], F32, "ones")
                glr = [sbt(ph, [16, TB], BF16, "glr") for _ in range(2)]
                qT = [sbt(ph, [128, TB], F32, "qT") for _ in range(2)]
                kT = [sbt(ph, [128, TB], F32, "kT") for _ in range(2)]
                vv = [sbt(ph, [128, NCH, 256], BF16, "vv") for _ in range(2)]
                go = [sbt(ph, [128, NCH, 256], F32, "go") for _ in range(2)]
                ee = sbt(ph, [128, TB], F32, "ee")
                cum = sbt(ph, [128, TB], F32, "cum")
                ex = sbt(ph, [128, TB], F32, "ex")
                dd = sbt(ph, [128, TB], F32, "dd")
                qd = sbt(ph, [128, TB], BF16, "qd")
                kdm = sbt(ph, [128, 4, TB], BF16, "kdm")
                kdec = sbt(ph, [128, TB], BF16, "kdec")
                dcol = sbt(ph, [128, NCH], F32, "dcol")
                kdtm = [sbt(ph, [128, 128], BF16, "kdtm") for _ in range(2)]
                am = [sbt(ph, [128, 4, 128], BF16, "am") for _ in range(2)]
                Sst = sbt(ph, [128, 256], F32, "Sst")
                Sbf = sbt(ph, [128, 256], BF16, "Sbf")
                kvm = sbt(ph, [128, 256], F32, "kvm")
                ob = sbt(ph, [128, NCH, 256], F32, "ob")
                osq = sbt(ph, [128, NCH, 256], F32, "osq")
                ssq = sbt(ph, [128, NCH * 4], F32, "gssq")
                rst = sbt(ph, [128, NCH * 4], F32, "grst")
                sg = sbt(ph, [128, NCH, 256], F32, "sg")
                yb = sbt(ph, [128, NCH, 256], BF16, "yb")
                yT = [sbt(ph, [128, 2, TB], BF16, "yT") for _ in range(2)]
                zps = [pst(ph, [128, 512], F32, "zps") for _ in range(1)]
                tps = pst(ph, [128, 1024], BF16, "gtps")
                aps_ = [pst(ph, [128, 512], F32, "gaps") for _ in range(1)]
                ops_ = [pst(ph, [128, 512], F32, "gops") for _ in range(2)]
                kvps = pst(ph, [128, 512], F32, "kvps")

                dma("sp", w2s[:], gw2_d[l], writes=["w2s"])
                kb.op("dve", lambda e: e.tensor_copy(out=w2b[:], in_=w2s[:]), reads=["w2s"], writes=["w2b"])
                dma("sp", negb[:], gb_d[l], writes=["negb"])
                kb.op("dve", lambda e: e.tensor_scalar(out=negb[:], in0=negb[:], scalar1=-1.0, scalar2=None, op0=ALU.mult), reads=["negb"], writes=["negb"])
                dma("sp", gn[:], gn_d[l:l + 1, :].partition_broadcast(128), writes=["gn"])
                dma("sp", tri[:], tri_d, writes=["tri"])
                dma("sp", bmask[:], bmask_d, writes=["bmask"])
                dma("sp", hm[:], hm_d, writes=["hm"])
                kb.op("pool", lambda e: e.memset(ones[:], 1.0), writes=["ones"])
                kb.op("pool", lambda e: e.memset(Sst[:], 0.0), writes=["Sst"])
                kb.op("pool", lambda e: e.memset(Sbf[:], 0.0), writes=["Sbf"])

                def load(b):
                    i = b % 2
                    t0 = b * TB
                    dma("sp", glr[i][:], glrT_d[:, t0:t0 + TB], writes=[("glr", i)], stream="x", n=3)
                    dma("sp", qT[i][:], gqT_d[:, t0:t0 + TB], writes=[("qT", i)], stream="x", n=3)
                    dma("sp", kT[i][:], gkT_d[:, t0:t0 + TB], writes=[("kT", i)], stream="x", n=3)
                    dma("sp", vv[i][:], gv_d[t0:t0 + TB, :].rearrange("(c p) f -> p c f", p=128), writes=[("vv", i)], stream="x", n=3)
                    dma("sp", go[i][:], gout_d[t0:t0 + TB, :].rearrange("(c p) f -> p c f", p=128), writes=[("go", i)], stream="x", n=3)

                nb = S // TB
                load(0)
                for b in range(nb):
                    if b + 1 < nb:
                        load(b + 1)
                    i = b % 2
                    t0 = b * TB
                    q_, k_, v_, g_, r_ = qT[i], kT[i], vv[i], go[i], glr[i]
                    kq, kk, kv, kg, kr = ("qT", i), ("kT", i), ("vv", i), ("go", i), ("glr", i)
                    for sblk in range(TB // 512):
                        cs = slice(sblk * 512, (sblk + 1) * 512)
                        zp = zps[0]
                        mm(zp[:], w2b[:], r_[:, cs], True, True, reads=["w2b", kr], writes=[("zps", 0)])
                        kb.op("act", lambda e, zp=zp, cs=cs: e.activation(out=ee[:, cs], in_=zp[:], func=AF.Exp, scale=-1.0, bias=negb[:]),
                              reads=[("zps", 0), "negb"], writes=["ee"])
                    yield
                    kb.op("act", lambda e: e.activation(out=ee[:], in_=ee[:], func=AF.Ln, bias=1.0), reads=["ee"], writes=["ee"])
                    for c in range(NCH):
                        cs = slice(c * 128, (c + 1) * 128)
                        kb.op("dve", lambda e, cs=cs: e.tensor_tensor_scan(out=cum[:, cs], data0=ones[:], data1=ee[:, cs], initial=0.0, op0=ALU.mult, op1=ALU.add),
                              reads=["ones", "ee"], writes=["cum"])
                    yield
                    kb.op("act", lambda e: e.activation(out=ex[:], in_=cum[:], func=AF.Exp, scale=-1.0 / 16.0), reads=["cum"], writes=["ex"])
                    kb.op("dve", lambda e, q_=q_: e.scalar_tensor_tensor(out=qd[:], in0=q_[:], scalar=32.0 ** -0.5, in1=ex[:], op0=ALU.mult, op1=ALU.mult),
                          reads=[kq, "ex"], writes=["qd"])
                    yield
                    kb.op("act", lambda e: e.activation(out=dcol[:], in_=cum[:].rearrange("p (c t) -> p c t", t=128)[:, :, 127], func=AF.Exp, scale=-1.0 / 16.0),
                          reads=["cum"], writes=["dcol"])
                    for c in range(NCH):
                        cs = slice(c * 128, (c + 1) * 128)
                        kb.op("pool", lambda e, cs=cs, c=c: e.tensor_scalar(out=dd[:, cs], in0=cum[:, cs], scalar1=cum[:, c * 128 + 127:c * 128 + 128], scalar2=None, op0=ALU.subtract),
                              reads=["cum"], writes=["dd"])
                    yield
                    kb.op("act", lambda e: e.activation(out=ex[:], in_=cum[:], func=AF.Exp, scale=1.0 / 16.0), reads=["cum", "qd"], writes=["ex"])
                    for hh_ in range(4):
                        kb.op("dve", lambda e, k_=k_, hh_=hh_: e.scalar_tensor_tensor(out=kdm[:, hh_, :], in0=k_[:], scalar=hm[:, hh_:hh_ + 1], in1=ex[:], op0=ALU.mult, op1=ALU.mult),
                              reads=[kk, "ex", "hm"], writes=["kdm"])
                    yield
                    kb.op("act", lambda e: e.activation(out=dd[:], in_=dd[:], func=AF.Exp, scale=1.0 / 16.0), reads=["dd"], writes=["dd"])
                    kb.op("pool", lambda e, k_=k_: e.tensor_tensor(out=kdec[:], in0=k_[:], in1=dd[:], op=ALU.mult), reads=[kk, "dd"], writes=["kdec"])
                    kb.op("act", lambda e, g_=g_: e.activation(out=sg[:], in_=g_[:], func=AF.Silu), reads=[kg], writes=["sg"])
                    def gla_s1(c):
                        cs = slice(c * 128, (c + 1) * 128)
                        j = c % 2
                        tp(tps[:, j * 128:(j + 1) * 128], kdec[:, cs], ident[:], reads=["kdec", "ident"], writes=["gtps"])
                        kb.op("act", lambda e, j=j: e.copy(out=kdtm[j][:], in_=tps[:, j * 128:(j + 1) * 128]), reads=["gtps"], writes=[("kdtm", j)])
                        ap_ = aps_[0]
                        for hh_ in range(4):
                            mm(ap_[:, hh_ * 128:(hh_ + 1) * 128], kdm[:, hh_, cs], qd[:, cs], True, True, reads=["kdm", "qd"], writes=[("gaps", 0)])
                        kb.op("dve", lambda e, ap_=ap_, j=j: e.tensor_tensor(out=am[j][:], in0=ap_[:].rearrange("p (h c) -> p h c", h=4),
                                                                             in1=tri[:].unsqueeze(1).broadcast_to([128, 4, 128]), op=ALU.mult),
                              reads=[("gaps", 0), "tri"], writes=[("am", j)])

                    def gla_s2(c):
                        cs = slice(c * 128, (c + 1) * 128)
                        j = c % 2
                        op_ = ops_[j]
                        mm(op_[:, 0:256], qd[:, cs], Sbf[:], True, True, reads=["qd", "Sbf"], writes=[("gops", j)])
                        for hh_ in range(4):
                            mm(op_[:, hh_ * 64:(hh_ + 1) * 64], am[j][:, hh_, :], v_[:, c, hh_ * 64:(hh_ + 1) * 64], False, True,
                               reads=[("am", j), kv], writes=[("gops", j)], skip_group_check=True)
                        kb.op("act", lambda e, op_=op_, c=c: e.copy(out=ob[:, c, :], in_=op_[:, 0:256]), reads=[("gops", j)], writes=["ob"])
                        mm(kvps[:, 0:256], kdtm[j][:], v_[:, c, :], True, True, reads=[("kdtm", j), kv], writes=["kvps"])
                        kb.op("dve", lambda e: e.tensor_tensor(out=kvm[:], in0=kvps[:, 0:256], in1=bmask[:], op=ALU.mult), reads=["kvps", "bmask"], writes=["kvm"])
                        kb.op("dve", lambda e, c=c: e.scalar_tensor_tensor(out=Sst[:], in0=Sst[:], scalar=dcol[:, c:c + 1], in1=kvm[:], op0=ALU.mult, op1=ALU.add),
                              reads=["Sst", "dcol", "kvm"], writes=["Sst"])
                        kb.op("pool", lambda e: e.tensor_copy(out=Sbf[:], in_=Sst[:]), reads=["Sst"], writes=["Sbf"])

                    gla_s1(0)
                    yield
                    for c in range(NCH):
                        if c + 1 < NCH:
                            gla_s1(c + 1)
                            yield
                        gla_s2(c)
                        yield
                    yield
                    kb.op("pool", lambda e: e.tensor_tensor(out=osq[:], in0=ob[:], in1=ob[:], op=ALU.mult), reads=["ob"], writes=["osq"])
                    kb.op("dve", lambda e: e.tensor_reduce(out=ssq[:], in_=osq[:].rearrange("p c (h v) -> p (c h) v", h=4), axis=AX.X, op=ALU.add),
                          reads=["osq"], writes=["p2bssq"])
                    rstd_from_ssq(ssq[:], rst[:], 64, "p2b")
                    kb.op("dve", lambda e: e.tensor_tensor(out=ob[:].rearrange("p c (h v) -> p (c h) v", h=4), in0=ob[:].rearrange("p c (h v) -> p (c h) v", h=4),
                                                           in1=rst[:].unsqueeze(2).broadcast_to([128, NCH * 4, 64]), op=ALU.mult),
                          reads=["ob", "p2brs"], writes=["ob"])
                    kb.op("pool", lambda e: e.tensor_tensor(out=sg[:], in0=sg[:], in1=gn[:].unsqueeze(1).broadcast_to([128, NCH, 256]), op=ALU.mult),
                          reads=["sg", "gn"], writes=["sg"])
                    kb.op("dve", lambda e: e.tensor_tensor(out=yb[:], in0=ob[:], in1=sg[:], op=ALU.mult), reads=["ob", "sg"], writes=["yb"])
                    yield
                    yTb = yT[b % 2]
                    for c in range(NCH):
                        for f in range(2):
                            jj = (c * 2 + f) % 4
                            tp(tps[:, jj * 128:(jj + 1) * 128], yb[:, c, f * 128:(f + 1) * 128], ident[:], reads=["yb", "ident"], writes=["gtps"])
                            kb.op("act", lambda e, jj=jj, c=c, f=f, yTb=yTb: e.copy(out=yTb[:, f, c * 128:(c + 1) * 128], in_=tps[:, jj * 128:(jj + 1) * 128]),
                                  reads=["gtps"], writes=[("yT", b % 2)])
                    dma("sp", mixT_d[256:512, t0:t0 + TB].rearrange("(f p) t -> p f t", p=128), yTb[:], reads=[("yT", b % 2)], stream="o", n=4)

        def phase2ab(l):
            with ExitStack() as ph:
                gens = [phase2a_gen(l, ph), phase2b_gen(l, ph)]
                while gens:
                    for g_ in list(gens):
                        try:
                            next(g_)
                        except StopIteration:
                            gens.remove(g_)
                end_phase()

        def phase2c(l):
            with ExitStack() as ph:
                kaug = sbt(ph, [128, 8, S], BF16, "kaug")
                vp = sbt(ph, [128, 32, 520], BF16, "vp")
                qaug = [sbt(ph, [128, 8, 512], BF16, "qaug") for _ in range(3)]
                km = sbt(ph, [64, 8, 16], F32, "km")
                kmb = sbt(ph, [64, 8, 16], BF16, "kmb")
                cm = sbt(ph, [128, 16, 16], F32, "cm")
                pm = sbt(ph, [128, 16, 16], F32, "pm")
                b31 = sbt(ph, [128, 8], F32, "b31")
                caus = sbt(ph, [128, 128], F32, "caus")
                tstage = sbt(ph, [128, 2, 8, 128], F32, "tstage")
                tdT = sbt(ph, [128, 8, 128], BF16, "tdT")
                toT = sbt(ph, [128, 8, 128], BF16, "toT")
                tomT = sbt(ph, [128, 8, 128], BF16, "tomT")
                zer = sbt(ph, [128, 260], BF16, "zer")
                gm = sbt(ph, [128, 4, 8, 16], F32, "gm")
                m8 = sbt(ph, [128, 4, 8, 8], F32, "m8")
                sel = sbt(ph, [128, 4, 8, 16], F32, "sel")
                mpad = [sbt(ph, [128, 4, 8, 80], BF16, "mpad") for _ in range(2)]
                pT = [sbt(ph, [128, 512], BF16, "pT") for _ in range(4)]
                rcp = sbt(ph, [128, 4], F32, "rcp")
                ymo = [sbt(ph, [128, 4, 512], BF16, "ymo") for _ in range(2)]
                ymT = [sbt(ph, [128, 4, 512], BF16, "ymT") for _ in range(2)]
                gps = pst(ph, [128, 512], F32, "mgps")
                mtps = [pst(ph, [128, 512], F32, "mtps") for _ in range(1)]
                mtps_b = [pst(ph, [128, 1024], BF16, "mtpsb") for _ in range(1)]
                sps = [pst(ph, [128, 512], F32, "sps") for _ in range(3)]
                accs_full = [pst(ph, [128, 512], F32, "acc") for _ in range(2)]
                accs = [a_[:, 0:260].rearrange("p (s d) -> p s d", s=4) for a_ in accs_full]

                for h in range(8):
                    dma("sp", kaug[0:64, h, :], mkT_d[h * 64:(h + 1) * 64, :], writes=["kaug"], stream="x", n=3)
                    dma("sp", kaug[64:80, h, :], e16_d, writes=["kaug"], stream="x", n=3)
                for c in range(4):
                    dma("sp", vp[:, c * 8:(c + 1) * 8, :], mvp_d[c * 1024:(c + 1) * 1024, :].rearrange("(c p) f -> p c f", p=128), writes=["vp"], stream="x", n=3)
                dma("sp", cm[:].rearrange("p a b -> p (a b)"), cm_d.partition_broadcast(128), writes=["cm"])
                dma("sp", pm[:].rearrange("p a b -> p (a b)"), pm_d.partition_broadcast(128), writes=["pm"])
                dma("sp", b31[:], rb31_d.partition_broadcast(128), writes=["b31"])
                dma("sp", caus[:], caus_d, writes=["caus"])
                dma("sp", tstage[:, 0], tdg_d, writes=["tstage"])
                dma("sp", tstage[:, 1], tof_d, writes=["tstage"])
                kb.op("dve", lambda e: e.tensor_tensor(out=tdT[:], in0=tstage[:, 0], in1=caus[:].unsqueeze(1).broadcast_to([128, 8, 128]), op=ALU.add),
                      reads=["tstage", "caus"], writes=["tdT"])
                kb.op("dve", lambda e: e.tensor_copy(out=toT[:], in_=tstage[:, 1]), reads=["tstage"], writes=["toT"])
                kb.op("dve", lambda e: e.tensor_tensor(out=tomT[:], in0=tstage[:, 1], in1=b31[:].unsqueeze(2).broadcast_to([128, 8, 128]), op=ALU.subtract),
                      reads=["tstage", "b31"], writes=["tomT"])
                kb.op("pool", lambda e: e.memset(zer[:], 0.0), writes=["zer"])
                for i in range(2):
                    kb.op("pool", lambda e, i=i: e.memset(mpad[i][:], 0.0), writes=[("mpad", i)])
                kb.op("dve", lambda e: e.tensor_reduce(out=km[:].rearrange("p h n -> p (h n)"), in_=kaug[0:64, :, :].rearrange("p h (n t) -> p (h n) t", t=256), axis=AX.X, op=ALU.add),
                      reads=["kaug"], writes=["km"])
                kb.op("dve", lambda e: e.tensor_scalar(out=kmb[:], in0=km[:], scalar1=1.0 / 256.0, scalar2=None, op0=ALU.mult), reads=["km"], writes=["kmb"])

                if l == 0:
                    casts_rest()

                def loadq(G):
                    i = G % 3
                    dma("sp", qaug[i][0:64, :, :], mqT_d.rearrange("(h d) t -> d h t", d=64)[:, :, G * 512:(G + 1) * 512], writes=[("qaug", i)], stream="q", n=3)

                NG = S // 512
                si = 0
                ai = 0

                def pre1(G):
                    qi = G % 3
                    qa = qaug[qi]
                    kqa = ("qaug", qi)
                    mp = mpad[G % 2]
                    for s in range(4):
                        for h in range(8):
                            mm(gps[:, (s * 8 + h) * 16:(s * 8 + h + 1) * 16], qa[0:64, h, s * 128:(s + 1) * 128], kmb[:, h, :], True, True,
                               reads=[kqa, "kmb"], writes=["mgps"])
                    np0 = 2 * G
                    for a in range(2):
                        cmv = cm[:, np0 + a, :].unsqueeze(1).unsqueeze(1).broadcast_to([128, 2, 8, 16])
                        kb.op("dve", lambda e, cmv=cmv, a=a: e.tensor_tensor(out=gm[:, 2 * a:2 * a + 2], in0=gps[:].rearrange("p (s h n) -> p s h n", s=4, h=8)[:, 2 * a:2 * a + 2],
                                                                             in1=cmv, op=ALU.add),
                              reads=["mgps", "cm"], writes=["gm"])
                    for s in range(4):
                        for h in range(8):
                            kb.op("dve", lambda e, s=s, h=h: e.max(out=m8[:, s, h, :], in_=gm[:, s, h, :]), reads=["gm"], writes=["m8"])
                    kb.op("dve", lambda e: e.tensor_tensor(out=sel[:], in0=gm[:], in1=m8[:, :, :, 2:3].broadcast_to([128, 4, 8, 16]), op=ALU.is_ge),
                          reads=["gm", "m8"], writes=["sel"])
                    kb.op("dve", lambda e: e.tensor_scalar(out=sel[:], in0=sel[:], scalar1=-NEG, scalar2=NEG, op0=ALU.mult, op1=ALU.add), reads=["sel"], writes=["sel"])
                    kb.op("dve", lambda e: e.tensor_tensor(out=sel[:], in0=sel[:], in1=b31[:].unsqueeze(1).unsqueeze(3).broadcast_to([128, 4, 8, 16]), op=ALU.add),
                          reads=["sel", "b31"], writes=["sel"])
                    for a in range(2):
                        pmv = pm[:, np0 + a, :].unsqueeze(1).unsqueeze(1).broadcast_to([128, 2, 8, 16])
                        kb.op("dve", lambda e, pmv=pmv, mp=mp, a=a: e.tensor_tensor(out=mp[:, 2 * a:2 * a + 2, :, 64:80], in0=sel[:, 2 * a:2 * a + 2], in1=pmv, op=ALU.mult),
                              reads=["sel", "pm"], writes=[("mpad", G % 2)])

                def pre2(G):
                    qi = G % 3
                    qa = qaug[qi]
                    kqa = ("qaug", qi)
                    mp = mpad[G % 2]
                    for h in range(8):
                        mt = mtps[0]
                        for s in range(4):
                            mm(mt[0:80, s * 128:(s + 1) * 128], mp[:, s, h, :], ident[:], True, True, reads=[("mpad", G % 2), "ident"], writes=[("mtps", 0)])
                        kb.op("act", lambda e, mt=mt, h=h, qa=qa: e.copy(out=qa[64:80, h, :], in_=mt[64:80, :]),
                              reads=[("mtps", 0)], writes=[kqa])

                loadq(0)
                if NG > 1:
                    loadq(1)
                pre1(0)
                pre2(0)
                for G in range(NG):
                    if G + 2 < NG:
                        loadq(G + 2)
                    if G + 1 < NG:
                        pre1(G + 1)
                    qi = G % 3
                    qa = qaug[qi]
                    kqa = ("qaug", qi)
                    ym = ymo[G % 2]
                    nj = 4 * G + 4
                    DEPTH = 2

                    def stageA(h, j):
                        nonlocal si
                        acc = accs[h % 2]
                        ka = ("acc", h % 2)
                        if j == 0:
                            mm(accs_full[h % 2][:, 0:260], zer[:, 0:128], zer[:, :], True, True, reads=["zer"], writes=[ka])
                        r = j - 4 * G
                        c0 = max(r, 0) * 128
                        sp_ = sps[si % 3]
                        ks = ("sps", si % 3)
                        pt = pT[si % 4]
                        kp = ("pT", si % 4)
                        si += 1
                        mm(sp_[:, c0:512], kaug[0:80, h, j * 128:(j + 1) * 128], qa[0:80, h, c0:512], True, True,
                           reads=["kaug", kqa], writes=[ks])
                        if r == -1:
                            mm(sp_[:, 0:128], ident[:], tomT[:, h, :], False, True, reads=["ident", "tomT"], writes=[ks], skip_group_check=True)
                        if r >= 0:
                            mm(sp_[:, r * 128:(r + 1) * 128], ident[:], tdT[:, h, :], False, True, reads=["ident", "tdT"], writes=[ks], skip_group_check=True)
                            if r < 3:
                                tt = toT if r % 2 == 0 else tomT
                                mm(sp_[:, (r + 1) * 128:(r + 2) * 128], ident[:], tt[:, h, :], False, True, reads=["ident", "toT", "tomT"], writes=[ks], skip_group_check=True)
                        kb.op("act", lambda e, pt=pt, sp_=sp_, c0=c0: e.activation(out=pt[:, c0:512], in_=sp_[:, c0:512], func=AF.Exp),
                              reads=[ks], writes=[kp])
                        return (h, j, r, pt, kp, acc, ka)

                    def stageB(info):
                        h, j, r, pt, kp, acc, ka = info
                        for s in range(max(r, 0), 4):
                            mm(acc[:, s, :], pt[:, s * 128:(s + 1) * 128], vp[:, j, h * 65:(h + 1) * 65], False, True,
                               reads=[kp, "vp"], writes=[ka], skip_group_check=True)
                        if j == nj - 1:
                            kb.op("dve", lambda e, acc=acc: e.reciprocal(out=rcp[:], in_=acc[:, :, 64]), reads=[ka], writes=["rcp"])
                            kb.op("dve", lambda e, acc=acc, h=h, ym=ym: e.tensor_tensor(out=ym[:, :, h * 64:(h + 1) * 64], in0=acc[:, :, 0:64],
                                                                                  in1=rcp[:].unsqueeze(2).broadcast_to([128, 4, 64]), op=ALU.mult),
                                  reads=[ka, "rcp"], writes=[("ymo", G % 2)])

                    pend = []
                    for h in range(8):
                        if h == 4 and G + 1 < NG:
                            pre2(G + 1)
                        for j in range(nj):
                            pend.append(stageA(h, j))
                            if len(pend) > DEPTH:
                                stageB(pend.pop(0))
                    while pend:
                        stageB(pend.pop(0))
                    yt = ymT[G % 2]
                    for s in range(4):
                        tpb = mtps_b[0]
                        for f in range(4):
                            tp(tpb[:, f * 128:(f + 1) * 128], ym[:, s, f * 128:(f + 1) * 128], ident[:], reads=[("ymo", G % 2), "ident"], writes=[("mtpsb", 0)])
                        kb.op("act", lambda e, tpb=tpb, yt=yt, s=s: e.copy(out=yt[:, :, s * 128:(s + 1) * 128], in_=tpb[:, 0:512].rearrange("p (f q) -> p f q", f=4)),
                              reads=[("mtpsb", 0)], writes=[("ymT", G % 2)])
                    dma("sp", mixT_d[512:1024, G * 512:(G + 1) * 512].rearrange("(f p) t -> p f t", p=128), yt[:], reads=[("ymT", G % 2)], stream="o", n=4)
                end_phase()

        def phase3(l, xin_d, xout_d):
            with ExitStack() as ph:
                wout = sbt(ph, [128, 8, D], BF16, "wout")
                wdn = sbt(ph, [128, NFC, D], BF16, "wdn")
                g3 = sbt(ph, [128, 3, D], F32, "g3")
                wgu = [sbt(ph, [128, 2, 8, 128], BF16, "wgu") for _ in range(4)]
                mixT = [sbt(ph, [128, 8, 512], BF16, "mixT") for _ in range(2)]
                xt = [sbt(ph, [128, D], F32, "xt3") for _ in range(2)]
                x1 = [sbt(ph, [128, D], F32, "x1") for _ in range(8)]
                tmp = [sbt(ph, [128, D], F32, "tmp3") for _ in range(2)]
                junk = sbt(ph, [128, D], BF16, "junk3")
                hb = [sbt(ph, [128, D], BF16, "hb3") for _ in range(4)]
                hT = sbt(ph, [128, 8, 512], BF16, "hT3")
                actT = sbt(ph, [128, NFC, 512], BF16, "actT")
                sgt = [sbt(ph, [128, 512], F32, "sgt") for _ in range(2)]
                ssq = sbt(ph, [128, 16], F32, "ssq3")
                rst = sbt(ph, [128, 16], F32, "rst3")
                xo = [sbt(ph, [128, D], F32, "xo") for _ in range(2)]
                ops_ = [pst(ph, [128, 2, 512], F32, "p3o") for _ in range(2)]
                tps = pst(ph, [128, D], BF16, "p3t")
                gus = [pst(ph, [128, 512], F32, "p3gu") for _ in range(3)]
                for kc in range(8):
                    dma("sp", wout[:, kc, :], woutb_d[l, kc * 128:(kc + 1) * 128, :], writes=["wout"], stream="w", n=4)
                for fc in range(NFC):
                    dma("sp", wdn[:, fc, :], wdb_d[l, fc * 128:(fc + 1) * 128, :], writes=["wdn"], stream="w", n=4)
                for i in range(3):
                    dma("sp", g3[:, i, :], norms_d[l, i + 1:i + 2, :].partition_broadcast(128), writes=["g3"])

                wi = [0]

                def loadw(fc):
                    i = wi[0] % 4
                    wi[0] += 1
                    dma("sp", wgu[i][:], wgub_d[l, fc], writes=[("wgu", i)], stream="wgu", n=4)
                    return i

                NG = S // 512
                sq = 0

                def loadg(G):
                    i = G % 2
                    dma("sp", mixT[i][:], mixT_d[:, G * 512:(G + 1) * 512].rearrange("(k p) t -> p k t", p=128), writes=[("mixT", i)], stream="m", n=2)

                def loadx(t):
                    dma("sp", xt[t % 2][:], xin_d[t * 128:(t + 1) * 128, :], writes=[("xt3", t % 2)], stream="x", n=3)

                loadg(0)
                loadx(0)
                wq = []
                PRE = 3
                def p3_front(G):
                    nonlocal sq
                    mT = mixT[G % 2]
                    for s in range(4):
                        t = G * 4 + s
                        if t + 1 < S // 128:
                            loadx(t + 1)
                        xs = xt[t % 2]
                        x1s = x1[(G % 2) * 4 + s]
                        op_ = ops_[s % 2]
                        ko = ("p3o", s % 2)
                        tm_ = tmp[s % 2]
                        kt = ("tmp3", s % 2)
                        for hf in range(2):
                            for kc in range(8):
                                mm(op_[:, hf, :], mT[:, kc, s * 128:(s + 1) * 128], wout[:, kc, hf * 512:(hf + 1) * 512], kc == 0, kc == 7,
                                   reads=[("mixT", G % 2), "wout"], writes=[ko])
                        c = sq % 16
                        sq += 1
                        kb.op("act", lambda e, c=c, op_=op_: e.activation(out=junk[:], in_=op_[:].rearrange("p a b -> p (a b)"), func=AF.Square, accum_out=ssq[:, c:c + 1]),
                              reads=[ko], writes=["junk3", "p3ssq"])
                        rstd_from_ssq(ssq[:, c:c + 1], rst[:, c:c + 1], D, "p3")
                        kb.op("dve", lambda e, c=c, op_=op_, tm_=tm_: e.scalar_tensor_tensor(out=tm_[:], in0=op_[:].rearrange("p a b -> p (a b)"), scalar=rst[:, c:c + 1], in1=g3[:, 0, :], op0=ALU.mult, op1=ALU.mult),
                              reads=[ko, "p3rs", "g3"], writes=[kt])
                        kb.op("pool", lambda e, xs=xs, x1s=x1s, tm_=tm_: e.tensor_tensor(out=x1s[:], in0=xs[:], in1=tm_[:], op=ALU.add),
                              reads=[("xt3", t % 2), kt], writes=[("x1", (G % 2) * 4 + s)])
                        c2 = sq % 16
                        sq += 1
                        kb.op("act", lambda e, c2=c2, x1s=x1s: e.activation(out=junk[:], in_=x1s[:], func=AF.Square, accum_out=ssq[:, c2:c2 + 1]),
                              reads=[("x1", (G % 2) * 4 + s)], writes=["junk3", "p3ssq"])
                        rstd_from_ssq(ssq[:, c2:c2 + 1], rst[:, c2:c2 + 1], D, "p3")
                        hbs = hb[s]
                        kb.op("dve", lambda e, c2=c2, x1s=x1s, hbs=hbs: e.scalar_tensor_tensor(out=hbs[:], in0=x1s[:], scalar=rst[:, c2:c2 + 1], in1=g3[:, 1, :], op0=ALU.mult, op1=ALU.mult),
                              reads=[("x1", (G % 2) * 4 + s), "p3rs", "g3"], writes=[("hb3", s)])

                def p3_trans(G):
                    for s in range(4):
                        hbs = hb[s]
                        for kc in range(8):
                            tp(tps[:, kc * 128:(kc + 1) * 128], hbs[:, kc * 128:(kc + 1) * 128], ident[:], reads=[("hb3", s), "ident"], writes=["p3t"])
                        kb.op("act", lambda e, s=s: e.copy(out=hT[:, :, s * 128:(s + 1) * 128], in_=tps[:].rearrange("p (k c) -> p k c", k=8)),
                              reads=["p3t"], writes=["hT3"])

                def p3_gateup(G):
                    for fc in range(NFC):
                        wslot = wq.pop(0)
                        nxt = fc + PRE
                        if nxt < NFC:
                            wq.append(loadw(nxt))
                        w_ = wgu[wslot]
                        gp, up = gus[(2 * fc) % 3], gus[(2 * fc + 1) % 3]
                        kgp, kup = ("p3gu", (2 * fc) % 3), ("p3gu", (2 * fc + 1) % 3)
                        for kc in range(8):
                            mm(gp[:], w_[:, 0, kc, :], hT[:, kc, :], kc == 0, kc == 7, reads=[("wgu", wslot), "hT3"], writes=[kgp])
                        for kc in range(8):
                            mm(up[:], w_[:, 1, kc, :], hT[:, kc, :], kc == 0, kc == 7, reads=[("wgu", wslot), "hT3"], writes=[kup])
                        sg_ = sgt[fc % 2]
                        kb.op("act", lambda e, sg_=sg_, gp=gp: e.activation(out=sg_[:], in_=gp[:], func=AF.Silu), reads=[kgp], writes=[("sgt", fc % 2)])
                        kb.op("dve", lambda e, sg_=sg_, up=up, fc=fc: e.tensor_tensor(out=actT[:, fc, :], in0=up[:], in1=sg_[:], op=ALU.mult),
                              reads=[kup, ("sgt", fc % 2)], writes=["actT"])

                def p3_down(G):
                    nonlocal sq
                    for s in range(4):
                        t = G * 4 + s
                        op_ = ops_[s % 2]
                        ko = ("p3o", s % 2)
                        tm_ = tmp[s % 2]
                        kt = ("tmp3", s % 2)
                        for hf in range(2):
                            for fc in range(NFC):
                                mm(op_[:, hf, :], actT[:, fc, s * 128:(s + 1) * 128], wdn[:, fc, hf * 512:(hf + 1) * 512], fc == 0, fc == NFC - 1,
                                   reads=["actT", "wdn"], writes=[ko])
                        c = sq % 16
                        sq += 1
                        kb.op("act", lambda e, c=c, op_=op_: e.activation(out=junk[:], in_=op_[:].rearrange("p a b -> p (a b)"), func=AF.Square, accum_out=ssq[:, c:c + 1]),
                              reads=[ko], writes=["junk3", "p3ssq"])
                        rstd_from_ssq(ssq[:, c:c + 1], rst[:, c:c + 1], D, "p3")
                        kb.op("dve", lambda e, c=c, op_=op_, tm_=tm_: e.scalar_tensor_tensor(out=tm_[:], in0=op_[:].rearrange("p a b -> p (a b)"), scalar=rst[:, c:c + 1], in1=g3[:, 2, :], op0=ALU.mult, op1=ALU.mult),
                              reads=[ko, "p3rs", "g3"], writes=[kt])
                        xos = xo[t % 2]
                        kb.op("pool", lambda e, xos=xos, s=s, tm_=tm_: e.tensor_tensor(out=xos[:], in0=x1[(G % 2) * 4 + s][:], in1=tm_[:], op=ALU.add),
                              reads=[("x1", (G % 2) * 4 + s), kt], writes=[("xo", t % 2)])
                        dma("sp", xout_d[t * 128:(t + 1) * 128, :], xos[:], reads=[("xo", t % 2)], stream="o", n=4)

                for G in range(NG):
                    if G + 1 < NG:
                        loadg(G + 1)
                    while len(wq) < PRE:
                        wq.append(loadw(len(wq)))
                    p3_front(G)
                    if G > 0:
                        p3_down(G - 1)
                    p3_trans(G)
                    p3_gateup(G)
                p3_down(NG - 1)
                end_phase()

        kb.barrier()
        import os as _os2
        if not _os2.environ.get("SKIP_P0"):
            phase0()
        done = stop_after == "p0"
        for l in range(L):
            if done:
                break
            xin = x_d if l == 0 else xs1_d
            xout = xs1_d if l == 0 else out_d
            for nm, fn in (("p1", lambda: phase1(l, xin)), ("p2b", lambda: phase2ab(l)),
                           ("p2c", lambda: phase2c(l)), ("p3", lambda: phase3(l, xin, xout))):
                fn()
                if stop_after == (l, nm):
                    done = True
                    break
            if done:
                break
        kb.barrier()
        kb.emit()

    return nc


def _host_inputs(inputs):
    f = lambda a: np.ascontiguousarray(np.asarray(a, dtype=np.float32))
    c = _consts()
    idx_diag, idx_off1 = _bias_idx()
    rel = f(inputs["rel_bias"])
    shared = {
        "norms": f(np.stack([inputs["pre_mix_norm"], inputs["post_mix_norm"], inputs["pre_ffn_norm"], inputs["post_ffn_norm"]], axis=1)),
        "w_in": f(inputs["w_in"]), "w_out": f(inputs["w_out"]),
        "w_ffn_gate": f(inputs["w_ffn_gate"]), "w_ffn_up": f(inputs["w_ffn_up"]), "w_ffn_down": f(inputs["w_ffn_down"]),
        "lru_wa": f(inputs["lru_wa"]), "lru_wx": f(inputs["lru_wx"]),
        "gla_gate_w2": f(inputs["gla_gate_w2"]),
        "gla_gate_b": f(np.asarray(inputs["gla_gate_b"]).reshape(L, 128, 1)),
        "gla_norm": f(inputs["gla_norm"]),
        "rb31": f(rel[31:32, :]),
        "tdg": f(np.transpose(rel[idx_diag], (0, 2, 1))),
        "tof": f(np.transpose(rel[idx_off1], (0, 2, 1))),
        "ident": c["ident"], "tri": c["tri"], "caus": c["caus"], "e16": c["e16"],
        "cm": c["cm"], "pm": c["pm"], "bmask": c["bmask"], "hm": c["hm"],
    }
    cw = np.transpose(np.asarray(inputs["lru_conv_w"], dtype=np.float32), (0, 2, 1))
    cols = np.concatenate([cw] + [np.asarray(inputs[k], dtype=np.float32)[:, :, None]
                                  for k in ("lru_conv_b", "lru_ba", "lru_bx", "lru_lambda")], axis=2)
    shared["lru_cols"] = f(cols.reshape(L, 2, 128, 8))
    x = np.asarray(inputs["x"], dtype=np.float32)
    return [dict(shared, x=np.ascontiguousarray(x[b])) for b in range(x.shape[0])]


_NC_CACHE = {}


def kernel(**inputs):
    in_maps = _host_inputs(inputs)
    if "nc" not in _NC_CACHE:
        _NC_CACHE["nc"] = build()
    nc = _NC_CACHE["nc"]
    n = len(in_maps)
    res = run_bass_kernel_spmd(nc, in_maps, core_ids=list(range(n)))
    return np.stack([np.asarray(r["out"], dtype=np.float32) for r in res.results], axis=0)
```

```python
from contextlib import ExitStack
import math
import numpy as np
import ml_dtypes
import concourse.bass as bass
import concourse.mybir as mybir
from concourse.bass_utils import run_bass_kernel_spmd

F32 = mybir.dt.float32
BF16 = mybir.dt.bfloat16
ALU = mybir.AluOpType
AF = mybir.ActivationFunctionType
AX = mybir.AxisListType

S = 4096
D = 1024
L = 2
DIN = 2832
DFF = 2816
NFC = DFF // 128
EPS = 1e-6
NEG = -30000.0
ENGS = ("pe", "act", "dve", "pool", "sp")


class KB:
    def __init__(self, nc, stack, sync_same=True):
        self.nc = nc
        self.stack = stack
        self.sync_same = sync_same
        self.ops = {e: [] for e in ENGS}
        self.sem = {}
        self.cnt = {}
        self.step = {}
        self.known = {e: {} for e in ENGS}
        self.lw = {}
        self.rd = {}
        for e in ENGS:
            self._dom(e, 1)

    def _dom(self, name, step):
        if name not in self.sem:
            self.sem[name] = self.stack.enter_context(self.nc.semaphore("s_" + name))
            self.cnt[name] = 0
            self.step[name] = step
        return name

    def op(self, eng, fn, reads=(), writes=(), dma=None):
        dom = eng if dma is None else self._dom("d_" + dma, 16)
        deps = {}

        def add(d):
            if d is not None and deps.get(d[0], 0) < d[1]:
                deps[d[0]] = d[1]

        for k in reads:
            add(self.lw.get(k))
        for k in writes:
            add(self.lw.get(k))
            for dm, c in self.rd.get(k, {}).items():
                add((dm, c))
        if dma is not None and self.cnt[dom] > 0:
            add((dom, self.cnt[dom]))
        kn = self.known[eng]
        for d, c in deps.items():
            if d == eng and (eng == "pe" or not self.sync_same):
                continue
            if kn.get(d, 0) >= c:
                continue
            self.ops[eng].append(("w", self.sem[d], c))
            kn[d] = c
        self.cnt[dom] += self.step[dom]
        me = (dom, self.cnt[dom])
        self.ops[eng].append(("o", fn, self.sem[dom], self.step[dom]))
        for k in writes:
            self.lw[k] = me
            self.rd[k] = {}
        for k in reads:
            r = self.rd.setdefault(k, {})
            if r.get(dom, 0) < me[1]:
                r[dom] = me[1]
        return me

    def barrier(self):
        for eng in ENGS:
            kn = self.known[eng]
            for dom, c in self.cnt.items():
                if c > 0 and dom != eng and kn.get(dom, 0) < c:
                    self.ops[eng].append(("w", self.sem[dom], c))
                    kn[dom] = c
        self.lw = {}
        self.rd = {}

    def emit(self):
        nc = self.nc
        ops = self.ops

        def run(lst, e):
            for it in lst:
                if it[0] == "w":
                    e.wait_ge(it[1], it[2])
                else:
                    it[1](e).then_inc(it[2], it[3])

        with nc.Block() as block:
            @block.tensor
            def _(e):
                run(ops["pe"], e)

            @block.scalar
            def _(e):
                run(ops["act"], e)

            @block.vector
            def _(e):
                run(ops["dve"], e)

            @block.gpsimd
            def _(e):
                run(ops["pool"], e)

            @block.sync
            def _(e):
                run(ops["sp"], e)
        self.ops = {e: [] for e in ENGS}


def _t5_bucket(n):
    n = np.maximum(n, 0)
    nf = np.maximum(n, 1).astype(np.float32)
    large = 16 + (np.log(nf / np.float32(16)) / np.float32(math.log(128 / 16)) * np.float32(16)).astype(np.int32)
    large = np.minimum(large, 31)
    return np.where(n < 16, n, large)


def _consts():
    c = {}
    c["ident"] = np.eye(128, dtype=np.float32).astype(ml_dtypes.bfloat16)
    e = np.arange(128)
    c["tri"] = (e[:, None] <= e[None, :]).astype(np.float32)
    c["caus"] = np.where(e[None, :] >= e[:, None], 0.0, NEG).astype(np.float32)
    keys = np.arange(S)
    c["e16"] = (keys[None, :] // 256 == np.arange(16)[:, None]).astype(np.float32).astype(ml_dtypes.bfloat16)
    npast = np.arange(16)[:, None]
    nn = np.arange(16)[None, :]
    c["cm"] = np.where(nn < npast, 0.0, -1e30).astype(np.float32).reshape(1, 256)
    c["pm"] = (nn < npast).astype(np.float32).reshape(1, 256)
    p = np.arange(128)[:, None]
    c["bmask"] = (p // 32 == (np.arange(256)[None, :] // 64)).astype(np.float32)
    c["hm"] = (p // 32 == np.arange(4)[None, :]).astype(np.float32)
    return c


def _bias_idx():
    k = np.arange(128)[:, None]
    q = np.arange(128)[None, :]
    idx_diag = _t5_bucket(q - k)
    idx_off1 = _t5_bucket(q + 128 - k)
    return idx_diag, idx_off1


def build(debug=False, stop_after=None):
    nc = bass.Bass("TRN2", target_bir_lowering=False)
    dr = lambda name, shape, dt, kind="Internal": nc.dram_tensor(name, list(shape), dt, kind=kind).ap()
    IN = "ExternalInput"
    x_d = dr("x", [S, D], F32, IN)
    norms_d = dr("norms", [L, 4, D], F32, IN)
    w_in_d = dr("w_in", [L, D, DIN], F32, IN)
    w_out_d = dr("w_out", [L, D, D], F32, IN)
    wg_d = dr("w_ffn_gate", [L, D, DFF], F32, IN)
    wu_d = dr("w_ffn_up", [L, D, DFF], F32, IN)
    wd_d = dr("w_ffn_down", [L, DFF, D], F32, IN)
    lcols_d = dr("lru_cols", [L, 2, 128, 8], F32, IN)
    lwa_d = dr("lru_wa", [L, 4, 64, 64], F32, IN)
    lwx_d = dr("lru_wx", [L, 4, 64, 64], F32, IN)
    gw2_d = dr("gla_gate_w2", [L, 16, 128], F32, IN)
    gb_d = dr("gla_gate_b", [L, 128, 1], F32, IN)
    gn_d = dr("gla_norm", [L, 256], F32, IN)
    rb31_d = dr("rb31", [1, 8], F32, IN)
    tdg_d = dr("tdg", [128, 8, 128], F32, IN)
    tof_d = dr("tof", [128, 8, 128], F32, IN)
    ident_d = dr("ident", [128, 128], BF16, IN)
    tri_d = dr("tri", [128, 128], F32, IN)
    caus_d = dr("caus", [128, 128], F32, IN)
    e16_d = dr("e16", [16, S], BF16, IN)
    cm_d = dr("cm", [1, 256], F32, IN)
    pm_d = dr("pm", [1, 256], F32, IN)
    bmask_d = dr("bmask", [128, 256], F32, IN)
    hm_d = dr("hm", [128, 4], F32, IN)
    out_d = dr("out", [S, D], F32, "ExternalOutput")

    dk = "ExternalOutput" if debug else "Internal"
    winb_d = dr("winb", [L, D, DIN], BF16)
    woutb_d = dr("woutb", [L, D, D], BF16)
    wgub_d = dr("wgub", [L, NFC, 128, 2, 8, 128], BF16)
    wdb_d = dr("wdb", [L, DFF, D], BF16)
    xs1_d = dr("xs1", [S, D], F32, dk)
    lruT_d = dr("lruT", [512, S], F32, dk)
    gqT_d = dr("gqT", [128, S], F32, dk)
    gkT_d = dr("gkT", [128, S], F32, dk)
    glrT_d = dr("glrT", [16, S], BF16, dk)
    mqT_d = dr("mqT", [512, S], BF16, dk)
    mkT_d = dr("mkT", [512, S], BF16, dk)
    gv_d = dr("gv", [S, 256], BF16, dk)
    gout_d = dr("gout", [S, 256], F32, dk)
    mvp_d = dr("mvp", [S, 520], BF16, dk)
    mixT_d = dr("mixT", [D, S], BF16, dk)

    with ExitStack() as st:
        kb = KB(nc, st)
        uid = [0]

        def sbt(ctx, shape, dt, name=None):
            uid[0] += 1
            return ctx.enter_context(nc.sbuf_tensor("%s_%d" % (name or "t", uid[0]), list(shape), dt))

        def pst(ctx, shape, dt, name=None):
            uid[0] += 1
            return ctx.enter_context(nc.psum_tensor("%s_%d" % (name or "p", uid[0]), list(shape), dt))

        rr = {}

        def dmaname(stream, n):
            i = rr.get(stream, 0)
            rr[stream] = i + 1
            return "%s%d" % (stream, i % n)

        def dma(eng, out, in_, reads=(), writes=(), stream="g", n=4):
            kb.op(eng, lambda e: e.dma_start(out=out, in_=in_), reads=reads, writes=writes, dma=dmaname(stream, n))

        def mm(out, lhsT, rhs, start, stop, reads, writes, **kw):
            kb.op("pe", lambda e: e.matmul(out, lhsT=lhsT, rhs=rhs, start=start, stop=stop, **kw),
                  reads=reads, writes=writes)

        def tp(out, in_, ident, reads, writes):
            kb.op("pe", lambda e: e.transpose(out, in_, ident), reads=reads, writes=writes)

        ident = sbt(st, [128, 128], BF16, "ident")
        dma("sp", ident[:], ident_d, writes=["ident"])

        def end_phase():
            kb.barrier()
            kb.emit()

        def phase0():
            l = 0
            for c0 in range(0, DIN, 944):
                for kc in range(8):
                    r0 = kc * 128
                    dma("pool", winb_d[l, r0:r0 + 128, c0:c0 + 944], w_in_d[l, r0:r0 + 128, c0:c0 + 944], writes=[("winb", l, kc, c0)], stream="cast", n=4)

        def casts_rest():
            for l in range(L):
                for kc in range(8):
                    r0 = kc * 128
                    dma("pool", woutb_d[l, r0:r0 + 128, :], w_out_d[l, r0:r0 + 128, :], stream="cast", n=4)
                for fc in range(NFC):
                    for gu, wsrc in enumerate((wg_d, wu_d)):
                        dma("pool", wgub_d[l, fc, :, gu, :, :],
                            wsrc[l].rearrange("(kc p) f -> p kc f", p=128)[:, :, fc * 128:(fc + 1) * 128],
                            stream="cast", n=4)
                    dma("pool", wdb_d[l, fc * 128:(fc + 1) * 128, :], wd_d[l, fc * 128:(fc + 1) * 128, :], stream="cast", n=4)
            l = 1
            for c0 in range(0, DIN, 944):
                for kc in range(8):
                    r0 = kc * 128
                    dma("pool", winb_d[l, r0:r0 + 128, c0:c0 + 944], w_in_d[l, r0:r0 + 128, c0:c0 + 944], stream="cast", n=4)

        def rstd_from_ssq(ssq, rstd, n, tag):
            kb.op("dve", lambda e: e.tensor_scalar(out=rstd, in0=ssq, scalar1=1.0 / n, scalar2=EPS, op0=ALU.mult, op1=ALU.add),
                  reads=[tag + "ssq"], writes=[tag + "rs"])
            kb.op("act", lambda e: e.sqrt(out=rstd, in_=rstd), reads=[tag + "rs"], writes=[tag + "rs"])
            kb.op("dve", lambda e: e.reciprocal(out=rstd, in_=rstd), reads=[tag + "rs"], writes=[tag + "rs"])

        def phase1(l, xin_d):
            with ExitStack() as ph:
                win = sbt(ph, [128, 8, DIN], BF16, "win")
                gpre = sbt(ph, [128, D], F32, "gpre")
                xt = [sbt(ph, [128, D], F32, "xt") for _ in range(8)]
                hb = [sbt(ph, [128, D], BF16, "hb") for _ in range(4)]
                junk = sbt(ph, [128, D], BF16, "junk")
                hT = [sbt(ph, [128, 8, 512], BF16, "hT") for _ in range(2)]
                ssq = sbt(ph, [128, 8], F32, "ssq")
                rst = sbt(ph, [128, 8], F32, "rst")
                sf = [sbt(ph, [128, 512], F32, "sf") for _ in range(6)]
                sbf = [sbt(ph, [128, 512], BF16, "sbf") for _ in range(6)]
                sgv = [sbt(ph, [128, 256], BF16, "sgv") for _ in range(4)]
                sgo = [sbt(ph, [128, 256], F32, "sgo") for _ in range(4)]
                smv = [sbt(ph, [128, 8, 65], BF16, "smv") for _ in range(4)]
                tps = [pst(ph, [128, D], BF16, "tps") for _ in range(2)]
                aps = [pst(ph, [128, 512], F32, "aps") for _ in range(5)]
                for c0_ in range(0, DIN, 944):
                    for kc in range(8):
                        dma("sp", win[:, kc, c0_:c0_ + 944], winb_d[l, kc * 128:(kc + 1) * 128, c0_:c0_ + 944], reads=[("winb", l, kc, c0_)], writes=[("win", kc, c0_)], stream="w", n=8)

                def wink(c_lo, c_hi):
                    return [("win", kc_, cb_) for kc_ in range(8) for cb_ in range(0, DIN, 944) if cb_ < c_hi and cb_ + 944 > c_lo]

                dma("sp", gpre[:], norms_d[l, 0:1, :].partition_broadcast(128), writes=["gpre"])
                for i in range(4):
                    kb.op("dve", lambda e, i=i: e.memset(smv[i][:], 1.0), writes=[("smv", i)])

                flist = [("lru", lruT_d, 0, 0, 128, F32), ("lru", lruT_d, 128, 128, 128, F32),
                         ("lru", lruT_d, 256, 256, 128, F32), ("lru", lruT_d, 384, 384, 128, F32),
                         ("gq", gqT_d, 0, 512, 128, F32), ("gk", gkT_d, 0, 640, 128, F32),
                         ("glr", glrT_d, 0, 1024, 16, BF16)]
                for i in range(4):
                    flist.append(("mq", mqT_d, i * 128, 1296 + i * 128, 128, BF16))
                for i in range(4):
                    flist.append(("mk", mkT_d, i * 128, 1808 + i * 128, 128, BF16))

                def load(t):
                    dma("sp", xt[t % 8][:], xin_d[t * 128:(t + 1) * 128, :], writes=[("xt", t % 8)], stream="x", n=4)

                NT = S // 128
                pi = 0
                ev = 0
                import os as _os
                _ng = int(_os.environ.get("P1_GROUPS", S // 512))
                _parts = int(_os.environ.get("P1_PARTS", 7))

                def chain(g):
                    for s in range(4):
                        t = g * 4 + s
                        xs = xt[t % 8]
                        hbs = hb[s]
                        c = t % 8
                        kb.op("act", lambda e, xs=xs, c=c: e.activation(out=junk[:], in_=xs[:], func=AF.Square, accum_out=ssq[:, c:c + 1]),
                              reads=[("xt", t % 8)], writes=["junk", "p1ssq"])
                        rstd_from_ssq(ssq[:, c:c + 1], rst[:, c:c + 1], D, "p1")
                        kb.op("dve", lambda e, xs=xs, hbs=hbs, c=c: e.scalar_tensor_tensor(out=hbs[:], in0=xs[:], scalar=rst[:, c:c + 1], in1=gpre[:], op0=ALU.mult, op1=ALU.mult),
                              reads=[("xt", t % 8), "p1rs", "gpre"], writes=[("hb", s)])

                def transp(g):
                    hTg_ = hT[g % 2]
                    for s in range(4):
                        t = g * 4 + s
                        hbs = hb[s]
                        tpp = tps[t % 2]
                        for kc in range(8):
                            tp(tpp[:, kc * 128:(kc + 1) * 128], hbs[:, kc * 128:(kc + 1) * 128], ident[:],
                               reads=[("hb", s), "ident"], writes=[("tps", t % 2)])
                        kb.op("act", lambda e, tpp=tpp, hTg_=hTg_, s=s: e.copy(out=hTg_[:, :, s * 128:(s + 1) * 128], in_=tpp[:].rearrange("p (k c) -> p k c", k=8)),
                              reads=[("tps", t % 2)], writes=[("hT", g % 2)])

                for t in range(8):
                    load(t)
                chain(0)
                transp(0)
                for g in range(_ng):
                    hTg = hT[g % 2]
                    if g + 1 < _ng:
                        chain(g + 1)
                    if g + 2 < _ng:
                        for s in range(4):
                            load((g + 2) * 4 + s)
                    for (nm, dst, drow, wcol, wid, dt) in (flist if _parts & 2 else []):
                        ps = aps[pi % 5]
                        pk = ("aps", pi % 5)
                        pi += 1
                        for kc in range(8):
                            mm(ps[0:wid, :], win[:, kc, wcol:wcol + wid], hTg[:, kc, :], kc == 0, kc == 7,
                               reads=wink(wcol, wcol + wid) + [("hT", g % 2)], writes=[pk])
                        if dt == F32:
                            stg = sf[ev % 6]
                            sk = ("sf", ev % 6)
                        else:
                            stg = sbf[ev % 6]
                            sk = ("sbf", ev % 6)
                        eng = "act" if ev % 2 == 0 else "dve"
                        ev += 1
                        if nm == "mq":
                            if eng == "act":
                                kb.op("act", lambda e, stg=stg, ps=ps, wid=wid: e.mul(out=stg[0:wid, :], in_=ps[0:wid, :], mul=0.125), reads=[pk], writes=[sk])
                            else:
                                kb.op("dve", lambda e, stg=stg, ps=ps, wid=wid: e.tensor_scalar(out=stg[0:wid, :], in0=ps[0:wid, :], scalar1=0.125, scalar2=None, op0=ALU.mult), reads=[pk], writes=[sk])
                        else:
                            if eng == "act":
                                kb.op("act", lambda e, stg=stg, ps=ps, wid=wid: e.copy(out=stg[0:wid, :], in_=ps[0:wid, :]), reads=[pk], writes=[sk])
                            else:
                                kb.op("dve", lambda e, stg=stg, ps=ps, wid=wid: e.tensor_copy(out=stg[0:wid, :], in_=ps[0:wid, :]), reads=[pk], writes=[sk])
                        dma("sp", dst[drow:drow + wid, g * 512:(g + 1) * 512], stg[0:wid, :], reads=[sk], stream="o1", n=12)
                    if g + 1 < _ng:
                        transp(g + 1)
                    for s in (range(4) if _parts & 4 else []):
                        t = g * 4 + s
                        _tm = int(_os.environ.get("TM_SKIP", 0))
                        if not _tm & 1:
                            ps = aps[pi % 5]
                            pk = ("aps", pi % 5)
                            pi += 1
                            ps2 = aps[pi % 5]
                            pk2 = ("aps", pi % 5)
                            pi += 1
                            for kc in range(8):
                                mm(ps[:, 0:256], hTg[:, kc, s * 128:(s + 1) * 128], win[:, kc, 768:1024], kc == 0, kc == 7,
                                   reads=wink(768, 1024) + [("hT", g % 2)], writes=[pk])
                            for kc in range(8):
                                mm(ps2[:, 0:256], hTg[:, kc, s * 128:(s + 1) * 128], win[:, kc, 1040:1296], kc == 0, kc == 7,
                                   reads=wink(1040, 1296) + [("hT", g % 2)], writes=[pk2])
                            a, b = sgv[t % 4], sgo[t % 4]
                            kb.op("act", lambda e, a=a, ps=ps: e.copy(out=a[:], in_=ps[:, 0:256]), reads=[pk], writes=[("sgv", t % 4)])
                            kb.op("dve", lambda e, b=b, ps2=ps2: e.tensor_copy(out=b[:], in_=ps2[:, 0:256]), reads=[pk2], writes=[("sgo", t % 4)])
                            dma("sp", gv_d[t * 128:(t + 1) * 128, :], a[:], reads=[("sgv", t % 4)], stream="o1", n=12)
                            dma("sp", gout_d[t * 128:(t + 1) * 128, :], b[:], reads=[("sgo", t % 4)], stream="o1", n=12)
                        if not _tm & 2:
                            ps = aps[pi % 5]
                            pk = ("aps", pi % 5)
                            pi += 1
                            for kc in range(8):
                                mm(ps[:, :], hTg[:, kc, s * 128:(s + 1) * 128], win[:, kc, 2320:2832], kc == 0, kc == 7,
                                   reads=wink(2320, 2832) + [("hT", g % 2)], writes=[pk])
                            m = smv[t % 4]
                            psv = ps[:].rearrange("p (h d) -> p h d", h=8)
                            if _tm & 4:
                                pass
                            elif t % 2:
                                kb.op("act", lambda e, m=m, psv=psv: e.copy(out=m[:, :, 0:64], in_=psv), reads=[pk], writes=[("smv", t % 4)])
                            else:
                                kb.op("dve", lambda e, m=m, psv=psv: e.tensor_copy(out=m[:, :, 0:64], in_=psv), reads=[pk], writes=[("smv", t % 4)])
                            if not _tm & 8:
                                dma("sp", mvp_d[t * 128:(t + 1) * 128, :], m[:].rearrange("p h d -> p (h d)"), reads=[("smv", t % 4)], stream="o1", n=12)
                if _os.environ.get("P1_TAILSTORE"):
                    dma("sp", lruT_d[0:128, 0:8], rst[:], reads=["p1rs"], stream="o1", n=12)
                end_phase()

        def phase2a_gen(l, ph):
            TB = 1024
            if True:
                cols = sbt(ph, [128, 2, 8], F32, "lcols")
                ccol = sbt(ph, [128, 2], F32, "ccol")
                wstage = sbt(ph, [128, 2, 2, 128], F32, "wstage")
                wbd = sbt(ph, [128, 2, 2, 128], BF16, "wbd")
                xin = [sbt(ph, [128, TB + 3], F32, "xin") for _ in range(2)]
                gin = [sbt(ph, [128, TB], F32, "gin") for _ in range(2)]
                xc = sbt(ph, [128, TB], F32, "xc")
                xcb = sbt(ph, [128, TB], BF16, "xcb")
                rr_ = sbt(ph, [128, TB], F32, "r")
                ii_ = sbt(ph, [128, TB], F32, "i")
                aa = sbt(ph, [128, TB], F32, "a")
                mmul = sbt(ph, [128, TB], F32, "mult")
                uu = sbt(ph, [128, TB], F32, "u")
                hh = [sbt(ph, [128, TB], F32, "h") for _ in range(2)]
                gt = sbt(ph, [128, TB], F32, "gt")
                gs = sbt(ph, [128, TB], F32, "gs")
                yb = [sbt(ph, [128, TB], BF16, "yb") for _ in range(2)]
                gps = [pst(ph, [128, 512], F32, "gps") for _ in range(2)]
                for h in range(2):
                    dma("sp", cols[:, h, :], lcols_d[l, h], writes=["lcols"])
                kb.op("pool", lambda e: e.memset(wstage[:], 0.0), writes=["wstage"])
                for ax, src in enumerate((lwa_d, lwx_d)):
                    for h in range(2):
                        for b in range(2):
                            dma("sp", wstage[b * 64:(b + 1) * 64, ax, h, b * 64:(b + 1) * 64], src[l, 2 * h + b],
                                reads=[], writes=["wstage"])
                kb.op("dve", lambda e: e.tensor_copy(out=wbd[:], in_=wstage[:]), reads=["wstage"], writes=["wbd"])
                kb.op("act", lambda e: e.activation(out=ccol[:], in_=cols[:, :, 7], func=AF.Exp, scale=-1.0), reads=["lcols"], writes=["ccol"])
                kb.op("act", lambda e: e.activation(out=ccol[:], in_=ccol[:], func=AF.Ln, bias=1.0), reads=["ccol"], writes=["ccol"])
                kb.op("dve", lambda e: e.tensor_scalar(out=ccol[:], in0=ccol[:], scalar1=-8.0, scalar2=None, op0=ALU.mult), reads=["ccol"], writes=["ccol"])

                nb = S // TB
                it = 0
                for h in range(2):
                    for b in range(nb):
                        t0 = b * TB
                        xi = xin[it % 2]
                        gi = gin[it % 2]
                        hcur = hh[it % 2]
                        hprev = hh[(it + 1) % 2]
                        ybs = yb[it % 2]
                        kx, kg, ky = ("xin", it % 2), ("gin", it % 2), ("yb", it % 2)
                        kh, khp = ("h", it % 2), ("h", (it + 1) % 2)
                        it += 1
                        if b == 0:
                            kb.op("pool", lambda e, xi=xi: e.memset(xi[:, 0:3], 0.0), writes=[kx])
                            dma("sp", xi[:, 3:], lruT_d[h * 128:(h + 1) * 128, 0:TB], writes=[kx], stream="x", n=3)
                        else:
                            dma("sp", xi[:], lruT_d[h * 128:(h + 1) * 128, t0 - 3:t0 + TB], writes=[kx], stream="x", n=3)
                        dma("sp", gi[:], lruT_d[256 + h * 128:256 + (h + 1) * 128, t0:t0 + TB], writes=[kg], stream="x", n=3)
                        yield
                        kb.op("dve", lambda e, xi=xi, h=h: e.tensor_scalar(out=xc[:], in0=xi[:, 3:TB + 3], scalar1=cols[:, h, 3:4], scalar2=cols[:, h, 4:5], op0=ALU.mult, op1=ALU.add),
                              reads=[kx, "lcols"], writes=["xc"])
                        for j in range(3):
                            kb.op("dve", lambda e, xi=xi, h=h, j=j: e.scalar_tensor_tensor(out=xc[:], in0=xi[:, j:TB + j], scalar=cols[:, h, j:j + 1], in1=xc[:], op0=ALU.mult, op1=ALU.add),
                                  reads=[kx, "lcols", "xc"], writes=["xc"])
                        yield
                        kb.op("pool", lambda e: e.tensor_copy(out=xcb[:], in_=xc[:]), reads=["xc"], writes=["xcb"])
                        for sblk in range(TB // 512):
                            cs = slice(sblk * 512, (sblk + 1) * 512)
                            pa, px = gps[0], gps[1]
                            ka, kx_ = ("gps", 0), ("gps", 1)
                            mm(pa[:], wbd[:, 0, h, :], xcb[:, cs], True, True, reads=["wbd", "xcb"], writes=[ka])
                            mm(px[:], wbd[:, 1, h, :], xcb[:, cs], True, True, reads=["wbd", "xcb"], writes=[kx_])
                            kb.op("act", lambda e, pa=pa, cs=cs, h=h: e.activation(out=rr_[:, cs], in_=pa[:], func=AF.Sigmoid, bias=cols[:, h, 5:6]),
                                  reads=[ka, "lcols"], writes=["r"])
                            kb.op("act", lambda e, px=px, cs=cs, h=h: e.activation(out=ii_[:, cs], in_=px[:], func=AF.Sigmoid, bias=cols[:, h, 6:7]),
                                  reads=[kx_, "lcols"], writes=["i"])
                        yield
                        kb.op("pool", lambda e, gi=gi: e.tensor_tensor(out=gt[:], in0=gi[:], in1=gi[:], op=ALU.mult), reads=[kg], writes=["gt"])
                        kb.op("pool", lambda e: e.tensor_scalar(out=gt[:], in0=gt[:], scalar1=0.044715, scalar2=1.0, op0=ALU.mult, op1=ALU.add), reads=["gt"], writes=["gt"])
                        kb.op("pool", lambda e, gi=gi: e.tensor_tensor(out=gt[:], in0=gt[:], in1=gi[:], op=ALU.mult), reads=["gt", kg], writes=["gt"])
                        kb.op("act", lambda e: e.activation(out=gs[:], in_=gt[:], func=AF.Sigmoid, scale=1.5957691216057308), reads=["gt"], writes=["gs"])
                        kb.op("pool", lambda e, gi=gi: e.tensor_tensor(out=gs[:], in0=gs[:], in1=gi[:], op=ALU.mult), reads=["gs", kg], writes=["gs"])
                        yield
                        kb.op("act", lambda e, h=h: e.activation(out=aa[:], in_=rr_[:], func=AF.Exp, scale=ccol[:, h:h + 1]), reads=["r", "ccol"], writes=["a"])
                        kb.op("pool", lambda e: e.tensor_tensor(out=mmul[:], in0=aa[:], in1=aa[:], op=ALU.mult), reads=["a"], writes=["mult"])
                        kb.op("act", lambda e: e.activation(out=mmul[:], in_=mmul[:], func=AF.Sqrt, scale=-1.0, bias=1.0), reads=["mult"], writes=["mult"])
                        yield
                        if b == 0:
                            kb.op("dve", lambda e: e.memset(mmul[:, 0:1], 1.0), reads=["mult"], writes=["mult"])
                        kb.op("dve", lambda e: e.tensor_tensor(out=uu[:], in0=ii_[:], in1=xc[:], op=ALU.mult), reads=["i", "xc"], writes=["u"])
                        kb.op("dve", lambda e: e.tensor_tensor(out=uu[:], in0=uu[:], in1=mmul[:], op=ALU.mult), reads=["u", "mult"], writes=["u"])
                        yield
                        if b == 0:
                            kb.op("dve", lambda e, hcur=hcur: e.tensor_tensor_scan(out=hcur[:], data0=aa[:], data1=uu[:], initial=0.0, op0=ALU.mult, op1=ALU.add),
                                  reads=["a", "u"], writes=[kh])
                        else:
                            kb.op("dve", lambda e, hcur=hcur, hprev=hprev: e.tensor_tensor_scan(out=hcur[:], data0=aa[:], data1=uu[:], initial=hprev[:, TB - 1:TB], op0=ALU.mult, op1=ALU.add),
                                  reads=["a", "u", khp], writes=[kh])
                        yield
                        kb.op("dve", lambda e, hcur=hcur, ybs=ybs: e.tensor_tensor(out=ybs[:], in0=hcur[:], in1=gs[:], op=ALU.mult), reads=[kh, "gs"], writes=[ky])
                        dma("sp", mixT_d[h * 128:(h + 1) * 128, t0:t0 + TB], ybs[:], reads=[ky], stream="o", n=4)
                        yield

        def phase2b_gen(l, ph):
            TB = 1024
            NCH = TB // 128
            if True:
                w2s = sbt(ph, [16, 128], F32, "w2s")
                w2b = sbt(ph, [16, 128], BF16, "w2b")
                negb = sbt(ph, [128, 1], F32, "negb")
                gn = sbt(ph, [128, 256], F32, "gn")
                tri = sbt(ph, [128, 128], F32, "tri")
                bmask = sbt(ph, [128, 256], F32, "bmask")
                hm = sbt(ph, [128, 4], F32, "hm")
                ones = sbt(ph, [128, 128], F32, "ones")
                glr = [sbt(ph, [16, TB], BF16, "glr") for _ in range(2)]
                qT = [sbt(ph, [128, TB], F32, "qT") for _ in range(2)]
                kT = [sbt(ph, [128, TB], F32, "kT") for _ in range(2)]
                vv = [sbt(ph, [128, NCH, 256], BF16, "vv") for _ in range(2)]
                go = [sbt(ph, [128, NCH, 256], F32, "go") for _ in range(2)]
                ee = sbt(ph, [128, TB], F32, "ee")
                cum = sbt(ph, [128, TB], F32, "cum")
                ex = sbt(ph, [128, TB], F32, "ex")
                dd = sbt(ph, [128, TB], F32, "dd")
                qd = sbt(ph, [128, TB], BF16, "qd")
                kdm = sbt(ph, [128, 4, TB], BF16, "kdm")
                kdec = sbt(ph, [128, TB], BF16, "kdec")
                dcol = sbt(ph, [128, NCH], F32, "dcol")
                kdtm = [sbt(ph, [128, 128], BF16, "kdtm") for _ in range(2)]
                am = [sbt(ph, [128, 4, 128], BF16, "am") for _ in range(2)]
                Sst = sbt(ph, [128, 256], F32, "Sst")
                Sbf = sbt(ph, [128, 256], BF16, "Sbf")
                kvm = sbt(ph, [128, 256], F32, "kvm")
                ob = sbt(ph, [128, NCH, 256], F32, "ob")
                osq = sbt(ph, [128, NCH, 256], F32, "osq")
                ssq = sbt(ph, [128, NCH * 4], F32, "gssq")
                rst = sbt(ph, [128, NCH * 4], F32, "grst")
                sg = sbt(ph, [128, NCH, 256], F32, "sg")
                yb = sbt(ph, [128, NCH, 256], BF16, "yb")
                yT = [sbt(ph, [128, 2, TB], BF16, "yT") for _ in range(2)]
                zps = [pst(ph, [128, 512], F32, "zps") for _ in range(1)]
                tps = pst(ph, [128, 1024], BF16, "gtps")
                aps_ = [pst(ph, [128, 512], F32, "gaps") for _ in range(1)]
                ops_ = [pst(ph, [128, 512], F32, "gops") for _ in range(2)]
                kvps = pst(ph, [128, 512], F32, "kvps")

                dma("sp", w2s[:], gw2_d[l], writes=["w2s"])
                kb.op("dve", lambda e: e.tensor_copy(out=w2b[:], in_=w2s[:]), reads=["w2s"], writes=["w2b"])
                dma("sp", negb[:], gb_d[l], writes=["negb"])
                kb.op("dve", lambda e: e.tensor_scalar(out=negb[:], in0=negb[:], scalar1=-1.0, scalar2=None, op0=ALU.mult), reads=["negb"], writes=["negb"])
                dma("sp", gn[:], gn_d[l:l + 1, :].partition_broadcast(128), writes=["gn"])
                dma("sp", tri[:], tri_d, writes=["tri"])
                dma("sp", bmask[:], bmask_d, writes=["bmask"])
                dma("sp", hm[:], hm_d, writes=["hm"])
                kb.op("pool", lambda e: e.memset(ones[:], 1.0), writes=["ones"])
                kb.op("pool", lambda e: e.memset(Sst[:], 0.0), writes=["Sst"])
                kb.op("pool", lambda e: e.memset(Sbf[:], 0.0), writes=["Sbf"])

                def load(b):
                    i = b % 2
                    t0 = b * TB
                    dma("sp", glr[i][:], glrT_d[:, t0:t0 + TB], writes=[("glr", i)], stream="x", n=3)
                    dma("sp", qT[i][:], gqT_d[:, t0:t0 + TB], writes=[("qT", i)], stream="x", n=3)
                    dma("sp", kT[i][:], gkT_d[:, t0:t0 + TB], writes=[("kT", i)], stream="x", n=3)
                    dma("sp", vv[i][:], gv_d[t0:t0 + TB, :].rearrange("(c p) f -> p c f", p=128), writes=[("vv", i)], stream="x", n=3)
                    dma("sp", go[i][:], gout_d[t0:t0 + TB, :].rearrange("(c p) f -> p c f", p=128), writes=[("go", i)], stream="x", n=3)

                nb = S // TB
                load(0)
                for b in range(nb):
                    if b + 1 < nb:
                        load(b + 1)
                    i = b % 2
                    t0 = b * TB
                    q_, k_, v_, g_, r_ = qT[i], kT[i], vv[i], go[i], glr[i]
                    kq, kk, kv, kg, kr = ("qT", i), ("kT", i), ("vv", i), ("go", i), ("glr", i)
                    for sblk in range(TB // 512):
                        cs = slice(sblk * 512, (sblk + 1) * 512)
                        zp = zps[0]
                        mm(zp[:], w2b[:], r_[:, cs], True, True, reads=["w2b", kr], writes=[("zps", 0)])
                        kb.op("act", lambda e, zp=zp, cs=cs: e.activation(out=ee[:, cs], in_=zp[:], func=AF.Exp, scale=-1.0, bias=negb[:]),
                              reads=[("zps", 0), "negb"], writes=["ee"])
                    yield
                    kb.op("act", lambda e: e.activation(out=ee[:], in_=ee[:], func=AF.Ln, bias=1.0), reads=["ee"], writes=["ee"])
                    for c in range(NCH):
                        cs = slice(c * 128, (c + 1) * 128)
                        kb.op("dve", lambda e, cs=cs: e.tensor_tensor_scan(out=cum[:, cs], data0=ones[:], data1=ee[:, cs], initial=0.0, op0=ALU.mult, op1=ALU.add),
                              reads=["ones", "ee"], writes=["cum"])
                    yield
                    kb.op("act", lambda e: e.activation(out=ex[:], in_=cum[:], func=AF.Exp, scale=-1.0 / 16.0), reads=["cum"], writes=["ex"])
                    kb.op("dve", lambda e, q_=q_: e.scalar_tensor_tensor(out=qd[:], in0=q_[:], scalar=32.0 ** -0.5, in1=ex[:], op0=ALU.mult, op1=ALU.mult),
                          reads=[kq, "ex"], writes=["qd"])
                    yield
                    kb.op("act", lambda e: e.activation(out=dcol[:], in_=cum[:].rearrange("p (c t) -> p c t", t=128)[:, :, 127], func=AF.Exp, scale=-1.0 / 16.0),
                          reads=["cum"], writes=["dcol"])
                    for c in range(NCH):
                        cs = slice(c * 128, (c + 1) * 128)
                        kb.op("pool", lambda e, cs=cs, c=c: e.tensor_scalar(out=dd[:, cs], in0=cum[:, cs], scalar1=cum[:, c * 128 + 127:c * 128 + 128], scalar2=None, op0=ALU.subtract),
                              reads=["cum"], writes=["dd"])
                    yield
                    kb.op("act", lambda e: e.activation(out=ex[:], in_=cum[:], func=AF.Exp, scale=1.0 / 16.0), reads=["cum", "qd"], writes=["ex"])
                    for hh_ in range(4):
                        kb.op("dve", lambda e, k_=k_, hh_=hh_: e.scalar_tensor_tensor(out=kdm[:, hh_, :], in0=k_[:], scalar=hm[:, hh_:hh_ + 1], in1=ex[:], op0=ALU.mult, op1=ALU.mult),
                              reads=[kk, "ex", "hm"], writes=["kdm"])
                    yield
                    kb.op("act", lambda e: e.activation(out=dd[:], in_=dd[:], func=AF.Exp, scale=1.0 / 16.0), reads=["dd"], writes=["dd"])
                    kb.op("pool", lambda e, k_=k_: e.tensor_tensor(out=kdec[:], in0=k_[:], in1=dd[:], op=ALU.mult), reads=[kk, "dd"], writes=["kdec"])
                    kb.op("act", lambda e, g_=g_: e.activation(out=sg[:], in_=g_[:], func=AF.Silu), reads=[kg], writes=["sg"])
                    def gla_s1(c):
                        cs = slice(c * 128, (c + 1) * 128)
                        j = c % 2
                        tp(tps[:, j * 128:(j + 1) * 128], kdec[:, cs], ident[:], reads=["kdec", "ident"], writes=["gtps"])
                        kb.op("act", lambda e, j=j: e.copy(out=kdtm[j][:], in_=tps[:, j * 128:(j + 1) * 128]), reads=["gtps"], writes=[("kdtm", j)])
                        ap_ = aps_[0]
                        for hh_ in range(4):
                            mm(ap_[:, hh_ * 128:(hh_ + 1) * 128], kdm[:, hh_, cs], qd[:, cs], True, True, reads=["kdm", "qd"], writes=[("gaps", 0)])
                        kb.op("dve", lambda e, ap_=ap_, j=j: e.tensor_tensor(out=am[j][:], in0=ap_[:].rearrange("p (h c) -> p h c", h=4),
                                                                             in1=tri[:].unsqueeze(1).broadcast_to([128, 4, 128]), op=ALU.mult),
                              reads=[("gaps", 0), "tri"], writes=[("am", j)])

                    def gla_s2(c):
                        cs = slice(c * 128, (c + 1) * 128)
                        j = c % 2
                        op_ = ops_[j]
                        mm(op_[:, 0:256], qd[:, cs], Sbf[:], True, True, reads=["qd", "Sbf"], writes=[("gops", j)])
                        for hh_ in range(4):
                            mm(op_[:, hh_ * 64:(hh_ + 1) * 64], am[j][:, hh_, :], v_[:, c, hh_ * 64:(hh_ + 1) * 64], False, True,
                               reads=[("am", j), kv], writes=[("gops", j)], skip_group_check=True)
                        kb.op("act", lambda e, op_=op_, c=c: e.copy(out=ob[:, c, :], in_=op_[:, 0:256]), reads=[("gops", j)], writes=["ob"])
                        mm(kvps[:, 0:256], kdtm[j][:], v_[:, c, :], True, True, reads=[("kdtm", j), kv], writes=["kvps"])
                        kb.op("dve", lambda e: e.tensor_tensor(out=kvm[:], in0=kvps[:, 0:256], in1=bmask[:], op=ALU.mult), reads=["kvps", "bmask"], writes=["kvm"])
                        kb.op("dve", lambda e, c=c: e.scalar_tensor_tensor(out=Sst[:], in0=Sst[:], scalar=dcol[:, c:c + 1], in1=kvm[:], op0=ALU.mult, op1=ALU.add),
                              reads=["Sst", "dcol", "kvm"], writes=["Sst"])
                        kb.op("pool", lambda e: e.tensor_copy(out=Sbf[:], in_=Sst[:]), reads=["Sst"], writes=["Sbf"])

                    gla_s1(0)
                    yield
                    for c in range(NCH):
                        if c + 1 < NCH:
                            gla_s1(c + 1)
                            yield
                        gla_s2(c)
                        yield
                    yield
                    kb.op("pool", lambda e: e.tensor_tensor(out=osq[:], in0=ob[:], in1=ob[:], op=ALU.mult), reads=["ob"], writes=["osq"])
                    kb.op("dve", lambda e: e.tensor_reduce(out=ssq[:], in_=osq[:].rearrange("p c (h v) -> p (c h) v", h=4), axis=AX.X, op=ALU.add),
                          reads=["osq"], writes=["p2bssq"])
                    rstd_from_ssq(ssq[:], rst[:], 64, "p2b")
                    kb.op("dve", lambda e: e.tensor_tensor(out=ob[:].rearrange("p c (h v) -> p (c h) v", h=4), in0=ob[:].rearrange("p c (h v) -> p (c h) v", h=4),
                                                           in1=rst[:].unsqueeze(2).broadcast_to([128, NCH * 4, 64]), op=ALU.mult),
                          reads=["ob", "p2brs"], writes=["ob"])
                    kb.op("pool", lambda e: e.tensor_tensor(out=sg[:], in0=sg[:], in1=gn[:].unsqueeze(1).broadcast_to([128, NCH, 256]), op=ALU.mult),
                          reads=["sg", "gn"], writes=["sg"])
                    kb.op("dve", lambda e: e.tensor_tensor(out=yb[:], in0=ob[:], in1=sg[:], op=ALU.mult), reads=["ob", "sg"], writes=["yb"])
                    yield
                    yTb = yT[b % 2]
                    for c in range(NCH):
                        for f in range(2):
                            jj = (c * 2 + f) % 4
                            tp(tps[:, jj * 128:(jj + 1) * 128], yb[:, c, f * 128:(f + 1) * 128], ident[:], reads=["yb", "ident"], writes=["gtps"])
                            kb.op("act", lambda e, jj=jj, c=c, f=f, yTb=yTb: e.copy(out=yTb[:, f, c * 128:(c + 1) * 128], in_=tps[:, jj * 128:(jj + 1) * 128]),
                                  reads=["gtps"], writes=[("yT", b % 2)])
                    dma("sp", mixT_d[256:512, t0:t0 + TB].rearrange("(f p) t -> p f t", p=128), yTb[:], reads=[("yT", b % 2)], stream="o", n=4)

        def phase2ab(l):
            with ExitStack() as ph:
                gens = [phase2a_gen(l, ph), phase2b_gen(l, ph)]
                while gens:
                    for g_ in list(gens):
                        try:
                            next(g_)
                        except StopIteration:
                            gens.remove(g_)
                end_phase()

        def phase2c(l):
            with ExitStack() as ph:
                kaug = sbt(ph, [128, 8, S], BF16, "kaug")
                vp = sbt(ph, [128, 32, 520], BF16, "vp")
                qaug = [sbt(ph, [128, 8, 512], BF16, "qaug") for _ in range(3)]
                km = sbt(ph, [64, 8, 16], F32, "km")
                kmb = sbt(ph, [64, 8, 16], BF16, "kmb")
                cm = sbt(ph, [128, 16, 16], F32, "cm")
                pm = sbt(ph, [128, 16, 16], F32, "pm")
                b31 = sbt(ph, [128, 8], F32, "b31")
                caus = sbt(ph, [128, 128], F32, "caus")
                tstage = sbt(ph, [128, 2, 8, 128], F32, "tstage")
                tdT = sbt(ph, [128, 8, 128], BF16, "tdT")
                toT = sbt(ph, [128, 8, 128], BF16, "toT")
                tomT = sbt(ph, [128, 8, 128], BF16, "tomT")
                zer = sbt(ph, [128, 260], BF16, "zer")
                gm = sbt(ph, [128, 4, 8, 16], F32, "gm")
                m8 = sbt(ph, [128, 4, 8, 8], F32, "m8")
                sel = sbt(ph, [128, 4, 8, 16], F32, "sel")
                mpad = [sbt(ph, [128, 4, 8, 80], BF16, "mpad") for _ in range(2)]
                pT = [sbt(ph, [128, 512], BF16, "pT") for _ in range(4)]
                rcp = sbt(ph, [128, 4], F32, "rcp")
                ymo = [sbt(ph, [128, 4, 512], BF16, "ymo") for _ in range(2)]
                ymT = [sbt(ph, [128, 4, 512], BF16, "ymT") for _ in range(2)]
                gps = pst(ph, [128, 512], F32, "mgps")
                mtps = [pst(ph, [128, 512], F32, "mtps") for _ in range(1)]
                mtps_b = [pst(ph, [128, 1024], BF16, "mtpsb") for _ in range(1)]
                sps = [pst(ph, [128, 512], F32, "sps") for _ in range(3)]
                accs_full = [pst(ph, [128, 512], F32, "acc") for _ in range(2)]
                accs = [a_[:, 0:260].rearrange("p (s d) -> p s d", s=4) for a_ in accs_full]

                for h in range(8):
                    dma("sp", kaug[0:64, h, :], mkT_d[h * 64:(h + 1) * 64, :], writes=["kaug"], stream="x", n=3)
                    dma("sp", kaug[64:80, h, :], e16_d, writes=["kaug"], stream="x", n=3)
                for c in range(4):
                    dma("sp", vp[:, c * 8:(c + 1) * 8, :], mvp_d[c * 1024:(c + 1) * 1024, :].rearrange("(c p) f -> p c f", p=128), writes=["vp"], stream="x", n=3)
                dma("sp", cm[:].rearrange("p a b -> p (a b)"), cm_d.partition_broadcast(128), writes=["cm"])
                dma("sp", pm[:].rearrange("p a b -> p (a b)"), pm_d.partition_broadcast(128), writes=["pm"])
                dma("sp", b31[:], rb31_d.partition_broadcast(128), writes=["b31"])
                dma("sp", caus[:], caus_d, writes=["caus"])
                dma("sp", tstage[:, 0], tdg_d, writes=["tstage"])
                dma("sp", tstage[:, 1], tof_d, writes=["tstage"])
                kb.op("dve", lambda e: e.tensor_tensor(out=tdT[:], in0=tstage[:, 0], in1=caus[:].unsqueeze(1).broadcast_to([128, 8, 128]), op=ALU.add),
                      reads=["tstage", "caus"], writes=["tdT"])
                kb.op("dve", lambda e: e.tensor_copy(out=toT[:], in_=tstage[:, 1]), reads=["tstage"], writes=["toT"])
                kb.op("dve", lambda e: e.tensor_tensor(out=tomT[:], in0=tstage[:, 1], in1=b31[:].unsqueeze(2).broadcast_to([128, 8, 128]), op=ALU.subtract),
                      reads=["tstage", "b31"], writes=["tomT"])
                kb.op("pool", lambda e: e.memset(zer[:], 0.0), writes=["zer"])
                for i in range(2):
                    kb.op("pool", lambda e, i=i: e.memset(mpad[i][:], 0.0), writes=[("mpad", i)])
                kb.op("dve", lambda e: e.tensor_reduce(out=km[:].rearrange("p h n -> p (h n)"), in_=kaug[0:64, :, :].rearrange("p h (n t) -> p (h n) t", t=256), axis=AX.X, op=ALU.add),
                      reads=["kaug"], writes=["km"])
                kb.op("dve", lambda e: e.tensor_scalar(out=kmb[:], in0=km[:], scalar1=1.0 / 256.0, scalar2=None, op0=ALU.mult), reads=["km"], writes=["kmb"])

                if l == 0:
                    casts_rest()

                def loadq(G):
                    i = G % 3
                    dma("sp", qaug[i][0:64, :, :], mqT_d.rearrange("(h d) t -> d h t", d=64)[:, :, G * 512:(G + 1) * 512], writes=[("qaug", i)], stream="q", n=3)

                NG = S // 512
                si = 0
                ai = 0

                def pre1(G):
                    qi = G % 3
                    qa = qaug[qi]
                    kqa = ("qaug", qi)
                    mp = mpad[G % 2]
                    for s in range(4):
                        for h in range(8):
                            mm(gps[:, (s * 8 + h) * 16:(s * 8 + h + 1) * 16], qa[0:64, h, s * 128:(s + 1) * 128], kmb[:, h, :], True, True,
                               reads=[kqa, "kmb"], writes=["mgps"])
                    np0 = 2 * G
                    for a in range(2):
                        cmv = cm[:, np0 + a, :].unsqueeze(1).unsqueeze(1).broadcast_to([128, 2, 8, 16])
                        kb.op("dve", lambda e, cmv=cmv, a=a: e.tensor_tensor(out=gm[:, 2 * a:2 * a + 2], in0=gps[:].rearrange("p (s h n) -> p s h n", s=4, h=8)[:, 2 * a:2 * a + 2],
                                                                             in1=cmv, op=ALU.add),
                              reads=["mgps", "cm"], writes=["gm"])
                    for s in range(4):
                        for h in range(8):
                            kb.op("dve", lambda e, s=s, h=h: e.max(out=m8[:, s, h, :], in_=gm[:, s, h, :]), reads=["gm"], writes=["m8"])
                    kb.op("dve", lambda e: e.tensor_tensor(out=sel[:], in0=gm[:], in1=m8[:, :, :, 2:3].broadcast_to([128, 4, 8, 16]), op=ALU.is_ge),
                          reads=["gm", "m8"], writes=["sel"])
                    kb.op("dve", lambda e: e.tensor_scalar(out=sel[:], in0=sel[:], scalar1=-NEG, scalar2=NEG, op0=ALU.mult, op1=ALU.add), reads=["sel"], writes=["sel"])
                    kb.op("dve", lambda e: e.tensor_tensor(out=sel[:], in0=sel[:], in1=b31[:].unsqueeze(1).unsqueeze(3).broadcast_to([128, 4, 8, 16]), op=ALU.add),
                          reads=["sel", "b31"], writes=["sel"])
                    for a in range(2):
                        pmv = pm[:, np0 + a, :].unsqueeze(1).unsqueeze(1).broadcast_to([128, 2, 8, 16])
                        kb.op("dve", lambda e, pmv=pmv, mp=mp, a=a: e.tensor_tensor(out=mp[:, 2 * a:2 * a + 2, :, 64:80], in0=sel[:, 2 * a:2 * a + 2], in1=pmv, op=ALU.mult),
                              reads=["sel", "pm"], writes=[("mpad", G % 2)])

                def pre2(G):
                    qi = G % 3
                    qa = qaug[qi]
                    kqa = ("qaug", qi)
                    mp = mpad[G % 2]
                    for h in range(8):
                        mt = mtps[0]
                        for s in range(4):
                            mm(mt[0:80, s * 128:(s + 1) * 128], mp[:, s, h, :], ident[:], True, True, reads=[("mpad", G % 2), "ident"], writes=[("mtps", 0)])
                        kb.op("dve", lambda e, mt=mt, h=h, qa=qa: e.tensor_copy(out=qa[64:80, h, :], in_=mt[64:80, :]),
                              reads=[("mtps", 0)], writes=[kqa])

                loadq(0)
                if NG > 1:
                    loadq(1)
                pre1(0)
                pre2(0)
                for G in range(NG):
                    if G + 2 < NG:
                        loadq(G + 2)
                    if G + 1 < NG:
                        pre1(G + 1)
                    qi = G % 3
                    qa = qaug[qi]
                    kqa = ("qaug", qi)
                    ym = ymo[G % 2]
                    nj = 4 * G + 4
                    DEPTH = 2

                    def stageA(h, j):
                        nonlocal si
                        acc = accs[h % 2]
                        ka = ("acc", h % 2)
                        if j == 0:
                            mm(accs_full[h % 2][:, 0:260], zer[:, 0:128], zer[:, :], True, True, reads=["zer"], writes=[ka])
                        r = j - 4 * G
                        c0 = max(r, 0) * 128
                        sp_ = sps[si % 3]
                        ks = ("sps", si % 3)
                        pt = pT[si % 4]
                        kp = ("pT", si % 4)
                        si += 1
                        mm(sp_[:, c0:512], kaug[0:80, h, j * 128:(j + 1) * 128], qa[0:80, h, c0:512], True, True,
                           reads=["kaug", kqa], writes=[ks])
                        if r == -1:
                            mm(sp_[:, 0:128], ident[:], tomT[:, h, :], False, True, reads=["ident", "tomT"], writes=[ks], skip_group_check=True)
                        if r >= 0:
                            mm(sp_[:, r * 128:(r + 1) * 128], ident[:], tdT[:, h, :], False, True, reads=["ident", "tdT"], writes=[ks], skip_group_check=True)
                            if r < 3:
                                tt = toT if r % 2 == 0 else tomT
                                mm(sp_[:, (r + 1) * 128:(r + 2) * 128], ident[:], tt[:, h, :], False, True, reads=["ident", "toT", "tomT"], writes=[ks], skip_group_check=True)
                        kb.op("act", lambda e, pt=pt, sp_=sp_, c0=c0: e.activation(out=pt[:, c0:512], in_=sp_[:, c0:512], func=AF.Exp),
                              reads=[ks], writes=[kp])
                        return (h, j, r, pt, kp, acc, ka)

                    def stageB(info):
                        h, j, r, pt, kp, acc, ka = info
                        for s in range(max(r, 0), 4):
                            mm(acc[:, s, :], pt[:, s * 128:(s + 1) * 128], vp[:, j, h * 65:(h + 1) * 65], False, True,
                               reads=[kp, "vp"], writes=[ka], skip_group_check=True)
                        if j == nj - 1:
                            kb.op("dve", lambda e, acc=acc: e.reciprocal(out=rcp[:], in_=acc[:, :, 64]), reads=[ka], writes=["rcp"])
                            kb.op("dve", lambda e, acc=acc, h=h, ym=ym: e.tensor_tensor(out=ym[:, :, h * 64:(h + 1) * 64], in0=acc[:, :, 0:64],
                                                                                  in1=rcp[:].unsqueeze(2).broadcast_to([128, 4, 64]), op=ALU.mult),
                                  reads=[ka, "rcp"], writes=[("ymo", G % 2)])

                    pend = []
                    for h in range(8):
                        if h == 4 and G + 1 < NG:
                            pre2(G + 1)
                        for j in range(nj):
                            pend.append(stageA(h, j))
                            if len(pend) > DEPTH:
                                stageB(pend.pop(0))
                    while pend:
                        stageB(pend.pop(0))
                    yt = ymT[G % 2]
                    for s in range(4):
                        tpb = mtps_b[0]
                        for f in range(4):
                            tp(tpb[:, f * 128:(f + 1) * 128], ym[:, s, f * 128:(f + 1) * 128], ident[:], reads=[("ymo", G % 2), "ident"], writes=[("mtpsb", 0)])
                        kb.op("dve", lambda e, tpb=tpb, yt=yt, s=s: e.tensor_copy(out=yt[:, :, s * 128:(s + 1) * 128], in_=tpb[:, 0:512].rearrange("p (f q) -> p f q", f=4)),
                              reads=[("mtpsb", 0)], writes=[("ymT", G % 2)])
                    dma("sp", mixT_d[512:1024, G * 512:(G + 1) * 512].rearrange("(f p) t -> p f t", p=128), yt[:], reads=[("ymT", G % 2)], stream="o", n=4)
                end_phase()

        def phase3(l, xin_d, xout_d):
            with ExitStack() as ph:
                wout = sbt(ph, [128, 8, D], BF16, "wout")
                wdn = sbt(ph, [128, NFC, D], BF16, "wdn")
                g3 = sbt(ph, [128, 3, D], F32, "g3")
                wgu = [sbt(ph, [128, 2, 8, 128], BF16, "wgu") for _ in range(4)]
                mixT = [sbt(ph, [128, 8, 512], BF16, "mixT") for _ in range(2)]
                xt = [sbt(ph, [128, D], F32, "xt3") for _ in range(2)]
                x1 = [sbt(ph, [128, D], F32, "x1") for _ in range(8)]
                tmp = [sbt(ph, [128, D], F32, "tmp3") for _ in range(2)]
                junk = sbt(ph, [128, D], BF16, "junk3")
                hb = [sbt(ph, [128, D], BF16, "hb3") for _ in range(4)]
                hT = sbt(ph, [128, 8, 512], BF16, "hT3")
                actT = sbt(ph, [128, NFC, 512], BF16, "actT")
                sgt = [sbt(ph, [128, 512], F32, "sgt") for _ in range(2)]
                ssq = sbt(ph, [128, 16], F32, "ssq3")
                rst = sbt(ph, [128, 16], F32, "rst3")
                xo = [sbt(ph, [128, D], F32, "xo") for _ in range(2)]
                ops_ = [pst(ph, [128, 2, 512], F32, "p3o") for _ in range(2)]
                tps = pst(ph, [128, D], BF16, "p3t")
                gus = [pst(ph, [128, 512], F32, "p3gu") for _ in range(3)]
                for kc in range(8):
                    dma("sp", wout[:, kc, :], woutb_d[l, kc * 128:(kc + 1) * 128, :], writes=["wout"], stream="w", n=4)
                for fc in range(NFC):
                    dma("sp", wdn[:, fc, :], wdb_d[l, fc * 128:(fc + 1) * 128, :], writes=["wdn"], stream="w", n=4)
                for i in range(3):
                    dma("sp", g3[:, i, :], norms_d[l, i + 1:i + 2, :].partition_broadcast(128), writes=["g3"])

                wi = [0]

                def loadw(fc):
                    i = wi[0] % 4
                    wi[0] += 1
                    dma("sp", wgu[i][:], wgub_d[l, fc], writes=[("wgu", i)], stream="wgu", n=4)
                    return i

                NG = S // 512
                sq = 0

                def loadg(G):
                    i = G % 2
                    dma("sp", mixT[i][:], mixT_d[:, G * 512:(G + 1) * 512].rearrange("(k p) t -> p k t", p=128), writes=[("mixT", i)], stream="m", n=2)

                def loadx(t):
                    dma("sp", xt[t % 2][:], xin_d[t * 128:(t + 1) * 128, :], writes=[("xt3", t % 2)], stream="x", n=3)

                loadg(0)
                loadx(0)
                wq = []
                PRE = 3
                def p3_front(G):
                    nonlocal sq
                    mT = mixT[G % 2]
                    for s in range(4):
                        t = G * 4 + s
                        if t + 1 < S // 128:
                            loadx(t + 1)
                        xs = xt[t % 2]
                        x1s = x1[(G % 2) * 4 + s]
                        op_ = ops_[s % 2]
                        ko = ("p3o", s % 2)
                        tm_ = tmp[s % 2]
                        kt = ("tmp3", s % 2)
                        for hf in range(2):
                            for kc in range(8):
                                mm(op_[:, hf, :], mT[:, kc, s * 128:(s + 1) * 128], wout[:, kc, hf * 512:(hf + 1) * 512], kc == 0, kc == 7,
                                   reads=[("mixT", G % 2), "wout"], writes=[ko])
                        c = sq % 16
                        sq += 1
                        kb.op("act", lambda e, c=c, op_=op_: e.activation(out=junk[:], in_=op_[:].rearrange("p a b -> p (a b)"), func=AF.Square, accum_out=ssq[:, c:c + 1]),
                              reads=[ko], writes=["junk3", "p3ssq"])
                        rstd_from_ssq(ssq[:, c:c + 1], rst[:, c:c + 1], D, "p3")
                        kb.op("dve", lambda e, c=c, op_=op_, tm_=tm_: e.scalar_tensor_tensor(out=tm_[:], in0=op_[:].rearrange("p a b -> p (a b)"), scalar=rst[:, c:c + 1], in1=g3[:, 0, :], op0=ALU.mult, op1=ALU.mult),
                              reads=[ko, "p3rs", "g3"], writes=[kt])
                        kb.op("pool", lambda e, xs=xs, x1s=x1s, tm_=tm_: e.tensor_tensor(out=x1s[:], in0=xs[:], in1=tm_[:], op=ALU.add),
                              reads=[("xt3", t % 2), kt], writes=[("x1", (G % 2) * 4 + s)])
                        c2 = sq % 16
                        sq += 1
                        kb.op("act", lambda e, c2=c2, x1s=x1s: e.activation(out=junk[:], in_=x1s[:], func=AF.Square, accum_out=ssq[:, c2:c2 + 1]),
                              reads=[("x1", (G % 2) * 4 + s)], writes=["junk3", "p3ssq"])
                        rstd_from_ssq(ssq[:, c2:c2 + 1], rst[:, c2:c2 + 1], D, "p3")
                        hbs = hb[s]
                        kb.op("dve", lambda e, c2=c2, x1s=x1s, hbs=hbs: e.scalar_tensor_tensor(out=hbs[:], in0=x1s[:], scalar=rst[:, c2:c2 + 1], in1=g3[:, 1, :], op0=ALU.mult, op1=ALU.mult),
                              reads=[("x1", (G % 2) * 4 + s), "p3rs", "g3"], writes=[("hb3", s)])

                def p3_trans(G):
                    for s in range(4):
                        hbs = hb[s]
                        for kc in range(8):
                            tp(tps[:, kc * 128:(kc + 1) * 128], hbs[:, kc * 128:(kc + 1) * 128], ident[:], reads=[("hb3", s), "ident"], writes=["p3t"])
                        kb.op("act", lambda e, s=s: e.copy(out=hT[:, :, s * 128:(s + 1) * 128], in_=tps[:].rearrange("p (k c) -> p k c", k=8)),
                              reads=["p3t"], writes=["hT3"])

                def p3_gateup(G):
                    for fc in range(NFC):
                        wslot = wq.pop(0)
                        nxt = fc + PRE
                        if nxt < NFC:
                            wq.append(loadw(nxt))
                        w_ = wgu[wslot]
                        gp, up = gus[(2 * fc) % 3], gus[(2 * fc + 1) % 3]
                        kgp, kup = ("p3gu", (2 * fc) % 3), ("p3gu", (2 * fc + 1) % 3)
                        for kc in range(8):
                            mm(gp[:], w_[:, 0, kc, :], hT[:, kc, :], kc == 0, kc == 7, reads=[("wgu", wslot), "hT3"], writes=[kgp])
                        for kc in range(8):
                            mm(up[:], w_[:, 1, kc, :], hT[:, kc, :], kc == 0, kc == 7, reads=[("wgu", wslot), "hT3"], writes=[kup])
                        sg_ = sgt[fc % 2]
                        kb.op("act", lambda e, sg_=sg_, gp=gp: e.activation(out=sg_[:], in_=gp[:], func=AF.Silu), reads=[kgp], writes=[("sgt", fc % 2)])
                        kb.op("dve", lambda e, sg_=sg_, up=up, fc=fc: e.tensor_tensor(out=actT[:, fc, :], in0=up[:], in1=sg_[:], op=ALU.mult),
                              reads=[kup, ("sgt", fc % 2)], writes=["actT"])

                def p3_down(G):
                    nonlocal sq
                    for s in range(4):
                        t = G * 4 + s
                        op_ = ops_[s % 2]
                        ko = ("p3o", s % 2)
                        tm_ = tmp[s % 2]
                        kt = ("tmp3", s % 2)
                        for hf in range(2):
                            for fc in range(NFC):
                                mm(op_[:, hf, :], actT[:, fc, s * 128:(s + 1) * 128], wdn[:, fc, hf * 512:(hf + 1) * 512], fc == 0, fc == NFC - 1,
                                   reads=["actT", "wdn"], writes=[ko])
                        c = sq % 16
                        sq += 1
                        kb.op("act", lambda e, c=c, op_=op_: e.activation(out=junk[:], in_=op_[:].rearrange("p a b -> p (a b)"), func=AF.Square, accum_out=ssq[:, c:c + 1]),
                              reads=[ko], writes=["junk3", "p3ssq"])
                        rstd_from_ssq(ssq[:, c:c + 1], rst[:, c:c + 1], D, "p3")
                        kb.op("dve", lambda e, c=c, op_=op_, tm_=tm_: e.scalar_tensor_tensor(out=tm_[:], in0=op_[:].rearrange("p a b -> p (a b)"), scalar=rst[:, c:c + 1], in1=g3[:, 2, :], op0=ALU.mult, op1=ALU.mult),
                              reads=[ko, "p3rs", "g3"], writes=[kt])
                        xos = xo[t % 2]
                        kb.op("pool", lambda e, xos=xos, s=s, tm_=tm_: e.tensor_tensor(out=xos[:], in0=x1[(G % 2) * 4 + s][:], in1=tm_[:], op=ALU.add),
                              reads=[("x1", (G % 2) * 4 + s), kt], writes=[("xo", t % 2)])
                        dma("sp", xout_d[t * 128:(t + 1) * 128, :], xos[:], reads=[("xo", t % 2)], stream="o", n=4)

                for G in range(NG):
                    if G + 1 < NG:
                        loadg(G + 1)
                    while len(wq) < PRE:
                        wq.append(loadw(len(wq)))
                    p3_front(G)
                    if G > 0:
                        p3_down(G - 1)
                    p3_trans(G)
                    p3_gateup(G)
                p3_down(NG - 1)
                end_phase()

        kb.barrier()
        import os as _os2
        if not _os2.environ.get("SKIP_P0"):
            phase0()
        done = stop_after == "p0"
        for l in range(L):
            if done:
                break
            xin = x_d if l == 0 else xs1_d
            xout = xs1_d if l == 0 else out_d
            for nm, fn in (("p1", lambda: phase1(l, xin)), ("p2b", lambda: phase2ab(l)),
                           ("p2c", lambda: phase2c(l)), ("p3", lambda: phase3(l, xin, xout))):
                fn()
                if stop_after == (l, nm):
                    done = True
                    break
            if done:
                break
        kb.barrier()
        kb.emit()

    return nc


def _host_inputs(inputs):
    f = lambda a: np.ascontiguousarray(np.asarray(a, dtype=np.float32))
    c = _consts()
    idx_diag, idx_off1 = _bias_idx()
    rel = f(inputs["rel_bias"])
    shared = {
        "norms": f(np.stack([inputs["pre_mix_norm"], inputs["post_mix_norm"], inputs["pre_ffn_norm"], inputs["post_ffn_norm"]], axis=1)),
        "w_in": f(inputs["w_in"]), "w_out": f(inputs["w_out"]),
        "w_ffn_gate": f(inputs["w_ffn_gate"]), "w_ffn_up": f(inputs["w_ffn_up"]), "w_ffn_down": f(inputs["w_ffn_down"]),
        "lru_wa": f(inputs["lru_wa"]), "lru_wx": f(inputs["lru_wx"]),
        "gla_gate_w2": f(inputs["gla_gate_w2"]),
        "gla_gate_b": f(np.asarray(inputs["gla_gate_b"]).reshape(L, 128, 1)),
        "gla_norm": f(inputs["gla_norm"]),
        "rb31": f(rel[31:32, :]),
        "tdg": f(np.transpose(rel[idx_diag], (0, 2, 1))),
        "tof": f(np.transpose(rel[idx_off1], (0, 2, 1))),
        "ident": c["ident"], "tri": c["tri"], "caus": c["caus"], "e16": c["e16"],
        "cm": c["cm"], "pm": c["pm"], "bmask": c["bmask"], "hm": c["hm"],
    }
    cw = np.transpose(np.asarray(inputs["lru_conv_w"], dtype=np.float32), (0, 2, 1))
    cols = np.concatenate([cw] + [np.asarray(inputs[k], dtype=np.float32)[:, :, None]
                                  for k in ("lru_conv_b", "lru_ba", "lru_bx", "lru_lambda")], axis=2)
    shared["lru_cols"] = f(cols.reshape(L, 2, 128, 8))
    x = np.asarray(inputs["x"], dtype=np.float32)
    return [dict(shared, x=np.ascontiguousarray(x[b])) for b in range(x.shape[0])]


_NC_CACHE = {}


def kernel(**inputs):
    in_maps = _host_inputs(inputs)
    if "nc" not in _NC_CACHE:
        _NC_CACHE["nc"] = build()
    nc = _NC_CACHE["nc"]
    n = len(in_maps)
    res = run_bass_kernel_spmd(nc, in_maps, core_ids=list(range(n)))
    return np.stack([np.asarray(r["out"], dtype=np.float32) for r in res.results], axis=0)
```

```python
from contextlib import ExitStack
import math
import numpy as np
import ml_dtypes
import concourse.bass as bass
import concourse.mybir as mybir
from concourse.bass_utils import run_bass_kernel_spmd

F32 = mybir.dt.float32
BF16 = mybir.dt.bfloat16
ALU = mybir.AluOpType
AF = mybir.ActivationFunctionType
AX = mybir.AxisListType

S = 4096
D = 1024
L = 2
DIN = 2832
DFF = 2816
NFC = DFF // 128
EPS = 1e-6
NEG = -30000.0
ENGS = ("pe", "act", "dve", "pool", "sp")


class KB:
    def __init__(self, nc, stack, sync_same=True):
        self.nc = nc
        self.stack = stack
        self.sync_same = sync_same
        self.ops = {e: [] for e in ENGS}
        self.sem = {}
        self.cnt = {}
        self.step = {}
        self.known = {e: {} for e in ENGS}
        self.lw = {}
        self.rd = {}
        for e in ENGS:
            self._dom(e, 1)

    def _dom(self, name, step):
        if name not in self.sem:
            self.sem[name] = self.stack.enter_context(self.nc.semaphore("s_" + name))
            self.cnt[name] = 0
            self.step[name] = step
        return name

    def op(self, eng, fn, reads=(), writes=(), dma=None):
        dom = eng if dma is None else self._dom("d_" + dma, 16)
        deps = {}

        def add(d):
            if d is not None and deps.get(d[0], 0) < d[1]:
                deps[d[0]] = d[1]

        for k in reads:
            add(self.lw.get(k))
        for k in writes:
            add(self.lw.get(k))
            for dm, c in self.rd.get(k, {}).items():
                add((dm, c))
        if dma is not None and self.cnt[dom] > 0:
            add((dom, self.cnt[dom]))
        kn = self.known[eng]
        for d, c in deps.items():
            if d == eng and (eng == "pe" or not self.sync_same):
                continue
            if kn.get(d, 0) >= c:
                continue
            self.ops[eng].append(("w", self.sem[d], c))
            kn[d] = c
        self.cnt[dom] += self.step[dom]
        me = (dom, self.cnt[dom])
        self.ops[eng].append(("o", fn, self.sem[dom], self.step[dom]))
        for k in writes:
            self.lw[k] = me
            self.rd[k] = {}
        for k in reads:
            r = self.rd.setdefault(k, {})
            if r.get(dom, 0) < me[1]:
                r[dom] = me[1]
        return me

    def barrier(self):
        for eng in ENGS:
            kn = self.known[eng]
            for dom, c in self.cnt.items():
                if c > 0 and dom != eng and kn.get(dom, 0) < c:
                    self.ops[eng].append(("w", self.sem[dom], c))
                    kn[dom] = c
        self.lw = {}
        self.rd = {}

    def emit(self):
        nc = self.nc
        ops = self.ops

        def run(lst, e):
            for it in lst:
                if it[0] == "w":
                    e.wait_ge(it[1], it[2])
                else:
                    it[1](e).then_inc(it[2], it[3])

        with nc.Block() as block:
            @block.tensor
            def _(e):
                run(ops["pe"], e)

            @block.scalar
            def _(e):
                run(ops["act"], e)

            @block.vector
            def _(e):
                run(ops["dve"], e)

            @block.gpsimd
            def _(e):
                run(ops["pool"], e)

            @block.sync
            def _(e):
                run(ops["sp"], e)
        self.ops = {e: [] for e in ENGS}


def _t5_bucket(n):
    n = np.maximum(n, 0)
    nf = np.maximum(n, 1).astype(np.float32)
    large = 16 + (np.log(nf / np.float32(16)) / np.float32(math.log(128 / 16)) * np.float32(16)).astype(np.int32)
    large = np.minimum(large, 31)
    return np.where(n < 16, n, large)


def _consts():
    c = {}
    c["ident"] = np.eye(128, dtype=np.float32).astype(ml_dtypes.bfloat16)
    e = np.arange(128)
    c["tri"] = (e[:, None] <= e[None, :]).astype(np.float32)
    c["caus"] = np.where(e[None, :] >= e[:, None], 0.0, NEG).astype(np.float32)
    keys = np.arange(S)
    c["e16"] = (keys[None, :] // 256 == np.arange(16)[:, None]).astype(np.float32).astype(ml_dtypes.bfloat16)
    npast = np.arange(16)[:, None]
    nn = np.arange(16)[None, :]
    c["cm"] = np.where(nn < npast, 0.0, -1e30).astype(np.float32).reshape(1, 256)
    c["pm"] = (nn < npast).astype(np.float32).reshape(1, 256)
    p = np.arange(128)[:, None]
    c["bmask"] = (p // 32 == (np.arange(256)[None, :] // 64)).astype(np.float32)
    c["hm"] = (p // 32 == np.arange(4)[None, :]).astype(np.float32)
    return c


def _bias_idx():
    k = np.arange(128)[:, None]
    q = np.arange(128)[None, :]
    idx_diag = _t5_bucket(q - k)
    idx_off1 = _t5_bucket(q + 128 - k)
    return idx_diag, idx_off1


def build(debug=False, stop_after=None):
    nc = bass.Bass("TRN2", target_bir_lowering=False)
    dr = lambda name, shape, dt, kind="Internal": nc.dram_tensor(name, list(shape), dt, kind=kind).ap()
    IN = "ExternalInput"
    x_d = dr("x", [S, D], F32, IN)
    norms_d = dr("norms", [L, 4, D], F32, IN)
    w_in_d = dr("w_in", [L, D, DIN], F32, IN)
    w_out_d = dr("w_out", [L, D, D], F32, IN)
    wg_d = dr("w_ffn_gate", [L, D, DFF], F32, IN)
    wu_d = dr("w_ffn_up", [L, D, DFF], F32, IN)
    wd_d = dr("w_ffn_down", [L, DFF, D], F32, IN)
    lcols_d = dr("lru_cols", [L, 2, 128, 8], F32, IN)
    lwa_d = dr("lru_wa", [L, 4, 64, 64], F32, IN)
    lwx_d = dr("lru_wx", [L, 4, 64, 64], F32, IN)
    gw2_d = dr("gla_gate_w2", [L, 16, 128], F32, IN)
    gb_d = dr("gla_gate_b", [L, 128, 1], F32, IN)
    gn_d = dr("gla_norm", [L, 256], F32, IN)
    rb31_d = dr("rb31", [1, 8], F32, IN)
    tdg_d = dr("tdg", [128, 8, 128], F32, IN)
    tof_d = dr("tof", [128, 8, 128], F32, IN)
    ident_d = dr("ident", [128, 128], BF16, IN)
    tri_d = dr("tri", [128, 128], F32, IN)
    caus_d = dr("caus", [128, 128], F32, IN)
    e16_d = dr("e16", [16, S], BF16, IN)
    cm_d = dr("cm", [1, 256], F32, IN)
    pm_d = dr("pm", [1, 256], F32, IN)
    bmask_d = dr("bmask", [128, 256], F32, IN)
    hm_d = dr("hm", [128, 4], F32, IN)
    out_d = dr("out", [S, D], F32, "ExternalOutput")

    dk = "ExternalOutput" if debug else "Internal"
    winb_d = dr("winb", [L, D, DIN], BF16)
    woutb_d = dr("woutb", [L, D, D], BF16)
    wgub_d = dr("wgub", [L, NFC, 128, 2, 8, 128], BF16)
    wdb_d = dr("wdb", [L, DFF, D], BF16)
    xs1_d = dr("xs1", [S, D], F32, dk)
    lruT_d = dr("lruT", [512, S], F32, dk)
    gqT_d = dr("gqT", [128, S], F32, dk)
    gkT_d = dr("gkT", [128, S], F32, dk)
    glrT_d = dr("glrT", [16, S], BF16, dk)
    mqT_d = dr("mqT", [512, S], BF16, dk)
    mkT_d = dr("mkT", [512, S], BF16, dk)
    gv_d = dr("gv", [S, 256], BF16, dk)
    gout_d = dr("gout", [S, 256], F32, dk)
    mvp_d = dr("mvp", [S, 520], BF16, dk)
    mixT_d = dr("mixT", [D, S], BF16, dk)

    with ExitStack() as st:
        kb = KB(nc, st)
        uid = [0]

        def sbt(ctx, shape, dt, name=None):
            uid[0] += 1
            return ctx.enter_context(nc.sbuf_tensor("%s_%d" % (name or "t", uid[0]), list(shape), dt))

        def pst(ctx, shape, dt, name=None):
            uid[0] += 1
            return ctx.enter_context(nc.psum_tensor("%s_%d" % (name or "p", uid[0]), list(shape), dt))

        rr = {}

        def dmaname(stream, n):
            i = rr.get(stream, 0)
            rr[stream] = i + 1
            return "%s%d" % (stream, i % n)

        def dma(eng, out, in_, reads=(), writes=(), stream="g", n=4):
            kb.op(eng, lambda e: e.dma_start(out=out, in_=in_), reads=reads, writes=writes, dma=dmaname(stream, n))

        def mm(out, lhsT, rhs, start, stop, reads, writes, **kw):
            kb.op("pe", lambda e: e.matmul(out, lhsT=lhsT, rhs=rhs, start=start, stop=stop, **kw),
                  reads=reads, writes=writes)

        def tp(out, in_, ident, reads, writes):
            kb.op("pe", lambda e: e.transpose(out, in_, ident), reads=reads, writes=writes)

        ident = sbt(st, [128, 128], BF16, "ident")
        dma("sp", ident[:], ident_d, writes=["ident"])

        def end_phase():
            kb.barrier()
            kb.emit()

        def phase0():
            l = 0
            for c0 in range(0, DIN, 944):
                for kc in range(8):
                    r0 = kc * 128
                    dma("pool", winb_d[l, r0:r0 + 128, c0:c0 + 944], w_in_d[l, r0:r0 + 128, c0:c0 + 944], writes=[("winb", l, kc, c0)], stream="cast", n=4)

        def casts_rest():
            for l in range(L):
                for kc in range(8):
                    r0 = kc * 128
                    dma("pool", woutb_d[l, r0:r0 + 128, :], w_out_d[l, r0:r0 + 128, :], stream="cast", n=4)
                for fc in range(NFC):
                    for gu, wsrc in enumerate((wg_d, wu_d)):
                        dma("pool", wgub_d[l, fc, :, gu, :, :],
                            wsrc[l].rearrange("(kc p) f -> p kc f", p=128)[:, :, fc * 128:(fc + 1) * 128],
                            stream="cast", n=4)
                    dma("pool", wdb_d[l, fc * 128:(fc + 1) * 128, :], wd_d[l, fc * 128:(fc + 1) * 128, :], stream="cast", n=4)
            l = 1
            for c0 in range(0, DIN, 944):
                for kc in range(8):
                    r0 = kc * 128
                    dma("pool", winb_d[l, r0:r0 + 128, c0:c0 + 944], w_in_d[l, r0:r0 + 128, c0:c0 + 944], stream="cast", n=4)

        def rstd_from_ssq(ssq, rstd, n, tag, col=None):
            ks_ = tag + "ssq" if col is None else (tag + "ssq", col)
            kr_ = tag + "rs" if col is None else (tag + "rs", col)
            kb.op("dve", lambda e: e.tensor_scalar(out=rstd, in0=ssq, scalar1=1.0 / n, scalar2=EPS, op0=ALU.mult, op1=ALU.add),
                  reads=[ks_], writes=[kr_])
            kb.op("act", lambda e: e.sqrt(out=rstd, in_=rstd), reads=[kr_], writes=[kr_])
            kb.op("dve", lambda e: e.reciprocal(out=rstd, in_=rstd), reads=[kr_], writes=[kr_])

        def phase1(l, xin_d):
            with ExitStack() as ph:
                win = sbt(ph, [128, 8, DIN], BF16, "win")
                gpre = sbt(ph, [128, D], F32, "gpre")
                xt = [sbt(ph, [128, D], F32, "xt") for _ in range(8)]
                hb = [sbt(ph, [128, D], BF16, "hb") for _ in range(4)]
                junk = sbt(ph, [128, D], BF16, "junk")
                hT = [sbt(ph, [128, 8, 512], BF16, "hT") for _ in range(2)]
                ssq = sbt(ph, [128, 8], F32, "ssq")
                rst = sbt(ph, [128, 8], F32, "rst")
                sf = [sbt(ph, [128, 512], F32, "sf") for _ in range(6)]
                sbf = [sbt(ph, [128, 512], BF16, "sbf") for _ in range(6)]
                sgv = [sbt(ph, [128, 256], BF16, "sgv") for _ in range(4)]
                sgo = [sbt(ph, [128, 256], F32, "sgo") for _ in range(4)]
                smv = [sbt(ph, [128, 8, 65], BF16, "smv") for _ in range(4)]
                tps = [pst(ph, [128, D], BF16, "tps") for _ in range(2)]
                aps = [pst(ph, [128, 512], F32, "aps") for _ in range(5)]
                for c0_ in range(0, DIN, 944):
                    for kc in range(8):
                        dma("sp", win[:, kc, c0_:c0_ + 944], winb_d[l, kc * 128:(kc + 1) * 128, c0_:c0_ + 944], reads=[("winb", l, kc, c0_)], writes=[("win", kc, c0_)], stream="w", n=8)

                def wink(c_lo, c_hi):
                    return [("win", kc_, cb_) for kc_ in range(8) for cb_ in range(0, DIN, 944) if cb_ < c_hi and cb_ + 944 > c_lo]

                dma("sp", gpre[:], norms_d[l, 0:1, :].partition_broadcast(128), writes=["gpre"])
                for i in range(4):
                    kb.op("dve", lambda e, i=i: e.memset(smv[i][:], 1.0), writes=[("smv", i)])

                flist = [("lru", lruT_d, 0, 0, 128, F32), ("lru", lruT_d, 128, 128, 128, F32),
                         ("lru", lruT_d, 256, 256, 128, F32), ("lru", lruT_d, 384, 384, 128, F32),
                         ("gq", gqT_d, 0, 512, 128, F32), ("gk", gkT_d, 0, 640, 128, F32),
                         ("glr", glrT_d, 0, 1024, 16, BF16)]
                for i in range(4):
                    flist.append(("mq", mqT_d, i * 128, 1296 + i * 128, 128, BF16))
                for i in range(4):
                    flist.append(("mk", mkT_d, i * 128, 1808 + i * 128, 128, BF16))

                def load(t):
                    dma("sp", xt[t % 8][:], xin_d[t * 128:(t + 1) * 128, :], writes=[("xt", t % 8)], stream="x", n=4)

                NT = S // 128
                pi = 0
                ev = 0
                import os as _os
                _ng = int(_os.environ.get("P1_GROUPS", S // 512))
                _parts = int(_os.environ.get("P1_PARTS", 7))

                def chain(g):
                    for s in range(4):
                        t = g * 4 + s
                        xs = xt[t % 8]
                        hbs = hb[s]
                        c = t % 8
                        kb.op("act", lambda e, xs=xs, c=c: e.activation(out=junk[:], in_=xs[:], func=AF.Square, accum_out=ssq[:, c:c + 1]),
                              reads=[("xt", t % 8)], writes=[("p1ssq", c)])
                        rstd_from_ssq(ssq[:, c:c + 1], rst[:, c:c + 1], D, "p1", c)
                        kb.op("dve", lambda e, xs=xs, hbs=hbs, c=c: e.scalar_tensor_tensor(out=hbs[:], in0=xs[:], scalar=rst[:, c:c + 1], in1=gpre[:], op0=ALU.mult, op1=ALU.mult),
                              reads=[("xt", t % 8), ("p1rs", c), "gpre"], writes=[("hb", s)])

                def transp(g):
                    hTg_ = hT[g % 2]
                    for s in range(4):
                        t = g * 4 + s
                        hbs = hb[s]
                        tpp = tps[t % 2]
                        for kc in range(8):
                            tp(tpp[:, kc * 128:(kc + 1) * 128], hbs[:, kc * 128:(kc + 1) * 128], ident[:],
                               reads=[("hb", s), "ident"], writes=[("tps", t % 2)])
                        kb.op("act", lambda e, tpp=tpp, hTg_=hTg_, s=s: e.copy(out=hTg_[:, :, s * 128:(s + 1) * 128], in_=tpp[:].rearrange("p (k c) -> p k c", k=8)),
                              reads=[("tps", t % 2)], writes=[("hT", g % 2)])

                for t in range(8):
                    load(t)
                chain(0)
                transp(0)
                for g in range(_ng):
                    hTg = hT[g % 2]
                    if g + 1 < _ng:
                        chain(g + 1)
                    if g + 2 < _ng:
                        for s in range(4):
                            load((g + 2) * 4 + s)
                    for (nm, dst, drow, wcol, wid, dt) in (flist if _parts & 2 else []):
                        ps = aps[pi % 5]
                        pk = ("aps", pi % 5)
                        pi += 1
                        for kc in range(8):
                            mm(ps[0:wid, :], win[:, kc, wcol:wcol + wid], hTg[:, kc, :], kc == 0, kc == 7,
                               reads=wink(wcol, wcol + wid) + [("hT", g % 2)], writes=[pk])
                        if dt == F32:
                            stg = sf[ev % 6]
                            sk = ("sf", ev % 6)
                        else:
                            stg = sbf[ev % 6]
                            sk = ("sbf", ev % 6)
                        eng = "act" if ev % 2 == 0 else "dve"
                        ev += 1
                        if nm == "mq":
                            if eng == "act":
                                kb.op("act", lambda e, stg=stg, ps=ps, wid=wid: e.mul(out=stg[0:wid, :], in_=ps[0:wid, :], mul=0.125), reads=[pk], writes=[sk])
                            else:
                                kb.op("dve", lambda e, stg=stg, ps=ps, wid=wid: e.tensor_scalar(out=stg[0:wid, :], in0=ps[0:wid, :], scalar1=0.125, scalar2=None, op0=ALU.mult), reads=[pk], writes=[sk])
                        else:
                            if eng == "act":
                                kb.op("act", lambda e, stg=stg, ps=ps, wid=wid: e.copy(out=stg[0:wid, :], in_=ps[0:wid, :]), reads=[pk], writes=[sk])
                            else:
                                kb.op("dve", lambda e, stg=stg, ps=ps, wid=wid: e.tensor_copy(out=stg[0:wid, :], in_=ps[0:wid, :]), reads=[pk], writes=[sk])
                        dma("sp", dst[drow:drow + wid, g * 512:(g + 1) * 512], stg[0:wid, :], reads=[sk], stream="o1", n=12)
                    if g + 1 < _ng:
                        transp(g + 1)
                    for s in (range(4) if _parts & 4 else []):
                        t = g * 4 + s
                        _tm = int(_os.environ.get("TM_SKIP", 0))
                        if not _tm & 1:
                            ps = aps[pi % 5]
                            pk = ("aps", pi % 5)
                            pi += 1
                            ps2 = aps[pi % 5]
                            pk2 = ("aps", pi % 5)
                            pi += 1
                            for kc in range(8):
                                mm(ps[:, 0:256], hTg[:, kc, s * 128:(s + 1) * 128], win[:, kc, 768:1024], kc == 0, kc == 7,
                                   reads=wink(768, 1024) + [("hT", g % 2)], writes=[pk])
                            for kc in range(8):
                                mm(ps2[:, 0:256], hTg[:, kc, s * 128:(s + 1) * 128], win[:, kc, 1040:1296], kc == 0, kc == 7,
                                   reads=wink(1040, 1296) + [("hT", g % 2)], writes=[pk2])
                            a, b = sgv[t % 4], sgo[t % 4]
                            kb.op("act", lambda e, a=a, ps=ps: e.copy(out=a[:], in_=ps[:, 0:256]), reads=[pk], writes=[("sgv", t % 4)])
                            kb.op("dve", lambda e, b=b, ps2=ps2: e.tensor_copy(out=b[:], in_=ps2[:, 0:256]), reads=[pk2], writes=[("sgo", t % 4)])
                            dma("sp", gv_d[t * 128:(t + 1) * 128, :], a[:], reads=[("sgv", t % 4)], stream="o1", n=12)
                            dma("sp", gout_d[t * 128:(t + 1) * 128, :], b[:], reads=[("sgo", t % 4)], stream="o1", n=12)
                        if not _tm & 2:
                            ps = aps[pi % 5]
                            pk = ("aps", pi % 5)
                            pi += 1
                            for kc in range(8):
                                mm(ps[:, :], hTg[:, kc, s * 128:(s + 1) * 128], win[:, kc, 2320:2832], kc == 0, kc == 7,
                                   reads=wink(2320, 2832) + [("hT", g % 2)], writes=[pk])
                            m = smv[t % 4]
                            psv = ps[:].rearrange("p (h d) -> p h d", h=8)
                            if _tm & 4:
                                pass
                            elif t % 2:
                                kb.op("act", lambda e, m=m, psv=psv: e.copy(out=m[:, :, 0:64], in_=psv), reads=[pk], writes=[("smv", t % 4)])
                            else:
                                kb.op("dve", lambda e, m=m, psv=psv: e.tensor_copy(out=m[:, :, 0:64], in_=psv), reads=[pk], writes=[("smv", t % 4)])
                            if not _tm & 8:
                                dma("sp", mvp_d[t * 128:(t + 1) * 128, :], m[:].rearrange("p h d -> p (h d)"), reads=[("smv", t % 4)], stream="o1", n=12)
                if _os.environ.get("P1_TAILSTORE"):
                    dma("sp", lruT_d[0:128, 0:8], rst[:], reads=["p1rs"], stream="o1", n=12)
                end_phase()

        def phase2a_gen(l, ph):
            TB = 1024
            if True:
                cols = sbt(ph, [128, 2, 8], F32, "lcols")
                ccol = sbt(ph, [128, 2], F32, "ccol")
                wstage = sbt(ph, [128, 2, 2, 128], F32, "wstage")
                wbd = sbt(ph, [128, 2, 2, 128], BF16, "wbd")
                xin = [sbt(ph, [128, TB + 3], F32, "xin") for _ in range(2)]
                gin = [sbt(ph, [128, TB], F32, "gin") for _ in range(2)]
                xc = sbt(ph, [128, TB], F32, "xc")
                xcb = sbt(ph, [128, TB], BF16, "xcb")
                rr_ = sbt(ph, [128, TB], F32, "r")
                ii_ = sbt(ph, [128, TB], F32, "i")
                aa = sbt(ph, [128, TB], F32, "a")
                mmul = sbt(ph, [128, TB], F32, "mult")
                uu = sbt(ph, [128, TB], F32, "u")
                hh = [sbt(ph, [128, TB], F32, "h") for _ in range(2)]
                gt = sbt(ph, [128, TB], F32, "gt")
                gs = sbt(ph, [128, TB], F32, "gs")
                yb = [sbt(ph, [128, TB], BF16, "yb") for _ in range(2)]
                gps = [pst(ph, [128, 512], F32, "gps") for _ in range(2)]
                for h in range(2):
                    dma("sp", cols[:, h, :], lcols_d[l, h], writes=["lcols"])
                kb.op("pool", lambda e: e.memset(wstage[:], 0.0), writes=["wstage"])
                for ax, src in enumerate((lwa_d, lwx_d)):
                    for h in range(2):
                        for b in range(2):
                            dma("sp", wstage[b * 64:(b + 1) * 64, ax, h, b * 64:(b + 1) * 64], src[l, 2 * h + b],
                                reads=[], writes=["wstage"])
                kb.op("dve", lambda e: e.tensor_copy(out=wbd[:], in_=wstage[:]), reads=["wstage"], writes=["wbd"])
                kb.op("act", lambda e: e.activation(out=ccol[:], in_=cols[:, :, 7], func=AF.Exp, scale=-1.0), reads=["lcols"], writes=["ccol"])
                kb.op("act", lambda e: e.activation(out=ccol[:], in_=ccol[:], func=AF.Ln, bias=1.0), reads=["ccol"], writes=["ccol"])
                kb.op("dve", lambda e: e.tensor_scalar(out=ccol[:], in0=ccol[:], scalar1=-8.0, scalar2=None, op0=ALU.mult), reads=["ccol"], writes=["ccol"])

                nb = S // TB
                it = 0
                for h in range(2):
                    for b in range(nb):
                        t0 = b * TB
                        xi = xin[it % 2]
                        gi = gin[it % 2]
                        hcur = hh[it % 2]
                        hprev = hh[(it + 1) % 2]
                        ybs = yb[it % 2]
                        kx, kg, ky = ("xin", it % 2), ("gin", it % 2), ("yb", it % 2)
                        kh, khp = ("h", it % 2), ("h", (it + 1) % 2)
                        it += 1
                        if b == 0:
                            kb.op("pool", lambda e, xi=xi: e.memset(xi[:, 0:3], 0.0), writes=[kx])
                            dma("sp", xi[:, 3:], lruT_d[h * 128:(h + 1) * 128, 0:TB], writes=[kx], stream="x", n=3)
                        else:
                            dma("sp", xi[:], lruT_d[h * 128:(h + 1) * 128, t0 - 3:t0 + TB], writes=[kx], stream="x", n=3)
                        dma("sp", gi[:], lruT_d[256 + h * 128:256 + (h + 1) * 128, t0:t0 + TB], writes=[kg], stream="x", n=3)
                        yield
                        kb.op("dve", lambda e, xi=xi, h=h: e.tensor_scalar(out=xc[:], in0=xi[:, 3:TB + 3], scalar1=cols[:, h, 3:4], scalar2=cols[:, h, 4:5], op0=ALU.mult, op1=ALU.add),
                              reads=[kx, "lcols"], writes=["xc"])
                        for j in range(3):
                            kb.op("dve", lambda e, xi=xi, h=h, j=j: e.scalar_tensor_tensor(out=xc[:], in0=xi[:, j:TB + j], scalar=cols[:, h, j:j + 1], in1=xc[:], op0=ALU.mult, op1=ALU.add),
                                  reads=[kx, "lcols", "xc"], writes=["xc"])
                        yield
                        kb.op("pool", lambda e: e.tensor_copy(out=xcb[:], in_=xc[:]), reads=["xc"], writes=["xcb"])
                        for sblk in range(TB // 512):
                            cs = slice(sblk * 512, (sblk + 1) * 512)
                            pa, px = gps[0], gps[1]
                            ka, kx_ = ("gps", 0), ("gps", 1)
                            mm(pa[:], wbd[:, 0, h, :], xcb[:, cs], True, True, reads=["wbd", "xcb"], writes=[ka])
                            mm(px[:], wbd[:, 1, h, :], xcb[:, cs], True, True, reads=["wbd", "xcb"], writes=[kx_])
                            kb.op("act", lambda e, pa=pa, cs=cs, h=h: e.activation(out=rr_[:, cs], in_=pa[:], func=AF.Sigmoid, bias=cols[:, h, 5:6]),
                                  reads=[ka, "lcols"], writes=["r"])
                            kb.op("act", lambda e, px=px, cs=cs, h=h: e.activation(out=ii_[:, cs], in_=px[:], func=AF.Sigmoid, bias=cols[:, h, 6:7]),
                                  reads=[kx_, "lcols"], writes=["i"])
                        yield
                        kb.op("pool", lambda e, gi=gi: e.tensor_tensor(out=gt[:], in0=gi[:], in1=gi[:], op=ALU.mult), reads=[kg], writes=["gt"])
                        kb.op("pool", lambda e: e.tensor_scalar(out=gt[:], in0=gt[:], scalar1=0.044715, scalar2=1.0, op0=ALU.mult, op1=ALU.add), reads=["gt"], writes=["gt"])
                        kb.op("pool", lambda e, gi=gi: e.tensor_tensor(out=gt[:], in0=gt[:], in1=gi[:], op=ALU.mult), reads=["gt", kg], writes=["gt"])
                        kb.op("act", lambda e: e.activation(out=gs[:], in_=gt[:], func=AF.Sigmoid, scale=1.5957691216057308), reads=["gt"], writes=["gs"])
                        kb.op("pool", lambda e, gi=gi: e.tensor_tensor(out=gs[:], in0=gs[:], in1=gi[:], op=ALU.mult), reads=["gs", kg], writes=["gs"])
                        yield
                        kb.op("act", lambda e, h=h: e.activation(out=aa[:], in_=rr_[:], func=AF.Exp, scale=ccol[:, h:h + 1]), reads=["r", "ccol"], writes=["a"])
                        kb.op("pool", lambda e: e.tensor_tensor(out=mmul[:], in0=aa[:], in1=aa[:], op=ALU.mult), reads=["a"], writes=["mult"])
                        kb.op("act", lambda e: e.activation(out=mmul[:], in_=mmul[:], func=AF.Sqrt, scale=-1.0, bias=1.0), reads=["mult"], writes=["mult"])
                        yield
                        if b == 0:
                            kb.op("dve", lambda e: e.memset(mmul[:, 0:1], 1.0), reads=["mult"], writes=["mult"])
                        kb.op("dve", lambda e: e.tensor_tensor(out=uu[:], in0=ii_[:], in1=xc[:], op=ALU.mult), reads=["i", "xc"], writes=["u"])
                        kb.op("dve", lambda e: e.tensor_tensor(out=uu[:], in0=uu[:], in1=mmul[:], op=ALU.mult), reads=["u", "mult"], writes=["u"])
                        yield
                        if b == 0:
                            kb.op("dve", lambda e, hcur=hcur: e.tensor_tensor_scan(out=hcur[:], data0=aa[:], data1=uu[:], initial=0.0, op0=ALU.mult, op1=ALU.add),
                                  reads=["a", "u"], writes=[kh])
                        else:
                            kb.op("dve", lambda e, hcur=hcur, hprev=hprev: e.tensor_tensor_scan(out=hcur[:], data0=aa[:], data1=uu[:], initial=hprev[:, TB - 1:TB], op0=ALU.mult, op1=ALU.add),
                                  reads=["a", "u", khp], writes=[kh])
                        yield
                        kb.op("dve", lambda e, hcur=hcur, ybs=ybs: e.tensor_tensor(out=ybs[:], in0=hcur[:], in1=gs[:], op=ALU.mult), reads=[kh, "gs"], writes=[ky])
                        dma("sp", mixT_d[h * 128:(h + 1) * 128, t0:t0 + TB], ybs[:], reads=[ky], stream="o", n=4)
                        yield

        def phase2b_gen(l, ph):
            TB = 1024
            NCH = TB // 128
            if True:
                w2s = sbt(ph, [16, 128], F32, "w2s")
                w2b = sbt(ph, [16, 128], BF16, "w2b")
                negb = sbt(ph, [128, 1], F32, "negb")
                gn = sbt(ph, [128, 256], F32, "gn")
                tri = sbt(ph, [128, 128], F32, "tri")
                bmask = sbt(ph, [128, 256], F32, "bmask")
                hm = sbt(ph, [128, 4], F32, "hm")
                ones = sbt(ph, [128, 128], F32, "ones")
                glr = [sbt(ph, [16, TB], BF16, "glr") for _ in range(2)]
                qT = [sbt(ph, [128, TB], F32, "qT") for _ in range(2)]
                kT = [sbt(ph, [128, TB], F32, "kT") for _ in range(2)]
                vv = [sbt(ph, [128, NCH, 256], BF16, "vv") for _ in range(2)]
                go = [sbt(ph, [128, NCH, 256], F32, "go") for _ in range(2)]
                ee = sbt(ph, [128, TB], F32, "ee")
                cum = sbt(ph, [128, TB], F32, "cum")
                ex = sbt(ph, [128, TB], F32, "ex")
                dd = sbt(ph, [128, TB], F32, "dd")
                qd = sbt(ph, [128, TB], BF16, "qd")
                kdm = sbt(ph, [128, 4, TB], BF16, "kdm")
                kdec = sbt(ph, [128, TB], BF16, "kdec")
                dcol = sbt(ph, [128, NCH], F32, "dcol")
                kdtm = [sbt(ph, [128, 128], BF16, "kdtm") for _ in range(2)]
                am = [sbt(ph, [128, 4, 128], BF16, "am") for _ in range(2)]
                Sst = sbt(ph, [128, 256], F32, "Sst")
                Sbf = sbt(ph, [128, 256], BF16, "Sbf")
                kvm = sbt(ph, [128, 256], F32, "kvm")
                ob = sbt(ph, [128, NCH, 256], F32, "ob")
                osq = sbt(ph, [128, NCH, 256], F32, "osq")
                ssq = sbt(ph, [128, NCH * 4], F32, "gssq")
                rst = sbt(ph, [128, NCH * 4], F32, "grst")
                sg = sbt(ph, [128, NCH, 256], F32, "sg")
                yb = sbt(ph, [128, NCH, 256], BF16, "yb")
                yT = [sbt(ph, [128, 2, TB], BF16, "yT") for _ in range(2)]
                zps = [pst(ph, [128, 512], F32, "zps") for _ in range(1)]
                tps = pst(ph, [128, 1024], BF16, "gtps")
                aps_ = [pst(ph, [128, 512], F32, "gaps") for _ in range(1)]
                ops_ = [pst(ph, [128, 512], F32, "gops") for _ in range(2)]
                kvps = pst(ph, [128, 512], F32, "kvps")

                dma("sp", w2s[:], gw2_d[l], writes=["w2s"])
                kb.op("dve", lambda e: e.tensor_copy(out=w2b[:], in_=w2s[:]), reads=["w2s"], writes=["w2b"])
                dma("sp", negb[:], gb_d[l], writes=["negb"])
                kb.op("dve", lambda e: e.tensor_scalar(out=negb[:], in0=negb[:], scalar1=-1.0, scalar2=None, op0=ALU.mult), reads=["negb"], writes=["negb"])
                dma("sp", gn[:], gn_d[l:l + 1, :].partition_broadcast(128), writes=["gn"])
                dma("sp", tri[:], tri_d, writes=["tri"])
                dma("sp", bmask[:], bmask_d, writes=["bmask"])
                dma("sp", hm[:], hm_d, writes=["hm"])
                kb.op("pool", lambda e: e.memset(ones[:], 1.0), writes=["ones"])
                kb.op("pool", lambda e: e.memset(Sst[:], 0.0), writes=["Sst"])
                kb.op("pool", lambda e: e.memset(Sbf[:], 0.0), writes=["Sbf"])

                def load(b):
                    i = b % 2
                    t0 = b * TB
                    dma("sp", glr[i][:], glrT_d[:, t0:t0 + TB], writes=[("glr", i)], stream="x", n=3)
                    dma("sp", qT[i][:], gqT_d[:, t0:t0 + TB], writes=[("qT", i)], stream="x", n=3)
                    dma("sp", kT[i][:], gkT_d[:, t0:t0 + TB], writes=[("kT", i)], stream="x", n=3)
                    dma("sp", vv[i][:], gv_d[t0:t0 + TB, :].rearrange("(c p) f -> p c f", p=128), writes=[("vv", i)], stream="x", n=3)
                    dma("sp", go[i][:], gout_d[t0:t0 + TB, :].rearrange("(c p) f -> p c f", p=128), writes=[("go", i)], stream="x", n=3)

                nb = S // TB
                load(0)
                for b in range(nb):
                    if b + 1 < nb:
                        load(b + 1)
                    i = b % 2
                    t0 = b * TB
                    q_, k_, v_, g_, r_ = qT[i], kT[i], vv[i], go[i], glr[i]
                    kq, kk, kv, kg, kr = ("qT", i), ("kT", i), ("vv", i), ("go", i), ("glr", i)
                    for sblk in range(TB // 512):
                        cs = slice(sblk * 512, (sblk + 1) * 512)
                        zp = zps[0]
                        mm(zp[:], w2b[:], r_[:, cs], True, True, reads=["w2b", kr], writes=[("zps", 0)])
                        kb.op("act", lambda e, zp=zp, cs=cs: e.activation(out=ee[:, cs], in_=zp[:], func=AF.Exp, scale=-1.0, bias=negb[:]),
                              reads=[("zps", 0), "negb"], writes=["ee"])
                    yield
                    kb.op("act", lambda e: e.activation(out=ee[:], in_=ee[:], func=AF.Ln, bias=1.0), reads=["ee"], writes=["ee"])
                    for c in range(NCH):
                        cs = slice(c * 128, (c + 1) * 128)
                        kb.op("dve", lambda e, cs=cs: e.tensor_tensor_scan(out=cum[:, cs], data0=ones[:], data1=ee[:, cs], initial=0.0, op0=ALU.mult, op1=ALU.add),
                              reads=["ones", "ee"], writes=["cum"])
                    yield
                    kb.op("act", lambda e: e.activation(out=ex[:], in_=cum[:], func=AF.Exp, scale=-1.0 / 16.0), reads=["cum"], writes=["ex"])
                    kb.op("dve", lambda e, q_=q_: e.scalar_tensor_tensor(out=qd[:], in0=q_[:], scalar=32.0 ** -0.5, in1=ex[:], op0=ALU.mult, op1=ALU.mult),
                          reads=[kq, "ex"], writes=["qd"])
                    yield
                    kb.op("act", lambda e: e.activation(out=dcol[:], in_=cum[:].rearrange("p (c t) -> p c t", t=128)[:, :, 127], func=AF.Exp, scale=-1.0 / 16.0),
                          reads=["cum"], writes=["dcol"])
                    for c in range(NCH):
                        cs = slice(c * 128, (c + 1) * 128)
                        kb.op("pool", lambda e, cs=cs, c=c: e.tensor_scalar(out=dd[:, cs], in0=cum[:, cs], scalar1=cum[:, c * 128 + 127:c * 128 + 128], scalar2=None, op0=ALU.subtract),
                              reads=["cum"], writes=["dd"])
                    yield
                    kb.op("act", lambda e: e.activation(out=ex[:], in_=cum[:], func=AF.Exp, scale=1.0 / 16.0), reads=["cum", "qd"], writes=["ex"])
                    for hh_ in range(4):
                        kb.op("dve", lambda e, k_=k_, hh_=hh_: e.scalar_tensor_tensor(out=kdm[:, hh_, :], in0=k_[:], scalar=hm[:, hh_:hh_ + 1], in1=ex[:], op0=ALU.mult, op1=ALU.mult),
                              reads=[kk, "ex", "hm"], writes=["kdm"])
                    yield
                    kb.op("act", lambda e: e.activation(out=dd[:], in_=dd[:], func=AF.Exp, scale=1.0 / 16.0), reads=["dd"], writes=["dd"])
                    kb.op("pool", lambda e, k_=k_: e.tensor_tensor(out=kdec[:], in0=k_[:], in1=dd[:], op=ALU.mult), reads=[kk, "dd"], writes=["kdec"])
                    kb.op("act", lambda e, g_=g_: e.activation(out=sg[:], in_=g_[:], func=AF.Silu), reads=[kg], writes=["sg"])
                    def gla_s1(c):
                        cs = slice(c * 128, (c + 1) * 128)
                        j = c % 2
                        tp(tps[:, j * 128:(j + 1) * 128], kdec[:, cs], ident[:], reads=["kdec", "ident"], writes=["gtps"])
                        kb.op("act", lambda e, j=j: e.copy(out=kdtm[j][:], in_=tps[:, j * 128:(j + 1) * 128]), reads=["gtps"], writes=[("kdtm", j)])
                        ap_ = aps_[0]
                        for hh_ in range(4):
                            mm(ap_[:, hh_ * 128:(hh_ + 1) * 128], kdm[:, hh_, cs], qd[:, cs], True, True, reads=["kdm", "qd"], writes=[("gaps", 0)])
                        kb.op("dve", lambda e, ap_=ap_, j=j: e.tensor_tensor(out=am[j][:], in0=ap_[:].rearrange("p (h c) -> p h c", h=4),
                                                                             in1=tri[:].unsqueeze(1).broadcast_to([128, 4, 128]), op=ALU.mult),
                              reads=[("gaps", 0), "tri"], writes=[("am", j)])

                    def gla_s2(c):
                        cs = slice(c * 128, (c + 1) * 128)
                        j = c % 2
                        op_ = ops_[j]
                        mm(op_[:, 0:256], qd[:, cs], Sbf[:], True, True, reads=["qd", "Sbf"], writes=[("gops", j)])
                        for hh_ in range(4):
                            mm(op_[:, hh_ * 64:(hh_ + 1) * 64], am[j][:, hh_, :], v_[:, c, hh_ * 64:(hh_ + 1) * 64], False, True,
                               reads=[("am", j), kv], writes=[("gops", j)], skip_group_check=True)
                        kb.op("act", lambda e, op_=op_, c=c: e.copy(out=ob[:, c, :], in_=op_[:, 0:256]), reads=[("gops", j)], writes=["ob"])
                        mm(kvps[:, 0:256], kdtm[j][:], v_[:, c, :], True, True, reads=[("kdtm", j), kv], writes=["kvps"])
                        kb.op("dve", lambda e: e.tensor_tensor(out=kvm[:], in0=kvps[:, 0:256], in1=bmask[:], op=ALU.mult), reads=["kvps", "bmask"], writes=["kvm"])
                        kb.op("dve", lambda e, c=c: e.scalar_tensor_tensor(out=Sst[:], in0=Sst[:], scalar=dcol[:, c:c + 1], in1=kvm[:], op0=ALU.mult, op1=ALU.add),
                              reads=["Sst", "dcol", "kvm"], writes=["Sst"])
                        kb.op("pool", lambda e: e.tensor_copy(out=Sbf[:], in_=Sst[:]), reads=["Sst"], writes=["Sbf"])

                    gla_s1(0)
                    yield
                    for c in range(NCH):
                        if c + 1 < NCH:
                            gla_s1(c + 1)
                            yield
                        gla_s2(c)
                        yield
                    yield
                    kb.op("pool", lambda e: e.tensor_tensor(out=osq[:], in0=ob[:], in1=ob[:], op=ALU.mult), reads=["ob"], writes=["osq"])
                    kb.op("dve", lambda e: e.tensor_reduce(out=ssq[:], in_=osq[:].rearrange("p c (h v) -> p (c h) v", h=4), axis=AX.X, op=ALU.add),
                          reads=["osq"], writes=["p2bssq"])
                    rstd_from_ssq(ssq[:], rst[:], 64, "p2b")
                    kb.op("dve", lambda e: e.tensor_tensor(out=ob[:].rearrange("p c (h v) -> p (c h) v", h=4), in0=ob[:].rearrange("p c (h v) -> p (c h) v", h=4),
                                                           in1=rst[:].unsqueeze(2).broadcast_to([128, NCH * 4, 64]), op=ALU.mult),
                          reads=["ob", "p2brs"], writes=["ob"])
                    kb.op("pool", lambda e: e.tensor_tensor(out=sg[:], in0=sg[:], in1=gn[:].unsqueeze(1).broadcast_to([128, NCH, 256]), op=ALU.mult),
                          reads=["sg", "gn"], writes=["sg"])
                    kb.op("dve", lambda e: e.tensor_tensor(out=yb[:], in0=ob[:], in1=sg[:], op=ALU.mult), reads=["ob", "sg"], writes=["yb"])
                    yield
                    yTb = yT[b % 2]
                    for c in range(NCH):
                        for f in range(2):
                            jj = (c * 2 + f) % 4
                            tp(tps[:, jj * 128:(jj + 1) * 128], yb[:, c, f * 128:(f + 1) * 128], ident[:], reads=["yb", "ident"], writes=["gtps"])
                            kb.op("act", lambda e, jj=jj, c=c, f=f, yTb=yTb: e.copy(out=yTb[:, f, c * 128:(c + 1) * 128], in_=tps[:, jj * 128:(jj + 1) * 128]),
                                  reads=["gtps"], writes=[("yT", b % 2)])
                    dma("sp", mixT_d[256:512, t0:t0 + TB].rearrange("(f p) t -> p f t", p=128), yTb[:], reads=[("yT", b % 2)], stream="o", n=4)

        def phase2ab(l):
            with ExitStack() as ph:
                gens = [phase2a_gen(l, ph), phase2b_gen(l, ph)]
                while gens:
                    for g_ in list(gens):
                        try:
                            next(g_)
                        except StopIteration:
                            gens.remove(g_)
                end_phase()

        def phase2c(l):
            with ExitStack() as ph:
                kaug = sbt(ph, [128, 8, S], BF16, "kaug")
                vp = sbt(ph, [128, 32, 520], BF16, "vp")
                qaug = [sbt(ph, [128, 8, 512], BF16, "qaug") for _ in range(3)]
                km = sbt(ph, [64, 8, 16], F32, "km")
                kmb = sbt(ph, [64, 8, 16], BF16, "kmb")
                cm = sbt(ph, [128, 16, 16], F32, "cm")
                pm = sbt(ph, [128, 16, 16], F32, "pm")
                b31 = sbt(ph, [128, 8], F32, "b31")
                caus = sbt(ph, [128, 128], F32, "caus")
                tstage = sbt(ph, [128, 2, 8, 128], F32, "tstage")
                tdT = sbt(ph, [128, 8, 128], BF16, "tdT")
                toT = sbt(ph, [128, 8, 128], BF16, "toT")
                tomT = sbt(ph, [128, 8, 128], BF16, "tomT")
                zer = sbt(ph, [128, 260], BF16, "zer")
                gm = sbt(ph, [128, 4, 8, 16], F32, "gm")
                m8 = sbt(ph, [128, 4, 8, 8], F32, "m8")
                sel = sbt(ph, [128, 4, 8, 16], F32, "sel")
                mpad = [sbt(ph, [128, 4, 8, 80], BF16, "mpad") for _ in range(2)]
                pT = [sbt(ph, [128, 512], BF16, "pT") for _ in range(4)]
                rcp = sbt(ph, [128, 4], F32, "rcp")
                ymo = [sbt(ph, [128, 4, 512], BF16, "ymo") for _ in range(2)]
                ymT = [sbt(ph, [128, 4, 512], BF16, "ymT") for _ in range(2)]
                gps = pst(ph, [128, 512], F32, "mgps")
                mtps = [pst(ph, [128, 512], F32, "mtps") for _ in range(1)]
                mtps_b = [pst(ph, [128, 1024], BF16, "mtpsb") for _ in range(1)]
                sps = [pst(ph, [128, 512], F32, "sps") for _ in range(3)]
                accs_full = [pst(ph, [128, 512], F32, "acc") for _ in range(2)]
                accs = [a_[:, 0:260].rearrange("p (s d) -> p s d", s=4) for a_ in accs_full]

                for h in range(8):
                    dma("sp", kaug[0:64, h, :], mkT_d[h * 64:(h + 1) * 64, :], writes=["kaug"], stream="x", n=3)
                    dma("sp", kaug[64:80, h, :], e16_d, writes=["kaug"], stream="x", n=3)
                for c in range(4):
                    dma("sp", vp[:, c * 8:(c + 1) * 8, :], mvp_d[c * 1024:(c + 1) * 1024, :].rearrange("(c p) f -> p c f", p=128), writes=["vp"], stream="x", n=3)
                dma("sp", cm[:].rearrange("p a b -> p (a b)"), cm_d.partition_broadcast(128), writes=["cm"])
                dma("sp", pm[:].rearrange("p a b -> p (a b)"), pm_d.partition_broadcast(128), writes=["pm"])
                dma("sp", b31[:], rb31_d.partition_broadcast(128), writes=["b31"])
                dma("sp", caus[:], caus_d, writes=["caus"])
                dma("sp", tstage[:, 0], tdg_d, writes=["tstage"])
                dma("sp", tstage[:, 1], tof_d, writes=["tstage"])
                kb.op("dve", lambda e: e.tensor_tensor(out=tdT[:], in0=tstage[:, 0], in1=caus[:].unsqueeze(1).broadcast_to([128, 8, 128]), op=ALU.add),
                      reads=["tstage", "caus"], writes=["tdT"])
                kb.op("dve", lambda e: e.tensor_copy(out=toT[:], in_=tstage[:, 1]), reads=["tstage"], writes=["toT"])
                kb.op("dve", lambda e: e.tensor_tensor(out=tomT[:], in0=tstage[:, 1], in1=b31[:].unsqueeze(2).broadcast_to([128, 8, 128]), op=ALU.subtract),
                      reads=["tstage", "b31"], writes=["tomT"])
                kb.op("pool", lambda e: e.memset(zer[:], 0.0), writes=["zer"])
                for i in range(2):
                    kb.op("pool", lambda e, i=i: e.memset(mpad[i][:], 0.0), writes=[("mpad", i)])
                kb.op("dve", lambda e: e.tensor_reduce(out=km[:].rearrange("p h n -> p (h n)"), in_=kaug[0:64, :, :].rearrange("p h (n t) -> p (h n) t", t=256), axis=AX.X, op=ALU.add),
                      reads=["kaug"], writes=["km"])
                kb.op("dve", lambda e: e.tensor_scalar(out=kmb[:], in0=km[:], scalar1=1.0 / 256.0, scalar2=None, op0=ALU.mult), reads=["km"], writes=["kmb"])

                if l == 0:
                    casts_rest()

                def loadq(G):
                    i = G % 3
                    dma("sp", qaug[i][0:64, :, :], mqT_d.rearrange("(h d) t -> d h t", d=64)[:, :, G * 512:(G + 1) * 512], writes=[("qaug", i)], stream="q", n=3)

                NG = S // 512
                si = 0
                ai = 0

                def pre1(G):
                    qi = G % 3
                    qa = qaug[qi]
                    kqa = ("qaug", qi)
                    mp = mpad[G % 2]
                    for s in range(4):
                        for h in range(8):
                            mm(gps[:, (s * 8 + h) * 16:(s * 8 + h + 1) * 16], qa[0:64, h, s * 128:(s + 1) * 128], kmb[:, h, :], True, True,
                               reads=[kqa, "kmb"], writes=["mgps"])
                    np0 = 2 * G
                    for a in range(2):
                        cmv = cm[:, np0 + a, :].unsqueeze(1).unsqueeze(1).broadcast_to([128, 2, 8, 16])
                        kb.op("dve", lambda e, cmv=cmv, a=a: e.tensor_tensor(out=gm[:, 2 * a:2 * a + 2], in0=gps[:].rearrange("p (s h n) -> p s h n", s=4, h=8)[:, 2 * a:2 * a + 2],
                                                                             in1=cmv, op=ALU.add),
                              reads=["mgps", "cm"], writes=["gm"])
                    for s in range(4):
                        for h in range(8):
                            kb.op("dve", lambda e, s=s, h=h: e.max(out=m8[:, s, h, :], in_=gm[:, s, h, :]), reads=["gm"], writes=["m8"])
                    kb.op("dve", lambda e: e.tensor_tensor(out=sel[:], in0=gm[:], in1=m8[:, :, :, 2:3].broadcast_to([128, 4, 8, 16]), op=ALU.is_ge),
                          reads=["gm", "m8"], writes=["sel"])
                    kb.op("dve", lambda e: e.tensor_scalar(out=sel[:], in0=sel[:], scalar1=-NEG, scalar2=NEG, op0=ALU.mult, op1=ALU.add), reads=["sel"], writes=["sel"])
                    kb.op("dve", lambda e: e.tensor_tensor(out=sel[:], in0=sel[:], in1=b31[:].unsqueeze(1).unsqueeze(3).broadcast_to([128, 4, 8, 16]), op=ALU.add),
                          reads=["sel", "b31"], writes=["sel"])
                    for a in range(2):
                        pmv = pm[:, np0 + a, :].unsqueeze(1).unsqueeze(1).broadcast_to([128, 2, 8, 16])
                        kb.op("dve", lambda e, pmv=pmv, mp=mp, a=a: e.tensor_tensor(out=mp[:, 2 * a:2 * a + 2, :, 64:80], in0=sel[:, 2 * a:2 * a + 2], in1=pmv, op=ALU.mult),
                              reads=["sel", "pm"], writes=[("mpad", G % 2)])

                def pre2(G):
                    qi = G % 3
                    qa = qaug[qi]
                    kqa = ("qaug", qi)
                    mp = mpad[G % 2]
                    for h in range(8):
                        mt = mtps[0]
                        for s in range(4):
                            mm(mt[0:80, s * 128:(s + 1) * 128], mp[:, s, h, :], ident[:], True, True, reads=[("mpad", G % 2), "ident"], writes=[("mtps", 0)])
                        kb.op("act", lambda e, mt=mt, h=h, qa=qa: e.copy(out=qa[64:80, h, :], in_=mt[64:80, :]),
                              reads=[("mtps", 0)], writes=[kqa])

                loadq(0)
                if NG > 1:
                    loadq(1)
                pre1(0)
                pre2(0)
                for G in range(NG):
                    if G + 2 < NG:
                        loadq(G + 2)
                    if G + 1 < NG:
                        pre1(G + 1)
                    qi = G % 3
                    qa = qaug[qi]
                    kqa = ("qaug", qi)
                    ym = ymo[G % 2]
                    nj = 4 * G + 4
                    DEPTH = 2

                    def stageA(h, j):
                        nonlocal si
                        acc = accs[h % 2]
                        ka = ("acc", h % 2)
                        if j == 0:
                            mm(accs_full[h % 2][:, 0:260], zer[:, 0:128], zer[:, :], True, True, reads=["zer"], writes=[ka])
                        r = j - 4 * G
                        c0 = max(r, 0) * 128
                        sp_ = sps[si % 3]
                        ks = ("sps", si % 3)
                        pt = pT[si % 4]
                        kp = ("pT", si % 4)
                        si += 1
                        mm(sp_[:, c0:512], kaug[0:80, h, j * 128:(j + 1) * 128], qa[0:80, h, c0:512], True, True,
                           reads=["kaug", kqa], writes=[ks])
                        if r == -1:
                            mm(sp_[:, 0:128], ident[:], tomT[:, h, :], False, True, reads=["ident", "tomT"], writes=[ks], skip_group_check=True)
                        if r >= 0:
                            mm(sp_[:, r * 128:(r + 1) * 128], ident[:], tdT[:, h, :], False, True, reads=["ident", "tdT"], writes=[ks], skip_group_check=True)
                            if r < 3:
                                tt = toT if r % 2 == 0 else tomT
                                mm(sp_[:, (r + 1) * 128:(r + 2) * 128], ident[:], tt[:, h, :], False, True, reads=["ident", "toT", "tomT"], writes=[ks], skip_group_check=True)
                        kb.op("act", lambda e, pt=pt, sp_=sp_, c0=c0: e.activation(out=pt[:, c0:512], in_=sp_[:, c0:512], func=AF.Exp),
                              reads=[ks], writes=[kp])
                        return (h, j, r, pt, kp, acc, ka)

                    def stageB(info):
                        h, j, r, pt, kp, acc, ka = info
                        for s in range(max(r, 0), 4):
                            mm(acc[:, s, :], pt[:, s * 128:(s + 1) * 128], vp[:, j, h * 65:(h + 1) * 65], False, True,
                               reads=[kp, "vp"], writes=[ka], skip_group_check=True)
                        if j == nj - 1:
                            kb.op("dve", lambda e, acc=acc: e.reciprocal(out=rcp[:], in_=acc[:, :, 64]), reads=[ka], writes=["rcp"])
                            kb.op("dve", lambda e, acc=acc, h=h, ym=ym: e.tensor_tensor(out=ym[:, :, h * 64:(h + 1) * 64], in0=acc[:, :, 0:64],
                                                                                  in1=rcp[:].unsqueeze(2).broadcast_to([128, 4, 64]), op=ALU.mult),
                                  reads=[ka, "rcp"], writes=[("ymo", G % 2)])

                    pend = []
                    for h in range(8):
                        if h == 4 and G + 1 < NG:
                            pre2(G + 1)
                        for j in range(nj):
                            pend.append(stageA(h, j))
                            if len(pend) > DEPTH:
                                stageB(pend.pop(0))
                    while pend:
                        stageB(pend.pop(0))
                    yt = ymT[G % 2]
                    for s in range(4):
                        tpb = mtps_b[0]
                        for f in range(4):
                            tp(tpb[:, f * 128:(f + 1) * 128], ym[:, s, f * 128:(f + 1) * 128], ident[:], reads=[("ymo", G % 2), "ident"], writes=[("mtpsb", 0)])
                        kb.op("act", lambda e, tpb=tpb, yt=yt, s=s: e.copy(out=yt[:, :, s * 128:(s + 1) * 128], in_=tpb[:, 0:512].rearrange("p (f q) -> p f q", f=4)),
                              reads=[("mtpsb", 0)], writes=[("ymT", G % 2)])
                    dma("sp", mixT_d[512:1024, G * 512:(G + 1) * 512].rearrange("(f p) t -> p f t", p=128), yt[:], reads=[("ymT", G % 2)], stream="o", n=4)
                end_phase()

        def phase3(l, xin_d, xout_d):
            with ExitStack() as ph:
                wout = sbt(ph, [128, 8, D], BF16, "wout")
                wdn = sbt(ph, [128, NFC, D], BF16, "wdn")
                g3 = sbt(ph, [128, 3, D], F32, "g3")
                wgu = [sbt(ph, [128, 2, 8, 128], BF16, "wgu") for _ in range(4)]
                mixT = [sbt(ph, [128, 8, 512], BF16, "mixT") for _ in range(2)]
                xt = [sbt(ph, [128, D], F32, "xt3") for _ in range(2)]
                x1 = [sbt(ph, [128, D], F32, "x1") for _ in range(8)]
                tmp = [sbt(ph, [128, D], F32, "tmp3") for _ in range(2)]
                junk = sbt(ph, [128, D], BF16, "junk3")
                hb = [sbt(ph, [128, D], BF16, "hb3") for _ in range(4)]
                hT = sbt(ph, [128, 8, 512], BF16, "hT3")
                actT = sbt(ph, [128, NFC, 512], BF16, "actT")
                sgt = [sbt(ph, [128, 512], F32, "sgt") for _ in range(2)]
                ssq = sbt(ph, [128, 16], F32, "ssq3")
                rst = sbt(ph, [128, 16], F32, "rst3")
                xo = [sbt(ph, [128, D], F32, "xo") for _ in range(2)]
                ops_ = [pst(ph, [128, 2, 512], F32, "p3o") for _ in range(2)]
                tps = pst(ph, [128, D], BF16, "p3t")
                gus = [pst(ph, [128, 512], F32, "p3gu") for _ in range(3)]
                for kc in range(8):
                    dma("sp", wout[:, kc, :], woutb_d[l, kc * 128:(kc + 1) * 128, :], writes=["wout"], stream="w", n=4)
                for fc in range(NFC):
                    dma("sp", wdn[:, fc, :], wdb_d[l, fc * 128:(fc + 1) * 128, :], writes=["wdn"], stream="w", n=4)
                for i in range(3):
                    dma("sp", g3[:, i, :], norms_d[l, i + 1:i + 2, :].partition_broadcast(128), writes=["g3"])

                wi = [0]

                def loadw(fc):
                    i = wi[0] % 4
                    wi[0] += 1
                    dma("sp", wgu[i][:], wgub_d[l, fc], writes=[("wgu", i)], stream="wgu", n=4)
                    return i

                NG = S // 512
                sq = 0

                def loadg(G):
                    i = G % 2
                    dma("sp", mixT[i][:], mixT_d[:, G * 512:(G + 1) * 512].rearrange("(k p) t -> p k t", p=128), writes=[("mixT", i)], stream="m", n=2)

                def loadx(t):
                    dma("sp", xt[t % 2][:], xin_d[t * 128:(t + 1) * 128, :], writes=[("xt3", t % 2)], stream="x", n=3)

                loadg(0)
                loadx(0)
                wq = []
                PRE = 3
                def p3_front(G):
                    nonlocal sq
                    mT = mixT[G % 2]
                    for s in range(4):
                        t = G * 4 + s
                        if t + 1 < S // 128:
                            loadx(t + 1)
                        xs = xt[t % 2]
                        x1s = x1[(G % 2) * 4 + s]
                        op_ = ops_[s % 2]
                        ko = ("p3o", s % 2)
                        tm_ = tmp[s % 2]
                        kt = ("tmp3", s % 2)
                        for hf in range(2):
                            for kc in range(8):
                                mm(op_[:, hf, :], mT[:, kc, s * 128:(s + 1) * 128], wout[:, kc, hf * 512:(hf + 1) * 512], kc == 0, kc == 7,
                                   reads=[("mixT", G % 2), "wout"], writes=[ko])
                        c = sq % 16
                        sq += 1
                        kb.op("act", lambda e, c=c, op_=op_: e.activation(out=junk[:], in_=op_[:].rearrange("p a b -> p (a b)"), func=AF.Square, accum_out=ssq[:, c:c + 1]),
                              reads=[ko], writes=[("p3ssq", c)])
                        rstd_from_ssq(ssq[:, c:c + 1], rst[:, c:c + 1], D, "p3", c)
                        kb.op("dve", lambda e, c=c, op_=op_, tm_=tm_: e.scalar_tensor_tensor(out=tm_[:], in0=op_[:].rearrange("p a b -> p (a b)"), scalar=rst[:, c:c + 1], in1=g3[:, 0, :], op0=ALU.mult, op1=ALU.mult),
                              reads=[ko, ("p3rs", c), "g3"], writes=[kt])
                        kb.op("pool", lambda e, xs=xs, x1s=x1s, tm_=tm_: e.tensor_tensor(out=x1s[:], in0=xs[:], in1=tm_[:], op=ALU.add),
                              reads=[("xt3", t % 2), kt], writes=[("x1", (G % 2) * 4 + s)])
                        c2 = sq % 16
                        sq += 1
                        kb.op("act", lambda e, c2=c2, x1s=x1s: e.activation(out=junk[:], in_=x1s[:], func=AF.Square, accum_out=ssq[:, c2:c2 + 1]),
                              reads=[("x1", (G % 2) * 4 + s)], writes=[("p3ssq", c2)])
                        rstd_from_ssq(ssq[:, c2:c2 + 1], rst[:, c2:c2 + 1], D, "p3", c2)
                        hbs = hb[s]
                        kb.op("dve", lambda e, c2=c2, x1s=x1s, hbs=hbs: e.scalar_tensor_tensor(out=hbs[:], in0=x1s[:], scalar=rst[:, c2:c2 + 1], in1=g3[:, 1, :], op0=ALU.mult, op1=ALU.mult),
                              reads=[("x1", (G % 2) * 4 + s), ("p3rs", c2), "g3"], writes=[("hb3", s)])

                def p3_trans(G):
                    for s in range(4):
                        hbs = hb[s]
                        for kc in range(8):
                            tp(tps[:, kc * 128:(kc + 1) * 128], hbs[:, kc * 128:(kc + 1) * 128], ident[:], reads=[("hb3", s), "ident"], writes=["p3t"])
                        kb.op("act", lambda e, s=s: e.copy(out=hT[:, :, s * 128:(s + 1) * 128], in_=tps[:].rearrange("p (k c) -> p k c", k=8)),
                              reads=["p3t"], writes=["hT3"])

                def p3_gateup(G):
                    for fc in range(NFC):
                        wslot = wq.pop(0)
                        nxt = fc + PRE
                        if nxt < NFC:
                            wq.append(loadw(nxt))
                        w_ = wgu[wslot]
                        gp, up = gus[(2 * fc) % 3], gus[(2 * fc + 1) % 3]
                        kgp, kup = ("p3gu", (2 * fc) % 3), ("p3gu", (2 * fc + 1) % 3)
                        for kc in range(8):
                            mm(gp[:], w_[:, 0, kc, :], hT[:, kc, :], kc == 0, kc == 7, reads=[("wgu", wslot), "hT3"], writes=[kgp])
                        for kc in range(8):
                            mm(up[:], w_[:, 1, kc, :], hT[:, kc, :], kc == 0, kc == 7, reads=[("wgu", wslot), "hT3"], writes=[kup])
                        sg_ = sgt[fc % 2]
                        kb.op("act", lambda e, sg_=sg_, gp=gp: e.activation(out=sg_[:], in_=gp[:], func=AF.Silu), reads=[kgp], writes=[("sgt", fc % 2)])
                        kb.op("dve", lambda e, sg_=sg_, up=up, fc=fc: e.tensor_tensor(out=actT[:, fc, :], in0=up[:], in1=sg_[:], op=ALU.mult),
                              reads=[kup, ("sgt", fc % 2)], writes=["actT"])

                def p3_down(G):
                    nonlocal sq
                    for s in range(4):
                        t = G * 4 + s
                        op_ = ops_[s % 2]
                        ko = ("p3o", s % 2)
                        tm_ = tmp[s % 2]
                        kt = ("tmp3", s % 2)
                        for hf in range(2):
                            for fc in range(NFC):
                                mm(op_[:, hf, :], actT[:, fc, s * 128:(s + 1) * 128], wdn[:, fc, hf * 512:(hf + 1) * 512], fc == 0, fc == NFC - 1,
                                   reads=["actT", "wdn"], writes=[ko])
                        c = sq % 16
                        sq += 1
                        kb.op("act", lambda e, c=c, op_=op_: e.activation(out=junk[:], in_=op_[:].rearrange("p a b -> p (a b)"), func=AF.Square, accum_out=ssq[:, c:c + 1]),
                              reads=[ko], writes=[("p3ssq", c)])
                        rstd_from_ssq(ssq[:, c:c + 1], rst[:, c:c + 1], D, "p3", c)
                        kb.op("dve", lambda e, c=c, op_=op_, tm_=tm_: e.scalar_tensor_tensor(out=tm_[:], in0=op_[:].rearrange("p a b -> p (a b)"), scalar=rst[:, c:c + 1], in1=g3[:, 2, :], op0=ALU.mult, op1=ALU.mult),
                              reads=[ko, ("p3rs", c), "g3"], writes=[kt])
                        xos = xo[t % 2]
                        kb.op("pool", lambda e, xos=xos, s=s, tm_=tm_: e.tensor_tensor(out=xos[:], in0=x1[(G % 2) * 4 + s][:], in1=tm_[:], op=ALU.add),
                              reads=[("x1", (G % 2) * 4 + s), kt], writes=[("xo", t % 2)])
                        dma("sp", xout_d[t * 128:(t + 1) * 128, :], xos[:], reads=[("xo", t % 2)], stream="o", n=4)

                for G in range(NG):
                    if G + 1 < NG:
                        loadg(G + 1)
                    while len(wq) < PRE:
                        wq.append(loadw(len(wq)))
                    p3_front(G)
                    if G > 0:
                        p3_down(G - 1)
                    p3_trans(G)
                    p3_gateup(G)
                p3_down(NG - 1)
                end_phase()

        kb.barrier()
        import os as _os2
        if not _os2.environ.get("SKIP_P0"):
            phase0()
        done = stop_after == "p0"
        for l in range(L):
            if done:
                break
            xin = x_d if l == 0 else xs1_d
            xout = xs1_d if l == 0 else out_d
            for nm, fn in (("p1", lambda: phase1(l, xin)), ("p2b", lambda: phase2ab(l)),
                           ("p2c", lambda: phase2c(l)), ("p3", lambda: phase3(l, xin, xout))):
                fn()
                if stop_after == (l, nm):
                    done = True
                    break
            if done:
                break
        kb.barrier()
        kb.emit()

    return nc


def _host_inputs(inputs):
    f = lambda a: np.ascontiguousarray(np.asarray(a, dtype=np.float32))
    c = _consts()
    idx_diag, idx_off1 = _bias_idx()
    rel = f(inputs["rel_bias"])
    shared = {
        "norms": f(np.stack([inputs["pre_mix_norm"], inputs["post_mix_norm"], inputs["pre_ffn_norm"], inputs["post_ffn_norm"]], axis=1)),
        "w_in": f(inputs["w_in"]), "w_out": f(inputs["w_out"]),
        "w_ffn_gate": f(inputs["w_ffn_gate"]), "w_ffn_up": f(inputs["w_ffn_up"]), "w_ffn_down": f(inputs["w_ffn_down"]),
        "lru_wa": f(inputs["lru_wa"]), "lru_wx": f(inputs["lru_wx"]),
        "gla_gate_w2": f(inputs["gla_gate_w2"]),
        "gla_gate_b": f(np.asarray(inputs["gla_gate_b"]).reshape(L, 128, 1)),
        "gla_norm": f(inputs["gla_norm"]),
        "rb31": f(rel[31:32, :]),
        "tdg": f(np.transpose(rel[idx_diag], (0, 2, 1))),
        "tof": f(np.transpose(rel[idx_off1], (0, 2, 1))),
        "ident": c["ident"], "tri": c["tri"], "caus": c["caus"], "e16": c["e16"],
        "cm": c["cm"], "pm": c["pm"], "bmask": c["bmask"], "hm": c["hm"],
    }
    cw = np.transpose(np.asarray(inputs["lru_conv_w"], dtype=np.float32), (0, 2, 1))
    cols = np.concatenate([cw] + [np.asarray(inputs[k], dtype=np.float32)[:, :, None]
                                  for k in ("lru_conv_b", "lru_ba", "lru_bx", "lru_lambda")], axis=2)
    shared["lru_cols"] = f(cols.reshape(L, 2, 128, 8))
    x = np.asarray(inputs["x"], dtype=np.float32)
    return [dict(shared, x=np.ascontiguousarray(x[b])) for b in range(x.shape[0])]


_NC_CACHE = {}


def kernel(**inputs):
    in_maps = _host_inputs(inputs)
    if "nc" not in _NC_CACHE:
        _NC_CACHE["nc"] = build()
    nc = _NC_CACHE["nc"]
    n = len(in_maps)
    res = run_bass_kernel_spmd(nc, in_maps, core_ids=list(range(n)))
    return np.stack([np.asarray(r["out"], dtype=np.float32) for r in res.results], axis=0)
```

```python
from contextlib import ExitStack
import math
import numpy as np
import ml_dtypes
import concourse.bass as bass
import concourse.mybir as mybir
from concourse.bass_utils import run_bass_kernel_spmd

F32 = mybir.dt.float32
BF16 = mybir.dt.bfloat16
ALU = mybir.AluOpType
AF = mybir.ActivationFunctionType
AX = mybir.AxisListType

S = 4096
D = 1024
L = 2
DIN = 2832
DFF = 2816
NFC = DFF // 128
EPS = 1e-6
NEG = -30000.0
ENGS = ("pe", "act", "dve", "pool", "sp")


class KB:
    def __init__(self, nc, stack, sync_same=True):
        self.nc = nc
        self.stack = stack
        self.sync_same = sync_same
        self.ops = {e: [] for e in ENGS}
        self.sem = {}
        self.cnt = {}
        self.step = {}
        self.known = {e: {} for e in ENGS}
        self.lw = {}
        self.rd = {}
        for e in ENGS:
            self._dom(e, 1)

    def _dom(self, name, step):
        if name not in self.sem:
            self.sem[name] = self.stack.enter_context(self.nc.semaphore("s_" + name))
            self.cnt[name] = 0
            self.step[name] = step
        return name

    def op(self, eng, fn, reads=(), writes=(), dma=None):
        dom = eng if dma is None else self._dom("d_" + dma, 16)
        deps = {}

        def add(d):
            if d is not None and deps.get(d[0], 0) < d[1]:
                deps[d[0]] = d[1]

        for k in reads:
            add(self.lw.get(k))
        for k in writes:
            add(self.lw.get(k))
            for dm, c in self.rd.get(k, {}).items():
                add((dm, c))
        if dma is not None and self.cnt[dom] > 0:
            add((dom, self.cnt[dom]))
        kn = self.known[eng]
        for d, c in deps.items():
            if d == eng and (eng == "pe" or not self.sync_same):
                continue
            if kn.get(d, 0) >= c:
                continue
            self.ops[eng].append(("w", self.sem[d], c))
            kn[d] = c
        self.cnt[dom] += self.step[dom]
        me = (dom, self.cnt[dom])
        self.ops[eng].append(("o", fn, self.sem[dom], self.step[dom]))
        for k in writes:
            self.lw[k] = me
            self.rd[k] = {}
        for k in reads:
            r = self.rd.setdefault(k, {})
            if r.get(dom, 0) < me[1]:
                r[dom] = me[1]
        return me

    def barrier(self):
        for eng in ENGS:
            kn = self.known[eng]
            for dom, c in self.cnt.items():
                if c > 0 and dom != eng and kn.get(dom, 0) < c:
                    self.ops[eng].append(("w", self.sem[dom], c))
                    kn[dom] = c
        self.lw = {}
        self.rd = {}

    def emit(self):
        nc = self.nc
        ops = self.ops

        def run(lst, e):
            for it in lst:
                if it[0] == "w":
                    e.wait_ge(it[1], it[2])
                else:
                    it[1](e).then_inc(it[2], it[3])

        with nc.Block() as block:
            @block.tensor
            def _(e):
                run(ops["pe"], e)

            @block.scalar
            def _(e):
                run(ops["act"], e)

            @block.vector
            def _(e):
                run(ops["dve"], e)

            @block.gpsimd
            def _(e):
                run(ops["pool"], e)

            @block.sync
            def _(e):
                run(ops["sp"], e)
        self.ops = {e: [] for e in ENGS}


def _t5_bucket(n):
    n = np.maximum(n, 0)
    nf = np.maximum(n, 1).astype(np.float32)
    large = 16 + (np.log(nf / np.float32(16)) / np.float32(math.log(128 / 16)) * np.float32(16)).astype(np.int32)
    large = np.minimum(large, 31)
    return np.where(n < 16, n, large)


def _consts():
    c = {}
    c["ident"] = np.eye(128, dtype=np.float32).astype(ml_dtypes.bfloat16)
    e = np.arange(128)
    c["tri"] = (e[:, None] <= e[None, :]).astype(np.float32)
    c["caus"] = np.where(e[None, :] >= e[:, None], 0.0, NEG).astype(np.float32)
    keys = np.arange(S)
    c["e16"] = (keys[None, :] // 256 == np.arange(16)[:, None]).astype(np.float32).astype(ml_dtypes.bfloat16)
    npast = np.arange(16)[:, None]
    nn = np.arange(16)[None, :]
    c["cm"] = np.where(nn < npast, 0.0, -1e30).astype(np.float32).reshape(1, 256)
    c["pm"] = (nn < npast).astype(np.float32).reshape(1, 256)
    p = np.arange(128)[:, None]
    c["bmask"] = (p // 32 == (np.arange(256)[None, :] // 64)).astype(np.float32)
    c["hm"] = (p // 32 == np.arange(4)[None, :]).astype(np.float32)
    return c


def _bias_idx():
    k = np.arange(128)[:, None]
    q = np.arange(128)[None, :]
    idx_diag = _t5_bucket(q - k)
    idx_off1 = _t5_bucket(q + 128 - k)
    return idx_diag, idx_off1


def build(debug=False, stop_after=None):
    nc = bass.Bass("TRN2", target_bir_lowering=False)
    dr = lambda name, shape, dt, kind="Internal": nc.dram_tensor(name, list(shape), dt, kind=kind).ap()
    IN = "ExternalInput"
    x_d = dr("x", [S, D], F32, IN)
    norms_d = dr("norms", [L, 4, D], F32, IN)
    w_in_d = dr("w_in", [L, D, DIN], F32, IN)
    w_out_d = dr("w_out", [L, D, D], F32, IN)
    wg_d = dr("w_ffn_gate", [L, D, DFF], F32, IN)
    wu_d = dr("w_ffn_up", [L, D, DFF], F32, IN)
    wd_d = dr("w_ffn_down", [L, DFF, D], F32, IN)
    lcols_d = dr("lru_cols", [L, 2, 128, 8], F32, IN)
    lwa_d = dr("lru_wa", [L, 4, 64, 64], F32, IN)
    lwx_d = dr("lru_wx", [L, 4, 64, 64], F32, IN)
    gw2_d = dr("gla_gate_w2", [L, 16, 128], F32, IN)
    gb_d = dr("gla_gate_b", [L, 128, 1], F32, IN)
    gn_d = dr("gla_norm", [L, 256], F32, IN)
    rb31_d = dr("rb31", [1, 8], F32, IN)
    tdg_d = dr("tdg", [128, 8, 128], F32, IN)
    tof_d = dr("tof", [128, 8, 128], F32, IN)
    ident_d = dr("ident", [128, 128], BF16, IN)
    tri_d = dr("tri", [128, 128], F32, IN)
    caus_d = dr("caus", [128, 128], F32, IN)
    e16_d = dr("e16", [16, S], BF16, IN)
    cm_d = dr("cm", [1, 256], F32, IN)
    pm_d = dr("pm", [1, 256], F32, IN)
    bmask_d = dr("bmask", [128, 256], F32, IN)
    hm_d = dr("hm", [128, 4], F32, IN)
    out_d = dr("out", [S, D], F32, "ExternalOutput")

    dk = "ExternalOutput" if debug else "Internal"
    winb_d = dr("winb", [L, D, DIN], BF16)
    woutb_d = dr("woutb", [L, D, D], BF16)
    wgub_d = dr("wgub", [L, NFC, 128, 2, 8, 128], BF16)
    wdb_d = dr("wdb", [L, DFF, D], BF16)
    xs1_d = dr("xs1", [S, D], F32, dk)
    lruT_d = dr("lruT", [512, S], F32, dk)
    gqT_d = dr("gqT", [128, S], F32, dk)
    gkT_d = dr("gkT", [128, S], F32, dk)
    glrT_d = dr("glrT", [16, S], BF16, dk)
    mqT_d = dr("mqT", [512, S], BF16, dk)
    mkT_d = dr("mkT", [512, S], BF16, dk)
    gv_d = dr("gv", [S, 256], BF16, dk)
    gout_d = dr("gout", [S, 256], F32, dk)
    mvp_d = dr("mvp", [S, 520], BF16, dk)
    mixT_d = dr("mixT", [D, S], BF16, dk)

    with ExitStack() as st:
        kb = KB(nc, st)
        uid = [0]

        def sbt(ctx, shape, dt, name=None):
            uid[0] += 1
            return ctx.enter_context(nc.sbuf_tensor("%s_%d" % (name or "t", uid[0]), list(shape), dt))

        def pst(ctx, shape, dt, name=None):
            uid[0] += 1
            return ctx.enter_context(nc.psum_tensor("%s_%d" % (name or "p", uid[0]), list(shape), dt))

        rr = {}

        def dmaname(stream, n):
            i = rr.get(stream, 0)
            rr[stream] = i + 1
            return "%s%d" % (stream, i % n)

        def dma(eng, out, in_, reads=(), writes=(), stream="g", n=4):
            kb.op(eng, lambda e: e.dma_start(out=out, in_=in_), reads=reads, writes=writes, dma=dmaname(stream, n))

        def mm(out, lhsT, rhs, start, stop, reads, writes, **kw):
            kb.op("pe", lambda e: e.matmul(out, lhsT=lhsT, rhs=rhs, start=start, stop=stop, **kw),
                  reads=reads, writes=writes)

        def tp(out, in_, ident, reads, writes):
            kb.op("pe", lambda e: e.transpose(out, in_, ident), reads=reads, writes=writes)

        ident = sbt(st, [128, 128], BF16, "ident")
        dma("sp", ident[:], ident_d, writes=["ident"])

        def end_phase():
            kb.barrier()
            kb.emit()

        def phase0():
            l = 0
            for c0 in range(0, DIN, 944):
                for kc in range(8):
                    r0 = kc * 128
                    dma("pool", winb_d[l, r0:r0 + 128, c0:c0 + 944], w_in_d[l, r0:r0 + 128, c0:c0 + 944], writes=[("winb", l, kc, c0)], stream="cast", n=4)

        def casts_rest():
            for l in range(L):
                for kc in range(8):
                    r0 = kc * 128
                    dma("pool", woutb_d[l, r0:r0 + 128, :], w_out_d[l, r0:r0 + 128, :], stream="cast", n=4)
                for fc in range(NFC):
                    for gu, wsrc in enumerate((wg_d, wu_d)):
                        dma("pool", wgub_d[l, fc, :, gu, :, :],
                            wsrc[l].rearrange("(kc p) f -> p kc f", p=128)[:, :, fc * 128:(fc + 1) * 128],
                            stream="cast", n=4)
                    dma("pool", wdb_d[l, fc * 128:(fc + 1) * 128, :], wd_d[l, fc * 128:(fc + 1) * 128, :], stream="cast", n=4)
            l = 1
            for c0 in range(0, DIN, 944):
                for kc in range(8):
                    r0 = kc * 128
                    dma("pool", winb_d[l, r0:r0 + 128, c0:c0 + 944], w_in_d[l, r0:r0 + 128, c0:c0 + 944], stream="cast", n=4)

        def rstd_from_ssq(ssq, rstd, n, tag, col=None):
            ks_ = tag + "ssq" if col is None else (tag + "ssq", col)
            kr_ = tag + "rs" if col is None else (tag + "rs", col)
            kb.op("dve", lambda e: e.tensor_scalar(out=rstd, in0=ssq, scalar1=1.0 / n, scalar2=EPS, op0=ALU.mult, op1=ALU.add),
                  reads=[ks_], writes=[kr_])
            kb.op("act", lambda e: e.sqrt(out=rstd, in_=rstd), reads=[kr_], writes=[kr_])
            kb.op("dve", lambda e: e.reciprocal(out=rstd, in_=rstd), reads=[kr_], writes=[kr_])

        def phase1(l, xin_d):
            with ExitStack() as ph:
                win = sbt(ph, [128, 8, DIN], BF16, "win")
                gpre = sbt(ph, [128, D], F32, "gpre")
                xt = [sbt(ph, [128, D], F32, "xt") for _ in range(8)]
                hb = [sbt(ph, [128, D], BF16, "hb") for _ in range(4)]
                junk = sbt(ph, [128, D], BF16, "junk")
                hT = [sbt(ph, [128, 8, 512], BF16, "hT") for _ in range(2)]
                ssq = sbt(ph, [128, 8], F32, "ssq")
                rst = sbt(ph, [128, 8], F32, "rst")
                sf = [sbt(ph, [128, 512], F32, "sf") for _ in range(6)]
                sbf = [sbt(ph, [128, 512], BF16, "sbf") for _ in range(6)]
                sgv = [sbt(ph, [128, 256], BF16, "sgv") for _ in range(4)]
                sgo = [sbt(ph, [128, 256], F32, "sgo") for _ in range(4)]
                smv = [sbt(ph, [128, 8, 65], BF16, "smv") for _ in range(4)]
                tps = [pst(ph, [128, D], BF16, "tps") for _ in range(2)]
                aps = [pst(ph, [128, 512], F32, "aps") for _ in range(5)]

                def wink(c_lo, c_hi):
                    return [("win", kc_, cb_) for kc_ in range(8) for cb_ in range(0, DIN, 944) if cb_ < c_hi and cb_ + 944 > c_lo]

                dma("sp", gpre[:], norms_d[l, 0:1, :].partition_broadcast(128), writes=["gpre"])
                for i in range(4):
                    kb.op("dve", lambda e, i=i: e.memset(smv[i][:], 1.0), writes=[("smv", i)])

                flist = [("lru", lruT_d, 0, 0, 128, F32), ("lru", lruT_d, 128, 128, 128, F32),
                         ("lru", lruT_d, 256, 256, 128, F32), ("lru", lruT_d, 384, 384, 128, F32),
                         ("gq", gqT_d, 0, 512, 128, F32), ("gk", gkT_d, 0, 640, 128, F32),
                         ("glr", glrT_d, 0, 1024, 16, BF16)]
                for i in range(4):
                    flist.append(("mq", mqT_d, i * 128, 1296 + i * 128, 128, BF16))
                for i in range(4):
                    flist.append(("mk", mkT_d, i * 128, 1808 + i * 128, 128, BF16))

                def load(t):
                    dma("sp", xt[t % 8][:], xin_d[t * 128:(t + 1) * 128, :], writes=[("xt", t % 8)], stream="x", n=4)

                NT = S // 128
                pi = 0
                ev = 0
                import os as _os
                _ng = int(_os.environ.get("P1_GROUPS", S // 512))
                _parts = int(_os.environ.get("P1_PARTS", 7))

                def chain(g):
                    for s in range(4):
                        t = g * 4 + s
                        xs = xt[t % 8]
                        hbs = hb[s]
                        c = t % 8
                        kb.op("act", lambda e, xs=xs, c=c: e.activation(out=junk[:], in_=xs[:], func=AF.Square, accum_out=ssq[:, c:c + 1]),
                              reads=[("xt", t % 8)], writes=[("p1ssq", c)])
                        rstd_from_ssq(ssq[:, c:c + 1], rst[:, c:c + 1], D, "p1", c)
                        kb.op("dve", lambda e, xs=xs, hbs=hbs, c=c: e.scalar_tensor_tensor(out=hbs[:], in0=xs[:], scalar=rst[:, c:c + 1], in1=gpre[:], op0=ALU.mult, op1=ALU.mult),
                              reads=[("xt", t % 8), ("p1rs", c), "gpre"], writes=[("hb", s)])

                def transp(g):
                    hTg_ = hT[g % 2]
                    for s in range(4):
                        t = g * 4 + s
                        hbs = hb[s]
                        tpp = tps[t % 2]
                        for kc in range(8):
                            tp(tpp[:, kc * 128:(kc + 1) * 128], hbs[:, kc * 128:(kc + 1) * 128], ident[:],
                               reads=[("hb", s), "ident"], writes=[("tps", t % 2)])
                        kb.op("act", lambda e, tpp=tpp, hTg_=hTg_, s=s: e.copy(out=hTg_[:, :, s * 128:(s + 1) * 128], in_=tpp[:].rearrange("p (k c) -> p k c", k=8)),
                              reads=[("tps", t % 2)], writes=[("hT", g % 2)])

                for t in range(8):
                    load(t)
                for c0_ in range(0, DIN, 944):
                    for kc in range(8):
                        dma("sp", win[:, kc, c0_:c0_ + 944], winb_d[l, kc * 128:(kc + 1) * 128, c0_:c0_ + 944], reads=[("winb", l, kc, c0_)], writes=[("win", kc, c0_)], stream="w", n=8)
                chain(0)
                transp(0)
                for g in range(_ng):
                    hTg = hT[g % 2]
                    if g + 1 < _ng:
                        chain(g + 1)
                    if g + 2 < _ng:
                        for s in range(4):
                            load((g + 2) * 4 + s)
                    for (nm, dst, drow, wcol, wid, dt) in (flist if _parts & 2 else []):
                        ps = aps[pi % 5]
                        pk = ("aps", pi % 5)
                        pi += 1
                        for kc in range(8):
                            mm(ps[0:wid, :], win[:, kc, wcol:wcol + wid], hTg[:, kc, :], kc == 0, kc == 7,
                               reads=wink(wcol, wcol + wid) + [("hT", g % 2)], writes=[pk])
                        if dt == F32:
                            stg = sf[ev % 6]
                            sk = ("sf", ev % 6)
                        else:
                            stg = sbf[ev % 6]
                            sk = ("sbf", ev % 6)
                        eng = "act" if ev % 2 == 0 else "dve"
                        ev += 1
                        if nm == "mq":
                            if eng == "act":
                                kb.op("act", lambda e, stg=stg, ps=ps, wid=wid: e.mul(out=stg[0:wid, :], in_=ps[0:wid, :], mul=0.125), reads=[pk], writes=[sk])
                            else:
                                kb.op("dve", lambda e, stg=stg, ps=ps, wid=wid: e.tensor_scalar(out=stg[0:wid, :], in0=ps[0:wid, :], scalar1=0.125, scalar2=None, op0=ALU.mult), reads=[pk], writes=[sk])
                        else:
                            if eng == "act":
                                kb.op("act", lambda e, stg=stg, ps=ps, wid=wid: e.copy(out=stg[0:wid, :], in_=ps[0:wid, :]), reads=[pk], writes=[sk])
                            else:
                                kb.op("dve", lambda e, stg=stg, ps=ps, wid=wid: e.tensor_copy(out=stg[0:wid, :], in_=ps[0:wid, :]), reads=[pk], writes=[sk])
                        dma("sp", dst[drow:drow + wid, g * 512:(g + 1) * 512], stg[0:wid, :], reads=[sk], stream="o1", n=12)
                    if g + 1 < _ng:
                        transp(g + 1)
                    for s in (range(4) if _parts & 4 else []):
                        t = g * 4 + s
                        _tm = int(_os.environ.get("TM_SKIP", 0))
                        if not _tm & 1:
                            ps = aps[pi % 5]
                            pk = ("aps", pi % 5)
                            pi += 1
                            ps2 = aps[pi % 5]
                            pk2 = ("aps", pi % 5)
                            pi += 1
                            for kc in range(8):
                                mm(ps[:, 0:256], hTg[:, kc, s * 128:(s + 1) * 128], win[:, kc, 768:1024], kc == 0, kc == 7,
                                   reads=wink(768, 1024) + [("hT", g % 2)], writes=[pk])
                            for kc in range(8):
                                mm(ps2[:, 0:256], hTg[:, kc, s * 128:(s + 1) * 128], win[:, kc, 1040:1296], kc == 0, kc == 7,
                                   reads=wink(1040, 1296) + [("hT", g % 2)], writes=[pk2])
                            a, b = sgv[t % 4], sgo[t % 4]
                            kb.op("act", lambda e, a=a, ps=ps: e.copy(out=a[:], in_=ps[:, 0:256]), reads=[pk], writes=[("sgv", t % 4)])
                            kb.op("dve", lambda e, b=b, ps2=ps2: e.tensor_copy(out=b[:], in_=ps2[:, 0:256]), reads=[pk2], writes=[("sgo", t % 4)])
                            dma("sp", gv_d[t * 128:(t + 1) * 128, :], a[:], reads=[("sgv", t % 4)], stream="o1", n=12)
                            dma("sp", gout_d[t * 128:(t + 1) * 128, :], b[:], reads=[("sgo", t % 4)], stream="o1", n=12)
                        if not _tm & 2:
                            ps = aps[pi % 5]
                            pk = ("aps", pi % 5)
                            pi += 1
                            for kc in range(8):
                                mm(ps[:, :], hTg[:, kc, s * 128:(s + 1) * 128], win[:, kc, 2320:2832], kc == 0, kc == 7,
                                   reads=wink(2320, 2832) + [("hT", g % 2)], writes=[pk])
                            m = smv[t % 4]
                            psv = ps[:].rearrange("p (h d) -> p h d", h=8)
                            if _tm & 4:
                                pass
                            elif t % 2:
                                kb.op("act", lambda e, m=m, psv=psv: e.copy(out=m[:, :, 0:64], in_=psv), reads=[pk], writes=[("smv", t % 4)])
                            else:
                                kb.op("dve", lambda e, m=m, psv=psv: e.tensor_copy(out=m[:, :, 0:64], in_=psv), reads=[pk], writes=[("smv", t % 4)])
                            if not _tm & 8:
                                dma("sp", mvp_d[t * 128:(t + 1) * 128, :], m[:].rearrange("p h d -> p (h d)"), reads=[("smv", t % 4)], stream="o1", n=12)
                if _os.environ.get("P1_TAILSTORE"):
                    dma("sp", lruT_d[0:128, 0:8], rst[:], reads=["p1rs"], stream="o1", n=12)
                end_phase()

        def phase2a_gen(l, ph):
            TB = 1024
            if True:
                cols = sbt(ph, [128, 2, 8], F32, "lcols")
                ccol = sbt(ph, [128, 2], F32, "ccol")
                wstage = sbt(ph, [128, 2, 2, 128], F32, "wstage")
                wbd = sbt(ph, [128, 2, 2, 128], BF16, "wbd")
                xin = [sbt(ph, [128, TB + 3], F32, "xin") for _ in range(2)]
                gin = [sbt(ph, [128, TB], F32, "gin") for _ in range(2)]
                xc = sbt(ph, [128, TB], F32, "xc")
                xcb = sbt(ph, [128, TB], BF16, "xcb")
                rr_ = sbt(ph, [128, TB], F32, "r")
                ii_ = sbt(ph, [128, TB], F32, "i")
                aa = sbt(ph, [128, TB], F32, "a")
                mmul = sbt(ph, [128, TB], F32, "mult")
                uu = sbt(ph, [128, TB], F32, "u")
                hh = [sbt(ph, [128, TB], F32, "h") for _ in range(2)]
                gt = sbt(ph, [128, TB], F32, "gt")
                gs = sbt(ph, [128, TB], F32, "gs")
                yb = [sbt(ph, [128, TB], BF16, "yb") for _ in range(2)]
                gps = [pst(ph, [128, 512], F32, "gps") for _ in range(2)]
                for h in range(2):
                    dma("sp", cols[:, h, :], lcols_d[l, h], writes=["lcols"])
                kb.op("pool", lambda e: e.memset(wstage[:], 0.0), writes=["wstage"])
                for ax, src in enumerate((lwa_d, lwx_d)):
                    for h in range(2):
                        for b in range(2):
                            dma("sp", wstage[b * 64:(b + 1) * 64, ax, h, b * 64:(b + 1) * 64], src[l, 2 * h + b],
                                reads=[], writes=["wstage"])
                kb.op("dve", lambda e: e.tensor_copy(out=wbd[:], in_=wstage[:]), reads=["wstage"], writes=["wbd"])
                kb.op("act", lambda e: e.activation(out=ccol[:], in_=cols[:, :, 7], func=AF.Exp, scale=-1.0), reads=["lcols"], writes=["ccol"])
                kb.op("act", lambda e: e.activation(out=ccol[:], in_=ccol[:], func=AF.Ln, bias=1.0), reads=["ccol"], writes=["ccol"])
                kb.op("dve", lambda e: e.tensor_scalar(out=ccol[:], in0=ccol[:], scalar1=-8.0, scalar2=None, op0=ALU.mult), reads=["ccol"], writes=["ccol"])

                nb = S // TB
                it = 0
                for h in range(2):
                    for b in range(nb):
                        t0 = b * TB
                        xi = xin[it % 2]
                        gi = gin[it % 2]
                        hcur = hh[it % 2]
                        hprev = hh[(it + 1) % 2]
                        ybs = yb[it % 2]
                        kx, kg, ky = ("xin", it % 2), ("gin", it % 2), ("yb", it % 2)
                        kh, khp = ("h", it % 2), ("h", (it + 1) % 2)
                        it += 1
                        if b == 0:
                            kb.op("pool", lambda e, xi=xi: e.memset(xi[:, 0:3], 0.0), writes=[kx])
                            dma("sp", xi[:, 3:], lruT_d[h * 128:(h + 1) * 128, 0:TB], writes=[kx], stream="x", n=3)
                        else:
                            dma("sp", xi[:], lruT_d[h * 128:(h + 1) * 128, t0 - 3:t0 + TB], writes=[kx], stream="x", n=3)
                        dma("sp", gi[:], lruT_d[256 + h * 128:256 + (h + 1) * 128, t0:t0 + TB], writes=[kg], stream="x", n=3)
                        yield
                        kb.op("dve", lambda e, xi=xi, h=h: e.tensor_scalar(out=xc[:], in0=xi[:, 3:TB + 3], scalar1=cols[:, h, 3:4], scalar2=cols[:, h, 4:5], op0=ALU.mult, op1=ALU.add),
                              reads=[kx, "lcols"], writes=["xc"])
                        for j in range(3):
                            kb.op("dve", lambda e, xi=xi, h=h, j=j: e.scalar_tensor_tensor(out=xc[:], in0=xi[:, j:TB + j], scalar=cols[:, h, j:j + 1], in1=xc[:], op0=ALU.mult, op1=ALU.add),
                                  reads=[kx, "lcols", "xc"], writes=["xc"])
                        yield
                        kb.op("pool", lambda e: e.tensor_copy(out=xcb[:], in_=xc[:]), reads=["xc"], writes=["xcb"])
                        for sblk in range(TB // 512):
                            cs = slice(sblk * 512, (sblk + 1) * 512)
                            pa, px = gps[0], gps[1]
                            ka, kx_ = ("gps", 0), ("gps", 1)
                            mm(pa[:], wbd[:, 0, h, :], xcb[:, cs], True, True, reads=["wbd", "xcb"], writes=[ka])
                            mm(px[:], wbd[:, 1, h, :], xcb[:, cs], True, True, reads=["wbd", "xcb"], writes=[kx_])
                            kb.op("act", lambda e, pa=pa, cs=cs, h=h: e.activation(out=rr_[:, cs], in_=pa[:], func=AF.Sigmoid, bias=cols[:, h, 5:6]),
                                  reads=[ka, "lcols"], writes=["r"])
                            kb.op("act", lambda e, px=px, cs=cs, h=h: e.activation(out=ii_[:, cs], in_=px[:], func=AF.Sigmoid, bias=cols[:, h, 6:7]),
                                  reads=[kx_, "lcols"], writes=["i"])
                        yield
                        kb.op("pool", lambda e, gi=gi: e.tensor_tensor(out=gt[:], in0=gi[:], in1=gi[:], op=ALU.mult), reads=[kg], writes=["gt"])
                        kb.op("pool", lambda e: e.tensor_scalar(out=gt[:], in0=gt[:], scalar1=0.044715, scalar2=1.0, op0=ALU.mult, op1=ALU.add), reads=["gt"], writes=["gt"])
                        kb.op("pool", lambda e, gi=gi: e.tensor_tensor(out=gt[:], in0=gt[:], in1=gi[:], op=ALU.mult), reads=["gt", kg], writes=["gt"])
                        kb.op("act", lambda e: e.activation(out=gs[:], in_=gt[:], func=AF.Sigmoid, scale=1.5957691216057308), reads=["gt"], writes=["gs"])
                        kb.op("pool", lambda e, gi=gi: e.tensor_tensor(out=gs[:], in0=gs[:], in1=gi[:], op=ALU.mult), reads=["gs", kg], writes=["gs"])
                        yield
                        kb.op("act", lambda e, h=h: e.activation(out=aa[:], in_=rr_[:], func=AF.Exp, scale=ccol[:, h:h + 1]), reads=["r", "ccol"], writes=["a"])
                        kb.op("pool", lambda e: e.tensor_tensor(out=mmul[:], in0=aa[:], in1=aa[:], op=ALU.mult), reads=["a"], writes=["mult"])
                        kb.op("act", lambda e: e.activation(out=mmul[:], in_=mmul[:], func=AF.Sqrt, scale=-1.0, bias=1.0), reads=["mult"], writes=["mult"])
                        yield
                        if b == 0:
                            kb.op("dve", lambda e: e.memset(mmul[:, 0:1], 1.0), reads=["mult"], writes=["mult"])
                        kb.op("dve", lambda e: e.tensor_tensor(out=uu[:], in0=ii_[:], in1=xc[:], op=ALU.mult), reads=["i", "xc"], writes=["u"])
                        kb.op("dve", lambda e: e.tensor_tensor(out=uu[:], in0=uu[:], in1=mmul[:], op=ALU.mult), reads=["u", "mult"], writes=["u"])
                        yield
                        if b == 0:
                            kb.op("dve", lambda e, hcur=hcur: e.tensor_tensor_scan(out=hcur[:], data0=aa[:], data1=uu[:], initial=0.0, op0=ALU.mult, op1=ALU.add),
                                  reads=["a", "u"], writes=[kh])
                        else:
                            kb.op("dve", lambda e, hcur=hcur, hprev=hprev: e.tensor_tensor_scan(out=hcur[:], data0=aa[:], data1=uu[:], initial=hprev[:, TB - 1:TB], op0=ALU.mult, op1=ALU.add),
                                  reads=["a", "u", khp], writes=[kh])
                        yield
                        kb.op("dve", lambda e, hcur=hcur, ybs=ybs: e.tensor_tensor(out=ybs[:], in0=hcur[:], in1=gs[:], op=ALU.mult), reads=[kh, "gs"], writes=[ky])
                        dma("sp", mixT_d[h * 128:(h + 1) * 128, t0:t0 + TB], ybs[:], reads=[ky], stream="o", n=4)
                        yield

        def phase2b_gen(l, ph):
            TB = 1024
            NCH = TB // 128
            if True:
                w2s = sbt(ph, [16, 128], F32, "w2s")
                w2b = sbt(ph, [16, 128], BF16, "w2b")
                negb = sbt(ph, [128, 1], F32, "negb")
                gn = sbt(ph, [128, 256], F32, "gn")
                tri = sbt(ph, [128, 128], F32, "tri")
                bmask = sbt(ph, [128, 256], F32, "bmask")
                hm = sbt(ph, [128, 4], F32, "hm")
                ones = sbt(ph, [128, 128], F32, "ones")
                glr = [sbt(ph, [16, TB], BF16, "glr") for _ in range(2)]
                qT = [sbt(ph, [128, TB], F32, "qT") for _ in range(2)]
                kT = [sbt(ph, [128, TB], F32, "kT") for _ in range(2)]
                vv = [sbt(ph, [128, NCH, 256], BF16, "vv") for _ in range(2)]
                go = [sbt(ph, [128, NCH, 256], F32, "go") for _ in range(2)]
                ee = sbt(ph, [128, TB], F32, "ee")
                cum = sbt(ph, [128, TB], F32, "cum")
                ex = sbt(ph, [128, TB], F32, "ex")
                dd = sbt(ph, [128, TB], F32, "dd")
                qd = sbt(ph, [128, TB], BF16, "qd")
                kdm = sbt(ph, [128, 4, TB], BF16, "kdm")
                kdec = sbt(ph, [128, TB], BF16, "kdec")
                dcol = sbt(ph, [128, NCH], F32, "dcol")
                kdtm = [sbt(ph, [128, 128], BF16, "kdtm") for _ in range(2)]
                am = [sbt(ph, [128, 4, 128], BF16, "am") for _ in range(2)]
                Sst = sbt(ph, [128, 256], F32, "Sst")
                Sbf = sbt(ph, [128, 256], BF16, "Sbf")
                kvm = sbt(ph, [128, 256], F32, "kvm")
                ob = sbt(ph, [128, NCH, 256], F32, "ob")
                osq = sbt(ph, [128, NCH, 256], F32, "osq")
                ssq = sbt(ph, [128, NCH * 4], F32, "gssq")
                rst = sbt(ph, [128, NCH * 4], F32, "grst")
                sg = sbt(ph, [128, NCH, 256], F32, "sg")
                yb = sbt(ph, [128, NCH, 256], BF16, "yb")
                yT = [sbt(ph, [128, 2, TB], BF16, "yT") for _ in range(2)]
                zps = [pst(ph, [128, 512], F32, "zps") for _ in range(1)]
                tps = pst(ph, [128, 1024], BF16, "gtps")
                aps_ = [pst(ph, [128, 512], F32, "gaps") for _ in range(1)]
                ops_ = [pst(ph, [128, 512], F32, "gops") for _ in range(2)]
                kvps = pst(ph, [128, 512], F32, "kvps")

                dma("sp", w2s[:], gw2_d[l], writes=["w2s"])
                kb.op("dve", lambda e: e.tensor_copy(out=w2b[:], in_=w2s[:]), reads=["w2s"], writes=["w2b"])
                dma("sp", negb[:], gb_d[l], writes=["negb"])
                kb.op("dve", lambda e: e.tensor_scalar(out=negb[:], in0=negb[:], scalar1=-1.0, scalar2=None, op0=ALU.mult), reads=["negb"], writes=["negb"])
                dma("sp", gn[:], gn_d[l:l + 1, :].partition_broadcast(128), writes=["gn"])
                dma("sp", tri[:], tri_d, writes=["tri"])
                dma("sp", bmask[:], bmask_d, writes=["bmask"])
                dma("sp", hm[:], hm_d, writes=["hm"])
                kb.op("pool", lambda e: e.memset(ones[:], 1.0), writes=["ones"])
                kb.op("pool", lambda e: e.memset(Sst[:], 0.0), writes=["Sst"])
                kb.op("pool", lambda e: e.memset(Sbf[:], 0.0), writes=["Sbf"])

                def load(b):
                    i = b % 2
                    t0 = b * TB
                    dma("sp", glr[i][:], glrT_d[:, t0:t0 + TB], writes=[("glr", i)], stream="x", n=3)
                    dma("sp", qT[i][:], gqT_d[:, t0:t0 + TB], writes=[("qT", i)], stream="x", n=3)
                    dma("sp", kT[i][:], gkT_d[:, t0:t0 + TB], writes=[("kT", i)], stream="x", n=3)
                    dma("sp", vv[i][:], gv_d[t0:t0 + TB, :].rearrange("(c p) f -> p c f", p=128), writes=[("vv", i)], stream="x", n=3)
                    dma("sp", go[i][:], gout_d[t0:t0 + TB, :].rearrange("(c p) f -> p c f", p=128), writes=[("go", i)], stream="x", n=3)

                nb = S // TB
                load(0)
                for b in range(nb):
                    if b + 1 < nb:
                        load(b + 1)
                    i = b % 2
                    t0 = b * TB
                    q_, k_, v_, g_, r_ = qT[i], kT[i], vv[i], go[i], glr[i]
                    kq, kk, kv, kg, kr = ("qT", i), ("kT", i), ("vv", i), ("go", i), ("glr", i)
                    for sblk in range(TB // 512):
                        cs = slice(sblk * 512, (sblk + 1) * 512)
                        zp = zps[0]
                        mm(zp[:], w2b[:], r_[:, cs], True, True, reads=["w2b", kr], writes=[("zps", 0)])
                        kb.op("act", lambda e, zp=zp, cs=cs: e.activation(out=ee[:, cs], in_=zp[:], func=AF.Exp, scale=-1.0, bias=negb[:]),
                              reads=[("zps", 0), "negb"], writes=["ee"])
                    yield
                    kb.op("act", lambda e: e.activation(out=ee[:], in_=ee[:], func=AF.Ln, bias=1.0), reads=["ee"], writes=["ee"])
                    for c in range(NCH):
                        cs = slice(c * 128, (c + 1) * 128)
                        kb.op("dve", lambda e, cs=cs: e.tensor_tensor_scan(out=cum[:, cs], data0=ones[:], data1=ee[:, cs], initial=0.0, op0=ALU.mult, op1=ALU.add),
                              reads=["ones", "ee"], writes=["cum"])
                    yield
                    kb.op("act", lambda e: e.activation(out=ex[:], in_=cum[:], func=AF.Exp, scale=-1.0 / 16.0), reads=["cum"], writes=["ex"])
                    kb.op("dve", lambda e, q_=q_: e.scalar_tensor_tensor(out=qd[:], in0=q_[:], scalar=32.0 ** -0.5, in1=ex[:], op0=ALU.mult, op1=ALU.mult),
                          reads=[kq, "ex"], writes=["qd"])
                    yield
                    kb.op("act", lambda e: e.activation(out=dcol[:], in_=cum[:].rearrange("p (c t) -> p c t", t=128)[:, :, 127], func=AF.Exp, scale=-1.0 / 16.0),
                          reads=["cum"], writes=["dcol"])
                    for c in range(NCH):
                        cs = slice(c * 128, (c + 1) * 128)
                        kb.op("pool", lambda e, cs=cs, c=c: e.tensor_scalar(out=dd[:, cs], in0=cum[:, cs], scalar1=cum[:, c * 128 + 127:c * 128 + 128], scalar2=None, op0=ALU.subtract),
                              reads=["cum"], writes=["dd"])
                    yield
                    kb.op("act", lambda e: e.activation(out=ex[:], in_=cum[:], func=AF.Exp, scale=1.0 / 16.0), reads=["cum", "qd"], writes=["ex"])
                    for hh_ in range(4):
                        kb.op("dve", lambda e, k_=k_, hh_=hh_: e.scalar_tensor_tensor(out=kdm[:, hh_, :], in0=k_[:], scalar=hm[:, hh_:hh_ + 1], in1=ex[:], op0=ALU.mult, op1=ALU.mult),
                              reads=[kk, "ex", "hm"], writes=["kdm"])
                    yield
                    kb.op("act", lambda e: e.activation(out=dd[:], in_=dd[:], func=AF.Exp, scale=1.0 / 16.0), reads=["dd"], writes=["dd"])
                    kb.op("pool", lambda e, k_=k_: e.tensor_tensor(out=kdec[:], in0=k_[:], in1=dd[:], op=ALU.mult), reads=[kk, "dd"], writes=["kdec"])
                    kb.op("act", lambda e, g_=g_: e.activation(out=sg[:], in_=g_[:], func=AF.Silu), reads=[kg], writes=["sg"])
                    def gla_s1(c):
                        cs = slice(c * 128, (c + 1) * 128)
                        j = c % 2
                        tp(tps[:, j * 128:(j + 1) * 128], kdec[:, cs], ident[:], reads=["kdec", "ident"], writes=["gtps"])
                        kb.op("act", lambda e, j=j: e.copy(out=kdtm[j][:], in_=tps[:, j * 128:(j + 1) * 128]), reads=["gtps"], writes=[("kdtm", j)])
                        ap_ = aps_[0]
                        for hh_ in range(4):
                            mm(ap_[:, hh_ * 128:(hh_ + 1) * 128], kdm[:, hh_, cs], qd[:, cs], True, True, reads=["kdm", "qd"], writes=[("gaps", 0)])
                        kb.op("dve", lambda e, ap_=ap_, j=j: e.tensor_tensor(out=am[j][:], in0=ap_[:].rearrange("p (h c) -> p h c", h=4),
                                                                             in1=tri[:].unsqueeze(1).broadcast_to([128, 4, 128]), op=ALU.mult),
                              reads=[("gaps", 0), "tri"], writes=[("am", j)])

                    def gla_s2(c):
                        cs = slice(c * 128, (c + 1) * 128)
                        j = c % 2
                        op_ = ops_[j]
                        mm(op_[:, 0:256], qd[:, cs], Sbf[:], True, True, reads=["qd", "Sbf"], writes=[("gops", j)])
                        for hh_ in range(4):
                            mm(op_[:, hh_ * 64:(hh_ + 1) * 64], am[j][:, hh_, :], v_[:, c, hh_ * 64:(hh_ + 1) * 64], False, True,
                               reads=[("am", j), kv], writes=[("gops", j)], skip_group_check=True)
                        kb.op("act", lambda e, op_=op_, c=c: e.copy(out=ob[:, c, :], in_=op_[:, 0:256]), reads=[("gops", j)], writes=["ob"])
                        mm(kvps[:, 0:256], kdtm[j][:], v_[:, c, :], True, True, reads=[("kdtm", j), kv], writes=["kvps"])
                        kb.op("dve", lambda e: e.tensor_tensor(out=kvm[:], in0=kvps[:, 0:256], in1=bmask[:], op=ALU.mult), reads=["kvps", "bmask"], writes=["kvm"])
                        kb.op("dve", lambda e, c=c: e.scalar_tensor_tensor(out=Sst[:], in0=Sst[:], scalar=dcol[:, c:c + 1], in1=kvm[:], op0=ALU.mult, op1=ALU.add),
                              reads=["Sst", "dcol", "kvm"], writes=["Sst"])
                        kb.op("pool", lambda e: e.tensor_copy(out=Sbf[:], in_=Sst[:]), reads=["Sst"], writes=["Sbf"])

                    gla_s1(0)
                    yield
                    for c in range(NCH):
                        if c + 1 < NCH:
                            gla_s1(c + 1)
                            yield
                        gla_s2(c)
                        yield
                    yield
                    kb.op("pool", lambda e: e.tensor_tensor(out=osq[:], in0=ob[:], in1=ob[:], op=ALU.mult), reads=["ob"], writes=["osq"])
                    kb.op("dve", lambda e: e.tensor_reduce(out=ssq[:], in_=osq[:].rearrange("p c (h v) -> p (c h) v", h=4), axis=AX.X, op=ALU.add),
                          reads=["osq"], writes=["p2bssq"])
                    rstd_from_ssq(ssq[:], rst[:], 64, "p2b")
                    kb.op("dve", lambda e: e.tensor_tensor(out=ob[:].rearrange("p c (h v) -> p (c h) v", h=4), in0=ob[:].rearrange("p c (h v) -> p (c h) v", h=4),
                                                           in1=rst[:].unsqueeze(2).broadcast_to([128, NCH * 4, 64]), op=ALU.mult),
                          reads=["ob", "p2brs"], writes=["ob"])
                    kb.op("pool", lambda e: e.tensor_tensor(out=sg[:], in0=sg[:], in1=gn[:].unsqueeze(1).broadcast_to([128, NCH, 256]), op=ALU.mult),
                          reads=["sg", "gn"], writes=["sg"])
                    kb.op("dve", lambda e: e.tensor_tensor(out=yb[:], in0=ob[:], in1=sg[:], op=ALU.mult), reads=["ob", "sg"], writes=["yb"])
                    yield
                    yTb = yT[b % 2]
                    for c in range(NCH):
                        for f in range(2):
                            jj = (c * 2 + f) % 4
                            tp(tps[:, jj * 128:(jj + 1) * 128], yb[:, c, f * 128:(f + 1) * 128], ident[:], reads=["yb", "ident"], writes=["gtps"])
                            kb.op("act", lambda e, jj=jj, c=c, f=f, yTb=yTb: e.copy(out=yTb[:, f, c * 128:(c + 1) * 128], in_=tps[:, jj * 128:(jj + 1) * 128]),
                                  reads=["gtps"], writes=[("yT", b % 2)])
                    dma("sp", mixT_d[256:512, t0:t0 + TB].rearrange("(f p) t -> p f t", p=128), yTb[:], reads=[("yT", b % 2)], stream="o", n=4)

        def phase2ab(l):
            with ExitStack() as ph:
                gens = [phase2a_gen(l, ph), phase2b_gen(l, ph)]
                while gens:
                    for g_ in list(gens):
                        try:
                            next(g_)
                        except StopIteration:
                            gens.remove(g_)
                end_phase()

        def phase2c(l):
            with ExitStack() as ph:
                kaug = sbt(ph, [128, 8, S], BF16, "kaug")
                vp = sbt(ph, [128, 32, 520], BF16, "vp")
                qaug = [sbt(ph, [128, 8, 512], BF16, "qaug") for _ in range(3)]
                km = sbt(ph, [64, 8, 16], F32, "km")
                kmb = sbt(ph, [64, 8, 16], BF16, "kmb")
                cm = sbt(ph, [128, 16, 16], F32, "cm")
                pm = sbt(ph, [128, 16, 16], F32, "pm")
                b31 = sbt(ph, [128, 8], F32, "b31")
                caus = sbt(ph, [128, 128], F32, "caus")
                tstage = sbt(ph, [128, 2, 8, 128], F32, "tstage")
                tdT = sbt(ph, [128, 8, 128], BF16, "tdT")
                toT = sbt(ph, [128, 8, 128], BF16, "toT")
                tomT = sbt(ph, [128, 8, 128], BF16, "tomT")
                zer = sbt(ph, [128, 260], BF16, "zer")
                gm = sbt(ph, [128, 4, 8, 16], F32, "gm")
                m8 = sbt(ph, [128, 4, 8, 8], F32, "m8")
                sel = sbt(ph, [128, 4, 8, 16], F32, "sel")
                mpad = [sbt(ph, [128, 4, 8, 80], BF16, "mpad") for _ in range(2)]
                pT = [sbt(ph, [128, 512], BF16, "pT") for _ in range(5)]
                rcp = sbt(ph, [128, 4], F32, "rcp")
                ymo = [sbt(ph, [128, 4, 512], BF16, "ymo") for _ in range(2)]
                ymT = [sbt(ph, [128, 4, 512], BF16, "ymT") for _ in range(2)]
                gps = pst(ph, [128, 512], F32, "mgps")
                mtps = [gps]
                mtps_b = [pst(ph, [128, 1024], BF16, "mtpsb") for _ in range(1)]
                sps = [pst(ph, [128, 512], F32, "sps") for _ in range(4)]
                accs_full = [pst(ph, [128, 512], F32, "acc") for _ in range(2)]
                accs = [a_[:, 0:260].rearrange("p (s d) -> p s d", s=4) for a_ in accs_full]

                for h in range(8):
                    dma("sp", kaug[0:64, h, :], mkT_d[h * 64:(h + 1) * 64, :], writes=["kaug"], stream="x", n=3)
                    dma("sp", kaug[64:80, h, :], e16_d, writes=["kaug"], stream="x", n=3)
                for c in range(4):
                    dma("sp", vp[:, c * 8:(c + 1) * 8, :], mvp_d[c * 1024:(c + 1) * 1024, :].rearrange("(c p) f -> p c f", p=128), writes=["vp"], stream="x", n=3)
                dma("sp", cm[:].rearrange("p a b -> p (a b)"), cm_d.partition_broadcast(128), writes=["cm"])
                dma("sp", pm[:].rearrange("p a b -> p (a b)"), pm_d.partition_broadcast(128), writes=["pm"])
                dma("sp", b31[:], rb31_d.partition_broadcast(128), writes=["b31"])
                dma("sp", caus[:], caus_d, writes=["caus"])
                dma("sp", tstage[:, 0], tdg_d, writes=["tstage"])
                dma("sp", tstage[:, 1], tof_d, writes=["tstage"])
                kb.op("dve", lambda e: e.tensor_tensor(out=tdT[:], in0=tstage[:, 0], in1=caus[:].unsqueeze(1).broadcast_to([128, 8, 128]), op=ALU.add),
                      reads=["tstage", "caus"], writes=["tdT"])
                kb.op("dve", lambda e: e.tensor_copy(out=toT[:], in_=tstage[:, 1]), reads=["tstage"], writes=["toT"])
                kb.op("dve", lambda e: e.tensor_tensor(out=tomT[:], in0=tstage[:, 1], in1=b31[:].unsqueeze(2).broadcast_to([128, 8, 128]), op=ALU.subtract),
                      reads=["tstage", "b31"], writes=["tomT"])
                kb.op("pool", lambda e: e.memset(zer[:], 0.0), writes=["zer"])
                for i in range(2):
                    kb.op("pool", lambda e, i=i: e.memset(mpad[i][:], 0.0), writes=[("mpad", i)])
                kb.op("dve", lambda e: e.tensor_reduce(out=km[:].rearrange("p h n -> p (h n)"), in_=kaug[0:64, :, :].rearrange("p h (n t) -> p (h n) t", t=256), axis=AX.X, op=ALU.add),
                      reads=["kaug"], writes=["km"])
                kb.op("dve", lambda e: e.tensor_scalar(out=kmb[:], in0=km[:], scalar1=1.0 / 256.0, scalar2=None, op0=ALU.mult), reads=["km"], writes=["kmb"])

                if l == 0:
                    casts_rest()

                def loadq(G):
                    i = G % 3
                    dma("sp", qaug[i][0:64, :, :], mqT_d.rearrange("(h d) t -> d h t", d=64)[:, :, G * 512:(G + 1) * 512], writes=[("qaug", i)], stream="q", n=3)

                NG = S // 512
                si = 0
                ai = 0

                def pre1(G):
                    qi = G % 3
                    qa = qaug[qi]
                    kqa = ("qaug", qi)
                    mp = mpad[G % 2]
                    for s in range(4):
                        for h in range(8):
                            mm(gps[:, (s * 8 + h) * 16:(s * 8 + h + 1) * 16], qa[0:64, h, s * 128:(s + 1) * 128], kmb[:, h, :], True, True,
                               reads=[kqa, "kmb"], writes=["mgps"])
                    np0 = 2 * G
                    for a in range(2):
                        cmv = cm[:, np0 + a, :].unsqueeze(1).unsqueeze(1).broadcast_to([128, 2, 8, 16])
                        kb.op("dve", lambda e, cmv=cmv, a=a: e.tensor_tensor(out=gm[:, 2 * a:2 * a + 2], in0=gps[:].rearrange("p (s h n) -> p s h n", s=4, h=8)[:, 2 * a:2 * a + 2],
                                                                             in1=cmv, op=ALU.add),
                              reads=["mgps", "cm"], writes=["gm"])
                    for s in range(4):
                        for h in range(8):
                            kb.op("dve", lambda e, s=s, h=h: e.max(out=m8[:, s, h, :], in_=gm[:, s, h, :]), reads=["gm"], writes=["m8"])
                    kb.op("dve", lambda e: e.tensor_tensor(out=sel[:], in0=gm[:], in1=m8[:, :, :, 2:3].broadcast_to([128, 4, 8, 16]), op=ALU.is_ge),
                          reads=["gm", "m8"], writes=["sel"])
                    kb.op("dve", lambda e: e.tensor_scalar(out=sel[:], in0=sel[:], scalar1=-NEG, scalar2=NEG, op0=ALU.mult, op1=ALU.add), reads=["sel"], writes=["sel"])
                    kb.op("dve", lambda e: e.tensor_tensor(out=sel[:], in0=sel[:], in1=b31[:].unsqueeze(1).unsqueeze(3).broadcast_to([128, 4, 8, 16]), op=ALU.add),
                          reads=["sel", "b31"], writes=["sel"])
                    for a in range(2):
                        pmv = pm[:, np0 + a, :].unsqueeze(1).unsqueeze(1).broadcast_to([128, 2, 8, 16])
                        kb.op("dve", lambda e, pmv=pmv, mp=mp, a=a: e.tensor_tensor(out=mp[:, 2 * a:2 * a + 2, :, 64:80], in0=sel[:, 2 * a:2 * a + 2], in1=pmv, op=ALU.mult),
                              reads=["sel", "pm"], writes=[("mpad", G % 2)])

                def pre2(G):
                    qi = G % 3
                    qa = qaug[qi]
                    kqa = ("qaug", qi)
                    mp = mpad[G % 2]
                    for h in range(8):
                        mt = mtps[0]
                        for s in range(4):
                            mm(mt[0:80, s * 128:(s + 1) * 128], mp[:, s, h, :], ident[:], True, True, reads=[("mpad", G % 2), "ident"], writes=["mgps"])
                        kb.op("act", lambda e, mt=mt, h=h, qa=qa: e.copy(out=qa[64:80, h, :], in_=mt[64:80, :]),
                              reads=["mgps"], writes=[kqa])

                loadq(0)
                if NG > 1:
                    loadq(1)
                pre1(0)
                pre2(0)
                for G in range(NG):
                    if G + 2 < NG:
                        loadq(G + 2)
                    if G + 1 < NG:
                        pre1(G + 1)
                    qi = G % 3
                    qa = qaug[qi]
                    kqa = ("qaug", qi)
                    ym = ymo[G % 2]
                    nj = 4 * G + 4
                    DEPTH = 3

                    def stageA(h, j):
                        nonlocal si
                        acc = accs[h % 2]
                        ka = ("acc", h % 2)
                        if j == 0:
                            mm(accs_full[h % 2][:, 0:260], zer[:, 0:128], zer[:, :], True, True, reads=["zer"], writes=[ka])
                        r = j - 4 * G
                        c0 = max(r, 0) * 128
                        sp_ = sps[si % 4]
                        ks = ("sps", si % 4)
                        pt = pT[si % 5]
                        kp = ("pT", si % 5)
                        si += 1
                        mm(sp_[:, c0:512], kaug[0:80, h, j * 128:(j + 1) * 128], qa[0:80, h, c0:512], True, True,
                           reads=["kaug", kqa], writes=[ks])
                        if r == -1:
                            mm(sp_[:, 0:128], ident[:], tomT[:, h, :], False, True, reads=["ident", "tomT"], writes=[ks], skip_group_check=True)
                        if r >= 0:
                            mm(sp_[:, r * 128:(r + 1) * 128], ident[:], tdT[:, h, :], False, True, reads=["ident", "tdT"], writes=[ks], skip_group_check=True)
                            if r < 3:
                                tt = toT if r % 2 == 0 else tomT
                                mm(sp_[:, (r + 1) * 128:(r + 2) * 128], ident[:], tt[:, h, :], False, True, reads=["ident", "toT", "tomT"], writes=[ks], skip_group_check=True)
                        kb.op("act", lambda e, pt=pt, sp_=sp_, c0=c0: e.activation(out=pt[:, c0:512], in_=sp_[:, c0:512], func=AF.Exp),
                              reads=[ks], writes=[kp])
                        return (h, j, r, pt, kp, acc, ka)

                    def stageB(info):
                        h, j, r, pt, kp, acc, ka = info
                        for s in range(max(r, 0), 4):
                            mm(acc[:, s, :], pt[:, s * 128:(s + 1) * 128], vp[:, j, h * 65:(h + 1) * 65], False, True,
                               reads=[kp, "vp"], writes=[ka], skip_group_check=True)
                        if j == nj - 1:
                            kb.op("dve", lambda e, acc=acc: e.reciprocal(out=rcp[:], in_=acc[:, :, 64]), reads=[ka], writes=["rcp"])
                            kb.op("dve", lambda e, acc=acc, h=h, ym=ym: e.tensor_tensor(out=ym[:, :, h * 64:(h + 1) * 64], in0=acc[:, :, 0:64],
                                                                                  in1=rcp[:].unsqueeze(2).broadcast_to([128, 4, 64]), op=ALU.mult),
                                  reads=[ka, "rcp"], writes=[("ymo", G % 2)])

                    pend = []
                    for h in range(8):
                        if h == 4 and G + 1 < NG:
                            pre2(G + 1)
                        for j in range(nj):
                            pend.append(stageA(h, j))
                            if len(pend) > DEPTH:
                                stageB(pend.pop(0))
                    while pend:
                        stageB(pend.pop(0))
                    yt = ymT[G % 2]
                    for s in range(4):
                        tpb = mtps_b[0]
                        for f in range(4):
                            tp(tpb[:, f * 128:(f + 1) * 128], ym[:, s, f * 128:(f + 1) * 128], ident[:], reads=[("ymo", G % 2), "ident"], writes=[("mtpsb", 0)])
                        kb.op("act", lambda e, tpb=tpb, yt=yt, s=s: e.copy(out=yt[:, :, s * 128:(s + 1) * 128], in_=tpb[:, 0:512].rearrange("p (f q) -> p f q", f=4)),
                              reads=[("mtpsb", 0)], writes=[("ymT", G % 2)])
                    dma("sp", mixT_d[512:1024, G * 512:(G + 1) * 512].rearrange("(f p) t -> p f t", p=128), yt[:], reads=[("ymT", G % 2)], stream="o", n=4)
                end_phase()

        def phase3(l, xin_d, xout_d):
            with ExitStack() as ph:
                wout = sbt(ph, [128, 8, D], BF16, "wout")
                wdn = sbt(ph, [128, NFC, D], BF16, "wdn")
                g3 = sbt(ph, [128, 3, D], F32, "g3")
                wgu = [sbt(ph, [128, 2, 8, 128], BF16, "wgu") for _ in range(4)]
                mixT = [sbt(ph, [128, 8, 512], BF16, "mixT") for _ in range(2)]
                xt = [sbt(ph, [128, D], F32, "xt3") for _ in range(2)]
                x1 = [sbt(ph, [128, D], F32, "x1") for _ in range(8)]
                tmp = [sbt(ph, [128, D], F32, "tmp3") for _ in range(2)]
                junk = sbt(ph, [128, D], BF16, "junk3")
                hb = [sbt(ph, [128, D], BF16, "hb3") for _ in range(4)]
                hT = sbt(ph, [128, 8, 512], BF16, "hT3")
                actT = sbt(ph, [128, NFC, 512], BF16, "actT")
                sgt = [sbt(ph, [128, 512], F32, "sgt") for _ in range(2)]
                ssq = sbt(ph, [128, 16], F32, "ssq3")
                rst = sbt(ph, [128, 16], F32, "rst3")
                xo = [sbt(ph, [128, D], F32, "xo") for _ in range(2)]
                ops_ = [pst(ph, [128, 2, 512], F32, "p3o") for _ in range(2)]
                tps = pst(ph, [128, D], BF16, "p3t")
                gus = [pst(ph, [128, 512], F32, "p3gu") for _ in range(3)]
                def load_resident():
                    for kc in range(8):
                        dma("sp", wout[:, kc, :], woutb_d[l, kc * 128:(kc + 1) * 128, :], writes=[("wout", kc)], stream="w", n=4)
                    for i in range(3):
                        dma("sp", g3[:, i, :], norms_d[l, i + 1:i + 2, :].partition_broadcast(128), writes=[("g3", i)])
                    for fc in range(NFC):
                        dma("sp", wdn[:, fc, :], wdb_d[l, fc * 128:(fc + 1) * 128, :], writes=[("wdn", fc)], stream="w", n=4)

                G3K = [("g3", i_) for i_ in range(3)]
                wi = [0]

                def loadw(fc):
                    i = wi[0] % 4
                    wi[0] += 1
                    dma("sp", wgu[i][:], wgub_d[l, fc], writes=[("wgu", i)], stream="wgu", n=4)
                    return i

                NG = S // 512
                sq = 0

                def loadg(G):
                    i = G % 2
                    dma("sp", mixT[i][:], mixT_d[:, G * 512:(G + 1) * 512].rearrange("(k p) t -> p k t", p=128), writes=[("mixT", i)], stream="m", n=2)

                def loadx(t):
                    dma("sp", xt[t % 2][:], xin_d[t * 128:(t + 1) * 128, :], writes=[("xt3", t % 2)], stream="x", n=3)

                loadg(0)
                loadx(0)
                load_resident()
                wq = []
                PRE = 3
                def p3_front(G):
                    nonlocal sq
                    mT = mixT[G % 2]
                    for s in range(4):
                        t = G * 4 + s
                        if t + 1 < S // 128:
                            loadx(t + 1)
                        xs = xt[t % 2]
                        x1s = x1[(G % 2) * 4 + s]
                        op_ = ops_[s % 2]
                        ko = ("p3o", s % 2)
                        tm_ = tmp[s % 2]
                        kt = ("tmp3", s % 2)
                        for hf in range(2):
                            for kc in range(8):
                                mm(op_[:, hf, :], mT[:, kc, s * 128:(s + 1) * 128], wout[:, kc, hf * 512:(hf + 1) * 512], kc == 0, kc == 7,
                                   reads=[("mixT", G % 2), ("wout", kc)], writes=[ko])
                        c = sq % 16
                        sq += 1
                        kb.op("act", lambda e, c=c, op_=op_: e.activation(out=junk[:], in_=op_[:].rearrange("p a b -> p (a b)"), func=AF.Square, accum_out=ssq[:, c:c + 1]),
                              reads=[ko], writes=[("p3ssq", c)])
                        rstd_from_ssq(ssq[:, c:c + 1], rst[:, c:c + 1], D, "p3", c)
                        kb.op("dve", lambda e, c=c, op_=op_, tm_=tm_: e.scalar_tensor_tensor(out=tm_[:], in0=op_[:].rearrange("p a b -> p (a b)"), scalar=rst[:, c:c + 1], in1=g3[:, 0, :], op0=ALU.mult, op1=ALU.mult),
                              reads=[ko, ("p3rs", c)] + G3K, writes=[kt])
                        kb.op("pool", lambda e, xs=xs, x1s=x1s, tm_=tm_: e.tensor_tensor(out=x1s[:], in0=xs[:], in1=tm_[:], op=ALU.add),
                              reads=[("xt3", t % 2), kt], writes=[("x1", (G % 2) * 4 + s)])
                        c2 = sq % 16
                        sq += 1
                        kb.op("act", lambda e, c2=c2, x1s=x1s: e.activation(out=junk[:], in_=x1s[:], func=AF.Square, accum_out=ssq[:, c2:c2 + 1]),
                              reads=[("x1", (G % 2) * 4 + s)], writes=[("p3ssq", c2)])
                        rstd_from_ssq(ssq[:, c2:c2 + 1], rst[:, c2:c2 + 1], D, "p3", c2)
                        hbs = hb[s]
                        kb.op("dve", lambda e, c2=c2, x1s=x1s, hbs=hbs: e.scalar_tensor_tensor(out=hbs[:], in0=x1s[:], scalar=rst[:, c2:c2 + 1], in1=g3[:, 1, :], op0=ALU.mult, op1=ALU.mult),
                              reads=[("x1", (G % 2) * 4 + s), ("p3rs", c2)] + G3K, writes=[("hb3", s)])

                def p3_trans(G):
                    for s in range(4):
                        hbs = hb[s]
                        for kc in range(8):
                            tp(tps[:, kc * 128:(kc + 1) * 128], hbs[:, kc * 128:(kc + 1) * 128], ident[:], reads=[("hb3", s), "ident"], writes=["p3t"])
                        kb.op("act", lambda e, s=s: e.copy(out=hT[:, :, s * 128:(s + 1) * 128], in_=tps[:].rearrange("p (k c) -> p k c", k=8)),
                              reads=["p3t"], writes=["hT3"])

                def p3_gateup(G):
                    for fc in range(NFC):
                        wslot = wq.pop(0)
                        nxt = fc + PRE
                        if nxt < NFC:
                            wq.append(loadw(nxt))
                        w_ = wgu[wslot]
                        gp, up = gus[(2 * fc) % 3], gus[(2 * fc + 1) % 3]
                        kgp, kup = ("p3gu", (2 * fc) % 3), ("p3gu", (2 * fc + 1) % 3)
                        for kc in range(8):
                            mm(gp[:], w_[:, 0, kc, :], hT[:, kc, :], kc == 0, kc == 7, reads=[("wgu", wslot), "hT3"], writes=[kgp])
                        for kc in range(8):
                            mm(up[:], w_[:, 1, kc, :], hT[:, kc, :], kc == 0, kc == 7, reads=[("wgu", wslot), "hT3"], writes=[kup])
                        sg_ = sgt[fc % 2]
                        kb.op("act", lambda e, sg_=sg_, gp=gp: e.activation(out=sg_[:], in_=gp[:], func=AF.Silu), reads=[kgp], writes=[("sgt", fc % 2)])
                        kb.op("dve", lambda e, sg_=sg_, up=up, fc=fc: e.tensor_tensor(out=actT[:, fc, :], in0=up[:], in1=sg_[:], op=ALU.mult),
                              reads=[kup, ("sgt", fc % 2)], writes=["actT"])

                def p3_down(G):
                    nonlocal sq
                    for s in range(4):
                        t = G * 4 + s
                        op_ = ops_[s % 2]
                        ko = ("p3o", s % 2)
                        tm_ = tmp[s % 2]
                        kt = ("tmp3", s % 2)
                        for hf in range(2):
                            for fc in range(NFC):
                                mm(op_[:, hf, :], actT[:, fc, s * 128:(s + 1) * 128], wdn[:, fc, hf * 512:(hf + 1) * 512], fc == 0, fc == NFC - 1,
                                   reads=["actT", ("wdn", fc)], writes=[ko])
                        c = sq % 16
                        sq += 1
                        kb.op("act", lambda e, c=c, op_=op_: e.activation(out=junk[:], in_=op_[:].rearrange("p a b -> p (a b)"), func=AF.Square, accum_out=ssq[:, c:c + 1]),
                              reads=[ko], writes=[("p3ssq", c)])
                        rstd_from_ssq(ssq[:, c:c + 1], rst[:, c:c + 1], D, "p3", c)
                        kb.op("dve", lambda e, c=c, op_=op_, tm_=tm_: e.scalar_tensor_tensor(out=tm_[:], in0=op_[:].rearrange("p a b -> p (a b)"), scalar=rst[:, c:c + 1], in1=g3[:, 2, :], op0=ALU.mult, op1=ALU.mult),
                              reads=[ko, ("p3rs", c)] + G3K, writes=[kt])
                        xos = xo[t % 2]
                        kb.op("pool", lambda e, xos=xos, s=s, tm_=tm_: e.tensor_tensor(out=xos[:], in0=x1[(G % 2) * 4 + s][:], in1=tm_[:], op=ALU.add),
                              reads=[("x1", (G % 2) * 4 + s), kt], writes=[("xo", t % 2)])
                        dma("sp", xout_d[t * 128:(t + 1) * 128, :], xos[:], reads=[("xo", t % 2)], stream="o", n=4)

                for G in range(NG):
                    if G + 1 < NG:
                        loadg(G + 1)
                    while len(wq) < PRE:
                        wq.append(loadw(len(wq)))
                    p3_front(G)
                    if G > 0:
                        p3_down(G - 1)
                    p3_trans(G)
                    p3_gateup(G)
                p3_down(NG - 1)
                end_phase()

        kb.barrier()
        import os as _os2
        if not _os2.environ.get("SKIP_P0"):
            phase0()
        done = stop_after == "p0"
        for l in range(L):
            if done:
                break
            xin = x_d if l == 0 else xs1_d
            xout = xs1_d if l == 0 else out_d
            for nm, fn in (("p1", lambda: phase1(l, xin)), ("p2b", lambda: phase2ab(l)),
                           ("p2c", lambda: phase2c(l)), ("p3", lambda: phase3(l, xin, xout))):
                fn()
                if stop_after == (l, nm):
                    done = True
                    break
            if done:
                break
        kb.barrier()
        kb.emit()

    return nc


def _host_inputs(inputs):
    f = lambda a: np.ascontiguousarray(np.asarray(a, dtype=np.float32))
    c = _consts()
    idx_diag, idx_off1 = _bias_idx()
    rel = f(inputs["rel_bias"])
    shared = {
        "norms": f(np.stack([inputs["pre_mix_norm"], inputs["post_mix_norm"], inputs["pre_ffn_norm"], inputs["post_ffn_norm"]], axis=1)),
        "w_in": f(inputs["w_in"]), "w_out": f(inputs["w_out"]),
        "w_ffn_gate": f(inputs["w_ffn_gate"]), "w_ffn_up": f(inputs["w_ffn_up"]), "w_ffn_down": f(inputs["w_ffn_down"]),
        "lru_wa": f(inputs["lru_wa"]), "lru_wx": f(inputs["lru_wx"]),
        "gla_gate_w2": f(inputs["gla_gate_w2"]),
        "gla_gate_b": f(np.asarray(inputs["gla_gate_b"]).reshape(L, 128, 1)),
        "gla_norm": f(inputs["gla_norm"]),
        "rb31": f(rel[31:32, :]),
        "tdg": f(np.transpose(rel[idx_diag], (0, 2, 1))),
        "tof": f(np.transpose(rel[idx_off1], (0, 2, 1))),
        "ident": c["ident"], "tri": c["tri"], "caus": c["caus"], "e16": c["e16"],
        "cm": c["cm"], "pm": c["pm"], "bmask": c["bmask"], "hm": c["hm"],
    }
    cw = np.transpose(np.asarray(inputs["lru_conv_w"], dtype=np.float32), (0, 2, 1))
    cols = np.concatenate([cw] + [np.asarray(inputs[k], dtype=np.float32)[:, :, None]
                                  for k in ("lru_conv_b", "lru_ba", "lru_bx", "lru_lambda")], axis=2)
    shared["lru_cols"] = f(cols.reshape(L, 2, 128, 8))
    x = np.asarray(inputs["x"], dtype=np.float32)
    return [dict(shared, x=np.ascontiguousarray(x[b])) for b in range(x.shape[0])]


_NC_CACHE = {}


def kernel(**inputs):
    in_maps = _host_inputs(inputs)
    if "nc" not in _NC_CACHE:
        _NC_CACHE["nc"] = build()
    nc = _NC_CACHE["nc"]
    n = len(in_maps)
    res = run_bass_kernel_spmd(nc, in_maps, core_ids=list(range(n)))
    return np.stack([np.asarray(r["out"], dtype=np.float32) for r in res.results], axis=0)
```

```python
from contextlib import ExitStack
import math
import numpy as np
import ml_dtypes
import concourse.bass as bass
import concourse.mybir as mybir
from concourse.bass_utils import run_bass_kernel_spmd

F32 = mybir.dt.float32
BF16 = mybir.dt.bfloat16
ALU = mybir.AluOpType
AF = mybir.ActivationFunctionType
AX = mybir.AxisListType

S = 4096
D = 1024
L = 2
DIN = 2832
DFF = 2816
NFC = DFF // 128
EPS = 1e-6
NEG = -30000.0
ENGS = ("pe", "act", "dve", "pool", "sp")


class KB:
    def __init__(self, nc, stack, sync_same=True):
        self.nc = nc
        self.stack = stack
        self.sync_same = sync_same
        self.ops = {e: [] for e in ENGS}
        self.sem = {}
        self.cnt = {}
        self.step = {}
        self.known = {e: {} for e in ENGS}
        self.lw = {}
        self.rd = {}
        for e in ENGS:
            self._dom(e, 1)

    def _dom(self, name, step):
        if name not in self.sem:
            self.sem[name] = self.stack.enter_context(self.nc.semaphore("s_" + name))
            self.cnt[name] = 0
            self.step[name] = step
        return name

    def op(self, eng, fn, reads=(), writes=(), dma=None):
        dom = eng if dma is None else self._dom("d_" + dma, 16)
        deps = {}

        def add(d):
            if d is not None and deps.get(d[0], 0) < d[1]:
                deps[d[0]] = d[1]

        for k in reads:
            add(self.lw.get(k))
        for k in writes:
            add(self.lw.get(k))
            for dm, c in self.rd.get(k, {}).items():
                add((dm, c))
        if dma is not None and self.cnt[dom] > 0:
            add((dom, self.cnt[dom]))
        kn = self.known[eng]
        for d, c in deps.items():
            if d == eng and (eng == "pe" or not self.sync_same):
                continue
            if kn.get(d, 0) >= c:
                continue
            self.ops[eng].append(("w", self.sem[d], c))
            kn[d] = c
        self.cnt[dom] += self.step[dom]
        me = (dom, self.cnt[dom])
        self.ops[eng].append(("o", fn, self.sem[dom], self.step[dom]))
        for k in writes:
            self.lw[k] = me
            self.rd[k] = {}
        for k in reads:
            r = self.rd.setdefault(k, {})
            if r.get(dom, 0) < me[1]:
                r[dom] = me[1]
        return me

    def barrier(self):
        for eng in ENGS:
            kn = self.known[eng]
            for dom, c in self.cnt.items():
                if c > 0 and dom != eng and kn.get(dom, 0) < c:
                    self.ops[eng].append(("w", self.sem[dom], c))
                    kn[dom] = c
        self.lw = {}
        self.rd = {}

    def emit(self):
        nc = self.nc
        ops = self.ops

        def run(lst, e):
            for it in lst:
                if it[0] == "w":
                    e.wait_ge(it[1], it[2])
                else:
                    it[1](e).then_inc(it[2], it[3])

        with nc.Block() as block:
            @block.tensor
            def _(e):
                run(ops["pe"], e)

            @block.scalar
            def _(e):
                run(ops["act"], e)

            @block.vector
            def _(e):
                run(ops["dve"], e)

            @block.gpsimd
            def _(e):
                run(ops["pool"], e)

            @block.sync
            def _(e):
                run(ops["sp"], e)
        self.ops = {e: [] for e in ENGS}


def _t5_bucket(n):
    n = np.maximum(n, 0)
    nf = np.maximum(n, 1).astype(np.float32)
    large = 16 + (np.log(nf / np.float32(16)) / np.float32(math.log(128 / 16)) * np.float32(16)).astype(np.int32)
    large = np.minimum(large, 31)
    return np.where(n < 16, n, large)


def _consts():
    c = {}
    c["ident"] = np.eye(128, dtype=np.float32).astype(ml_dtypes.bfloat16)
    e = np.arange(128)
    c["tri"] = (e[:, None] <= e[None, :]).astype(np.float32)
    c["caus"] = np.where(e[None, :] >= e[:, None], 0.0, NEG).astype(np.float32)
    keys = np.arange(S)
    c["e16"] = (keys[None, :] // 256 == np.arange(16)[:, None]).astype(np.float32).astype(ml_dtypes.bfloat16)
    npast = np.arange(16)[:, None]
    nn = np.arange(16)[None, :]
    c["cm"] = np.where(nn < npast, 0.0, -1e30).astype(np.float32).reshape(1, 256)
    c["pm"] = (nn < npast).astype(np.float32).reshape(1, 256)
    p = np.arange(128)[:, None]
    c["bmask"] = (p // 32 == (np.arange(256)[None, :] // 64)).astype(np.float32)
    c["hm"] = (p // 32 == np.arange(4)[None, :]).astype(np.float32)
    return c


def _bias_idx():
    k = np.arange(128)[:, None]
    q = np.arange(128)[None, :]
    idx_diag = _t5_bucket(q - k)
    idx_off1 = _t5_bucket(q + 128 - k)
    return idx_diag, idx_off1


def build(debug=False, stop_after=None):
    nc = bass.Bass("TRN2", target_bir_lowering=False)
    dr = lambda name, shape, dt, kind="Internal": nc.dram_tensor(name, list(shape), dt, kind=kind).ap()
    IN = "ExternalInput"
    x_d = dr("x", [S, D], F32, IN)
    norms_d = dr("norms", [L, 4, D], F32, IN)
    w_in_d = dr("w_in", [L, D, DIN], F32, IN)
    w_out_d = dr("w_out", [L, D, D], F32, IN)
    wg_d = dr("w_ffn_gate", [L, D, DFF], F32, IN)
    wu_d = dr("w_ffn_up", [L, D, DFF], F32, IN)
    wd_d = dr("w_ffn_down", [L, DFF, D], F32, IN)
    lcols_d = dr("lru_cols", [L, 2, 128, 8], F32, IN)
    lwa_d = dr("lru_wa", [L, 4, 64, 64], F32, IN)
    lwx_d = dr("lru_wx", [L, 4, 64, 64], F32, IN)
    gw2_d = dr("gla_gate_w2", [L, 16, 128], F32, IN)
    gb_d = dr("gla_gate_b", [L, 128, 1], F32, IN)
    gn_d = dr("gla_norm", [L, 256], F32, IN)
    rb31_d = dr("rb31", [1, 8], F32, IN)
    tdg_d = dr("tdg", [128, 8, 128], F32, IN)
    tof_d = dr("tof", [128, 8, 128], F32, IN)
    ident_d = dr("ident", [128, 128], BF16, IN)
    tri_d = dr("tri", [128, 128], F32, IN)
    caus_d = dr("caus", [128, 128], F32, IN)
    e16_d = dr("e16", [16, S], BF16, IN)
    cm_d = dr("cm", [1, 256], F32, IN)
    pm_d = dr("pm", [1, 256], F32, IN)
    bmask_d = dr("bmask", [128, 256], F32, IN)
    hm_d = dr("hm", [128, 4], F32, IN)
    out_d = dr("out", [S, D], F32, "ExternalOutput")

    dk = "ExternalOutput" if debug else "Internal"
    winb_d = dr("winb", [L, D, DIN], BF16)
    woutb_d = dr("woutb", [L, D, D], BF16)
    wgub_d = dr("wgub", [L, NFC, 128, 2, 8, 128], BF16)
    wdb_d = dr("wdb", [L, DFF, D], BF16)
    xs1_d = dr("xs1", [S, D], F32, dk)
    lruT_d = dr("lruT", [512, S], F32, dk)
    gqT_d = dr("gqT", [128, S], F32, dk)
    gkT_d = dr("gkT", [128, S], F32, dk)
    glrT_d = dr("glrT", [16, S], BF16, dk)
    mqT_d = dr("mqT", [512, S], BF16, dk)
    mkT_d = dr("mkT", [512, S], BF16, dk)
    gv_d = dr("gv", [S, 256], BF16, dk)
    gout_d = dr("gout", [S, 256], F32, dk)
    mvp_d = dr("mvp", [S, 520], BF16, dk)
    mixT_d = dr("mixT", [D, S], BF16, dk)

    with ExitStack() as st:
        kb = KB(nc, st)
        uid = [0]

        def sbt(ctx, shape, dt, name=None):
            uid[0] += 1
            return ctx.enter_context(nc.sbuf_tensor("%s_%d" % (name or "t", uid[0]), list(shape), dt))

        def pst(ctx, shape, dt, name=None):
            uid[0] += 1
            return ctx.enter_context(nc.psum_tensor("%s_%d" % (name or "p", uid[0]), list(shape), dt))

        rr = {}

        def dmaname(stream, n):
            i = rr.get(stream, 0)
            rr[stream] = i + 1
            return "%s%d" % (stream, i % n)

        def dma(eng, out, in_, reads=(), writes=(), stream="g", n=4):
            kb.op(eng, lambda e: e.dma_start(out=out, in_=in_), reads=reads, writes=writes, dma=dmaname(stream, n))

        def mm(out, lhsT, rhs, start, stop, reads, writes, **kw):
            kb.op("pe", lambda e: e.matmul(out, lhsT=lhsT, rhs=rhs, start=start, stop=stop, **kw),
                  reads=reads, writes=writes)

        def tp(out, in_, ident, reads, writes):
            kb.op("pe", lambda e: e.transpose(out, in_, ident), reads=reads, writes=writes)

        ident = sbt(st, [128, 128], BF16, "ident")
        dma("sp", ident[:], ident_d, writes=["ident"])

        def end_phase():
            kb.barrier()
            kb.emit()

        def phase0():
            l = 0
            for c0 in range(0, DIN, 944):
                for kc in range(8):
                    r0 = kc * 128
                    dma("pool", winb_d[l, r0:r0 + 128, c0:c0 + 944], w_in_d[l, r0:r0 + 128, c0:c0 + 944], writes=[("winb", l, kc, c0)], stream="cast", n=4)

        def casts_rest():
            for l in range(L):
                for kc in range(8):
                    r0 = kc * 128
                    dma("pool", woutb_d[l, r0:r0 + 128, :], w_out_d[l, r0:r0 + 128, :], stream="cast", n=4)
                for fc in range(NFC):
                    for gu, wsrc in enumerate((wg_d, wu_d)):
                        dma("pool", wgub_d[l, fc, :, gu, :, :],
                            wsrc[l].rearrange("(kc p) f -> p kc f", p=128)[:, :, fc * 128:(fc + 1) * 128],
                            stream="cast", n=4)
                    dma("pool", wdb_d[l, fc * 128:(fc + 1) * 128, :], wd_d[l, fc * 128:(fc + 1) * 128, :], stream="cast", n=4)
            l = 1
            for c0 in range(0, DIN, 944):
                for kc in range(8):
                    r0 = kc * 128
                    dma("pool", winb_d[l, r0:r0 + 128, c0:c0 + 944], w_in_d[l, r0:r0 + 128, c0:c0 + 944], stream="cast", n=4)

        def rstd_from_ssq(ssq, rstd, n, tag, col=None):
            ks_ = tag + "ssq" if col is None else (tag + "ssq", col)
            kr_ = tag + "rs" if col is None else (tag + "rs", col)
            kb.op("dve", lambda e: e.tensor_scalar(out=rstd, in0=ssq, scalar1=1.0 / n, scalar2=EPS, op0=ALU.mult, op1=ALU.add),
                  reads=[ks_], writes=[kr_])
            kb.op("act", lambda e: e.sqrt(out=rstd, in_=rstd), reads=[kr_], writes=[kr_])
            kb.op("dve", lambda e: e.reciprocal(out=rstd, in_=rstd), reads=[kr_], writes=[kr_])

        def phase1(l, xin_d):
            with ExitStack() as ph:
                win = sbt(ph, [128, 8, DIN], BF16, "win")
                gpre = sbt(ph, [128, D], F32, "gpre")
                xt = [sbt(ph, [128, D], F32, "xt") for _ in range(8)]
                hb = [sbt(ph, [128, D], BF16, "hb") for _ in range(4)]
                junk = sbt(ph, [128, D], BF16, "junk")
                hT = [sbt(ph, [128, 8, 512], BF16, "hT") for _ in range(2)]
                ssq = sbt(ph, [128, 8], F32, "ssq")
                rst = sbt(ph, [128, 8], F32, "rst")
                sf = [sbt(ph, [128, 512], F32, "sf") for _ in range(6)]
                sbf = [sbt(ph, [128, 512], BF16, "sbf") for _ in range(6)]
                sgv = [sbt(ph, [128, 256], BF16, "sgv") for _ in range(4)]
                sgo = [sbt(ph, [128, 256], F32, "sgo") for _ in range(4)]
                smv = [sbt(ph, [128, 8, 65], BF16, "smv") for _ in range(4)]
                tps = [pst(ph, [128, D], BF16, "tps") for _ in range(2)]
                aps = [pst(ph, [128, 512], F32, "aps") for _ in range(5)]

                def wink(c_lo, c_hi):
                    return [("win", kc_, cb_) for kc_ in range(8) for cb_ in range(0, DIN, 944) if cb_ < c_hi and cb_ + 944 > c_lo]

                dma("sp", gpre[:], norms_d[l, 0:1, :].partition_broadcast(128), writes=["gpre"])
                for i in range(4):
                    kb.op("dve", lambda e, i=i: e.memset(smv[i][:], 1.0), writes=[("smv", i)])

                flist = [("lru", lruT_d, 0, 0, 128, F32), ("lru", lruT_d, 128, 128, 128, F32),
                         ("lru", lruT_d, 256, 256, 128, F32), ("lru", lruT_d, 384, 384, 128, F32),
                         ("gq", gqT_d, 0, 512, 128, F32), ("gk", gkT_d, 0, 640, 128, F32),
                         ("glr", glrT_d, 0, 1024, 16, BF16)]
                for i in range(4):
                    flist.append(("mq", mqT_d, i * 128, 1296 + i * 128, 128, BF16))
                for i in range(4):
                    flist.append(("mk", mkT_d, i * 128, 1808 + i * 128, 128, BF16))

                def load(t):
                    dma("sp", xt[t % 8][:], xin_d[t * 128:(t + 1) * 128, :], writes=[("xt", t % 8)], stream="x", n=4)

                NT = S // 128
                pi = 0
                ev = 0
                import os as _os
                _ng = int(_os.environ.get("P1_GROUPS", S // 512))
                _parts = int(_os.environ.get("P1_PARTS", 7))

                def chain(g):
                    for s in range(4):
                        t = g * 4 + s
                        xs = xt[t % 8]
                        hbs = hb[s]
                        c = t % 8
                        kb.op("act", lambda e, xs=xs, c=c: e.activation(out=junk[:], in_=xs[:], func=AF.Square, accum_out=ssq[:, c:c + 1]),
                              reads=[("xt", t % 8)], writes=[("p1ssq", c)])
                        rstd_from_ssq(ssq[:, c:c + 1], rst[:, c:c + 1], D, "p1", c)
                        kb.op("dve", lambda e, xs=xs, hbs=hbs, c=c: e.scalar_tensor_tensor(out=hbs[:], in0=xs[:], scalar=rst[:, c:c + 1], in1=gpre[:], op0=ALU.mult, op1=ALU.mult),
                              reads=[("xt", t % 8), ("p1rs", c), "gpre"], writes=[("hb", s)])

                def transp(g):
                    hTg_ = hT[g % 2]
                    for s in range(4):
                        t = g * 4 + s
                        hbs = hb[s]
                        tpp = tps[t % 2]
                        for kc in range(8):
                            tp(tpp[:, kc * 128:(kc + 1) * 128], hbs[:, kc * 128:(kc + 1) * 128], ident[:],
                               reads=[("hb", s), "ident"], writes=[("tps", t % 2)])
                        kb.op("act", lambda e, tpp=tpp, hTg_=hTg_, s=s: e.copy(out=hTg_[:, :, s * 128:(s + 1) * 128], in_=tpp[:].rearrange("p (k c) -> p k c", k=8)),
                              reads=[("tps", t % 2)], writes=[("hT", g % 2)])

                for t in range(8):
                    load(t)
                for c0_ in range(0, DIN, 944):
                    for kc in range(8):
                        dma("sp", win[:, kc, c0_:c0_ + 944], winb_d[l, kc * 128:(kc + 1) * 128, c0_:c0_ + 944], reads=[("winb", l, kc, c0_)], writes=[("win", kc, c0_)], stream="w", n=8)
                chain(0)
                transp(0)
                for g in range(_ng):
                    hTg = hT[g % 2]
                    if g + 1 < _ng:
                        chain(g + 1)
                    if g + 2 < _ng:
                        for s in range(4):
                            load((g + 2) * 4 + s)
                    for (nm, dst, drow, wcol, wid, dt) in (flist if _parts & 2 else []):
                        ps = aps[pi % 5]
                        pk = ("aps", pi % 5)
                        pi += 1
                        for kc in range(8):
                            mm(ps[0:wid, :], win[:, kc, wcol:wcol + wid], hTg[:, kc, :], kc == 0, kc == 7,
                               reads=wink(wcol, wcol + wid) + [("hT", g % 2)], writes=[pk])
                        if dt == F32:
                            stg = sf[ev % 6]
                            sk = ("sf", ev % 6)
                        else:
                            stg = sbf[ev % 6]
                            sk = ("sbf", ev % 6)
                        eng = "act" if ev % 2 == 0 else "dve"
                        ev += 1
                        if nm == "mq":
                            if eng == "act":
                                kb.op("act", lambda e, stg=stg, ps=ps, wid=wid: e.mul(out=stg[0:wid, :], in_=ps[0:wid, :], mul=0.125), reads=[pk], writes=[sk])
                            else:
                                kb.op("dve", lambda e, stg=stg, ps=ps, wid=wid: e.tensor_scalar(out=stg[0:wid, :], in0=ps[0:wid, :], scalar1=0.125, scalar2=None, op0=ALU.mult), reads=[pk], writes=[sk])
                        else:
                            if eng == "act":
                                kb.op("act", lambda e, stg=stg, ps=ps, wid=wid: e.copy(out=stg[0:wid, :], in_=ps[0:wid, :]), reads=[pk], writes=[sk])
                            else:
                                kb.op("dve", lambda e, stg=stg, ps=ps, wid=wid: e.tensor_copy(out=stg[0:wid, :], in_=ps[0:wid, :]), reads=[pk], writes=[sk])
                        dma("sp", dst[drow:drow + wid, g * 512:(g + 1) * 512], stg[0:wid, :], reads=[sk], stream="o1", n=12)
                    if g + 1 < _ng:
                        transp(g + 1)
                    for s in (range(4) if _parts & 4 else []):
                        t = g * 4 + s
                        _tm = int(_os.environ.get("TM_SKIP", 0))
                        if not _tm & 1:
                            ps = aps[pi % 5]
                            pk = ("aps", pi % 5)
                            pi += 1
                            ps2 = aps[pi % 5]
                            pk2 = ("aps", pi % 5)
                            pi += 1
                            for kc in range(8):
                                mm(ps[:, 0:256], hTg[:, kc, s * 128:(s + 1) * 128], win[:, kc, 768:1024], kc == 0, kc == 7,
                                   reads=wink(768, 1024) + [("hT", g % 2)], writes=[pk])
                            for kc in range(8):
                                mm(ps2[:, 0:256], hTg[:, kc, s * 128:(s + 1) * 128], win[:, kc, 1040:1296], kc == 0, kc == 7,
                                   reads=wink(1040, 1296) + [("hT", g % 2)], writes=[pk2])
                            a, b = sgv[t % 4], sgo[t % 4]
                            kb.op("act", lambda e, a=a, ps=ps: e.copy(out=a[:], in_=ps[:, 0:256]), reads=[pk], writes=[("sgv", t % 4)])
                            kb.op("dve", lambda e, b=b, ps2=ps2: e.tensor_copy(out=b[:], in_=ps2[:, 0:256]), reads=[pk2], writes=[("sgo", t % 4)])
                            dma("sp", gv_d[t * 128:(t + 1) * 128, :], a[:], reads=[("sgv", t % 4)], stream="o1", n=12)
                            dma("sp", gout_d[t * 128:(t + 1) * 128, :], b[:], reads=[("sgo", t % 4)], stream="o1", n=12)
                        if not _tm & 2:
                            ps = aps[pi % 5]
                            pk = ("aps", pi % 5)
                            pi += 1
                            for kc in range(8):
                                mm(ps[:, :], hTg[:, kc, s * 128:(s + 1) * 128], win[:, kc, 2320:2832], kc == 0, kc == 7,
                                   reads=wink(2320, 2832) + [("hT", g % 2)], writes=[pk])
                            m = smv[t % 4]
                            psv = ps[:].rearrange("p (h d) -> p h d", h=8)
                            if _tm & 4:
                                pass
                            elif t % 2:
                                kb.op("act", lambda e, m=m, psv=psv: e.copy(out=m[:, :, 0:64], in_=psv), reads=[pk], writes=[("smv", t % 4)])
                            else:
                                kb.op("dve", lambda e, m=m, psv=psv: e.tensor_copy(out=m[:, :, 0:64], in_=psv), reads=[pk], writes=[("smv", t % 4)])
                            if not _tm & 8:
                                dma("sp", mvp_d[t * 128:(t + 1) * 128, :], m[:].rearrange("p h d -> p (h d)"), reads=[("smv", t % 4)], stream="o1", n=12)
                if _os.environ.get("P1_TAILSTORE"):
                    dma("sp", lruT_d[0:128, 0:8], rst[:], reads=["p1rs"], stream="o1", n=12)
                end_phase()

        def phase2a_gen(l, ph):
            TB = 1024
            if True:
                cols = sbt(ph, [128, 2, 8], F32, "lcols")
                ccol = sbt(ph, [128, 2], F32, "ccol")
                wstage = sbt(ph, [128, 2, 2, 128], F32, "wstage")
                wbd = sbt(ph, [128, 2, 2, 128], BF16, "wbd")
                xin = [sbt(ph, [128, TB + 3], F32, "xin") for _ in range(2)]
                gin = [sbt(ph, [128, TB], F32, "gin") for _ in range(2)]
                xc = sbt(ph, [128, TB], F32, "xc")
                xcb = sbt(ph, [128, TB], BF16, "xcb")
                rr_ = sbt(ph, [128, TB], F32, "r")
                ii_ = sbt(ph, [128, TB], F32, "i")
                aa = sbt(ph, [128, TB], F32, "a")
                mmul = sbt(ph, [128, TB], F32, "mult")
                uu = sbt(ph, [128, TB], F32, "u")
                hh = [sbt(ph, [128, TB], F32, "h") for _ in range(2)]
                gt = sbt(ph, [128, TB], F32, "gt")
                gs = sbt(ph, [128, TB], F32, "gs")
                yb = [sbt(ph, [128, TB], BF16, "yb") for _ in range(2)]
                gps = [pst(ph, [128, 512], F32, "gps") for _ in range(2)]
                for h in range(2):
                    dma("sp", cols[:, h, :], lcols_d[l, h], writes=["lcols"])
                kb.op("pool", lambda e: e.memset(wstage[:], 0.0), writes=["wstage"])
                for ax, src in enumerate((lwa_d, lwx_d)):
                    for h in range(2):
                        for b in range(2):
                            dma("sp", wstage[b * 64:(b + 1) * 64, ax, h, b * 64:(b + 1) * 64], src[l, 2 * h + b],
                                reads=[], writes=["wstage"])
                kb.op("dve", lambda e: e.tensor_copy(out=wbd[:], in_=wstage[:]), reads=["wstage"], writes=["wbd"])
                kb.op("act", lambda e: e.activation(out=ccol[:], in_=cols[:, :, 7], func=AF.Exp, scale=-1.0), reads=["lcols"], writes=["ccol"])
                kb.op("act", lambda e: e.activation(out=ccol[:], in_=ccol[:], func=AF.Ln, bias=1.0), reads=["ccol"], writes=["ccol"])
                kb.op("dve", lambda e: e.tensor_scalar(out=ccol[:], in0=ccol[:], scalar1=-8.0, scalar2=None, op0=ALU.mult), reads=["ccol"], writes=["ccol"])

                nb = S // TB
                it = 0
                for h in range(2):
                    for b in range(nb):
                        t0 = b * TB
                        xi = xin[it % 2]
                        gi = gin[it % 2]
                        hcur = hh[it % 2]
                        hprev = hh[(it + 1) % 2]
                        ybs = yb[it % 2]
                        kx, kg, ky = ("xin", it % 2), ("gin", it % 2), ("yb", it % 2)
                        kh, khp = ("h", it % 2), ("h", (it + 1) % 2)
                        it += 1
                        if b == 0:
                            kb.op("pool", lambda e, xi=xi: e.memset(xi[:, 0:3], 0.0), writes=[kx])
                            dma("sp", xi[:, 3:], lruT_d[h * 128:(h + 1) * 128, 0:TB], writes=[kx], stream="x", n=3)
                        else:
                            dma("sp", xi[:], lruT_d[h * 128:(h + 1) * 128, t0 - 3:t0 + TB], writes=[kx], stream="x", n=3)
                        dma("sp", gi[:], lruT_d[256 + h * 128:256 + (h + 1) * 128, t0:t0 + TB], writes=[kg], stream="x", n=3)
                        yield
                        kb.op("dve", lambda e, xi=xi, h=h: e.tensor_scalar(out=xc[:], in0=xi[:, 3:TB + 3], scalar1=cols[:, h, 3:4], scalar2=cols[:, h, 4:5], op0=ALU.mult, op1=ALU.add),
                              reads=[kx, "lcols"], writes=["xc"])
                        for j in range(3):
                            kb.op("dve", lambda e, xi=xi, h=h, j=j: e.scalar_tensor_tensor(out=xc[:], in0=xi[:, j:TB + j], scalar=cols[:, h, j:j + 1], in1=xc[:], op0=ALU.mult, op1=ALU.add),
                                  reads=[kx, "lcols", "xc"], writes=["xc"])
                        yield
                        kb.op("pool", lambda e: e.tensor_copy(out=xcb[:], in_=xc[:]), reads=["xc"], writes=["xcb"])
                        for sblk in range(TB // 512):
                            cs = slice(sblk * 512, (sblk + 1) * 512)
                            pa, px = gps[0], gps[1]
                            ka, kx_ = ("gps", 0), ("gps", 1)
                            mm(pa[:], wbd[:, 0, h, :], xcb[:, cs], True, True, reads=["wbd", "xcb"], writes=[ka])
                            mm(px[:], wbd[:, 1, h, :], xcb[:, cs], True, True, reads=["wbd", "xcb"], writes=[kx_])
                            kb.op("act", lambda e, pa=pa, cs=cs, h=h: e.activation(out=rr_[:, cs], in_=pa[:], func=AF.Sigmoid, bias=cols[:, h, 5:6]),
                                  reads=[ka, "lcols"], writes=["r"])
                            kb.op("act", lambda e, px=px, cs=cs, h=h: e.activation(out=ii_[:, cs], in_=px[:], func=AF.Sigmoid, bias=cols[:, h, 6:7]),
                                  reads=[kx_, "lcols"], writes=["i"])
                        yield
                        kb.op("pool", lambda e, gi=gi: e.tensor_tensor(out=gt[:], in0=gi[:], in1=gi[:], op=ALU.mult), reads=[kg], writes=["gt"])
                        kb.op("pool", lambda e: e.tensor_scalar(out=gt[:], in0=gt[:], scalar1=0.044715, scalar2=1.0, op0=ALU.mult, op1=ALU.add), reads=["gt"], writes=["gt"])
                        kb.op("pool", lambda e, gi=gi: e.tensor_tensor(out=gt[:], in0=gt[:], in1=gi[:], op=ALU.mult), reads=["gt", kg], writes=["gt"])
                        kb.op("act", lambda e: e.activation(out=gs[:], in_=gt[:], func=AF.Sigmoid, scale=1.5957691216057308), reads=["gt"], writes=["gs"])
                        kb.op("pool", lambda e, gi=gi: e.tensor_tensor(out=gs[:], in0=gs[:], in1=gi[:], op=ALU.mult), reads=["gs", kg], writes=["gs"])
                        yield
                        kb.op("act", lambda e, h=h: e.activation(out=aa[:], in_=rr_[:], func=AF.Exp, scale=ccol[:, h:h + 1]), reads=["r", "ccol"], writes=["a"])
                        kb.op("pool", lambda e: e.tensor_tensor(out=mmul[:], in0=aa[:], in1=aa[:], op=ALU.mult), reads=["a"], writes=["mult"])
                        kb.op("act", lambda e: e.activation(out=mmul[:], in_=mmul[:], func=AF.Sqrt, scale=-1.0, bias=1.0), reads=["mult"], writes=["mult"])
                        yield
                        if b == 0:
                            kb.op("dve", lambda e: e.memset(mmul[:, 0:1], 1.0), reads=["mult"], writes=["mult"])
                        kb.op("dve", lambda e: e.tensor_tensor(out=uu[:], in0=ii_[:], in1=xc[:], op=ALU.mult), reads=["i", "xc"], writes=["u"])
                        kb.op("dve", lambda e: e.tensor_tensor(out=uu[:], in0=uu[:], in1=mmul[:], op=ALU.mult), reads=["u", "mult"], writes=["u"])
                        yield
                        if b == 0:
                            kb.op("dve", lambda e, hcur=hcur: e.tensor_tensor_scan(out=hcur[:], data0=aa[:], data1=uu[:], initial=0.0, op0=ALU.mult, op1=ALU.add),
                                  reads=["a", "u"], writes=[kh])
                        else:
                            kb.op("dve", lambda e, hcur=hcur, hprev=hprev: e.tensor_tensor_scan(out=hcur[:], data0=aa[:], data1=uu[:], initial=hprev[:, TB - 1:TB], op0=ALU.mult, op1=ALU.add),
                                  reads=["a", "u", khp], writes=[kh])
                        yield
                        kb.op("dve", lambda e, hcur=hcur, ybs=ybs: e.tensor_tensor(out=ybs[:], in0=hcur[:], in1=gs[:], op=ALU.mult), reads=[kh, "gs"], writes=[ky])
                        dma("sp", mixT_d[h * 128:(h + 1) * 128, t0:t0 + TB], ybs[:], reads=[ky], stream="o", n=4)
                        yield

        def phase2b_gen(l, ph):
            TB = 1024
            NCH = TB // 128
            if True:
                w2s = sbt(ph, [16, 128], F32, "w2s")
                w2b = sbt(ph, [16, 128], BF16, "w2b")
                negb = sbt(ph, [128, 1], F32, "negb")
                gn = sbt(ph, [128, 256], F32, "gn")
                tri = sbt(ph, [128, 128], F32, "tri")
                bmask = sbt(ph, [128, 256], F32, "bmask")
                hm = sbt(ph, [128, 4], F32, "hm")
                ones = sbt(ph, [128, 128], F32, "ones")
                glr = [sbt(ph, [16, TB], BF16, "glr") for _ in range(2)]
                qT = [sbt(ph, [128, TB], F32, "qT") for _ in range(2)]
                kT = [sbt(ph, [128, TB], F32, "kT") for _ in range(2)]
                vv = [sbt(ph, [128, NCH, 256], BF16, "vv") for _ in range(2)]
                go = [sbt(ph, [128, NCH, 256], F32, "go") for _ in range(2)]
                ee = sbt(ph, [128, TB], F32, "ee")
                cum = sbt(ph, [128, TB], F32, "cum")
                ex = sbt(ph, [128, TB], F32, "ex")
                dd = sbt(ph, [128, TB], F32, "dd")
                qd = sbt(ph, [128, TB], BF16, "qd")
                kdm = sbt(ph, [128, 4, TB], BF16, "kdm")
                kdec = sbt(ph, [128, TB], BF16, "kdec")
                dcol = sbt(ph, [128, NCH], F32, "dcol")
                kdtm = [sbt(ph, [128, 128], BF16, "kdtm") for _ in range(2)]
                am = [sbt(ph, [128, 4, 128], BF16, "am") for _ in range(2)]
                Sst = sbt(ph, [128, 256], F32, "Sst")
                Sbf = sbt(ph, [128, 256], BF16, "Sbf")
                kvm = sbt(ph, [128, 256], F32, "kvm")
                ob = sbt(ph, [128, NCH, 256], F32, "ob")
                osq = sbt(ph, [128, NCH, 256], F32, "osq")
                ssq = sbt(ph, [128, NCH * 4], F32, "gssq")
                rst = sbt(ph, [128, NCH * 4], F32, "grst")
                sg = sbt(ph, [128, NCH, 256], F32, "sg")
                yb = sbt(ph, [128, NCH, 256], BF16, "yb")
                yT = [sbt(ph, [128, 2, TB], BF16, "yT") for _ in range(2)]
                zps = [pst(ph, [128, 512], F32, "zps") for _ in range(1)]
                tps = pst(ph, [128, 1024], BF16, "gtps")
                aps_ = [pst(ph, [128, 512], F32, "gaps") for _ in range(1)]
                ops_ = [pst(ph, [128, 512], F32, "gops") for _ in range(2)]
                kvps = pst(ph, [128, 512], F32, "kvps")

                dma("sp", w2s[:], gw2_d[l], writes=["w2s"])
                kb.op("dve", lambda e: e.tensor_copy(out=w2b[:], in_=w2s[:]), reads=["w2s"], writes=["w2b"])
                dma("sp", negb[:], gb_d[l], writes=["negb"])
                kb.op("dve", lambda e: e.tensor_scalar(out=negb[:], in0=negb[:], scalar1=-1.0, scalar2=None, op0=ALU.mult), reads=["negb"], writes=["negb"])
                dma("sp", gn[:], gn_d[l:l + 1, :].partition_broadcast(128), writes=["gn"])
                dma("sp", tri[:], tri_d, writes=["tri"])
                dma("sp", bmask[:], bmask_d, writes=["bmask"])
                dma("sp", hm[:], hm_d, writes=["hm"])
                kb.op("pool", lambda e: e.memset(ones[:], 1.0), writes=["ones"])
                kb.op("pool", lambda e: e.memset(Sst[:], 0.0), writes=["Sst"])
                kb.op("pool", lambda e: e.memset(Sbf[:], 0.0), writes=["Sbf"])

                def load(b):
                    i = b % 2
                    t0 = b * TB
                    dma("sp", glr[i][:], glrT_d[:, t0:t0 + TB], writes=[("glr", i)], stream="x", n=3)
                    dma("sp", qT[i][:], gqT_d[:, t0:t0 + TB], writes=[("qT", i)], stream="x", n=3)
                    dma("sp", kT[i][:], gkT_d[:, t0:t0 + TB], writes=[("kT", i)], stream="x", n=3)
                    dma("sp", vv[i][:], gv_d[t0:t0 + TB, :].rearrange("(c p) f -> p c f", p=128), writes=[("vv", i)], stream="x", n=3)
                    dma("sp", go[i][:], gout_d[t0:t0 + TB, :].rearrange("(c p) f -> p c f", p=128), writes=[("go", i)], stream="x", n=3)

                nb = S // TB
                load(0)
                for b in range(nb):
                    if b + 1 < nb:
                        load(b + 1)
                    i = b % 2
                    t0 = b * TB
                    q_, k_, v_, g_, r_ = qT[i], kT[i], vv[i], go[i], glr[i]
                    kq, kk, kv, kg, kr = ("qT", i), ("kT", i), ("vv", i), ("go", i), ("glr", i)
                    for sblk in range(TB // 512):
                        cs = slice(sblk * 512, (sblk + 1) * 512)
                        zp = zps[0]
                        mm(zp[:], w2b[:], r_[:, cs], True, True, reads=["w2b", kr], writes=[("zps", 0)])
                        kb.op("act", lambda e, zp=zp, cs=cs: e.activation(out=ee[:, cs], in_=zp[:], func=AF.Exp, scale=-1.0, bias=negb[:]),
                              reads=[("zps", 0), "negb"], writes=["ee"])
                    yield
                    kb.op("act", lambda e: e.activation(out=ee[:], in_=ee[:], func=AF.Ln, bias=1.0), reads=["ee"], writes=["ee"])
                    for c in range(NCH):
                        cs = slice(c * 128, (c + 1) * 128)
                        kb.op("dve", lambda e, cs=cs: e.tensor_tensor_scan(out=cum[:, cs], data0=ones[:], data1=ee[:, cs], initial=0.0, op0=ALU.mult, op1=ALU.add),
                              reads=["ones", "ee"], writes=["cum"])
                    yield
                    kb.op("act", lambda e: e.activation(out=ex[:], in_=cum[:], func=AF.Exp, scale=-1.0 / 16.0), reads=["cum"], writes=["ex"])
                    kb.op("dve", lambda e, q_=q_: e.scalar_tensor_tensor(out=qd[:], in0=q_[:], scalar=32.0 ** -0.5, in1=ex[:], op0=ALU.mult, op1=ALU.mult),
                          reads=[kq, "ex"], writes=["qd"])
                    yield
                    kb.op("act", lambda e: e.activation(out=dcol[:], in_=cum[:].rearrange("p (c t) -> p c t", t=128)[:, :, 127], func=AF.Exp, scale=-1.0 / 16.0),
                          reads=["cum"], writes=["dcol"])
                    for c in range(NCH):
                        cs = slice(c * 128, (c + 1) * 128)
                        kb.op("pool", lambda e, cs=cs, c=c: e.tensor_scalar(out=dd[:, cs], in0=cum[:, cs], scalar1=cum[:, c * 128 + 127:c * 128 + 128], scalar2=None, op0=ALU.subtract),
                              reads=["cum"], writes=["dd"])
                    yield
                    kb.op("act", lambda e: e.activation(out=ex[:], in_=cum[:], func=AF.Exp, scale=1.0 / 16.0), reads=["cum", "qd"], writes=["ex"])
                    for hh_ in range(4):
                        kb.op("dve", lambda e, k_=k_, hh_=hh_: e.scalar_tensor_tensor(out=kdm[:, hh_, :], in0=k_[:], scalar=hm[:, hh_:hh_ + 1], in1=ex[:], op0=ALU.mult, op1=ALU.mult),
                              reads=[kk, "ex", "hm"], writes=["kdm"])
                    yield
                    kb.op("act", lambda e: e.activation(out=dd[:], in_=dd[:], func=AF.Exp, scale=1.0 / 16.0), reads=["dd"], writes=["dd"])
                    kb.op("pool", lambda e, k_=k_: e.tensor_tensor(out=kdec[:], in0=k_[:], in1=dd[:], op=ALU.mult), reads=[kk, "dd"], writes=["kdec"])
                    kb.op("act", lambda e, g_=g_: e.activation(out=sg[:], in_=g_[:], func=AF.Silu), reads=[kg], writes=["sg"])
                    def gla_s1(c):
                        cs = slice(c * 128, (c + 1) * 128)
                        j = c % 2
                        tp(tps[:, j * 128:(j + 1) * 128], kdec[:, cs], ident[:], reads=["kdec", "ident"], writes=["gtps"])
                        kb.op("act", lambda e, j=j: e.copy(out=kdtm[j][:], in_=tps[:, j * 128:(j + 1) * 128]), reads=["gtps"], writes=[("kdtm", j)])
                        ap_ = aps_[0]
                        for hh_ in range(4):
                            mm(ap_[:, hh_ * 128:(hh_ + 1) * 128], kdm[:, hh_, cs], qd[:, cs], True, True, reads=["kdm", "qd"], writes=[("gaps", 0)])
                        kb.op("dve", lambda e, ap_=ap_, j=j: e.tensor_tensor(out=am[j][:], in0=ap_[:].rearrange("p (h c) -> p h c", h=4),
                                                                             in1=tri[:].unsqueeze(1).broadcast_to([128, 4, 128]), op=ALU.mult),
                              reads=[("gaps", 0), "tri"], writes=[("am", j)])

                    def gla_s2(c):
                        cs = slice(c * 128, (c + 1) * 128)
                        j = c % 2
                        op_ = ops_[j]
                        mm(op_[:, 0:256], qd[:, cs], Sbf[:], True, True, reads=["qd", "Sbf"], writes=[("gops", j)])
                        for hh_ in range(4):
                            mm(op_[:, hh_ * 64:(hh_ + 1) * 64], am[j][:, hh_, :], v_[:, c, hh_ * 64:(hh_ + 1) * 64], False, True,
                               reads=[("am", j), kv], writes=[("gops", j)], skip_group_check=True)
                        kb.op("act", lambda e, op_=op_, c=c: e.copy(out=ob[:, c, :], in_=op_[:, 0:256]), reads=[("gops", j)], writes=["ob"])
                        mm(kvps[:, 0:256], kdtm[j][:], v_[:, c, :], True, True, reads=[("kdtm", j), kv], writes=["kvps"])
                        kb.op("dve", lambda e: e.tensor_tensor(out=kvm[:], in0=kvps[:, 0:256], in1=bmask[:], op=ALU.mult), reads=["kvps", "bmask"], writes=["kvm"])
                        kb.op("dve", lambda e, c=c: e.scalar_tensor_tensor(out=Sst[:], in0=Sst[:], scalar=dcol[:, c:c + 1], in1=kvm[:], op0=ALU.mult, op1=ALU.add),
                              reads=["Sst", "dcol", "kvm"], writes=["Sst"])
                        kb.op("pool", lambda e: e.tensor_copy(out=Sbf[:], in_=Sst[:]), reads=["Sst"], writes=["Sbf"])

                    gla_s1(0)
                    yield
                    for c in range(NCH):
                        if c + 1 < NCH:
                            gla_s1(c + 1)
                            yield
                        gla_s2(c)
                        yield
                    yield
                    kb.op("pool", lambda e: e.tensor_tensor(out=osq[:], in0=ob[:], in1=ob[:], op=ALU.mult), reads=["ob"], writes=["osq"])
                    kb.op("dve", lambda e: e.tensor_reduce(out=ssq[:], in_=osq[:].rearrange("p c (h v) -> p (c h) v", h=4), axis=AX.X, op=ALU.add),
                          reads=["osq"], writes=["p2bssq"])
                    rstd_from_ssq(ssq[:], rst[:], 64, "p2b")
                    kb.op("dve", lambda e: e.tensor_tensor(out=ob[:].rearrange("p c (h v) -> p (c h) v", h=4), in0=ob[:].rearrange("p c (h v) -> p (c h) v", h=4),
                                                           in1=rst[:].unsqueeze(2).broadcast_to([128, NCH * 4, 64]), op=ALU.mult),
                          reads=["ob", "p2brs"], writes=["ob"])
                    kb.op("pool", lambda e: e.tensor_tensor(out=sg[:], in0=sg[:], in1=gn[:].unsqueeze(1).broadcast_to([128, NCH, 256]), op=ALU.mult),
                          reads=["sg", "gn"], writes=["sg"])
                    kb.op("dve", lambda e: e.tensor_tensor(out=yb[:], in0=ob[:], in1=sg[:], op=ALU.mult), reads=["ob", "sg"], writes=["yb"])
                    yield
                    yTb = yT[b % 2]
                    for c in range(NCH):
                        for f in range(2):
                            jj = (c * 2 + f) % 4
                            tp(tps[:, jj * 128:(jj + 1) * 128], yb[:, c, f * 128:(f + 1) * 128], ident[:], reads=["yb", "ident"], writes=["gtps"])
                            kb.op("act", lambda e, jj=jj, c=c, f=f, yTb=yTb: e.copy(out=yTb[:, f, c * 128:(c + 1) * 128], in_=tps[:, jj * 128:(jj + 1) * 128]),
                                  reads=["gtps"], writes=[("yT", b % 2)])
                    dma("sp", mixT_d[256:512, t0:t0 + TB].rearrange("(f p) t -> p f t", p=128), yTb[:], reads=[("yT", b % 2)], stream="o", n=4)

        def phase2ab(l):
            with ExitStack() as ph:
                gens = [phase2a_gen(l, ph), phase2b_gen(l, ph)]
                while gens:
                    for g_ in list(gens):
                        try:
                            next(g_)
                        except StopIteration:
                            gens.remove(g_)
                end_phase()

        def phase2c(l):
            with ExitStack() as ph:
                kaug = sbt(ph, [128, 8, S], BF16, "kaug")
                vp = sbt(ph, [128, 32, 520], BF16, "vp")
                qaug = [sbt(ph, [128, 8, 512], BF16, "qaug") for _ in range(3)]
                km = sbt(ph, [64, 8, 16], F32, "km")
                kmb = sbt(ph, [64, 8, 16], BF16, "kmb")
                cm = sbt(ph, [128, 16, 16], F32, "cm")
                pm = sbt(ph, [128, 16, 16], F32, "pm")
                b31 = sbt(ph, [128, 8], F32, "b31")
                caus = sbt(ph, [128, 128], F32, "caus")
                tstage = sbt(ph, [128, 2, 8, 128], F32, "tstage")
                tdT = sbt(ph, [128, 8, 128], BF16, "tdT")
                toT = sbt(ph, [128, 8, 128], BF16, "toT")
                tomT = sbt(ph, [128, 8, 128], BF16, "tomT")
                zer = sbt(ph, [128, 260], BF16, "zer")
                gm = sbt(ph, [128, 4, 8, 16], F32, "gm")
                m8 = sbt(ph, [128, 4, 8, 8], F32, "m8")
                sel = sbt(ph, [128, 4, 8, 16], F32, "sel")
                mpad = [sbt(ph, [128, 4, 8, 80], BF16, "mpad") for _ in range(2)]
                pT = [sbt(ph, [128, 512], BF16, "pT") for _ in range(5)]
                rcp = sbt(ph, [128, 4], F32, "rcp")
                ymo = [sbt(ph, [128, 4, 512], BF16, "ymo") for _ in range(2)]
                ymT = [sbt(ph, [128, 4, 512], BF16, "ymT") for _ in range(2)]
                gps = pst(ph, [128, 512], F32, "mgps")
                mtps = [gps]
                mtps_b = [pst(ph, [128, 1024], BF16, "mtpsb") for _ in range(1)]
                sps = [pst(ph, [128, 512], F32, "sps") for _ in range(4)]
                accs_full = [pst(ph, [128, 512], F32, "acc") for _ in range(2)]
                accs = [a_[:, 0:260].rearrange("p (s d) -> p s d", s=4) for a_ in accs_full]

                for h in range(8):
                    dma("sp", kaug[0:64, h, :], mkT_d[h * 64:(h + 1) * 64, :], writes=[("kaug", h)], stream="x", n=4)
                    dma("sp", kaug[64:80, h, :], e16_d, writes=[("kaugE", h)], stream="x", n=4)
                for c in range(4):
                    dma("sp", vp[:, c * 8:(c + 1) * 8, :], mvp_d[c * 1024:(c + 1) * 1024, :].rearrange("(c p) f -> p c f", p=128), writes=[("vp", c)], stream="x", n=4)
                dma("sp", cm[:].rearrange("p a b -> p (a b)"), cm_d.partition_broadcast(128), writes=["cm"])
                dma("sp", pm[:].rearrange("p a b -> p (a b)"), pm_d.partition_broadcast(128), writes=["pm"])
                dma("sp", b31[:], rb31_d.partition_broadcast(128), writes=["b31"])
                dma("sp", caus[:], caus_d, writes=["caus"])
                dma("sp", tstage[:, 0], tdg_d, writes=["tstage"])
                dma("sp", tstage[:, 1], tof_d, writes=["tstage"])
                kb.op("dve", lambda e: e.tensor_tensor(out=tdT[:], in0=tstage[:, 0], in1=caus[:].unsqueeze(1).broadcast_to([128, 8, 128]), op=ALU.add),
                      reads=["tstage", "caus"], writes=["tdT"])
                kb.op("dve", lambda e: e.tensor_copy(out=toT[:], in_=tstage[:, 1]), reads=["tstage"], writes=["toT"])
                kb.op("dve", lambda e: e.tensor_tensor(out=tomT[:], in0=tstage[:, 1], in1=b31[:].unsqueeze(2).broadcast_to([128, 8, 128]), op=ALU.subtract),
                      reads=["tstage", "b31"], writes=["tomT"])
                kb.op("pool", lambda e: e.memset(zer[:], 0.0), writes=["zer"])
                for i in range(2):
                    kb.op("pool", lambda e, i=i: e.memset(mpad[i][:], 0.0), writes=[("mpad", i)])
                kb.op("dve", lambda e: e.tensor_reduce(out=km[:].rearrange("p h n -> p (h n)"), in_=kaug[0:64, :, :].rearrange("p h (n t) -> p (h n) t", t=256), axis=AX.X, op=ALU.add),
                      reads=[("kaug", h_) for h_ in range(8)], writes=["km"])
                kb.op("dve", lambda e: e.tensor_scalar(out=kmb[:], in0=km[:], scalar1=1.0 / 256.0, scalar2=None, op0=ALU.mult), reads=["km"], writes=["kmb"])

                if l == 0:
                    casts_rest()

                def loadq(G):
                    i = G % 3
                    dma("sp", qaug[i][0:64, :, :], mqT_d.rearrange("(h d) t -> d h t", d=64)[:, :, G * 512:(G + 1) * 512], writes=[("qaug", i)], stream="q", n=3)

                NG = S // 512
                si = 0
                ai = 0

                def pre1(G):
                    qi = G % 3
                    qa = qaug[qi]
                    kqa = ("qaug", qi)
                    mp = mpad[G % 2]
                    for s in range(4):
                        for h in range(8):
                            mm(gps[:, (s * 8 + h) * 16:(s * 8 + h + 1) * 16], qa[0:64, h, s * 128:(s + 1) * 128], kmb[:, h, :], True, True,
                               reads=[kqa, "kmb"], writes=["mgps"])
                    np0 = 2 * G
                    for a in range(2):
                        cmv = cm[:, np0 + a, :].unsqueeze(1).unsqueeze(1).broadcast_to([128, 2, 8, 16])
                        kb.op("dve", lambda e, cmv=cmv, a=a: e.tensor_tensor(out=gm[:, 2 * a:2 * a + 2], in0=gps[:].rearrange("p (s h n) -> p s h n", s=4, h=8)[:, 2 * a:2 * a + 2],
                                                                             in1=cmv, op=ALU.add),
                              reads=["mgps", "cm"], writes=["gm"])
                    for s in range(4):
                        for h in range(8):
                            kb.op("dve", lambda e, s=s, h=h: e.max(out=m8[:, s, h, :], in_=gm[:, s, h, :]), reads=["gm"], writes=["m8"])
                    kb.op("dve", lambda e: e.tensor_tensor(out=sel[:], in0=gm[:], in1=m8[:, :, :, 2:3].broadcast_to([128, 4, 8, 16]), op=ALU.is_ge),
                          reads=["gm", "m8"], writes=["sel"])
                    kb.op("dve", lambda e: e.tensor_scalar(out=sel[:], in0=sel[:], scalar1=-NEG, scalar2=NEG, op0=ALU.mult, op1=ALU.add), reads=["sel"], writes=["sel"])
                    kb.op("dve", lambda e: e.tensor_tensor(out=sel[:], in0=sel[:], in1=b31[:].unsqueeze(1).unsqueeze(3).broadcast_to([128, 4, 8, 16]), op=ALU.add),
                          reads=["sel", "b31"], writes=["sel"])
                    for a in range(2):
                        pmv = pm[:, np0 + a, :].unsqueeze(1).unsqueeze(1).broadcast_to([128, 2, 8, 16])
                        kb.op("dve", lambda e, pmv=pmv, mp=mp, a=a: e.tensor_tensor(out=mp[:, 2 * a:2 * a + 2, :, 64:80], in0=sel[:, 2 * a:2 * a + 2], in1=pmv, op=ALU.mult),
                              reads=["sel", "pm"], writes=[("mpad", G % 2)])

                def pre2(G):
                    qi = G % 3
                    qa = qaug[qi]
                    kqa = ("qaug", qi)
                    mp = mpad[G % 2]
                    for h in range(8):
                        mt = mtps[0]
                        for s in range(4):
                            mm(mt[0:80, s * 128:(s + 1) * 128], mp[:, s, h, :], ident[:], True, True, reads=[("mpad", G % 2), "ident"], writes=["mgps"])
                        kb.op("act", lambda e, mt=mt, h=h, qa=qa: e.copy(out=qa[64:80, h, :], in_=mt[64:80, :]),
                              reads=["mgps"], writes=[kqa])

                loadq(0)
                if NG > 1:
                    loadq(1)
                pre1(0)
                pre2(0)
                for G in range(NG):
                    if G + 2 < NG:
                        loadq(G + 2)
                    if G + 1 < NG:
                        pre1(G + 1)
                    qi = G % 3
                    qa = qaug[qi]
                    kqa = ("qaug", qi)
                    ym = ymo[G % 2]
                    nj = 4 * G + 4
                    DEPTH = 3

                    def stageA(h, j):
                        nonlocal si
                        acc = accs[h % 2]
                        ka = ("acc", h % 2)
                        if j == 0:
                            mm(accs_full[h % 2][:, 0:260], zer[:, 0:128], zer[:, :], True, True, reads=["zer"], writes=[ka])
                        r = j - 4 * G
                        c0 = max(r, 0) * 128
                        sp_ = sps[si % 4]
                        ks = ("sps", si % 4)
                        pt = pT[si % 5]
                        kp = ("pT", si % 5)
                        si += 1
                        mm(sp_[:, c0:512], kaug[0:80, h, j * 128:(j + 1) * 128], qa[0:80, h, c0:512], True, True,
                           reads=[("kaug", h), ("kaugE", h), kqa], writes=[ks])
                        if r == -1:
                            mm(sp_[:, 0:128], ident[:], tomT[:, h, :], False, True, reads=["ident", "tomT"], writes=[ks], skip_group_check=True)
                        if r >= 0:
                            mm(sp_[:, r * 128:(r + 1) * 128], ident[:], tdT[:, h, :], False, True, reads=["ident", "tdT"], writes=[ks], skip_group_check=True)
                            if r < 3:
                                tt = toT if r % 2 == 0 else tomT
                                mm(sp_[:, (r + 1) * 128:(r + 2) * 128], ident[:], tt[:, h, :], False, True, reads=["ident", "toT", "tomT"], writes=[ks], skip_group_check=True)
                        kb.op("act", lambda e, pt=pt, sp_=sp_, c0=c0: e.activation(out=pt[:, c0:512], in_=sp_[:, c0:512], func=AF.Exp),
                              reads=[ks], writes=[kp])
                        return (h, j, r, pt, kp, acc, ka)

                    def stageB(info):
                        h, j, r, pt, kp, acc, ka = info
                        for s in range(max(r, 0), 4):
                            mm(acc[:, s, :], pt[:, s * 128:(s + 1) * 128], vp[:, j, h * 65:(h + 1) * 65], False, True,
                               reads=[kp, ("vp", j // 8)], writes=[ka], skip_group_check=True)
                        if j == nj - 1:
                            kb.op("dve", lambda e, acc=acc: e.reciprocal(out=rcp[:], in_=acc[:, :, 64]), reads=[ka], writes=["rcp"])
                            kb.op("dve", lambda e, acc=acc, h=h, ym=ym: e.tensor_tensor(out=ym[:, :, h * 64:(h + 1) * 64], in0=acc[:, :, 0:64],
                                                                                  in1=rcp[:].unsqueeze(2).broadcast_to([128, 4, 64]), op=ALU.mult),
                                  reads=[ka, "rcp"], writes=[("ymo", G % 2)])

                    pend = []
                    for h in range(8):
                        if h == 4 and G + 1 < NG:
                            pre2(G + 1)
                        for j in range(nj):
                            pend.append(stageA(h, j))
                            if len(pend) > DEPTH:
                                stageB(pend.pop(0))
                    while pend:
                        stageB(pend.pop(0))
                    yt = ymT[G % 2]
                    for s in range(4):
                        tpb = mtps_b[0]
                        for f in range(4):
                            tp(tpb[:, f * 128:(f + 1) * 128], ym[:, s, f * 128:(f + 1) * 128], ident[:], reads=[("ymo", G % 2), "ident"], writes=[("mtpsb", 0)])
                        kb.op("act", lambda e, tpb=tpb, yt=yt, s=s: e.copy(out=yt[:, :, s * 128:(s + 1) * 128], in_=tpb[:, 0:512].rearrange("p (f q) -> p f q", f=4)),
                              reads=[("mtpsb", 0)], writes=[("ymT", G % 2)])
                    dma("sp", mixT_d[512:1024, G * 512:(G + 1) * 512].rearrange("(f p) t -> p f t", p=128), yt[:], reads=[("ymT", G % 2)], stream="o", n=4)
                end_phase()

        def phase3(l, xin_d, xout_d):
            with ExitStack() as ph:
                wout = sbt(ph, [128, 8, D], BF16, "wout")
                wdn = sbt(ph, [128, NFC, D], BF16, "wdn")
                g3 = sbt(ph, [128, 3, D], F32, "g3")
                wgu = [sbt(ph, [128, 2, 8, 128], BF16, "wgu") for _ in range(4)]
                mixT = [sbt(ph, [128, 8, 512], BF16, "mixT") for _ in range(2)]
                xt = [sbt(ph, [128, D], F32, "xt3") for _ in range(2)]
                x1 = [sbt(ph, [128, D], F32, "x1") for _ in range(8)]
                tmp = [sbt(ph, [128, D], F32, "tmp3") for _ in range(2)]
                junk = sbt(ph, [128, D], BF16, "junk3")
                hb = [sbt(ph, [128, D], BF16, "hb3") for _ in range(4)]
                hT = sbt(ph, [128, 8, 512], BF16, "hT3")
                actT = sbt(ph, [128, NFC, 512], BF16, "actT")
                sgt = [sbt(ph, [128, 512], F32, "sgt") for _ in range(2)]
                ssq = sbt(ph, [128, 16], F32, "ssq3")
                rst = sbt(ph, [128, 16], F32, "rst3")
                xo = [sbt(ph, [128, D], F32, "xo") for _ in range(2)]
                ops_ = [pst(ph, [128, 2, 512], F32, "p3o") for _ in range(2)]
                tps = pst(ph, [128, D], BF16, "p3t")
                gus = [pst(ph, [128, 512], F32, "p3gu") for _ in range(3)]
                def load_resident():
                    for kc in range(8):
                        dma("sp", wout[:, kc, :], woutb_d[l, kc * 128:(kc + 1) * 128, :], writes=[("wout", kc)], stream="w", n=4)
                    for i in range(3):
                        dma("sp", g3[:, i, :], norms_d[l, i + 1:i + 2, :].partition_broadcast(128), writes=[("g3", i)])
                    for fc in range(NFC):
                        dma("sp", wdn[:, fc, :], wdb_d[l, fc * 128:(fc + 1) * 128, :], writes=[("wdn", fc)], stream="w", n=4)

                G3K = [("g3", i_) for i_ in range(3)]
                wi = [0]

                def loadw(fc):
                    i = wi[0] % 4
                    wi[0] += 1
                    dma("sp", wgu[i][:], wgub_d[l, fc], writes=[("wgu", i)], stream="wgu", n=4)
                    return i

                NG = S // 512
                sq = 0

                def loadg(G):
                    i = G % 2
                    dma("sp", mixT[i][:], mixT_d[:, G * 512:(G + 1) * 512].rearrange("(k p) t -> p k t", p=128), writes=[("mixT", i)], stream="m", n=2)

                def loadx(t):
                    dma("sp", xt[t % 2][:], xin_d[t * 128:(t + 1) * 128, :], writes=[("xt3", t % 2)], stream="x", n=3)

                loadg(0)
                loadx(0)
                load_resident()
                wq = []
                PRE = 3
                def p3_front(G):
                    nonlocal sq
                    mT = mixT[G % 2]
                    for s in range(4):
                        t = G * 4 + s
                        if t + 1 < S // 128:
                            loadx(t + 1)
                        xs = xt[t % 2]
                        x1s = x1[(G % 2) * 4 + s]
                        op_ = ops_[s % 2]
                        ko = ("p3o", s % 2)
                        tm_ = tmp[s % 2]
                        kt = ("tmp3", s % 2)
                        for hf in range(2):
                            for kc in range(8):
                                mm(op_[:, hf, :], mT[:, kc, s * 128:(s + 1) * 128], wout[:, kc, hf * 512:(hf + 1) * 512], kc == 0, kc == 7,
                                   reads=[("mixT", G % 2), ("wout", kc)], writes=[ko])
                        c = sq % 16
                        sq += 1
                        kb.op("act", lambda e, c=c, op_=op_: e.activation(out=junk[:], in_=op_[:].rearrange("p a b -> p (a b)"), func=AF.Square, accum_out=ssq[:, c:c + 1]),
                              reads=[ko], writes=[("p3ssq", c)])
                        rstd_from_ssq(ssq[:, c:c + 1], rst[:, c:c + 1], D, "p3", c)
                        kb.op("dve", lambda e, c=c, op_=op_, tm_=tm_: e.scalar_tensor_tensor(out=tm_[:], in0=op_[:].rearrange("p a b -> p (a b)"), scalar=rst[:, c:c + 1], in1=g3[:, 0, :], op0=ALU.mult, op1=ALU.mult),
                              reads=[ko, ("p3rs", c)] + G3K, writes=[kt])
                        kb.op("pool", lambda e, xs=xs, x1s=x1s, tm_=tm_: e.tensor_tensor(out=x1s[:], in0=xs[:], in1=tm_[:], op=ALU.add),
                              reads=[("xt3", t % 2), kt], writes=[("x1", (G % 2) * 4 + s)])
                        c2 = sq % 16
                        sq += 1
                        kb.op("act", lambda e, c2=c2, x1s=x1s: e.activation(out=junk[:], in_=x1s[:], func=AF.Square, accum_out=ssq[:, c2:c2 + 1]),
                              reads=[("x1", (G % 2) * 4 + s)], writes=[("p3ssq", c2)])
                        rstd_from_ssq(ssq[:, c2:c2 + 1], rst[:, c2:c2 + 1], D, "p3", c2)
                        hbs = hb[s]
                        kb.op("dve", lambda e, c2=c2, x1s=x1s, hbs=hbs: e.scalar_tensor_tensor(out=hbs[:], in0=x1s[:], scalar=rst[:, c2:c2 + 1], in1=g3[:, 1, :], op0=ALU.mult, op1=ALU.mult),
                              reads=[("x1", (G % 2) * 4 + s), ("p3rs", c2)] + G3K, writes=[("hb3", s)])

                def p3_trans(G):
                    for s in range(4):
                        hbs = hb[s]
                        for kc in range(8):
                            tp(tps[:, kc * 128:(kc + 1) * 128], hbs[:, kc * 128:(kc + 1) * 128], ident[:], reads=[("hb3", s), "ident"], writes=["p3t"])
                        kb.op("act", lambda e, s=s: e.copy(out=hT[:, :, s * 128:(s + 1) * 128], in_=tps[:].rearrange("p (k c) -> p k c", k=8)),
                              reads=["p3t"], writes=["hT3"])

                def p3_gateup(G):
                    for fc in range(NFC):
                        wslot = wq.pop(0)
                        nxt = fc + PRE
                        if nxt < NFC:
                            wq.append(loadw(nxt))
                        w_ = wgu[wslot]
                        gp, up = gus[(2 * fc) % 3], gus[(2 * fc + 1) % 3]
                        kgp, kup = ("p3gu", (2 * fc) % 3), ("p3gu", (2 * fc + 1) % 3)
                        for kc in range(8):
                            mm(gp[:], w_[:, 0, kc, :], hT[:, kc, :], kc == 0, kc == 7, reads=[("wgu", wslot), "hT3"], writes=[kgp])
                        for kc in range(8):
                            mm(up[:], w_[:, 1, kc, :], hT[:, kc, :], kc == 0, kc == 7, reads=[("wgu", wslot), "hT3"], writes=[kup])
                        sg_ = sgt[fc % 2]
                        kb.op("act", lambda e, sg_=sg_, gp=gp: e.activation(out=sg_[:], in_=gp[:], func=AF.Silu), reads=[kgp], writes=[("sgt", fc % 2)])
                        kb.op("dve", lambda e, sg_=sg_, up=up, fc=fc: e.tensor_tensor(out=actT[:, fc, :], in0=up[:], in1=sg_[:], op=ALU.mult),
                              reads=[kup, ("sgt", fc % 2)], writes=["actT"])

                def p3_down(G):
                    nonlocal sq
                    for s in range(4):
                        t = G * 4 + s
                        op_ = ops_[s % 2]
                        ko = ("p3o", s % 2)
                        tm_ = tmp[s % 2]
                        kt = ("tmp3", s % 2)
                        for hf in range(2):
                            for fc in range(NFC):
                                mm(op_[:, hf, :], actT[:, fc, s * 128:(s + 1) * 128], wdn[:, fc, hf * 512:(hf + 1) * 512], fc == 0, fc == NFC - 1,
                                   reads=["actT", ("wdn", fc)], writes=[ko])
                        c = sq % 16
                        sq += 1
                        kb.op("act", lambda e, c=c, op_=op_: e.activation(out=junk[:], in_=op_[:].rearrange("p a b -> p (a b)"), func=AF.Square, accum_out=ssq[:, c:c + 1]),
                              reads=[ko], writes=[("p3ssq", c)])
                        rstd_from_ssq(ssq[:, c:c + 1], rst[:, c:c + 1], D, "p3", c)
                        kb.op("dve", lambda e, c=c, op_=op_, tm_=tm_: e.scalar_tensor_tensor(out=tm_[:], in0=op_[:].rearrange("p a b -> p (a b)"), scalar=rst[:, c:c + 1], in1=g3[:, 2, :], op0=ALU.mult, op1=ALU.mult),
                              reads=[ko, ("p3rs", c)] + G3K, writes=[kt])
                        xos = xo[t % 2]
                        kb.op("pool", lambda e, xos=xos, s=s, tm_=tm_: e.tensor_tensor(out=xos[:], in0=x1[(G % 2) * 4 + s][:], in1=tm_[:], op=ALU.add),
                              reads=[("x1", (G % 2) * 4 + s), kt], writes=[("xo", t % 2)])
                        dma("sp", xout_d[t * 128:(t + 1) * 128, :], xos[:], reads=[("xo", t % 2)], stream="o", n=4)

                for G in range(NG):
                    if G + 1 < NG:
                        loadg(G + 1)
                    while len(wq) < PRE:
                        wq.append(loadw(len(wq)))
                    p3_front(G)
                    if G > 0:
                        p3_down(G - 1)
                    p3_trans(G)
                    p3_gateup(G)
                p3_down(NG - 1)
                end_phase()

        kb.barrier()
        import os as _os2
        if not _os2.environ.get("SKIP_P0"):
            phase0()
        done = stop_after == "p0"
        for l in range(L):
            if done:
                break
            xin = x_d if l == 0 else xs1_d
            xout = xs1_d if l == 0 else out_d
            for nm, fn in (("p1", lambda: phase1(l, xin)), ("p2b", lambda: phase2ab(l)),
                           ("p2c", lambda: phase2c(l)), ("p3", lambda: phase3(l, xin, xout))):
                fn()
                if stop_after == (l, nm):
                    done = True
                    break
            if done:
                break
        kb.barrier()
        kb.emit()

    return nc


def _host_inputs(inputs):
    f = lambda a: np.ascontiguousarray(np.asarray(a, dtype=np.float32))
    c = _consts()
    idx_diag, idx_off1 = _bias_idx()
    rel = f(inputs["rel_bias"])
    shared = {
        "norms": f(np.stack([inputs["pre_mix_norm"], inputs["post_mix_norm"], inputs["pre_ffn_norm"], inputs["post_ffn_norm"]], axis=1)),
        "w_in": f(inputs["w_in"]), "w_out": f(inputs["w_out"]),
        "w_ffn_gate": f(inputs["w_ffn_gate"]), "w_ffn_up": f(inputs["w_ffn_up"]), "w_ffn_down": f(inputs["w_ffn_down"]),
        "lru_wa": f(inputs["lru_wa"]), "lru_wx": f(inputs["lru_wx"]),
        "gla_gate_w2": f(inputs["gla_gate_w2"]),
        "gla_gate_b": f(np.asarray(inputs["gla_gate_b"]).reshape(L, 128, 1)),
        "gla_norm": f(inputs["gla_norm"]),
        "rb31": f(rel[31:32, :]),
        "tdg": f(np.transpose(rel[idx_diag], (0, 2, 1))),
        "tof": f(np.transpose(rel[idx_off1], (0, 2, 1))),
        "ident": c["ident"], "tri": c["tri"], "caus": c["caus"], "e16": c["e16"],
        "cm": c["cm"], "pm": c["pm"], "bmask": c["bmask"], "hm": c["hm"],
    }
    cw = np.transpose(np.asarray(inputs["lru_conv_w"], dtype=np.float32), (0, 2, 1))
    cols = np.concatenate([cw] + [np.asarray(inputs[k], dtype=np.float32)[:, :, None]
                                  for k in ("lru_conv_b", "lru_ba", "lru_bx", "lru_lambda")], axis=2)
    shared["lru_cols"] = f(cols.reshape(L, 2, 128, 8))
    x = np.asarray(inputs["x"], dtype=np.float32)
    return [dict(shared, x=np.ascontiguousarray(x[b])) for b in range(x.shape[0])]


_NC_CACHE = {}


def kernel(**inputs):
    in_maps = _host_inputs(inputs)
    if "nc" not in _NC_CACHE:
        _NC_CACHE["nc"] = build()
    nc = _NC_CACHE["nc"]
    n = len(in_maps)
    res = run_bass_kernel_spmd(nc, in_maps, core_ids=list(range(n)))
    return np.stack([np.asarray(r["out"], dtype=np.float32) for r in res.results], axis=0)
```
